# Optimizing a Trainium2 kernel written in Bass

```python
import math
import jax, jax.numpy as jnp
from jax import lax
import numpy as np

D_MODEL = 1024
BATCH = 8
SEQ = 4096
DEPTH = 1

PLE_DIM = 256
NORM_EPS = 1e-6

MLA_HEADS = 8
MLA_Q_RANK = 256
MLA_KV_RANK = 128
MLA_NOPE_DIM = 64
MLA_ROPE_DIM = 32
MLA_V_DIM = 64
MLA_OUT = MLA_HEADS * MLA_V_DIM
ROPE_THETA = 10000.0
Q_BLOCK = 128

SSD_HEADS = 8
SSD_HEAD_DIM = 64
SSD_GROUPS = 2
SSD_HEADS_PER_GROUP = SSD_HEADS // SSD_GROUPS
SSD_STATE = 64
SSD_CONV = 5
SSD_CHUNK = 128
SSD_INNER = SSD_HEADS * SSD_HEAD_DIM
SSD_XBC = SSD_INNER + 2 * SSD_GROUPS * SSD_STATE
SSD_DIRECTIONS = 2

D_MIX = MLA_OUT + SSD_INNER
IN_PROJ_COLS = MLA_Q_RANK + MLA_KV_RANK + MLA_ROPE_DIM + SSD_INNER + SSD_XBC + SSD_DIRECTIONS * SSD_HEADS

N_EXPERT_GROUPS = 4
EXPERTS_PER_GROUP = 8
N_EXPERTS = N_EXPERT_GROUPS * EXPERTS_PER_GROUP
TOP_K_IN_GROUP = 2
D_EXPERT = 256

kernel_name = "hybrid_mla_ssd_hmoe_ple_encoder"


def rmsnorm(x, w):
    xf = x.astype(jnp.float32)
    xf = xf * lax.rsqrt(jnp.mean(xf * xf, axis=-1, keepdims=True) + NORM_EPS)
    return xf.astype(x.dtype) * w


def rotary(t, cos, sin):
    half = t.shape[-1] // 2
    t1, t2 = t[..., :half], t[..., half:]
    return jnp.concatenate([t1 * cos - t2 * sin, t2 * cos + t1 * sin], axis=-1).astype(t.dtype)


def rotary_tables(positions):
    inv_freq = 1.0 / (ROPE_THETA ** (jnp.arange(0, MLA_ROPE_DIM, 2, dtype=jnp.float32) / MLA_ROPE_DIM))
    ang = positions.astype(jnp.float32)[..., None] * inv_freq
    return jnp.cos(ang), jnp.sin(ang)


def mla_bidirectional_attention(q_nope, q_rope, k_nope, k_rope, v):
    b, s, h, _ = q_nope.shape
    nb = s // Q_BLOCK
    scale = (MLA_NOPE_DIM + MLA_ROPE_DIM) ** -0.5

    def to_blocks(t):
        return jnp.moveaxis(t.reshape(b, nb, Q_BLOCK, *t.shape[2:]), 1, 0)

    def block(qs):
        qn, qr = qs
        sc = (jnp.einsum("bqhd,bkhd->bhqk", qn, k_nope)
              + jnp.einsum("bqhr,bkr->bhqk", qr, k_rope))
        pr = jax.nn.softmax(sc.astype(jnp.float32) * scale, axis=-1).astype(v.dtype)
        return jnp.einsum("bhqk,bkhd->bqhd", pr, v)

    out = lax.map(block, (to_blocks(q_nope), to_blocks(q_rope)))
    return jnp.moveaxis(out, 0, 1).reshape(b, s, h * MLA_V_DIM)


def centred_depthwise_conv(x, w, bias):
    c = x.shape[-1]
    out = lax.conv_general_dilated(
        x, w[:, None, :].astype(x.dtype), window_strides=(1,),
        padding=[(SSD_CONV // 2, SSD_CONV // 2)],
        dimension_numbers=("NWC", "WIO", "NWC"), feature_group_count=c)
    return out + bias


def ssd_chunked_scan(xh, dt, a, bm, cm):
    b, s, g, r, p = xh.shape
    n = bm.shape[-1]
    c = s // SSD_CHUNK
    X = (xh * dt[..., None]).reshape(b, c, SSD_CHUNK, g, r, p)
    A = (dt * a).reshape(b, c, SSD_CHUNK, g, r).transpose(0, 3, 4, 1, 2)
    Bc = bm.reshape(b, c, SSD_CHUNK, g, n)
    Cc = cm.reshape(b, c, SSD_CHUNK, g, n)
    a_cs = jnp.cumsum(A, axis=-1)
    tri = jnp.tril(jnp.ones((SSD_CHUNK, SSD_CHUNK), dtype=bool))
    seg = a_cs[..., :, None] - a_cs[..., None, :]
    Lmat = jnp.exp(jnp.where(tri, seg, -jnp.inf))
    y_diag = jnp.einsum("bclgn,bcsgn,bgrcls,bcsgrp->bclgrp", Cc, Bc, Lmat, X)
    decay_states = jnp.exp(a_cs[..., -1:] - a_cs)
    states = jnp.einsum("bclgn,bgrcl,bclgrp->bcgrpn", Bc, decay_states, X)
    chunk_decay = jnp.exp(a_cs[..., -1])

    def step(hstate, inp):
        st, dc = inp
        return hstate * dc[..., None, None] + st, hstate

    _, h_prev = lax.scan(step, jnp.zeros_like(states[:, 0]),
                         (jnp.moveaxis(states, 1, 0), jnp.moveaxis(chunk_decay, -1, 0)))
    h_prev = jnp.moveaxis(h_prev, 0, 1)
    y_off = jnp.einsum("bclgn,bcgrpn,bgrcl->bclgrp", Cc, h_prev, jnp.exp(a_cs))
    return (y_diag + y_off).reshape(b, s, g, r, p)


def hybrid_token_mixer(h, cos, sin, w_in, q_norm_w, w_uq, kv_norm_w, w_ukv, attn_out_norm_w,
                       conv_w, conv_b, dt_bias, a_log, ssd_d, ssd_norm_w, w_o):
    b, s, _ = h.shape
    proj = h @ w_in
    o1 = MLA_Q_RANK
    o2 = o1 + MLA_KV_RANK
    o3 = o2 + MLA_ROPE_DIM
    o4 = o3 + SSD_INNER
    o5 = o4 + SSD_XBC
    c_q, c_kv, k_rope, z, xbc, dt_raw = jnp.split(proj, [o1, o2, o3, o4, o5], axis=-1)

    q = (rmsnorm(c_q, q_norm_w) @ w_uq).reshape(b, s, MLA_HEADS, MLA_NOPE_DIM + MLA_ROPE_DIM)
    q_nope = q[..., :MLA_NOPE_DIM]
    q_rope = rotary(q[..., MLA_NOPE_DIM:], cos[:, :, None, :], sin[:, :, None, :])
    kv = (rmsnorm(c_kv, kv_norm_w) @ w_ukv).reshape(b, s, MLA_HEADS, MLA_NOPE_DIM + MLA_V_DIM)
    k_nope, v = kv[..., :MLA_NOPE_DIM], kv[..., MLA_NOPE_DIM:]
    k_rope = rotary(k_rope, cos, sin)
    attn = mla_bidirectional_attention(q_nope, q_rope, k_nope, k_rope, v)
    attn = rmsnorm(attn, attn_out_norm_w)

    xbc = jax.nn.silu(centred_depthwise_conv(xbc, conv_w, conv_b))
    xs, bm, cm = jnp.split(xbc, [SSD_INNER, SSD_INNER + SSD_GROUPS * SSD_STATE], axis=-1)
    xh = xs.reshape(b, s, SSD_GROUPS, SSD_HEADS_PER_GROUP, SSD_HEAD_DIM)
    bm = bm.reshape(b, s, SSD_GROUPS, SSD_STATE)
    cm = cm.reshape(b, s, SSD_GROUPS, SSD_STATE)
    dt = jax.nn.softplus(
        (dt_raw.reshape(b, s, SSD_DIRECTIONS, SSD_GROUPS, SSD_HEADS_PER_GROUP)
         + dt_bias.reshape(SSD_DIRECTIONS, SSD_GROUPS, SSD_HEADS_PER_GROUP)).astype(jnp.float32))
    a = -jnp.exp(a_log.astype(jnp.float32)).reshape(SSD_DIRECTIONS, SSD_GROUPS, SSD_HEADS_PER_GROUP)
    y_fwd = ssd_chunked_scan(xh, dt[:, :, 0], a[0], bm, cm)
    y_bwd = jnp.flip(ssd_chunked_scan(jnp.flip(xh, 1), jnp.flip(dt[:, :, 1], 1), a[1],
                                      jnp.flip(bm, 1), jnp.flip(cm, 1)), 1)
    y = y_fwd + y_bwd + xh * ssd_d.reshape(SSD_GROUPS, SSD_HEADS_PER_GROUP)[..., None]
    y = y.reshape(b, s, SSD_INNER).astype(h.dtype)
    y = rmsnorm(y * jax.nn.silu(z), ssd_norm_w)

    return jnp.concatenate([attn, y], axis=-1) @ w_o


def hierarchical_moe(h, w_router_group, b_router_group, w_router_expert, b_router_expert,
                     w_exp_gate, w_exp_up, w_exp_down):
    b, s, _ = h.shape
    group_logits = (h @ w_router_group).astype(jnp.float32) + b_router_group
    group_prob = jax.nn.softmax(group_logits, axis=-1)
    g_idx = jnp.argmax(group_logits, axis=-1)
    g_weight = jnp.max(group_prob, axis=-1, keepdims=True)
    exp_logits = ((h @ w_router_expert).astype(jnp.float32) + b_router_expert).reshape(
        b, s, N_EXPERT_GROUPS, EXPERTS_PER_GROUP)
    sel_logits = jnp.einsum("bsge,bsg->bse", exp_logits,
                            jax.nn.one_hot(g_idx, N_EXPERT_GROUPS, dtype=jnp.float32))
    top_v, top_i = lax.top_k(sel_logits, TOP_K_IN_GROUP)
    w_k = jax.nn.softmax(top_v, axis=-1) * g_weight
    e_idx = g_idx[..., None] * EXPERTS_PER_GROUP + top_i
    combine = jnp.einsum("bsk,bske->bse", w_k,
                         jax.nn.one_hot(e_idx, N_EXPERTS, dtype=jnp.float32)).astype(h.dtype)
    y = jnp.zeros_like(h)
    for e in range(N_EXPERTS):
        he = jax.nn.silu(h @ w_exp_gate[e]) * (h @ w_exp_up[e])
        y = y + combine[..., e:e + 1] * (he @ w_exp_down[e])
    return y


def setup_inputs(seed: int = 0) -> dict:
    key = jax.random.key(seed)
    ks = jax.random.split(key, 40)
    f32 = jnp.float32

    def nrm(k, shape, fan_in):
        return jax.random.normal(k, shape, f32) * fan_in ** -0.5

    def gain(k, shape):
        return 1.0 + 0.02 * jax.random.normal(k, shape, f32)

    L = DEPTH
    x = jax.random.normal(ks[0], (BATCH, SEQ, D_MODEL), f32)
    p = jax.random.normal(ks[1], (DEPTH, BATCH, SEQ, PLE_DIM), f32)
    offsets = jax.random.randint(ks[2], (BATCH, 1), 0, 1024, dtype=jnp.int32)
    positions = (jnp.arange(SEQ, dtype=jnp.int32)[None, :] + offsets).astype(jnp.int32)

    dt_init = jnp.exp(jax.random.uniform(ks[14], (L, SSD_DIRECTIONS * SSD_HEADS), f32)
                      * (math.log(0.1) - math.log(0.001)) + math.log(0.001))
    dt_bias = dt_init + jnp.log(-jnp.expm1(-dt_init))
    a_log = jnp.log(jax.random.uniform(ks[15], (L, SSD_DIRECTIONS * SSD_HEADS), f32, 1.0, 16.0))

    return {
        "x": x,
        "p": p,
        "positions": positions,
        "attn_norm_w": gain(ks[3], (L, D_MODEL)),
        "w_in": nrm(ks[4], (L, D_MODEL, IN_PROJ_COLS), D_MODEL),
        "q_norm_w": gain(ks[5], (L, MLA_Q_RANK)),
        "w_uq": nrm(ks[6], (L, MLA_Q_RANK, MLA_HEADS * (MLA_NOPE_DIM + MLA_ROPE_DIM)), MLA_Q_RANK),
        "kv_norm_w": gain(ks[7], (L, MLA_KV_RANK)),
        "w_ukv": nrm(ks[8], (L, MLA_KV_RANK, MLA_HEADS * (MLA_NOPE_DIM + MLA_V_DIM)), MLA_KV_RANK),
        "attn_out_norm_w": gain(ks[9], (L, MLA_OUT)),
        "conv_w": nrm(ks[10], (L, SSD_CONV, SSD_XBC), SSD_CONV),
        "conv_b": 0.02 * jax.random.normal(ks[11], (L, SSD_XBC), f32),
        "dt_bias": dt_bias,
        "a_log": a_log,
        "ssd_d": 1.0 + 0.1 * jax.random.normal(ks[12], (L, SSD_HEADS), f32),
        "ssd_norm_w": gain(ks[13], (L, SSD_INNER)),
        "w_o": nrm(ks[16], (L, D_MIX, D_MODEL), D_MIX),
        "ffn_norm_w": gain(ks[17], (L, D_MODEL)),
        "w_router_group": nrm(ks[18], (L, D_MODEL, N_EXPERT_GROUPS), D_MODEL),
        "b_router_group": 0.01 * jax.random.normal(ks[19], (L, N_EXPERT_GROUPS), f32),
        "w_router_expert": nrm(ks[20], (L, D_MODEL, N_EXPERTS), D_MODEL),
        "b_router_expert": 0.01 * jax.random.normal(ks[21], (L, N_EXPERTS), f32),
        "w_exp_gate": nrm(ks[22], (L, N_EXPERTS, D_MODEL, D_EXPERT), D_MODEL),
        "w_exp_up": nrm(ks[23], (L, N_EXPERTS, D_MODEL, D_EXPERT), D_MODEL),
        "w_exp_down": nrm(ks[24], (L, N_EXPERTS, D_EXPERT, D_MODEL), D_EXPERT),
        "ple_norm_w": gain(ks[25], (L, D_MODEL)),
        "w_ple_gate": nrm(ks[26], (L, D_MODEL, D_MODEL), D_MODEL),
        "b_ple_gate": 0.02 * jax.random.normal(ks[27], (L, D_MODEL), f32),
        "w_ple_proj": nrm(ks[28], (L, PLE_DIM, D_MODEL), PLE_DIM),
        "ple_post_norm_w": gain(ks[29], (L, D_MODEL)),
        "final_norm_w": gain(ks[30], (D_MODEL,)),
    }


def reference(x, p, positions, attn_norm_w, w_in, q_norm_w, w_uq, kv_norm_w, w_ukv,
              attn_out_norm_w, conv_w, conv_b, dt_bias, a_log, ssd_d, ssd_norm_w, w_o,
              ffn_norm_w, w_router_group, b_router_group, w_router_expert, b_router_expert,
              w_exp_gate, w_exp_up, w_exp_down, ple_norm_w, w_ple_gate, b_ple_gate,
              w_ple_proj, ple_post_norm_w, final_norm_w):
    cos, sin = rotary_tables(positions)
    cos = cos.astype(x.dtype)
    sin = sin.astype(x.dtype)
    for i in range(DEPTH):
        h = rmsnorm(x, attn_norm_w[i])
        x = x + hybrid_token_mixer(h, cos, sin, w_in[i], q_norm_w[i], w_uq[i], kv_norm_w[i],
                                   w_ukv[i], attn_out_norm_w[i], conv_w[i], conv_b[i],
                                   dt_bias[i], a_log[i], ssd_d[i], ssd_norm_w[i], w_o[i])
        h = rmsnorm(x, ffn_norm_w[i])
        x = x + hierarchical_moe(h, w_router_group[i], b_router_group[i], w_router_expert[i],
                                 b_router_expert[i], w_exp_gate[i], w_exp_up[i], w_exp_down[i])
        gate = jax.nn.sigmoid(rmsnorm(x, ple_norm_w[i]) @ w_ple_gate[i] + b_ple_gate[i])
        ple = rmsnorm(p[i] @ w_ple_proj[i], ple_post_norm_w[i])
        x = x + gate * ple
    return rmsnorm(x, final_norm_w)
```

```python
import numpy as np
import concourse.bass as bass
import concourse.mybir as mybir
from concourse.bass_utils import run_bass_kernel_spmd

F32 = mybir.dt.float32
BF16 = mybir.dt.bfloat16
I32 = mybir.dt.int32
AF = mybir.ActivationFunctionType
ALU = mybir.AluOpType
AX = mybir.AxisListType

ENGS = ("pe", "act", "dve", "pool", "sp")


class Buf:
    __slots__ = ("name", "w", "r")

    def __init__(self, name):
        self.name = name
        self.w = None
        self.r = []


class _Op:
    __slots__ = ("eng", "fn", "deps", "dma_key", "signal", "count", "kind")

    def __init__(self, eng, fn, deps, dma_key, kind):
        self.eng = eng
        self.fn = fn
        self.deps = deps
        self.dma_key = dma_key
        self.signal = False
        self.count = 0
        self.kind = kind


class Sched:
    def __init__(self):
        self.ops = []
        self.bufs = {}
        self.psum_names = set()

    def buf(self, name):
        b = self.bufs.get(name)
        if b is None:
            b = self.bufs[name] = Buf(name)
        return b

    def _norm(self, xs):
        out = []
        for x in xs:
            if x is None:
                continue
            out.append(self.buf(x) if isinstance(x, str) else x)
        return out

    def op(self, eng, fn, reads=(), writes=(), dma_key=None):
        reads = self._norm(reads)
        writes = self._norm(writes)
        deps = set()
        for b in reads:
            if b.w is not None:
                deps.add(b.w)
            if b.name in self.psum_names:
                deps.update(r for r in b.r if self.ops[r].eng != eng)
        for b in writes:
            if b.w is not None:
                deps.add(b.w)
            deps.update(b.r)
        if eng == "pe":
            deps = set(d for d in deps if self.ops[d].eng != "pe")
        i = len(self.ops)
        self.ops.append(_Op(eng, fn, deps, dma_key, "dma" if dma_key else "op"))
        for b in reads:
            b.r.append(i)
        for b in writes:
            b.w = i
            b.r = []
        return i

    def dma(self, eng, fn, key, reads=(), writes=()):
        return self.op(eng, fn, reads, writes, dma_key=eng + "_" + key)

    def barrier(self):
        self.ops.append(_Op(None, None, set(), None, "barrier"))

    def emit(self, nc, engines):
        ops = self.ops
        last = {e: None for e in ENGS}
        pend_dma = []
        bar_deps = {}
        for i, o in enumerate(ops):
            if o.kind == "barrier":
                d = set(v for v in last.values() if v is not None)
                d.update(pend_dma)
                bar_deps[i] = d
                pend_dma = []
            else:
                last[o.eng] = i
                if o.kind == "dma":
                    pend_dma.append(i)
        for i, o in enumerate(ops):
            for d in o.deps:
                if ops[d].kind == "op":
                    ops[d].signal = True
        for d in bar_deps.values():
            for j in d:
                if ops[j].kind == "op":
                    ops[j].signal = True
        cnt = {e: 0 for e in ENGS}
        dcnt = {}
        for o in ops:
            if o.kind == "op" and o.signal:
                cnt[o.eng] += 1
                o.count = cnt[o.eng]
            elif o.kind == "dma":
                dcnt[o.dma_key] = dcnt.get(o.dma_key, 0) + 16
                o.count = dcnt[o.dma_key]
        sem_names = ["e_" + e for e in ENGS] + ["d_" + k for k in dcnt]
        import contextlib
        with contextlib.ExitStack() as st:
            sems = {n: st.enter_context(nc.semaphore(n)) for n in sem_names}
            seen = {e: {} for e in ENGS}
            streams = {e: [] for e in ENGS}

            def need(e, d):
                p = ops[d]
                name = ("d_" + p.dma_key) if p.kind == "dma" else ("e_" + p.eng)
                if seen[e].get(name, 0) >= p.count:
                    return
                seen[e][name] = p.count
                streams[e].append(("wait", name, p.count))

            for i, o in enumerate(ops):
                if o.kind == "barrier":
                    best = {}
                    for d in bar_deps[i]:
                        p = ops[d]
                        name = ("d_" + p.dma_key) if p.kind == "dma" else ("e_" + p.eng)
                        if name not in best or ops[best[name]].count < p.count:
                            best[name] = d
                    for e in ENGS:
                        for d in sorted(best.values()):
                            need(e, d)
                    continue
                for d in sorted(o.deps):
                    need(o.eng, d)
                streams[o.eng].append(("op", o))
            self.streams = streams
            block = st.enter_context(nc.Block())

            def run(e, eng):
                for it in streams[e]:
                    if it[0] == "wait":
                        eng.wait_ge(sems[it[1]], it[2])
                    else:
                        o = it[1]
                        ins = o.fn(eng)
                        if o.kind == "dma":
                            ins.then_inc(sems["d_" + o.dma_key], 16)
                        elif o.signal:
                            ins.then_inc(sems["e_" + o.eng], 1)

            @block.tensor
            def _(eng):
                run("pe", eng)

            @block.scalar
            def _(eng):
                run("act", eng)

            @block.vector
            def _(eng):
                run("dve", eng)

            @block.gpsimd
            def _(eng):
                run("pool", eng)

            @block.sync
            def _(eng):
                run("sp", eng)
        return {e: len(streams[e]) for e in ENGS}


D_MODEL = 1024
PLE_DIM = 256
EPS = 1e-6
NH = 8
QR, KVR, ROPE, NOPE, VD = 256, 128, 32, 64, 64
SH, SP_, SG, SN, SCONV = 8, 64, 2, 64, 5
SINNER, SXBC = 512, 768
INCOLS = 1712
NEXP, NGRP, EPG, DEXP = 32, 4, 8, 256
C_CQ, C_CKV, C_KR, C_Z, C_XBC, C_DT = 0, 256, 384, 416, 928, 1696

WEIGHT_SPECS = [
    ("attn_norm_w", [1024]), ("w_in", [1024, 1712]), ("q_norm_w", [256]), ("w_uq", [256, 768]),
    ("kv_norm_w", [128]), ("w_ukv", [128, 1024]), ("attn_out_norm_w", [512]), ("conv_w", [5, 768]),
    ("conv_b", [768]), ("dt_bias", [1, 16]), ("a_log", [1, 16]), ("ssd_d", [1, 8]), ("ssd_norm_w", [512]),
    ("w_o", [1024, 1024]), ("ffn_norm_w", [1024]), ("w_router_group", [1024, 4]), ("b_router_group", [1, 4]),
    ("w_router_expert", [1024, 32]), ("b_router_expert", [1, 32]), ("w_exp_gate", [32 * 1024, 256]),
    ("w_exp_up", [32 * 1024, 256]), ("w_exp_down", [32 * 256, 1024]), ("ple_norm_w", [1024]),
    ("w_ple_gate", [1024, 1024]), ("b_ple_gate", [1, 1024]), ("w_ple_proj", [256, 1024]),
    ("ple_post_norm_w", [1024]), ("final_norm_w", [1024]),
]


class KB:
    def __init__(self, nc):
        import contextlib
        self.nc = nc
        self.S = Sched()
        self.root = contextlib.ExitStack()
        self.stack = [self.root]

    def push(self):
        import contextlib
        st = contextlib.ExitStack()
        self.stack.append(st)
        return st

    def pop(self):
        self.stack.pop().close()

    def sb(self, name, shape, dt):
        return self.stack[-1].enter_context(self.nc.sbuf_tensor(name, list(shape), dt))

    def ps(self, name, shape, dt=F32):
        self.S.psum_names.add(name)
        return self.stack[-1].enter_context(self.nc.psum_tensor(name, list(shape), dt))

    def act(self, out, in_, func, r, w, **kw):
        self.S.op("act", lambda e: e.activation(out=out, in_=in_, func=func, **kw), r, w)

    def ts(self, eng, out, in0, s1, s2, op0, op1, r, w):
        if op1 is None:
            self.S.op(eng, lambda e: e.tensor_scalar(out=out, in0=in0, scalar1=s1, scalar2=None, op0=op0), r, w)
        else:
            self.S.op(eng, lambda e: e.tensor_scalar(out=out, in0=in0, scalar1=s1, scalar2=s2, op0=op0, op1=op1), r, w)

    def tt(self, eng, out, in0, in1, op, r, w):
        self.S.op(eng, lambda e: e.tensor_tensor(out=out, in0=in0, in1=in1, op=op), r, w)

    def stt(self, out, in0, scalar, in1, op0, op1, r, w):
        self.S.op("dve", lambda e: e.scalar_tensor_tensor(out=out, in0=in0, scalar=scalar, in1=in1, op0=op0, op1=op1), r, w)

    def cp(self, eng, out, in_, r, w):
        if eng == "act":
            self.S.op("act", lambda e: e.activation(out=out, in_=in_, func=AF.Copy), r, w)
        else:
            self.S.op(eng, lambda e: e.tensor_copy(out=out, in_=in_), r, w)

    def memset(self, eng, ap, val, w):
        self.S.op(eng, lambda e: e.memset(ap, val), (), w)

    def mm(self, out, lhsT, rhs, start, stop, r, w, **kw):
        self.S.op("pe", lambda e: e.matmul(out, lhsT=lhsT, rhs=rhs, start=start, stop=stop, **kw), r, w)

    def tr(self, out, in_, ident, r, w):
        self.S.op("pe", lambda e: e.transpose(out=out, in_=in_, identity=ident), r, w)

    def dma(self, eng, out, in_, key, r, w, **kw):
        self.S.dma(eng, lambda e: e.dma_start(out=out, in_=in_, **kw), key, r, w)

    def barrier(self):
        self.S.barrier()

    def reduce(self, out, in_, op, r, w):
        self.S.op("dve", lambda e: e.tensor_reduce(out=out, in_=in_, axis=AX.X, op=op), r, w)

    def recip(self, out, in_, r, w):
        self.S.op("dve", lambda e: e.reciprocal(out=out, in_=in_), r, w)

    def max8(self, out, in_, r, w):
        self.S.op("dve", lambda e: e.max(out=out, in_=in_), r, w)


def build(S=4096, stop_after=None, serial_scatter=False):
    import math
    nc = bass.Bass("TRN2", target_bir_lowering=False)
    NT = S // 128
    GS = min(512, S)
    NG = S // GS
    TG = GS // 128
    D = {}
    D["x"] = nc.dram_tensor("x", [S, 1024], F32, kind="ExternalInput").ap()
    D["p"] = nc.dram_tensor("p", [S, 256], F32, kind="ExternalInput").ap()
    D["pos"] = nc.dram_tensor("pos", [NT, 128], I32, kind="ExternalInput").ap()
    for name, shape in WEIGHT_SPECS:
        shp = shape if len(shape) == 2 else [1, shape[0]]
        D[name] = nc.dram_tensor(name, shp, F32, kind="ExternalInput").ap()
    OUT = nc.dram_tensor("out", [S, 1024], F32, kind="ExternalOutput").ap()

    def scr(name, shape, dt):
        return nc.dram_tensor(name, shape, dt).ap()

    S_cqn = scr("S_cqn", [256, S], BF16)
    S_kvn = scr("S_kvn", [128, S], BF16)
    S_sz = scr("S_sz", [512, S], BF16)
    S_xbc = scr("S_xbc", [768, S], F32)
    S_xtm = scr("S_xtm", [S, 768], BF16)
    S_bcT = scr("S_bcT", [256, S], BF16)
    S_yf = scr("S_yf", [512, S], F32)
    S_yn = scr("S_yn", [512, S], BF16)

    import os
    KD = int(os.environ.get("KDBG", "0"))
    kb = KB(nc)
    Sd = kb.S
    mult, add, sub = ALU.mult, ALU.add, ALU.subtract
    _regs = {}

    def BC(e, val):
        if val not in _regs:
            _regs[val] = e.to_reg(val)
        return _regs[val]

    VROWS = [("anw", "attn_norm_w", 8), ("cb", "conv_b", 6), ("cw", "conv_w", 30), ("kvw", "kv_norm_w", 1),
             ("qw", "q_norm_w", 2), ("pnw", "ple_norm_w", 8)]
    voff = {}
    _o = 0
    for nm, _, k in VROWS:
        voff[nm] = (_o, k)
        _o += k
    NV = _o

    identf = kb.sb("identf", [128, 128], F32)
    ident = kb.sb("ident", [128, 128], BF16)
    onesf = kb.sb("onesf", [128, 128], F32)
    onesb = kb.sb("onesb", [128, 128], BF16)
    kb.memset("pool", identf[:], 0.0, ["identf"])
    Sd.op("pool", lambda e: e.affine_select(out=identf[:], in_=identf[:], pattern=[[-1, 128]],
                                            compare_op=ALU.not_equal, fill=1.0, base=0,
                                            channel_multiplier=1), ["identf"], ["identf"])
    kb.cp("dve", ident[:], identf[:], ["identf"], ["ident"])
    kb.memset("pool", onesf[:], 1.0, ["onesf"])
    c_eps = kb.sb("c_eps", [128, 1], F32)
    c_one = kb.sb("c_one", [128, 1], F32)
    kb.memset("pool", c_eps[:], EPS, ["c_eps"])
    kb.memset("pool", c_one[:], 1.0, ["c_one"])
    kb.memset("pool", onesb[:], 1.0, ["onesb"])

    vecs = kb.sb("vecs", [128, NV], F32)
    vecs64 = kb.sb("vecs64", [64, 16], F32)
    kb.push()
    vst = kb.sb("vst", [NV, 128], F32)
    vst64 = kb.sb("vst64", [16, 64], F32)
    pvec = kb.ps("pvec", [128, 512], F32)
    for nm, dn, k in VROWS:
        o_ = voff[nm][0]
        src = D[dn]
        src = src.rearrange("o (k p) -> (o k) p", p=128) if dn != "conv_w" else src.rearrange("t (j p) -> (t j) p", p=128)
        kb.dma("sp", vst[o_:o_ + k, :], src, "vst", [], ["vst"])
    kb.dma("sp", vst64[0:8, :], D["attn_out_norm_w"].rearrange("o (k p) -> (o k) p", p=64), "vst64", [], ["vst64"])
    kb.dma("sp", vst64[8:16, :], D["ssd_norm_w"].rearrange("o (k p) -> (o k) p", p=64), "vst64", [], ["vst64"])
    kb.tr(pvec[:, 0:NV], vst[:], identf[:NV, :NV], ["vst", "identf"], ["pvec"])
    kb.cp("dve", vecs[:], pvec[:, 0:NV], ["pvec"], ["vecs"])
    kb.tr(pvec[0:64, 64:80], vst64[:], identf[:16, :16], ["vst64", "identf", "vecs"], ["pvec"])
    kb.cp("dve", vecs64[:], pvec[0:64, 64:80], ["pvec"], ["vecs64"])
    kb.barrier()
    kb.pop()

    def vec_pk(name, dram=None, k=None):
        o_, k_ = voff[name]
        return vecs[:, o_:o_ + k_]

    cosT = kb.sb("cosT", [128, NT, 16], F32)
    sinT = kb.sb("sinT", [128, NT, 16], F32)
    krot = kb.sb("krot", [128, NT, 32], F32)
    dtp = kb.sb("dtp", [128, NT, 16], F32)
    dtb_bc = kb.sb("dtb_bc", [128, 16], F32)
    a_bc = kb.sb("a_bc", [128, 16], F32)
    kb.dma("sp", dtb_bc[:], D["dt_bias"].partition_broadcast(128), "dtb_bc", [], ["dtb_bc"])
    kb.dma("sp", a_bc[:], D["a_log"].partition_broadcast(128), "a_bc", [], ["a_bc"])
    kb.act(a_bc[:], a_bc[:], AF.Exp, ["a_bc"], ["a_bc"])
    kb.ts("dve", a_bc[:], a_bc[:], -1.0, None, mult, None, ["a_bc"], ["a_bc"])
    kb.push()
    posi = kb.sb("posi", [NT, 128], I32)
    posr = kb.sb("posr", [NT, 128], F32)
    posf = kb.sb("posf", [128, NT], F32)
    invf = kb.sb("invf", [128, 16], F32)
    rr = kb.sb("rr", [128, NT, 16], F32)
    rf = kb.sb("rf", [128, NT, 16], F32)
    ri = kb.sb("ri", [128, NT, 16], I32)
    rm = kb.sb("rm", [128, NT, 16], F32)
    ppos = kb.ps("ppos", [128, 512], F32)
    kb.dma("sp", posi[:], D["pos"], "posi", [], ["posi"])
    kb.cp("dve", posr[:], posi[:], ["posi"], ["posr"])
    kb.tr(ppos[:, :NT], posr[:], identf[:NT, :NT], ["posr", "identf"], ["ppos"])
    kb.cp("dve", posf[:], ppos[:, :NT], ["ppos"], ["posf"])
    for i in range(16):
        kb.memset("pool", invf[:, i:i + 1], (10000.0 ** (-(2.0 * i) / 32.0)) / (2 * math.pi), ["invf"])
    for t in range(NT):
        kb.ts("dve", rr[:, t, :], invf[:], posf[:, t:t + 1], None, mult, None, ["invf", "posf"], ["rr"])
    for shift, dst in ((0.0, "sinT"), (0.25, "cosT")):
        dstt = sinT if dst == "sinT" else cosT
        if shift:
            kb.ts("dve", rr[:], rr[:], shift, None, add, None, ["rr"], ["rr"])
        kb.cp("dve", ri[:], rr[:], ["rr"], ["ri"])
        kb.cp("dve", rf[:], ri[:], ["ri"], ["rf"])
        kb.tt("dve", rf[:], rr[:], rf[:], sub, ["rr", "rf"], ["rf"])
        kb.ts("dve", rm[:], rf[:], 0.5, None, ALU.is_gt, None, ["rf"], ["rm"])
        kb.tt("dve", rf[:], rf[:], rm[:], sub, ["rf", "rm"], ["rf"])
        kb.ts("dve", rm[:], rf[:], -0.5, None, ALU.is_lt, None, ["rf"], ["rm"])
        kb.tt("dve", rf[:], rf[:], rm[:], add, ["rf", "rm"], ["rf"])
        kb.act(dstt[:], rf[:], AF.Sin, ["rf"], [dst], scale=2 * math.pi)
    kb.barrier()
    kb.pop()

    if stop_after == "P0":
        return nc, kb
    kb.push()
    anw = vec_pk("anw")
    w_in_bf = kb.sb("w_in_bf", [128, 8, INCOLS], BF16)
    wst = [kb.sb(f"wst{i}", [128, INCOLS], F32) for i in range(2)]
    for k in range(8):
        kb.dma("sp", wst[k % 2][:], D["w_in"][k * 128:(k + 1) * 128, :], f"wst{k % 2}", [], [f"wst{k % 2}"])
        kb.ts("dve" if k % 2 == 0 else "pool", w_in_bf[:, k, :], wst[k % 2][:], anw[:, k:k + 1], None, mult, None,
              [f"wst{k % 2}", "vecs"], ["w_in_bf"])
    wsmall = kb.sb("wsmall", [128, 8, 48], BF16)
    kb.cp("dve", wsmall[:, :, 0:32], w_in_bf[:, :, C_KR:C_KR + 32], ["w_in_bf"], ["wsmall"])
    kb.cp("dve", wsmall[:, :, 32:48], w_in_bf[:, :, C_DT:C_DT + 16], ["w_in_bf"], ["wsmall"])

    NX = 3
    xt = [kb.sb(f"xt{i}", [128, 1024], F32) for i in range(NX)]
    hn = [kb.sb(f"hn{i}", [128, 1024], BF16) for i in range(NX)]
    ss = [kb.sb(f"ss{i}", [128, 1], F32) for i in range(NX)]
    junk = kb.sb("junk", [128, 1024], BF16)
    hT = [kb.sb(f"hT{i}", [128, 8, GS], BF16) for i in range(2)]
    sm = kb.sb("sm", [128, 48], F32)
    rot = [kb.sb(f"rot{i}", [128, 16], F32) for i in range(4)]
    dx = kb.sb("dx", [128, 16], F32)
    cqf = kb.sb("cqf", [128, 2, GS], F32)
    kvf = kb.sb("kvf", [128, GS], F32)
    sq = [kb.sb(f"sq{i}", [128, GS], F32) for i in range(3)]
    rs = [kb.sb(f"rs{i}", [128, GS], F32) for i in range(2)]
    cqn = [kb.sb(f"cqn{i}", [128, 2, GS], BF16) for i in range(2)]
    kvn = [kb.sb(f"kvn{i}", [128, GS], BF16) for i in range(2)]
    szs = [kb.sb(f"szs{i}", [128, 4, GS], BF16) for i in range(2)]
    xbs = [kb.sb(f"xbs{i}", [128, 6, GS], F32) for i in range(2)]
    pT = [kb.ps(f"pT{i}", [128, 8, 128], BF16) for i in range(2)]
    pS = kb.ps("pS", [128, 512], F32)
    pM = [kb.ps(f"pM{i}", [128, 512], F32) for i in range(3)]
    pST = [kb.ps(f"pST{i}", [128, 512], F32) for i in range(2)]

    chunks = [("cq", 0, C_CQ), ("cq", 1, C_CQ + 128), ("ckv", 0, C_CKV)]
    chunks += [("z", i, C_Z + 128 * i) for i in range(4)]
    chunks += [("xbc", i, C_XBC + 128 * i) for i in range(6)]
    ci_glob = 0
    for g in range(NG):
        gp = g % 2
        for t in range(TG):
            tt_ = g * TG + t
            s = tt_ % NX
            pp = tt_ % 2
            kb.dma("sp", xt[s][:], D["x"][tt_ * 128:(tt_ + 1) * 128, :], f"xt{s}", [], [f"xt{s}"])
            kb.act(junk[:], xt[s][:], AF.Square, [f"xt{s}"], ["junk", f"ss{s}"], accum_out=ss[s][:])
            kb.ts("dve", ss[s][:], ss[s][:], 1.0 / 1024, EPS, mult, add, [f"ss{s}"], [f"ss{s}"])
            kb.act(ss[s][:], ss[s][:], AF.Ln, [f"ss{s}"], [f"ss{s}"])
            kb.act(ss[s][:], ss[s][:], AF.Exp, [f"ss{s}"], [f"ss{s}"], scale=-0.5)
            kb.ts("dve", hn[s][:], xt[s][:], ss[s][:], None, mult, None, [f"xt{s}", f"ss{s}"], [f"hn{s}"])
            for k in range(8):
                kb.tr(pT[pp][:, k, :], hn[s][:, k * 128:(k + 1) * 128], ident[:], [f"hn{s}", "ident"], [f"pT{pp}"])
            kb.cp("act" if t % 2 else "dve", hT[gp][:, :, t * 128:(t + 1) * 128], pT[pp][:], [f"pT{pp}"], [f"hT{gp}_{t}"])
            if KD and KD < 2:
                continue
            for k in range(8):
                kb.mm(pS[:, :48], hT[gp][:, k, t * 128:(t + 1) * 128], wsmall[:, k, :], k == 0, k == 7,
                      [f"hT{gp}_{t}", "wsmall"], ["pS"])
            kb.cp("dve", sm[:], pS[:, :48], ["pS"], ["sm"])
            if KD and KD < 3:
                continue
            k1, k2 = sm[:, 0:16], sm[:, 16:32]
            cs_, sn_ = cosT[:, tt_, :], sinT[:, tt_, :]
            kb.tt("pool", rot[0][:], k1, cs_, mult, ["sm", "cosT"], ["rot0"])
            kb.tt("pool", rot[1][:], k2, sn_, mult, ["sm", "sinT"], ["rot1"])
            kb.tt("pool", krot[:, tt_, 0:16], rot[0][:], rot[1][:], sub, ["rot0", "rot1"], ["krot"])
            kb.tt("pool", rot[2][:], k2, cs_, mult, ["sm", "cosT"], ["rot2"])
            kb.tt("pool", rot[3][:], k1, sn_, mult, ["sm", "sinT"], ["rot3"])
            kb.tt("pool", krot[:, tt_, 16:32], rot[2][:], rot[3][:], add, ["rot2", "rot3"], ["krot"])
            kb.tt("dve", dx[:], sm[:, 32:48], dtb_bc[:], add, ["sm", "dtb_bc"], ["dx"])
            kb.act(dx[:], dx[:], AF.Exp, ["dx"], ["dx"])
            kb.act(dtp[:, tt_, :], dx[:], AF.Ln, ["dx", "c_one"], ["dtp"], bias=c_one[:])
        hT_bufs = [f"hT{gp}_{t}" for t in range(TG)]
        for (kind, i, c0) in (chunks if not KD else chunks[:max(0, KD - 3)]):
            pi = ci_glob % 3
            ci_glob += 1
            pm = pM[pi]
            for k in range(8):
                kb.mm(pm[:, :GS], w_in_bf[:, k, c0:c0 + 128], hT[gp][:, k, :], k == 0, k == 7,
                      ["w_in_bf"] + hT_bufs, [f"pM{pi}"])
            KD2 = int(os.environ.get("KDBG2", "9"))
            if kind == "cq":
                if KD2 >= 1 and os.environ.get("KNODVE") != "1":
                    kb.cp("dve", cqf[:, i, :], pm[:, :GS], [f"pM{pi}"], [f"cqf{i}"])
                if KD2 >= 2:
                    if os.environ.get("KSQ") == "dve":
                        kb.cp("dve", sq[i][:], pm[:, :GS], [f"pM{pi}"], [f"sq{i}"])
                    elif os.environ.get("KSQ") == "junk":
                        kb.act(junk[:, :GS], pm[:, :GS], AF.Square, [f"pM{pi}"], ["junk"])
                    else:
                        kb.act(sq[i][:], pm[:, :GS], AF.Square, [f"pM{pi}"], [f"sq{i}"])
                if KD2 >= 3:
                    kb.mm(pST[0][:, :GS], onesf[:], sq[i][:], i == 0, i == 1, ["onesf", f"sq{i}"], ["pST0"])
                if i == 1:
                    kb.act(rs[0][:], pST[0][:, :GS], AF.Ln, ["pST0", "c_eps"], ["rs0"], scale=1.0 / 256, bias=c_eps[:])
                    kb.act(rs[0][:], rs[0][:], AF.Exp, ["rs0"], ["rs0"], scale=-0.5)
                    for j in range(2):
                        kb.tt("dve", cqn[gp][:, j, :], cqf[:, j, :], rs[0][:], mult, [f"cqf{j}", "rs0"], [f"cqn{gp}_{j}"])
                    kb.dma("sp", S_cqn.rearrange("(c p) s -> p c s", p=128)[:, :, g * GS:(g + 1) * GS], cqn[gp][:],
                           f"cqn{gp}", [f"cqn{gp}_0", f"cqn{gp}_1"], [])
            elif kind == "ckv":
                kb.cp("dve", kvf[:], pm[:, :GS], [f"pM{pi}"], ["kvf"])
                kb.act(sq[2][:], pm[:, :GS], AF.Square, [f"pM{pi}"], ["sq2"])
                kb.mm(pST[1][:, :GS], onesf[:], sq[2][:], True, True, ["onesf", "sq2"], ["pST1"])
                kb.act(rs[1][:], pST[1][:, :GS], AF.Ln, ["pST1", "c_eps"], ["rs1"], scale=1.0 / 128, bias=c_eps[:])
                kb.act(rs[1][:], rs[1][:], AF.Exp, ["rs1"], ["rs1"], scale=-0.5)
                kb.tt("dve", kvn[gp][:], kvf[:], rs[1][:], mult, ["kvf", "rs1"], [f"kvn{gp}"])
                kb.dma("sp", S_kvn[:, g * GS:(g + 1) * GS], kvn[gp][:], f"kvn{gp}", [f"kvn{gp}"], [])
            elif kind == "z":
                kb.act(szs[gp][:, i, :], pm[:, :GS], AF.Silu, [f"pM{pi}"], [f"szs{gp}_{i}"])
                if i == 3:
                    kb.dma("sp", S_sz.rearrange("(c p) s -> p c s", p=128)[:, :, g * GS:(g + 1) * GS], szs[gp][:],
                           f"szs{gp}", [f"szs{gp}_{j}" for j in range(4)], [])
            else:
                kb.cp("dve" if i % 2 else "act", xbs[gp][:, i, :], pm[:, :GS], [f"pM{pi}"], [f"xbs{gp}_{i}"])
                if i == 5:
                    kb.dma("sp", S_xbc.rearrange("(c p) s -> p c s", p=128)[:, :, g * GS:(g + 1) * GS], xbs[gp][:],
                           f"xbs{gp}", [f"xbs{gp}_{j}" for j in range(6)], [])
    kb.barrier()
    kb.pop()
    if stop_after == "P1":
        return nc, kb

    kb.push()
    cw = vec_pk("cw").rearrange("p (t j) -> p t j", j=6)
    cb = vec_pk("cb")
    xin = [kb.sb(f"xin{i}", [128, 6, GS + 4], F32) for i in range(2)]
    acc = [kb.sb(f"acc{i}", [128, 6, GS], F32) for i in range(2)]
    xc = [kb.sb(f"xc{i}", [128, 6, GS], BF16) for i in range(2)]
    xts = [kb.sb(f"xts{i}", [128, 768], BF16) for i in range(2)]
    pX = [kb.ps(f"pX{i}", [128, 6, 128], BF16) for i in range(2)]
    xbc_v = S_xbc.rearrange("(c p) s -> p c s", p=128)
    for g in range(NG):
        gp = g % 2
        lo, hi = g * GS - 2, g * GS + GS + 2
        clo, chi = max(lo, 0), min(hi, S)
        if lo < 0:
            kb.memset("pool", xin[gp][:, :, 0:2], 0.0, [f"xin{gp}"])
        if hi > S:
            kb.memset("pool", xin[gp][:, :, GS + 2:GS + 4], 0.0, [f"xin{gp}"])
        kb.dma("sp", xin[gp][:, :, clo - lo:chi - lo], xbc_v[:, :, clo:chi], f"xin{gp}", [], [f"xin{gp}"])
        for j in range(6):
            kb.ts("dve", acc[gp][:, j, :], xin[gp][:, j, 0:GS], cw[:, 0, j:j + 1], cb[:, j:j + 1], mult, add,
                  [f"xin{gp}", "vecs"], [f"acc{gp}_{j}"])
            for k in range(1, 5):
                kb.stt(acc[gp][:, j, :], xin[gp][:, j, k:k + GS], cw[:, k, j:j + 1], acc[gp][:, j, :], mult, add,
                       [f"xin{gp}", "vecs", f"acc{gp}_{j}"], [f"acc{gp}_{j}"])
            kb.act(xc[gp][:, j, :], acc[gp][:, j, :], AF.Silu, [f"acc{gp}_{j}"], [f"xc{gp}_{j}"])
        xcb = [f"xc{gp}_{j}" for j in range(6)]
        kb.dma("sp", S_bcT.rearrange("(j p) s -> p j s", p=128)[:, :, g * GS:(g + 1) * GS], xc[gp][:, 4:6, :],
               f"xc{gp}", xcb, [])
        for t in range(TG):
            tt_ = g * TG + t
            pp = tt_ % 2
            for j in range(6):
                kb.tr(pX[pp][:, j, :], xc[gp][:, j, t * 128:(t + 1) * 128], ident[:], [f"xc{gp}_{j}", "ident"], [f"pX{pp}"])
            kb.cp("act" if t % 2 else "dve", xts[pp][:], pX[pp][:], [f"pX{pp}"], [f"xts{pp}"])
            kb.dma("sp", S_xtm[tt_ * 128:(tt_ + 1) * 128, :], xts[pp][:], f"xts{pp}", [f"xts{pp}"], [])
    kb.barrier()
    kb.pop()
    if stop_after == "P1b":
        return nc, kb

    kb.push()
    tri = [kb.sb("triU", [128, 128], F32), kb.sb("triL", [128, 128], F32)]
    for d_, (cm_, pat) in enumerate(((-1, 1), (1, -1))):
        kb.memset("pool", tri[d_][:], 1.0, [f"tri{d_}"])
        Sd.op("pool", lambda e, d_=d_, cm_=cm_, pat=pat: e.affine_select(
            out=tri[d_][:], in_=tri[d_][:], pattern=[[pat, 128]], compare_op=ALU.is_ge, fill=0.0, base=0,
            channel_multiplier=cm_), [f"tri{d_}"], [f"tri{d_}"])
    Esel = kb.sb("Esel", [8, 8, 128], F32)
    kb.memset("pool", Esel[:], 0.0, ["Esel"])
    Sd.op("pool", lambda e: e.affine_select(out=Esel[:], in_=Esel[:], pattern=[[-1, 8], [0, 128]],
                                            compare_op=ALU.not_equal, fill=1.0, base=0, channel_multiplier=1),
          ["Esel"], ["Esel"])
    d_bc = kb.sb("d_bc", [128, 8], F32)
    kb.dma("sp", d_bc[:], D["ssd_d"].partition_broadcast(128), "d_bc", [], ["d_bc"])
    diagD = kb.sb("diagD", [128, 8, 128], BF16)
    for r in range(8):
        kb.ts("dve", diagD[:, r, :], identf[:], d_bc[:, r:r + 1], None, mult, None, ["identf", "d_bc"], ["diagD"])
    xtm = [kb.sb(f"xtm{i}", [128, 768], BF16) for i in range(2)]
    bcT = [kb.sb(f"bcT{i}", [64, 4, 128], BF16) for i in range(2)]
    A_ = kb.sb("A_", [128, 8], F32)
    cs_sb = kb.sb("cs_sb", [128, 8], F32)
    csT_sb = kb.sb("csT_sb", [8, 128], F32)
    seg = kb.sb("seg", [128, 8, 128], F32)
    GTm = kb.sb("GTm", [128, 2, 128], F32)
    MT = kb.sb("MT", [128, 8, 128], BF16)
    ecs = kb.sb("ecs", [64, 8, 128], F32)
    Cdec = kb.sb("Cdec", [64, 8, 128], BF16)
    wex = kb.sb("wex", [128, 8], F32)
    xw = kb.sb("xw", [128, 8, 64], BF16)
    xdt = kb.sb("xdt", [128, 8, 64], BF16)
    dec = kb.sb("dec", [64, 8], F32)
    hst = kb.sb("hst", [64, 8, 64], F32)
    hbf = kb.sb("hbf", [64, 8, 64], BF16)
    ysb = [kb.sb(f"ysb{i}", [64, 8, 128], F32) for i in range(2)]
    yfl = [kb.sb(f"yfl{i}", [64, 8, 128], F32) for i in range(2)]
    szl = [kb.sb(f"szl{i}", [64, 8, 128], BF16) for i in range(2)]
    yg = kb.sb("yg", [64, 8, 128], F32)
    sqy = kb.sb("sqy", [64, 8, 128], F32)
    rsy = kb.sb("rsy", [64, 128], F32)
    ynb = [kb.sb(f"ynb{i}", [64, 8, 128], BF16) for i in range(2)]
    pA = kb.ps("pA", [128, 512], F32)
    pB = kb.ps("pB", [64, 8, 64], F32)
    pC = kb.ps("pC", [64, 512], F32)
    pCS = kb.ps("pCS", [128, 8, 128], F32)
    pG = kb.ps("pG", [128, 512], F32)
    pY = kb.ps("pY", [64, 8, 128], F32)
    yf_v = S_yf.rearrange("(r p) s -> p r s", p=64)
    yn_v = S_yn.rearrange("(r p) s -> p r s", p=64)
    sz_v = S_sz.rearrange("(r p) s -> p r s", p=64)
    bcT_v = S_bcT.rearrange("(q n) s -> n q s", n=64)
    for d_ in range(2):
        kb.memset("pool", hst[:], 0.0, ["hst"])
        kb.memset("pool", hbf[:], 0.0, ["hbf"])
        order = list(range(NT)) if d_ == 0 else list(range(NT - 1, -1, -1))
        for ci, c in enumerate(order):
            b2 = ci % 2
            cs0, cs1 = c * 128, (c + 1) * 128
            kb.dma("sp", xtm[b2][:], S_xtm[cs0:cs1, :], f"xtm{b2}", [], [f"xtm{b2}"])
            kb.dma("sp", bcT[b2][:], bcT_v[:, :, cs0:cs1], f"bcT{b2}", [], [f"bcT{b2}"])
            if d_ == 1:
                kb.dma("sp", yfl[b2][:], yf_v[:, :, cs0:cs1], f"yfl{b2}", [], [f"yfl{b2}"])
                kb.dma("sp", szl[b2][:], sz_v[:, :, cs0:cs1], f"szl{b2}", [], [f"szl{b2}"])
            dts = dtp[:, c, d_ * 8:(d_ + 1) * 8]
            kb.tt("dve", A_[:], dts, a_bc[:, d_ * 8:(d_ + 1) * 8], mult, ["dtp", "a_bc"], ["A_"])
            kb.mm(pA[:, 0:8], tri[d_][:], A_[:], True, True, [f"tri{d_}", "A_"], ["pA"])
            kb.mm(pA[:, 8:16], onesf[:], A_[:], True, True, ["onesf", "A_"], ["pA"])
            kb.cp("dve", cs_sb[:], pA[:, 0:8], ["pA"], ["cs_sb"])
            kb.tr(pA[0:8, 128:256], cs_sb[:], identf[:], ["cs_sb", "identf"], ["pA"])
            kb.cp("dve", csT_sb[:], pA[0:8, 128:256], ["pA"], ["csT_sb"])
            for r in range(8):
                kb.mm(pCS[:, r, :], Esel[:, r, :], csT_sb[:], True, True, ["Esel", "csT_sb"], ["pCS"])
            kb.tt("dve", seg[:], pCS[:], cs_sb[:, :, None].to_broadcast([128, 8, 128]), sub, ["pCS", "cs_sb"], ["seg"])
            kb.ts("pool", seg[:], seg[:], 0.0, None, ALU.min, None, ["seg"], ["seg"])
            kb.act(seg[:], seg[:], AF.Exp, ["seg"], ["seg"])
            kb.act(ecs[:], pCS[0:64, :, :], AF.Exp, ["pCS"], ["ecs"])
            for g_ in range(2):
                kb.mm(pG[:, g_ * 128:(g_ + 1) * 128], bcT[b2][:, g_, :], bcT[b2][:, 2 + g_, :], True, True,
                      [f"bcT{b2}"], ["pG"])
            kb.tt("dve", GTm[:], pG[:, 0:256].rearrange("p (g i) -> p g i", g=2), tri[d_][:, None, :].to_broadcast([128, 2, 128]),
                  mult, ["pG", f"tri{d_}"], ["GTm"])
            kb.tt("pool", xdt[:], xtm[b2][:, 0:512].rearrange("p (r q) -> p r q", r=8), dts[:, :, None].to_broadcast([128, 8, 64]),
                  mult, [f"xtm{b2}", "dtp"], ["xdt"])
            kb.tt("dve", MT[:].rearrange("p (g r) i -> p g r i", g=2), seg[:].rearrange("p (g r) i -> p g r i", g=2),
                  GTm[:, :, None, :].to_broadcast([128, 2, 4, 128]), mult, ["seg", "GTm"], ["MT"])
            kb.tt("pool", Cdec[:].rearrange("p (g r) i -> p g r i", g=2), ecs[:].rearrange("p (g r) i -> p g r i", g=2),
                  bcT[b2][:, 2:4, None, :].to_broadcast([64, 2, 4, 128]), mult, ["ecs", f"bcT{b2}"], ["Cdec"])
            for r in range(8):
                kb.mm(pY[:, r, :], xdt[:, r, :], MT[:, r, :], True, False, ["xdt", "MT"], ["pY"])
                if d_ == 0:
                    kb.mm(pY[:, r, :], xtm[b2][:, r * 64:(r + 1) * 64], diagD[:, r, :], False, False, [f"xtm{b2}", "diagD"], ["pY"])
                kb.mm(pY[:, r, :], hbf[:, r, :], Cdec[:, r, :], False, True, ["hbf", "Cdec"], ["pY"])
            kb.tt("dve", wex[:], pA[:, 8:16], cs_sb[:], sub, ["pA", "cs_sb"], ["wex"])
            kb.act(wex[:], wex[:], AF.Exp, ["wex"], ["wex"])
            kb.tt("pool", xw[:], xdt[:], wex[:, :, None].to_broadcast([128, 8, 64]), mult, ["xdt", "wex"], ["xw"])
            for g_ in range(2):
                kb.mm(pB[:, g_ * 4:(g_ + 1) * 4, :], xtm[b2][:, 512 + g_ * 64:512 + (g_ + 1) * 64],
                      xw[:, g_ * 4:(g_ + 1) * 4, :], True, True, [f"xtm{b2}", "xw"], ["pB"])
            kb.act(dec[:], pA[0:64, 8:16], AF.Exp, ["pA"], ["dec"])
            kb.tt("pool", hst[:], hst[:], dec[:, :, None].to_broadcast([64, 8, 64]), mult, ["hst", "dec"], ["hst"])
            kb.tt("dve", hst[:], hst[:], pB[:], add, ["hst", "pB"], ["hst"])
            kb.cp("pool", hbf[:], hst[:], ["hst"], ["hbf"])
            if d_ == 0:
                kb.cp("act", ysb[b2][:], pY[:], ["pY"], [f"ysb{b2}"])
                kb.dma("sp", yf_v[:, :, cs0:cs1], ysb[b2][:], f"ysb{b2}", [f"ysb{b2}"], [])
            else:
                kb.tt("dve", yg[:], pY[:], yfl[b2][:], add, ["pY", f"yfl{b2}"], ["yg"])
                kb.tt("pool", yg[:], yg[:], szl[b2][:], mult, ["yg", f"szl{b2}"], ["yg"])
                kb.act(sqy[:], yg[:], AF.Square, ["yg"], ["sqy"])
                for r in range(8):
                    kb.mm(pC[:, 0:128], onesf[0:64, 0:64], sqy[:, r, :], r == 0, r == 7, ["onesf", "sqy"], ["pC"])
                kb.act(rsy[:], pC[:, 0:128], AF.Ln, ["pC", "c_eps"], ["rsy"], scale=1.0 / 512, bias=c_eps[0:64, :])
                kb.act(rsy[:], rsy[:], AF.Exp, ["rsy"], ["rsy"], scale=-0.5)
                kb.tt("dve", ynb[b2][:], yg[:], rsy[:, None, :].to_broadcast([64, 8, 128]), mult, ["yg", "rsy"], [f"ynb{b2}"])
                kb.dma("sp", yn_v[:, :, cs0:cs1], ynb[b2][:], f"ynb{b2}", [f"ynb{b2}"], [])
        kb.barrier()
    kb.pop()
    if stop_after == "P2":
        return nc, kb

    S_attn = scr("S_attn", [512, S], BF16)
    SCALE = 96.0 ** -0.5
    kb.push()
    KT = kb.sb("KT", [128, 8, S], BF16)
    Vaug = kb.sb("Vaug", [128, NT, 8, 65], BF16)
    kmax = kb.sb("kmax", [128, 1], F32)
    kb.push()
    kvw = vec_pk("kvw")
    wkv_st = kb.sb("wkv_st", [128, 1024], F32)
    wkv = kb.sb("wkv", [128, 8, 128], BF16)
    kb.dma("sp", wkv_st[:], D["w_ukv"], "wkv_st", [], ["wkv_st"])
    kb.ts("dve", wkv[:].rearrange("p h d -> p (h d)"), wkv_st[:], kvw[:, 0:1], None, mult, None, ["wkv_st", "vecs"], ["wkv"])
    kvl = [kb.sb(f"kvl{i}", [128, GS], BF16) for i in range(2)]
    kpad = [kb.sb(f"kpad{i}", [128, 96], BF16) for i in range(2)]
    krs = [kb.sb(f"krs{i}", [128, 128], BF16) for i in range(2)]
    sqk = [kb.sb(f"sqk{i}", [96, GS], BF16) for i in range(2)]
    tmx = kb.sb("tmx", [128, 1], F32)
    pK = [kb.ps(f"pK{i}", [128, 512], F32) for i in range(2)]
    pV = [kb.ps(f"pV{i}", [128, 512], F32) for i in range(2)]
    pR = [kb.ps(f"pR{i}", [128, 1024], BF16) for i in range(2)]
    pN = [kb.ps(f"pN{i}", [128, 512], F32) for i in range(2)]
    kb.memset("pool", Vaug[:, :, :, 64:65], 1.0, ["V"])
    kb.memset("pool", kmax[:], 0.0, ["kmax"])
    for i in range(2):
        kb.memset("pool", kpad[i][:], 0.0, [f"kpad{i}"])
    for g in range(NG):
        gp = g % 2
        gs0, gs1 = g * GS, (g + 1) * GS
        kb.dma("sp", kvl[gp][:], S_kvn[:, gs0:gs1], f"kvl{gp}", [], [f"kvl{gp}"])
        for t in range(TG):
            tt_ = g * TG + t
            pp = tt_ % 2
            kb.cp("pool", kpad[pp][:, 64:96], krot[:, tt_, :], ["krot"], [f"kpad{pp}"])
            kb.tr(pR[pp][0:96, 0:128], kpad[pp][:], ident[:], [f"kpad{pp}", "ident"], [f"pR{pp}"])
            kb.cp("act", krs[pp][64:96, :], pR[pp][64:96, 0:128], [f"pR{pp}"], [f"krs{pp}"])
            kb.cp("pool", KT[64:96, :, tt_ * 128:(tt_ + 1) * 128], krs[pp][64:96, None, :].to_broadcast([32, 8, 128]),
                  [f"krs{pp}"], [f"KTr{g}"])
            kb.mm(pV[pp][:], kvl[gp][:, t * 128:(t + 1) * 128], wkv[:, :, 64:128], True, True, [f"kvl{gp}", "wkv"], [f"pV{pp}"])
            kb.cp("dve", Vaug[:, tt_, :, 0:64], pV[pp][:].rearrange("p (h d) -> p h d", h=8), [f"pV{pp}"], ["V"])
        for h in range(8):
            hp = h % 2
            kb.mm(pK[hp][0:64, :GS], wkv[:, h, 0:64], kvl[gp][:], True, True, ["wkv", f"kvl{gp}"], [f"pK{hp}"])
            kb.cp("act" if h % 2 else "dve", KT[0:64, h, gs0:gs1], pK[hp][0:64, :GS], [f"pK{hp}"], [f"KTn{g}_{h}"])
            kb.act(sqk[hp][:], KT[0:96, h, gs0:gs1], AF.Square, [f"KTn{g}_{h}", f"KTr{g}"], [f"sqk{hp}"])
            kb.mm(pN[hp][:, :GS], onesb[0:96, :], sqk[hp][:], True, True, ["onesb", f"sqk{hp}"], [f"pN{hp}"])
            Sd.op("dve", lambda e, hp=hp: e.tensor_reduce(out=tmx[:], in_=pN[hp][:, :GS], axis=AX.X, op=ALU.max),
                  [f"pN{hp}"], ["tmx"])
            kb.tt("dve", kmax[:], kmax[:], tmx[:], ALU.max, ["kmax", "tmx"], ["kmax"])
    kb.barrier()
    kb.pop()
    if stop_after == "P3":
        return nc, kb

    kb.push()
    qw = vec_pk("qw")
    wuq_st = kb.sb("wuq_st", [128, 2, 768], F32)
    wuq = kb.sb("wuq", [128, 2, 768], BF16)
    kb.dma("sp", wuq_st[:], D["w_uq"].rearrange("(c p) n -> p c n", p=128), "wuq_st", [], ["wuq_st"])
    for c in range(2):
        kb.ts("dve", wuq[:, c, :], wuq_st[:, c, :], qw[:, c:c + 1], None, mult, None, ["wuq_st", "vecs"], ["wuq"])
    sel65 = kb.sb("sel65", [65, 64], F32)
    kb.memset("pool", sel65[:], 0.0, ["sel65"])
    Sd.op("pool", lambda e: e.affine_select(out=sel65[:], in_=sel65[:], pattern=[[0, 64]], compare_op=ALU.not_equal,
                                            fill=1.0, base=-64, channel_multiplier=1), ["sel65"], ["sel65"])
    cql = [kb.sb(f"cql{i}", [128, 2, GS], BF16) for i in range(2)]
    qtm = kb.sb("qtm", [128, 8, 96], F32)
    qrt = [kb.sb(f"qrt{i}", [128, 16], F32) for i in range(4)]
    qrot = [kb.sb(f"qrot{i}", [128, 8, 96], BF16) for i in range(2)]
    qra = [kb.sb(f"qra{i}", [128, 8, 16], F32) for i in range(4)]
    qT = [kb.sb(f"qT{i}", [96, 8, GS], BF16) for i in range(2)]
    sqq = [kb.sb(f"sqq{i}", [96, GS], BF16) for i in range(2)]
    qmax = kb.sb("qmax", [128, 1], F32)
    tmq = kb.sb("tmq", [128, 1], F32)
    bias_g = [kb.sb(f"bias_g{i}", [128, 1], F32) for i in range(2)]
    NPT = 4
    pt = [kb.sb(f"pt{i}", [128, GS], BF16) for i in range(NPT)]
    osb = [kb.sb(f"osb{i}", [65, GS], F32) for i in range(2)]
    rden = [kb.sb(f"rden{i}", [64, GS], F32) for i in range(2)]
    ao = kb.sb("ao", [64, 8, GS], F32)
    sqa = [kb.sb(f"sqa{i}", [64, GS], F32) for i in range(2)]
    rsa = kb.sb("rsa", [64, GS], F32)
    aon = [kb.sb(f"aon{i}", [64, 8, GS], BF16) for i in range(2)]
    psc = [kb.ps(f"psc{i}", [128, 512], F32) for i in range(3)]
    po = [kb.ps(f"po{i}", [128, 512], F32) for i in range(2)]
    pq = [kb.ps(f"pq{i}", [128, 512], F32) for i in range(2)]
    pQT = kb.ps("pQT", [128, 8, 128], BF16)
    sci = 0
    for g in range(NG):
        gp = g % 2
        gs0, gs1 = g * GS, (g + 1) * GS
        kb.dma("sp", cql[gp][:], S_cqn.rearrange("(c p) s -> p c s", p=128)[:, :, gs0:gs1], f"cql{gp}", [], [f"cql{gp}"])
        for t in range(TG):
            tt_ = g * TG + t
            rp = tt_ % 2
            for c in range(2):
                kb.mm(pq[0][:, 0:512], cql[gp][:, c, t * 128:(t + 1) * 128], wuq[:, c, 0:512], c == 0, c == 1,
                      [f"cql{gp}", "wuq"], ["pq0"])
            for c in range(2):
                kb.mm(pq[1][:, 0:256], cql[gp][:, c, t * 128:(t + 1) * 128], wuq[:, c, 512:768], c == 0, c == 1,
                      [f"cql{gp}", "wuq"], ["pq1"])
            qflat = qtm[:].rearrange("p h d -> p (h d)")
            kb.cp("act", qflat[:, 0:512], pq[0][:, 0:512], ["pq0"], ["qtm"])
            kb.cp("dve", qflat[:, 512:768], pq[1][:, 0:256], ["pq1"], ["qtm"])
            cb_ = cosT[:, tt_:tt_ + 1, :].to_broadcast([128, 8, 16])
            sb_ = sinT[:, tt_:tt_ + 1, :].to_broadcast([128, 8, 16])
            q1, q2 = qtm[:, :, 64:80], qtm[:, :, 80:96]
            kb.cp("pool", qrot[rp][:, :, 0:64], qtm[:, :, 0:64], ["qtm"], [f"qrot{rp}"])
            kb.tt("pool", qra[0][:], q1, cb_, mult, ["qtm", "cosT"], ["qra0"])
            kb.tt("dve", qra[1][:], q2, sb_, mult, ["qtm", "sinT"], ["qra1"])
            kb.tt("pool", qrot[rp][:, :, 64:80], qra[0][:], qra[1][:], sub, ["qra0", "qra1"], [f"qrot{rp}"])
            kb.tt("pool", qra[2][:], q2, cb_, mult, ["qtm", "cosT"], ["qra2"])
            kb.tt("dve", qra[3][:], q1, sb_, mult, ["qtm", "sinT"], ["qra3"])
            kb.tt("dve", qrot[rp][:, :, 80:96], qra[2][:], qra[3][:], add, ["qra2", "qra3"], [f"qrot{rp}"])
            for h in range(8):
                kb.tr(pQT[0:96, h, :], qrot[rp][:, h, :], ident[:], [f"qrot{rp}", "ident"], ["pQT"])
            kb.cp("act", qT[gp][:, :, t * 128:(t + 1) * 128], pQT[0:96, :, :], ["pQT"], [f"qT{gp}_{t}"])
        qTb = [f"qT{gp}_{t}" for t in range(TG)]
        kb.memset("pool", qmax[:], 0.0, ["qmax"])
        for h in range(8):
            hp = h % 2
            kb.act(sqq[hp][:], qT[gp][:, h, :], AF.Square, qTb, [f"sqq{hp}"])
            kb.mm(pq[hp][:, :GS], onesb[0:96, :], sqq[hp][:], True, True, ["onesb", f"sqq{hp}"], [f"pq{hp}"])
            Sd.op("dve", lambda e, hp=hp: e.tensor_reduce(out=tmq[:], in_=pq[hp][:, :GS], axis=AX.X, op=ALU.max),
                  [f"pq{hp}"], ["tmq"])
            kb.tt("dve", qmax[:], qmax[:], tmq[:], ALU.max, ["qmax", "tmq"], ["qmax"])
        bg = bias_g[gp]
        kb.tt("dve", bg[:], qmax[:], kmax[:], mult, ["qmax", "kmax"], [f"bias_g{gp}"])
        kb.ts("dve", bg[:], bg[:], 1e-30, None, add, None, [f"bias_g{gp}"], [f"bias_g{gp}"])
        kb.act(bg[:], bg[:], AF.Ln, [f"bias_g{gp}"], [f"bias_g{gp}"])
        kb.act(bg[:], bg[:], AF.Exp, [f"bias_g{gp}"], [f"bias_g{gp}"], scale=0.5)
        kb.ts("dve", bg[:], bg[:], -SCALE * 1.02, None, mult, None, [f"bias_g{gp}"], [f"bias_g{gp}"])
        units = [(h, kbk) for h in range(8) for kbk in range(NT)]
        LOOK = 2
        NPS = 3

        def emit_qk(u):
            h, kbk = units[u]
            si = u % NPS
            kb.mm(psc[si][:, :GS], KT[0:96, h, kbk * 128:(kbk + 1) * 128], qT[gp][:, h, :], True, True,
                  ["KT"] + qTb, [f"psc{si}"])

        for u in range(min(LOOK, len(units))):
            emit_qk(u)
        for u, (h, kbk) in enumerate(units):
            hp = h % 2
            si = u % NPS
            pi = u % NPT
            if u + LOOK < len(units):
                emit_qk(u + LOOK)
            kb.act(pt[pi][:], psc[si][:, :GS], AF.Exp, [f"psc{si}", f"bias_g{gp}"], [f"pt{pi}"], scale=SCALE, bias=bg[:])
            kb.mm(po[hp][0:65, :GS], Vaug[:, kbk, h, :], pt[pi][:], kbk == 0, kbk == NT - 1, ["V", f"pt{pi}"], [f"po{hp}"])
            if kbk == NT - 1:
                kb.cp("dve", osb[hp][:], po[hp][0:65, :GS], [f"po{hp}"], [f"osb{hp}"])
                kb.mm(pq[hp][0:64, :GS], sel65[:], osb[hp][:], True, True, ["sel65", f"osb{hp}"], [f"pq{hp}"])
                Sd.op("dve", lambda e, hp=hp: e.reciprocal(out=rden[hp][:], in_=pq[hp][0:64, :GS]), [f"pq{hp}"], [f"rden{hp}"])
                kb.tt("pool", ao[:, h, :], osb[hp][0:64, :], rden[hp][:], mult, [f"osb{hp}", f"rden{hp}"], [f"ao{h}"])
        for h in range(8):
            hp = h % 2
            kb.act(sqa[hp][:], ao[:, h, :], AF.Square, [f"ao{h}"], [f"sqa{hp}"])
            kb.mm(pq[0][0:64, :GS], onesf[0:64, 0:64], sqa[hp][:], h == 0, h == 7, ["onesf", f"sqa{hp}"], ["pq0"])
        kb.act(rsa[:], pq[0][0:64, :GS], AF.Ln, ["pq0", "c_eps"], ["rsa"], scale=1.0 / 512, bias=c_eps[0:64, :])
        kb.act(rsa[:], rsa[:], AF.Exp, ["rsa"], ["rsa"], scale=-0.5)
        for h in range(8):
            kb.tt("pool" if h % 2 else "dve", aon[gp][:, h, :], ao[:, h, :], rsa[:], mult, [f"ao{h}", "rsa"], [f"aon{gp}_{h}"])
        kb.dma("sp", S_attn.rearrange("(h p) s -> p h s", p=64)[:, :, gs0:gs1], aon[gp][:], f"aon{gp}",
               [f"aon{gp}_{h}" for h in range(8)], [])
    kb.barrier()
    kb.pop()
    kb.pop()
    if stop_after == "P4":
        return nc, kb

    TS = min(512, S)
    NSUB = TS // 128
    NTILES = (2 * S) // TS + NEXP
    NSLOT = NTILES * TS
    BIG = float(1 << 22)
    S_x1 = scr("S_x1", [S, 1024], F32)
    S_h2 = scr("S_h2", [S, 1024], BF16)
    S_slot = scr("S_slot", [NSLOT, 4], F32)
    S_ymoe = scr("S_ymoe", [2 * S, 1024], F32)
    S_tile = scr("S_tile", [2, 128, NTILES], I32)
    kb.push()
    ohall = kb.sb("ohall", [128, NT * 2, 32], BF16)
    posn = kb.sb("posn", [128, NT * 2], F32)
    wcomb = kb.sb("wcomb", [128, NT * 2], F32)
    run_bc = kb.sb("run_bc", [128, 32], F32)
    stri = kb.sb("stri", [128, 128], BF16)
    kb.memset("pool", run_bc[:], 0.0, ["run_bc"])
    kb.memset("pool", stri[:], 1.0, ["stri"])
    Sd.op("pool", lambda e: e.affine_select(out=stri[:], in_=stri[:], pattern=[[1, 128]], compare_op=ALU.is_gt, fill=0.0,
                                            base=0, channel_multiplier=-1), ["stri"], ["stri"])
    kb.push()
    aow = vecs64[:, 0:8]
    snw = vecs64[:, 8:16]
    wo = kb.sb("wo", [64, 16, 1024], BF16)
    wo_st = [kb.sb(f"wo_st{i}", [64, 1024], F32) for i in range(2)]
    for c in range(16):
        kb.dma("sp", wo_st[c % 2][:], D["w_o"][c * 64:(c + 1) * 64, :], f"wo_st{c % 2}", [], [f"wo_st{c % 2}"])
        sc_ = aow[:, c:c + 1] if c < 8 else snw[:, c - 8:c - 7]
        kb.ts("dve" if c % 2 else "pool", wo[:, c, :], wo_st[c % 2][:], sc_, None, mult, None, [f"wo_st{c % 2}", "vecs64"], [f"wo{c}"])
    wob = [f"wo{c}" for c in range(16)]
    fnw_bc = kb.sb("fnw_bc", [128, 1024], F32)
    kb.dma("sp", fnw_bc[:], D["ffn_norm_w"].partition_broadcast(128), "fnw_bc", [], ["fnw_bc"])
    wr = kb.sb("wr", [128, 8, 36], F32)
    kb.dma("sp", wr[:, :, 0:4], D["w_router_group"].rearrange("(k p) n -> p k n", p=128), "wr", [], ["wr"])
    kb.dma("sp", wr[:, :, 4:36], D["w_router_expert"].rearrange("(k p) n -> p k n", p=128), "wr", [], ["wr"])
    br_bc = kb.sb("br_bc", [128, 36], F32)
    kb.dma("sp", br_bc[:, 0:4], D["b_router_group"].partition_broadcast(128), "br_bc", [], ["br_bc"])
    kb.dma("sp", br_bc[:, 4:36], D["b_router_expert"].partition_broadcast(128), "br_bc", [], ["br_bc"])
    xl = [kb.sb(f"xl{i}", [128, 1024], F32) for i in range(2)]
    atl = [kb.sb(f"atl{i}", [64, 8, 128], BF16) for i in range(2)]
    ynl = [kb.sb(f"ynl{i}", [64, 8, 128], BF16) for i in range(2)]
    x1t = [kb.sb(f"x1t{i}", [128, 1024], F32) for i in range(2)]
    jk_2 = [kb.sb(f"jk_{i}", [128, 1024], BF16) for i in range(2)]
    st5 = [kb.sb(f"st5_{i}", [128, 1], F32) for i in range(2)]
    h2f_2 = [kb.sb(f"h2f_{i}", [128, 1024], F32) for i in range(2)]
    h2b = [kb.sb(f"h2b{i}", [128, 1024], BF16) for i in range(2)]
    h2T_2 = [kb.sb(f"h2T_{i}", [128, 8, 128], F32) for i in range(2)]
    rl_2 = [kb.sb(f"rl_{i}", [128, 36], F32) for i in range(2)]
    gmx_2 = [kb.sb(f"gmx_{i}", [128, 1], F32) for i in range(2)]
    ngm_2 = [kb.sb(f"ngm_{i}", [128, 1], F32) for i in range(2)]
    ohg_2 = [kb.sb(f"ohg_{i}", [128, 4], F32) for i in range(2)]
    eg_2 = [kb.sb(f"eg_{i}", [128, 4], F32) for i in range(2)]
    sume_2 = [kb.sb(f"sume_{i}", [128, 1], F32) for i in range(2)]
    gw_2 = [kb.sb(f"gw_{i}", [128, 1], F32) for i in range(2)]
    selt_2 = [kb.sb(f"selt_{i}", [128, 4, 8], F32) for i in range(2)]
    sel_2 = [kb.sb(f"sel_{i}", [128, 8], F32) for i in range(2)]
    m8_2 = [kb.sb(f"m8_{i}", [128, 8], F32) for i in range(2)]
    dd_2 = [kb.sb(f"dd_{i}", [128, 1], F32) for i in range(2)]
    w12_2 = [kb.sb(f"w12_{i}", [128, 2], F32) for i in range(2)]
    ohe_2 = [kb.sb(f"ohe_{i}", [128, 2, 8], F32) for i in range(2)]
    pmat_2 = [kb.sb(f"pmat_{i}", [128, 32], F32) for i in range(2)]
    ptmp_2 = [kb.sb(f"ptmp_{i}", [128, 32], F32) for i in range(2)]
    pmx = kb.ps("pmx", [128, 1024], F32)
    pH = kb.ps("pH", [128, 8, 128], F32)
    pL = kb.ps("pL", [128, 512], F32)
    pP = kb.ps("pP", [128, 512], F32)
    at_v = S_attn.rearrange("(h p) s -> p h s", p=64)
    for tt_ in range(NT):
        b2 = tt_ % 2
        jk = jk_2[b2]
        h2f = h2f_2[b2]
        h2T = h2T_2[b2]
        rl = rl_2[b2]
        gmx = gmx_2[b2]
        ngm = ngm_2[b2]
        ohg = ohg_2[b2]
        eg = eg_2[b2]
        sume = sume_2[b2]
        gw = gw_2[b2]
        selt = selt_2[b2]
        sel = sel_2[b2]
        m8 = m8_2[b2]
        dd = dd_2[b2]
        w12 = w12_2[b2]
        ohe = ohe_2[b2]
        pmat = pmat_2[b2]
        ptmp = ptmp_2[b2]
        ts0, ts1 = tt_ * 128, (tt_ + 1) * 128
        kb.dma("sp", xl[b2][:], D["x"][ts0:ts1, :], f"xl{b2}", [], [f"xl{b2}"])
        kb.dma("sp", atl[b2][:], at_v[:, :, ts0:ts1], f"atl{b2}", [], [f"atl{b2}"])
        kb.dma("sp", ynl[b2][:], yn_v[:, :, ts0:ts1], f"ynl{b2}", [], [f"ynl{b2}"])
        for half in range(2):
            for c in range(16):
                lhs = atl[b2][:, c, :] if c < 8 else ynl[b2][:, c - 8, :]
                kb.mm(pmx[:, half * 512:(half + 1) * 512], lhs, wo[:, c, half * 512:(half + 1) * 512], c == 0, c == 15,
                      [f"atl{b2}", f"ynl{b2}"] + wob, ["pmx"])
        kb.tt("dve", x1t[b2][:], pmx[:], xl[b2][:], add, ["pmx", f"xl{b2}"], [f"x1t{b2}"])
        kb.dma("sp", S_x1[ts0:ts1, :], x1t[b2][:], f"x1t{b2}", [f"x1t{b2}"], [])
        st = st5[b2]
        kb.act(jk[:], x1t[b2][:], AF.Square, [f"x1t{b2}"], [f"jk_{b2}", f"st5_{b2}"], accum_out=st[:])
        kb.ts("dve", st[:], st[:], 1.0 / 1024, EPS, mult, add, [f"st5_{b2}"], [f"st5_{b2}"])
        kb.act(st[:], st[:], AF.Ln, [f"st5_{b2}"], [f"st5_{b2}"])
        kb.act(st[:], st[:], AF.Exp, [f"st5_{b2}"], [f"st5_{b2}"], scale=-0.5)
        kb.stt(h2f[:], x1t[b2][:], st[:, 0:1], fnw_bc[:], mult, mult, [f"x1t{b2}", f"st5_{b2}", "fnw_bc"], [f"h2f_{b2}"])
        kb.cp("pool", h2b[b2][:], h2f[:], [f"h2f_{b2}"], [f"h2b{b2}"])
        kb.dma("sp", S_h2[ts0:ts1, :], h2b[b2][:], f"h2b{b2}", [f"h2b{b2}"], [])
        for k in range(8):
            kb.tr(pH[:, k, :], h2f[:, k * 128:(k + 1) * 128], identf[:], [f"h2f_{b2}", "identf"], ["pH"])
        kb.cp("act", h2T[:], pH[:], ["pH"], [f"h2T_{b2}"])
        for k in range(8):
            kb.mm(pL[:, 0:36], h2T[:, k, :], wr[:, k, :], k == 0, k == 7, [f"h2T_{b2}", "wr"], ["pL"])
        kb.tt("dve", rl[:], pL[:, 0:36], br_bc[:], add, ["pL", "br_bc"], [f"rl_{b2}"])
        kb.reduce(gmx[:], rl[:, 0:4], ALU.max, [f"rl_{b2}"], [f"gmx_{b2}"])
        kb.ts("dve", ohg[:], rl[:, 0:4], gmx[:, 0:1], None, ALU.is_equal, None, [f"rl_{b2}", f"gmx_{b2}"], [f"ohg_{b2}"])
        kb.ts("dve", ngm[:], gmx[:], -1.0, None, mult, None, [f"gmx_{b2}"], [f"ngm_{b2}"])
        kb.act(eg[:], rl[:, 0:4], AF.Exp, [f"rl_{b2}", f"ngm_{b2}"], [f"eg_{b2}", f"sume_{b2}"], bias=ngm[:], accum_out=sume[:])
        kb.recip(gw[:], sume[:], [f"sume_{b2}"], [f"gw_{b2}"])
        kb.tt("dve", selt[:], rl[:, 4:36].rearrange("p (g e) -> p g e", g=4), ohg[:, :, None].to_broadcast([128, 4, 8]), mult,
              [f"rl_{b2}", f"ohg_{b2}"], [f"selt_{b2}"])
        kb.reduce(sel[:], selt[:].rearrange("p g e -> p e g"), ALU.add, [f"selt_{b2}"], [f"sel_{b2}"])
        kb.max8(m8[:], sel[:], [f"sel_{b2}"], [f"m8_{b2}"])
        kb.tt("dve", dd[:], m8[:, 1:2], m8[:, 0:1], sub, [f"m8_{b2}"], [f"dd_{b2}"])
        kb.act(dd[:], dd[:], AF.Exp, [f"dd_{b2}"], [f"dd_{b2}"])
        kb.ts("dve", w12[:, 0:1], dd[:], 1.0, None, add, None, [f"dd_{b2}"], [f"w12_{b2}"])
        kb.recip(w12[:, 0:1], w12[:, 0:1], [f"w12_{b2}"], [f"w12_{b2}"])
        kb.tt("dve", w12[:, 1:2], w12[:, 0:1], dd[:], mult, [f"w12_{b2}", f"dd_{b2}"], [f"w12_{b2}"])
        kb.ts("dve", wcomb[:, 2 * tt_:2 * tt_ + 2], w12[:], gw[:, 0:1], None, mult, None, [f"w12_{b2}", f"gw_{b2}"], ["wcomb"])
        for k in range(2):
            kb.ts("dve", ohe[:, k, :], sel[:], m8[:, k:k + 1], None, ALU.is_equal, None, [f"sel_{b2}", f"m8_{b2}"], [f"ohe_{b2}"])
        for k in range(2):
            u = 2 * tt_ + k
            kb.tt("dve", ohall[:, u, :].rearrange("p (g e) -> p g e", g=4), ohg[:, :, None].to_broadcast([128, 4, 8]),
                  ohe[:, k:k + 1, :].to_broadcast([128, 4, 8]), mult, [f"ohg_{b2}", f"ohe_{b2}"], [f"ohall{u}"])
            kb.mm(pP[:, 0:32], stri[:], ohall[:, u, :], True, True, ["stri", f"ohall{u}"], ["pP"])
            kb.mm(pP[:, 32:64], onesb[:], ohall[:, u, :], True, True, ["onesb", f"ohall{u}"], ["pP"])
            kb.tt("dve", pmat[:], pP[:, 0:32], run_bc[:], add, ["pP", "run_bc"], [f"pmat_{b2}"])
            kb.tt("dve", ptmp[:], pmat[:], ohall[:, u, :], mult, [f"pmat_{b2}", f"ohall{u}"], [f"ptmp_{b2}"])
            kb.reduce(posn[:, u:u + 1], ptmp[:], ALU.add, [f"ptmp_{b2}"], ["posn"])
            kb.tt("dve", run_bc[:], run_bc[:], pP[:, 32:64], add, ["run_bc", "pP"], ["run_bc"])
    kb.barrier()
    kb.pop()
    if stop_after == "P5a":
        return nc, kb

    import math as _m
    LOG_TS = int(_m.log2(TS))
    kb.push()
    cntf = kb.sb("cntf", [128, 32], F32)
    cnti = kb.sb("cnti", [128, 32], I32)
    ntf = kb.sb("ntf", [128, 32], F32)
    ones32 = kb.sb("ones32", [128, 32], F32)
    incl = kb.sb("incl", [128, 32], F32)
    base_bc = kb.sb("base_bc", [128, 32], F32)
    tmpb = kb.sb("tmpb", [128, NT * 2, 32], F32)
    slotf = kb.sb("slotf", [128, NT * 2], F32)
    sloti = kb.sb("sloti", [128, NT * 2], I32)
    rowdat = kb.sb("rowdat", [128, NT * 2, 4], F32)
    rowdat_i = rowdat[:].bitcast(I32)
    NDF = NSLOT // 128
    dflt = kb.sb("dflt", [128, NDF, 4], F32)
    dflt_i = dflt[:].bitcast(I32)
    jidx = kb.sb("jidx", [128, NTILES], F32)
    pidx = kb.sb("pidx", [128, 1], F32)
    cmpt = kb.sb("cmpt", [128, NTILES, 32], F32)
    ej = kb.sb("ej", [128, NTILES], F32)
    wgf = kb.sb("wgf", [128, NTILES], F32)
    wgi = kb.sb("wgi", [128, NTILES], I32)
    wdi = kb.sb("wdi", [128, NTILES], I32)
    kb.ts("dve", cntf[:], run_bc[:], float(TS - 1), None, add, None, ["run_bc"], ["cntf"])
    kb.cp("dve", cnti[:], cntf[:], ["cntf"], ["cnti"])
    kb.ts("dve", cnti[:], cnti[:], LOG_TS, None, ALU.arith_shift_right, None, ["cnti"], ["cnti"])
    kb.cp("dve", ntf[:], cnti[:], ["cnti"], ["ntf"])
    kb.memset("pool", ones32[:], 1.0, ["ones32"])
    Sd.op("dve", lambda e: e.tensor_tensor_scan(out=incl[:], data0=ones32[:], data1=ntf[:], initial=0.0, op0=mult, op1=add),
          ["ones32", "ntf"], ["incl"])
    kb.tt("dve", base_bc[:], incl[:], ntf[:], sub, ["incl", "ntf"], ["base_bc"])
    kb.ts("dve", base_bc[:], base_bc[:], float(TS), None, mult, None, ["base_bc"], ["base_bc"])
    kb.tt("pool", tmpb[:], ohall[:], base_bc[:, None, :].to_broadcast([128, NT * 2, 32]), mult,
          [f"ohall{u}" for u in range(NT * 2)] + ["base_bc"], ["tmpb"])
    Sd.op("dve", lambda e: e.tensor_reduce(out=slotf[:], in_=tmpb[:], axis=AX.X, op=ALU.add), ["tmpb"], ["slotf"])
    kb.tt("dve", slotf[:], slotf[:], posn[:], add, ["slotf", "posn"], ["slotf"])
    kb.cp("dve", sloti[:], slotf[:], ["slotf"], ["sloti"])
    kb.memset("pool", rowdat[:], 0.0, ["rowdat"])
    Sd.op("pool", lambda e: e.iota(rowdat_i[:, :, 0].rearrange("p (t k) -> p t k", k=2), pattern=[[128, NT], [0, 2]], base=0,
                                   channel_multiplier=1), ["rowdat"], ["rowdat"])
    Sd.op("pool", lambda e: e.iota(rowdat_i[:, :, 2].rearrange("p (t k) -> p t k", k=2), pattern=[[128, NT], [S, 2]], base=0,
                                   channel_multiplier=1), ["rowdat"], ["rowdat"])
    kb.cp("pool", rowdat[:, :, 1], wcomb[:], ["wcomb", "rowdat"], ["rowdat"])
    kb.memset("pool", dflt[:], 0.0, ["dflt"])
    kb.memset("pool", dflt_i[:, :, 0:1], 1 << 22, ["dflt"])
    kb.memset("pool", dflt_i[:, :, 2:3], 1 << 22, ["dflt"])
    kb.dma("sp", S_slot.rearrange("(n p) c -> p n c", p=128), dflt[:], "dflt", ["dflt"], ["S_slot_init"])
    for u in range(NT * 2):
        Sd.dma("pool", lambda e, u=u: e.indirect_dma_start(
            out=S_slot, out_offset=bass.IndirectOffsetOnAxis(ap=sloti[:, u:u + 1], axis=0), in_=rowdat[:, u, :], in_offset=None,
            bounds_check=BC(e, NSLOT - 1), oob_is_err=False), "slotsc", ["S_slot_init", "sloti", "rowdat"], [])
    Sd.op("pool", lambda e: e.iota(jidx[:], pattern=[[1, NTILES]], base=0, channel_multiplier=0,
                                   allow_small_or_imprecise_dtypes=True), [], ["jidx"])
    Sd.op("pool", lambda e: e.iota(pidx[:], pattern=[[0, 1]], base=0, channel_multiplier=1,
                                   allow_small_or_imprecise_dtypes=True), [], ["pidx"])
    kb.tt("dve", cmpt[:], incl[:, None, :].to_broadcast([128, NTILES, 32]), jidx[:, :, None].to_broadcast([128, NTILES, 32]),
          ALU.is_le, ["incl", "jidx"], ["cmpt"])
    Sd.op("dve", lambda e: e.tensor_reduce(out=ej[:], in_=cmpt[:], axis=AX.X, op=ALU.add), ["cmpt"], ["ej"])
    kb.ts("dve", wgf[:], ej[:], 128.0, pidx[:, 0:1], mult, add, ["ej", "pidx"], ["wgf"])
    kb.cp("dve", wgi[:], wgf[:], ["wgf"], ["wgi"])
    kb.barrier()
    if stop_after == "P5b":
        return nc, kb

    sl = [kb.sb(f"sl{i}", [128, NSUB * 4], F32) for i in range(2)]
    wg = [kb.sb(f"wg{i}", [128, 8, 256], BF16) for i in range(2)]
    wu = [kb.sb(f"wu{i}", [128, 8, 256], BF16) for i in range(2)]
    wd = [kb.sb(f"wd{i}", [128, 2, 1024], BF16) for i in range(2)]
    hg = [kb.sb(f"hg{i}", [128, 1024], BF16) for i in range(2)]
    hTt = [kb.sb(f"hTt{i}", [128, 8, TS], BF16) for i in range(2)]
    sg = [kb.sb(f"sg{i}", [128, TS], F32) for i in range(2)]
    heT = [kb.sb(f"heT{i}", [128, 2, TS], BF16) for i in range(2)]
    ysc = [kb.sb(f"ysc{i}", [128, 1024], F32) for i in range(2)]
    pHT = kb.ps("pHT", [128, 8, 128], BF16)
    pgu = [kb.ps(f"pgu{i}", [128, 512], F32) for i in range(4)]
    py = kb.ps("py", [128, 1024], F32)
    for i in range(2):
        kb.memset("pool", wg[i][:], 0.0, [f"wg{i}"])
        kb.memset("pool", wu[i][:], 0.0, [f"wu{i}"])
        kb.memset("pool", wd[i][:], 0.0, [f"wd{i}"])
        kb.memset("pool", hg[i][:], 0.0, [f"hg{i}"])
    sub_i = 0
    for j in range(NTILES):
        jp = j % 2
        kb.dma("sp", sl[jp][:].rearrange("p (n c) -> p n c", c=4), S_slot[j * TS:(j + 1) * TS, :].rearrange("(n p) c -> p n c", p=128), f"sl{jp}", [], [f"sl{jp}"])
        sl_i = sl[jp][:].bitcast(I32)
        for wt, wname, a_ in ((wg, "w_exp_gate", 8), (wu, "w_exp_up", 8), (wd, "w_exp_down", 2)):
            Sd.dma("pool", lambda e, j=j, jp=jp, wt=wt, wname=wname, a_=a_: e.indirect_dma_start(
                out=wt[jp][:].rearrange("p k c -> p (k c)"), out_offset=None,
                in_=D[wname].rearrange("(r a) c -> r (a c)", a=a_),
                in_offset=bass.IndirectOffsetOnAxis(ap=wgi[:, j:j + 1], axis=0),
                bounds_check=BC(e, 32 * 128 - 1), oob_is_err=False), f"{wname}{jp}", ["wgi"],
                [{"w_exp_gate": "wg", "w_exp_up": "wu", "w_exp_down": "wd"}[wname] + str(jp)])
        for sb_ in range(NSUB):
            s2 = sub_i % 2
            sub_i += 1
            Sd.dma("pool", lambda e, jp=jp, sb_=sb_, s2=s2, sl_i=sl_i: e.indirect_dma_start(
                out=hg[s2][:], out_offset=None, in_=S_h2, in_offset=bass.IndirectOffsetOnAxis(ap=sl_i[:, sb_ * 4:sb_ * 4 + 1], axis=0),
                bounds_check=BC(e, S - 1), oob_is_err=False), f"hg{s2}", [f"sl{jp}"], [f"hg{s2}"])
            for k in range(8):
                kb.tr(pHT[:, k, :], hg[s2][:].rearrange("p (j a) -> p a j", a=8)[:, k, :], ident[:], [f"hg{s2}", "ident"], ["pHT"])
            kb.cp("act" if sb_ % 2 else "dve", hTt[jp][:, :, sb_ * 128:(sb_ + 1) * 128], pHT[:], ["pHT"], [f"hTt{jp}_{sb_}"])
        hb_ = [f"hTt{jp}_{sb_}" for sb_ in range(NSUB)]
        for m in range(2):
            pg_, pu_ = pgu[2 * m], pgu[2 * m + 1]
            for kc in range(8):
                kb.mm(pg_[:, :TS], wg[jp][:].rearrange("p k (j a) -> p k a j", a=2)[:, kc, m, :], hTt[jp][:, kc, :], kc == 0, kc == 7,
                      [f"wg{jp}"] + hb_, [f"pgu{2 * m}"])
            for kc in range(8):
                kb.mm(pu_[:, :TS], wu[jp][:].rearrange("p k (j a) -> p k a j", a=2)[:, kc, m, :], hTt[jp][:, kc, :], kc == 0, kc == 7,
                      [f"wu{jp}"] + hb_, [f"pgu{2 * m + 1}"])
            kb.act(sg[m][:], pg_[:, :TS], AF.Silu, [f"pgu{2 * m}"], [f"sg{m}"])
            kb.tt("dve", heT[jp][:, m, :], sg[m][:], pu_[:, :TS], mult, [f"sg{m}", f"pgu{2 * m + 1}"], [f"heT{jp}_{m}"])
        for sb_ in range(NSUB):
            s2 = sub_i % 2
            sub_i += 1
            for half in range(2):
                for m in range(2):
                    kb.mm(py[:, half * 512:(half + 1) * 512], heT[jp][:, m, sb_ * 128:(sb_ + 1) * 128],
                          wd[jp][:, m, half * 512:(half + 1) * 512], m == 0, m == 1,
                          [f"heT{jp}_0", f"heT{jp}_1", f"wd{jp}"], ["py"])
            kb.act(ysc[s2][:], py[:], AF.Copy, ["py", f"sl{jp}"], [f"ysc{s2}"], scale=sl[jp][:, sb_ * 4 + 1:sb_ * 4 + 2])
            Sd.dma("pool", lambda e, sb_=sb_, s2=s2, sl_i=sl_i: e.indirect_dma_start(
                out=S_ymoe, out_offset=bass.IndirectOffsetOnAxis(ap=sl_i[:, sb_ * 4 + 2:sb_ * 4 + 3], axis=0), in_=ysc[s2][:], in_offset=None,
                bounds_check=BC(e, 2 * S - 1), oob_is_err=False), f"ysc{s2}", [f"ysc{s2}", f"sl{jp}"],
                ["S_ymoe"] if serial_scatter else [])
    kb.barrier()
    kb.pop()
    kb.pop()
    if stop_after == "P5c":
        return nc, kb

    kb.push()
    pnw = vec_pk("pnw")
    wpg = kb.sb("wpg", [128, 8, 1024], BF16)
    wpg_st = [kb.sb(f"wpg_st{i}", [128, 1024], F32) for i in range(2)]
    for k in range(8):
        kb.dma("sp", wpg_st[k % 2][:], D["w_ple_gate"][k * 128:(k + 1) * 128, :], f"wpg_st{k % 2}", [], [f"wpg_st{k % 2}"])
        kb.ts("dve" if k % 2 else "pool", wpg[:, k, :], wpg_st[k % 2][:], pnw[:, k:k + 1], None, mult, None,
              [f"wpg_st{k % 2}", "vecs"], [f"wpg{k}"])
    wpgb = [f"wpg{k}" for k in range(8)]
    wpp = kb.sb("wpp", [128, 2, 1024], BF16)
    Sd.dma("pool", lambda e: e.dma_start(out=wpp[:], in_=D["w_ple_proj"].rearrange("(c p) n -> p c n", p=128)), "wpp", [], ["wpp"])
    bpg_bc = kb.sb("bpg_bc", [128, 1024], F32)
    kb.dma("sp", bpg_bc[:], D["b_ple_gate"].partition_broadcast(128), "bpg_bc", [], ["bpg_bc"])
    ppw_bc = kb.sb("ppw_bc", [128, 1024], F32)
    kb.dma("sp", ppw_bc[:], D["ple_post_norm_w"].partition_broadcast(128), "ppw_bc", [], ["ppw_bc"])
    fin_bc = kb.sb("fin_bc", [128, 1024], F32)
    kb.dma("sp", fin_bc[:], D["final_norm_w"].partition_broadcast(128), "fin_bc", [], ["fin_bc"])
    x1l = [kb.sb(f"x1l{i}", [128, 1024], F32) for i in range(2)]
    y0l = [kb.sb(f"y0l{i}", [128, 1024], F32) for i in range(2)]
    y1l = [kb.sb(f"y1l{i}", [128, 1024], F32) for i in range(2)]
    pl = [kb.sb(f"pl{i}", [128, 256], F32) for i in range(2)]
    plb_2 = [kb.sb(f"plb_{i}", [128, 256], BF16) for i in range(2)]
    pTs_2 = [kb.sb(f"pTs_{i}", [128, 2, 128], BF16) for i in range(2)]
    x2_2 = [kb.sb(f"x2_{i}", [128, 1024], F32) for i in range(2)]
    jk2_2 = [kb.sb(f"jk2_{i}", [128, 1024], BF16) for i in range(2)]
    s6_2 = [[kb.sb(f"s6_{i}_{q}", [128, 1], F32) for i in range(3)] for q in range(2)]
    n3b_2 = [kb.sb(f"n3b_{i}", [128, 1024], BF16) for i in range(2)]
    n3T_2 = [kb.sb(f"n3T_{i}", [128, 8, 128], BF16) for i in range(2)]
    gate_2 = [kb.sb(f"gate_{i}", [128, 1024], F32) for i in range(2)]
    ple_2 = [kb.sb(f"ple_{i}", [128, 1024], F32) for i in range(2)]
    x3_2 = [kb.sb(f"x3_{i}", [128, 1024], F32) for i in range(2)]
    ot = [kb.sb(f"ot{i}", [128, 1024], F32) for i in range(2)]
    pGt = kb.ps("pGt", [128, 1024], F32)
    pPp = kb.ps("pPp", [128, 1024], F32)
    pN3 = kb.ps("pN3", [128, 8, 128], BF16)
    pPT = kb.ps("pPT", [128, 8, 128], BF16)

    def rstd_of(src_ap, src_bufs, st, stname, junk_ap, junkname):
        kb.act(junk_ap, src_ap, AF.Square, src_bufs, [junkname, stname], accum_out=st[:])
        kb.ts("dve", st[:], st[:], 1.0 / 1024, EPS, mult, add, [stname], [stname])
        kb.act(st[:], st[:], AF.Ln, [stname], [stname])
        kb.act(st[:], st[:], AF.Exp, [stname], [stname], scale=-0.5)

    for tt_ in range(NT):
        b2 = tt_ % 2
        plb = plb_2[b2]
        pTs = pTs_2[b2]
        x2 = x2_2[b2]
        jk2 = jk2_2[b2]
        n3b = n3b_2[b2]
        n3T = n3T_2[b2]
        gate = gate_2[b2]
        ple = ple_2[b2]
        x3 = x3_2[b2]
        s6 = s6_2[b2]
        ts0, ts1 = tt_ * 128, (tt_ + 1) * 128
        kb.dma("sp", x1l[b2][:], S_x1[ts0:ts1, :], f"x1l{b2}", [], [f"x1l{b2}"])
        kb.dma("sp", y0l[b2][:], S_ymoe[ts0:ts1, :], f"y0l{b2}", [], [f"y0l{b2}"])
        kb.dma("sp", y1l[b2][:], S_ymoe[S + ts0:S + ts1, :], f"y1l{b2}", [], [f"y1l{b2}"])
        kb.dma("sp", pl[b2][:], D["p"][ts0:ts1, :], f"pl{b2}", [], [f"pl{b2}"])
        kb.tt("pool", x2[:], x1l[b2][:], y0l[b2][:], add, [f"x1l{b2}", f"y0l{b2}"], [f"x2_{b2}"])
        kb.tt("pool", x2[:], x2[:], y1l[b2][:], add, [f"x2_{b2}", f"y1l{b2}"], [f"x2_{b2}"])
        rstd_of(x2[:], [f"x2_{b2}"], s6[0], f"s6_0_{b2}", jk2[:], f"jk2_{b2}")
        kb.ts("dve", n3b[:], x2[:], s6[0][:, 0:1], None, mult, None, [f"x2_{b2}", f"s6_0_{b2}"], [f"n3b_{b2}"])
        for k in range(8):
            kb.tr(pN3[:, k, :], n3b[:, k * 128:(k + 1) * 128], ident[:], [f"n3b_{b2}", "ident"], ["pN3"])
        kb.cp("act", n3T[:], pN3[:], ["pN3"], [f"n3T_{b2}"])
        for half in range(2):
            for k in range(8):
                kb.mm(pGt[:, half * 512:(half + 1) * 512], n3T[:, k, :], wpg[:, k, half * 512:(half + 1) * 512], k == 0, k == 7,
                      [f"n3T_{b2}"] + wpgb, ["pGt"])
        kb.tt("dve", gate[:], pGt[:], bpg_bc[:], add, ["pGt", "bpg_bc"], [f"gate_{b2}"])
        kb.act(gate[:], gate[:], AF.Sigmoid, [f"gate_{b2}"], [f"gate_{b2}"])
        kb.cp("pool", plb[:], pl[b2][:], [f"pl{b2}"], [f"plb_{b2}"])
        for c in range(2):
            kb.tr(pPT[:, c, :], plb[:, c * 128:(c + 1) * 128], ident[:], [f"plb_{b2}", "ident"], ["pPT"])
        kb.cp("dve", pTs[:], pPT[:, 0:2, :], ["pPT"], [f"pTs_{b2}"])
        for half in range(2):
            for c in range(2):
                kb.mm(pPp[:, half * 512:(half + 1) * 512], pTs[:, c, :], wpp[:, c, half * 512:(half + 1) * 512], c == 0, c == 1,
                      [f"pTs_{b2}", "wpp"], ["pPp"])
        rstd_of(pPp[:], ["pPp"], s6[1], f"s6_1_{b2}", jk2[:], f"jk2_{b2}")
        kb.stt(ple[:], pPp[:], s6[1][:, 0:1], ppw_bc[:], mult, mult, ["pPp", f"s6_1_{b2}", "ppw_bc"], [f"ple_{b2}"])
        kb.tt("pool", ple[:], ple[:], gate[:], mult, [f"ple_{b2}", f"gate_{b2}"], [f"ple_{b2}"])
        kb.tt("dve", x3[:], x2[:], ple[:], add, [f"x2_{b2}", f"ple_{b2}"], [f"x3_{b2}"])
        rstd_of(x3[:], [f"x3_{b2}"], s6[2], f"s6_2_{b2}", jk2[:], f"jk2_{b2}")
        kb.stt(ot[b2][:], x3[:], s6[2][:, 0:1], fin_bc[:], mult, mult, [f"x3_{b2}", f"s6_2_{b2}", "fin_bc"], [f"ot{b2}"])
        kb.dma("sp", OUT[ts0:ts1, :], ot[b2][:], f"ot{b2}", [f"ot{b2}"], [])
    kb.barrier()
    kb.pop()
    return nc, kb


_NC_CACHE = {}


def kernel(**inputs):
    x = np.asarray(inputs["x"], dtype=np.float32)
    B, S, _ = x.shape
    assert B == 8
    p = np.asarray(inputs["p"], dtype=np.float32)
    pos = np.asarray(inputs["positions"]).astype(np.int32)
    if S not in _NC_CACHE:
        nc, kb = build(S)
        kb.S.emit(nc, None)
        _NC_CACHE[S] = nc
    nc = _NC_CACHE[S]
    shared = {}
    for name, shape in WEIGHT_SPECS:
        a = np.asarray(inputs[name], dtype=np.float32)
        if name != "final_norm_w":
            a = a[0]
        shp = shape if len(shape) == 2 else [1, shape[0]]
        shared[name] = np.ascontiguousarray(a.reshape(shp))
    in_maps = []
    for b in range(B):
        m = dict(shared)
        m["x"] = np.ascontiguousarray(x[b])
        m["p"] = np.ascontiguousarray(p[0, b])
        m["pos"] = np.ascontiguousarray(pos[b].reshape(S // 128, 128))
        in_maps.append(m)
    res = run_bass_kernel_spmd(nc, in_maps, core_ids=list(range(B)))
    return np.stack([np.asarray(r["out"], dtype=np.float32) for r in res.results], axis=0)
```

```python
import numpy as np
import concourse.bass as bass
import concourse.mybir as mybir
from concourse.bass_utils import run_bass_kernel_spmd

F32 = mybir.dt.float32
BF16 = mybir.dt.bfloat16
I32 = mybir.dt.int32
AF = mybir.ActivationFunctionType
ALU = mybir.AluOpType
AX = mybir.AxisListType

ENGS = ("pe", "act", "dve", "pool", "sp")


class Buf:
    __slots__ = ("name", "w", "r")

    def __init__(self, name):
        self.name = name
        self.w = None
        self.r = []


class _Op:
    __slots__ = ("eng", "fn", "deps", "dma_key", "signal", "count", "kind")

    def __init__(self, eng, fn, deps, dma_key, kind):
        self.eng = eng
        self.fn = fn
        self.deps = deps
        self.dma_key = dma_key
        self.signal = False
        self.count = 0
        self.kind = kind


class Sched:
    def __init__(self):
        self.ops = []
        self.bufs = {}
        self.psum_names = set()

    def buf(self, name):
        b = self.bufs.get(name)
        if b is None:
            b = self.bufs[name] = Buf(name)
        return b

    def _norm(self, xs):
        out = []
        for x in xs:
            if x is None:
                continue
            out.append(self.buf(x) if isinstance(x, str) else x)
        return out

    def op(self, eng, fn, reads=(), writes=(), dma_key=None):
        reads = self._norm(reads)
        writes = self._norm(writes)
        deps = set()
        for b in reads:
            if b.w is not None:
                deps.add(b.w)
            if b.name in self.psum_names:
                deps.update(r for r in b.r if self.ops[r].eng != eng)
        for b in writes:
            if b.w is not None:
                deps.add(b.w)
            deps.update(b.r)
        if eng == "pe":
            deps = set(d for d in deps if self.ops[d].eng != "pe")
        i = len(self.ops)
        self.ops.append(_Op(eng, fn, deps, dma_key, "dma" if dma_key else "op"))
        for b in reads:
            b.r.append(i)
        for b in writes:
            b.w = i
            b.r = []
        return i

    def dma(self, eng, fn, key, reads=(), writes=()):
        return self.op(eng, fn, reads, writes, dma_key=eng + "_" + key)

    def barrier(self):
        self.ops.append(_Op(None, None, set(), None, "barrier"))

    def emit(self, nc, engines):
        ops = self.ops
        last = {e: None for e in ENGS}
        pend_dma = []
        bar_deps = {}
        for i, o in enumerate(ops):
            if o.kind == "barrier":
                d = set(v for v in last.values() if v is not None)
                d.update(pend_dma)
                bar_deps[i] = d
                pend_dma = []
            else:
                last[o.eng] = i
                if o.kind == "dma":
                    pend_dma.append(i)
        for i, o in enumerate(ops):
            for d in o.deps:
                if ops[d].kind == "op":
                    ops[d].signal = True
        for d in bar_deps.values():
            for j in d:
                if ops[j].kind == "op":
                    ops[j].signal = True
        cnt = {e: 0 for e in ENGS}
        dcnt = {}
        for o in ops:
            if o.kind == "op" and o.signal:
                cnt[o.eng] += 1
                o.count = cnt[o.eng]
            elif o.kind == "dma":
                dcnt[o.dma_key] = dcnt.get(o.dma_key, 0) + 16
                o.count = dcnt[o.dma_key]
        sem_names = ["e_" + e for e in ENGS] + ["d_" + k for k in dcnt]
        import contextlib
        with contextlib.ExitStack() as st:
            sems = {n: st.enter_context(nc.semaphore(n)) for n in sem_names}
            seen = {e: {} for e in ENGS}
            streams = {e: [] for e in ENGS}

            def need(e, d):
                p = ops[d]
                name = ("d_" + p.dma_key) if p.kind == "dma" else ("e_" + p.eng)
                if seen[e].get(name, 0) >= p.count:
                    return
                seen[e][name] = p.count
                streams[e].append(("wait", name, p.count))

            for i, o in enumerate(ops):
                if o.kind == "barrier":
                    best = {}
                    for d in bar_deps[i]:
                        p = ops[d]
                        name = ("d_" + p.dma_key) if p.kind == "dma" else ("e_" + p.eng)
                        if name not in best or ops[best[name]].count < p.count:
                            best[name] = d
                    for e in ENGS:
                        for d in sorted(best.values()):
                            need(e, d)
                    continue
                for d in sorted(o.deps):
                    need(o.eng, d)
                streams[o.eng].append(("op", o))
            self.streams = streams
            block = st.enter_context(nc.Block())

            def run(e, eng):
                for it in streams[e]:
                    if it[0] == "wait":
                        eng.wait_ge(sems[it[1]], it[2])
                    else:
                        o = it[1]
                        ins = o.fn(eng)
                        if o.kind == "dma":
                            ins.then_inc(sems["d_" + o.dma_key], 16)
                        elif o.signal:
                            ins.then_inc(sems["e_" + o.eng], 1)

            @block.tensor
            def _(eng):
                run("pe", eng)

            @block.scalar
            def _(eng):
                run("act", eng)

            @block.vector
            def _(eng):
                run("dve", eng)

            @block.gpsimd
            def _(eng):
                run("pool", eng)

            @block.sync
            def _(eng):
                run("sp", eng)
        return {e: len(streams[e]) for e in ENGS}


D_MODEL = 1024
PLE_DIM = 256
EPS = 1e-6
NH = 8
QR, KVR, ROPE, NOPE, VD = 256, 128, 32, 64, 64
SH, SP_, SG, SN, SCONV = 8, 64, 2, 64, 5
SINNER, SXBC = 512, 768
INCOLS = 1712
NEXP, NGRP, EPG, DEXP = 32, 4, 8, 256
C_CQ, C_CKV, C_KR, C_Z, C_XBC, C_DT = 0, 256, 384, 416, 928, 1696

WEIGHT_SPECS = [
    ("attn_norm_w", [1024]), ("w_in", [1024, 1712]), ("q_norm_w", [256]), ("w_uq", [256, 768]),
    ("kv_norm_w", [128]), ("w_ukv", [128, 1024]), ("attn_out_norm_w", [512]), ("conv_w", [5, 768]),
    ("conv_b", [768]), ("dt_bias", [1, 16]), ("a_log", [1, 16]), ("ssd_d", [1, 8]), ("ssd_norm_w", [512]),
    ("w_o", [1024, 1024]), ("ffn_norm_w", [1024]), ("w_router_group", [1024, 4]), ("b_router_group", [1, 4]),
    ("w_router_expert", [1024, 32]), ("b_router_expert", [1, 32]), ("w_exp_gate", [32 * 1024, 256]),
    ("w_exp_up", [32 * 1024, 256]), ("w_exp_down", [32 * 256, 1024]), ("ple_norm_w", [1024]),
    ("w_ple_gate", [1024, 1024]), ("b_ple_gate", [1, 1024]), ("w_ple_proj", [256, 1024]),
    ("ple_post_norm_w", [1024]), ("final_norm_w", [1024]),
]


class KB:
    def __init__(self, nc):
        import contextlib
        self.nc = nc
        self.S = Sched()
        self.root = contextlib.ExitStack()
        self.stack = [self.root]

    def push(self):
        import contextlib
        st = contextlib.ExitStack()
        self.stack.append(st)
        return st

    def pop(self):
        self.stack.pop().close()

    def sb(self, name, shape, dt):
        return self.stack[-1].enter_context(self.nc.sbuf_tensor(name, list(shape), dt))

    def ps(self, name, shape, dt=F32):
        self.S.psum_names.add(name)
        return self.stack[-1].enter_context(self.nc.psum_tensor(name, list(shape), dt))

    def act(self, out, in_, func, r, w, **kw):
        self.S.op("act", lambda e: e.activation(out=out, in_=in_, func=func, **kw), r, w)

    def ts(self, eng, out, in0, s1, s2, op0, op1, r, w):
        if op1 is None:
            self.S.op(eng, lambda e: e.tensor_scalar(out=out, in0=in0, scalar1=s1, scalar2=None, op0=op0), r, w)
        else:
            self.S.op(eng, lambda e: e.tensor_scalar(out=out, in0=in0, scalar1=s1, scalar2=s2, op0=op0, op1=op1), r, w)

    def tt(self, eng, out, in0, in1, op, r, w):
        self.S.op(eng, lambda e: e.tensor_tensor(out=out, in0=in0, in1=in1, op=op), r, w)

    def stt(self, out, in0, scalar, in1, op0, op1, r, w):
        self.S.op("dve", lambda e: e.scalar_tensor_tensor(out=out, in0=in0, scalar=scalar, in1=in1, op0=op0, op1=op1), r, w)

    def cp(self, eng, out, in_, r, w):
        if eng == "act":
            self.S.op("act", lambda e: e.activation(out=out, in_=in_, func=AF.Copy), r, w)
        else:
            self.S.op(eng, lambda e: e.tensor_copy(out=out, in_=in_), r, w)

    def memset(self, eng, ap, val, w):
        self.S.op(eng, lambda e: e.memset(ap, val), (), w)

    def mm(self, out, lhsT, rhs, start, stop, r, w, **kw):
        self.S.op("pe", lambda e: e.matmul(out, lhsT=lhsT, rhs=rhs, start=start, stop=stop, **kw), r, w)

    def tr(self, out, in_, ident, r, w):
        self.S.op("pe", lambda e: e.transpose(out=out, in_=in_, identity=ident), r, w)

    def dma(self, eng, out, in_, key, r, w, **kw):
        self.S.dma(eng, lambda e: e.dma_start(out=out, in_=in_, **kw), key, r, w)

    def barrier(self):
        self.S.barrier()

    def reduce(self, out, in_, op, r, w):
        self.S.op("dve", lambda e: e.tensor_reduce(out=out, in_=in_, axis=AX.X, op=op), r, w)

    def recip(self, out, in_, r, w):
        self.S.op("dve", lambda e: e.reciprocal(out=out, in_=in_), r, w)

    def max8(self, out, in_, r, w):
        self.S.op("dve", lambda e: e.max(out=out, in_=in_), r, w)


def build(S=4096, stop_after=None, serial_scatter=False):
    import math
    nc = bass.Bass("TRN2", target_bir_lowering=False)
    NT = S // 128
    GS = min(512, S)
    NG = S // GS
    TG = GS // 128
    D = {}
    D["x"] = nc.dram_tensor("x", [S, 1024], F32, kind="ExternalInput").ap()
    D["p"] = nc.dram_tensor("p", [S, 256], F32, kind="ExternalInput").ap()
    D["pos"] = nc.dram_tensor("pos", [NT, 128], I32, kind="ExternalInput").ap()
    for name, shape in WEIGHT_SPECS:
        shp = shape if len(shape) == 2 else [1, shape[0]]
        D[name] = nc.dram_tensor(name, shp, F32, kind="ExternalInput").ap()
    OUT = nc.dram_tensor("out", [S, 1024], F32, kind="ExternalOutput").ap()

    def scr(name, shape, dt):
        return nc.dram_tensor(name, shape, dt).ap()

    S_cqn = scr("S_cqn", [256, S], BF16)
    S_kvn = scr("S_kvn", [128, S], BF16)
    S_sz = scr("S_sz", [512, S], BF16)
    S_xbc = scr("S_xbc", [768, S], F32)
    S_xtm = scr("S_xtm", [S, 768], BF16)
    S_bcT = scr("S_bcT", [256, S], BF16)
    S_yf = scr("S_yf", [512, S], F32)
    S_yn = scr("S_yn", [512, S], BF16)

    import os
    KD = int(os.environ.get("KDBG", "0"))
    kb = KB(nc)
    Sd = kb.S
    mult, add, sub = ALU.mult, ALU.add, ALU.subtract
    _regs = {}

    def BC(e, val):
        if val not in _regs:
            _regs[val] = e.to_reg(val)
        return _regs[val]

    VROWS = [("anw", "attn_norm_w", 8), ("cb", "conv_b", 6), ("cw", "conv_w", 30), ("kvw", "kv_norm_w", 1),
             ("qw", "q_norm_w", 2), ("pnw", "ple_norm_w", 8)]
    voff = {}
    _o = 0
    for nm, _, k in VROWS:
        voff[nm] = (_o, k)
        _o += k
    NV = _o

    identf = kb.sb("identf", [128, 128], F32)
    ident = kb.sb("ident", [128, 128], BF16)
    onesf = kb.sb("onesf", [128, 128], F32)
    onesb = kb.sb("onesb", [128, 128], BF16)
    kb.memset("pool", identf[:], 0.0, ["identf"])
    Sd.op("pool", lambda e: e.affine_select(out=identf[:], in_=identf[:], pattern=[[-1, 128]],
                                            compare_op=ALU.not_equal, fill=1.0, base=0,
                                            channel_multiplier=1), ["identf"], ["identf"])
    kb.cp("dve", ident[:], identf[:], ["identf"], ["ident"])
    kb.memset("pool", onesf[:], 1.0, ["onesf"])
    c_eps = kb.sb("c_eps", [128, 1], F32)
    c_one = kb.sb("c_one", [128, 1], F32)
    kb.memset("pool", c_eps[:], EPS, ["c_eps"])
    kb.memset("pool", c_one[:], 1.0, ["c_one"])
    kb.memset("pool", onesb[:], 1.0, ["onesb"])

    vecs = kb.sb("vecs", [128, NV], F32)
    vecs64 = kb.sb("vecs64", [64, 16], F32)
    kb.push()
    vst = kb.sb("vst", [NV, 128], F32)
    vst64 = kb.sb("vst64", [16, 64], F32)
    pvec = kb.ps("pvec", [128, 512], F32)
    for nm, dn, k in VROWS:
        o_ = voff[nm][0]
        src = D[dn]
        src = src.rearrange("o (k p) -> (o k) p", p=128) if dn != "conv_w" else src.rearrange("t (j p) -> (t j) p", p=128)
        kb.dma("sp", vst[o_:o_ + k, :], src, "vst", [], ["vst"])
    kb.dma("sp", vst64[0:8, :], D["attn_out_norm_w"].rearrange("o (k p) -> (o k) p", p=64), "vst64", [], ["vst64"])
    kb.dma("sp", vst64[8:16, :], D["ssd_norm_w"].rearrange("o (k p) -> (o k) p", p=64), "vst64", [], ["vst64"])
    kb.tr(pvec[:, 0:NV], vst[:], identf[:NV, :NV], ["vst", "identf"], ["pvec"])
    kb.cp("dve", vecs[:], pvec[:, 0:NV], ["pvec"], ["vecs"])
    kb.tr(pvec[0:64, 64:80], vst64[:], identf[:16, :16], ["vst64", "identf", "vecs"], ["pvec"])
    kb.cp("dve", vecs64[:], pvec[0:64, 64:80], ["pvec"], ["vecs64"])
    kb.barrier()
    kb.pop()

    def vec_pk(name, dram=None, k=None):
        o_, k_ = voff[name]
        return vecs[:, o_:o_ + k_]

    cosT = kb.sb("cosT", [128, NT, 16], F32)
    sinT = kb.sb("sinT", [128, NT, 16], F32)
    krot = kb.sb("krot", [128, NT, 32], F32)
    dtp = kb.sb("dtp", [128, NT, 16], F32)
    dtb_bc = kb.sb("dtb_bc", [128, 16], F32)
    a_bc = kb.sb("a_bc", [128, 16], F32)
    kb.dma("sp", dtb_bc[:], D["dt_bias"].partition_broadcast(128), "dtb_bc", [], ["dtb_bc"])
    kb.dma("sp", a_bc[:], D["a_log"].partition_broadcast(128), "a_bc", [], ["a_bc"])
    kb.act(a_bc[:], a_bc[:], AF.Exp, ["a_bc"], ["a_bc"])
    kb.ts("dve", a_bc[:], a_bc[:], -1.0, None, mult, None, ["a_bc"], ["a_bc"])
    kb.push()
    posi = kb.sb("posi", [NT, 128], I32)
    posr = kb.sb("posr", [NT, 128], F32)
    posf = kb.sb("posf", [128, NT], F32)
    invf = kb.sb("invf", [128, 16], F32)
    rr = kb.sb("rr", [128, NT, 16], F32)
    rf = kb.sb("rf", [128, NT, 16], F32)
    ri = kb.sb("ri", [128, NT, 16], I32)
    rm = kb.sb("rm", [128, NT, 16], F32)
    ppos = kb.ps("ppos", [128, 512], F32)
    kb.dma("sp", posi[:], D["pos"], "posi", [], ["posi"])
    kb.cp("dve", posr[:], posi[:], ["posi"], ["posr"])
    kb.tr(ppos[:, :NT], posr[:], identf[:NT, :NT], ["posr", "identf"], ["ppos"])
    kb.cp("dve", posf[:], ppos[:, :NT], ["ppos"], ["posf"])
    for i in range(16):
        kb.memset("pool", invf[:, i:i + 1], (10000.0 ** (-(2.0 * i) / 32.0)) / (2 * math.pi), ["invf"])
    for t in range(NT):
        kb.ts("dve", rr[:, t, :], invf[:], posf[:, t:t + 1], None, mult, None, ["invf", "posf"], ["rr"])
    for shift, dst in ((0.0, "sinT"), (0.25, "cosT")):
        dstt = sinT if dst == "sinT" else cosT
        if shift:
            kb.ts("dve", rr[:], rr[:], shift, None, add, None, ["rr"], ["rr"])
        kb.cp("dve", ri[:], rr[:], ["rr"], ["ri"])
        kb.cp("dve", rf[:], ri[:], ["ri"], ["rf"])
        kb.tt("dve", rf[:], rr[:], rf[:], sub, ["rr", "rf"], ["rf"])
        kb.ts("dve", rm[:], rf[:], 0.5, None, ALU.is_gt, None, ["rf"], ["rm"])
        kb.tt("dve", rf[:], rf[:], rm[:], sub, ["rf", "rm"], ["rf"])
        kb.ts("dve", rm[:], rf[:], -0.5, None, ALU.is_lt, None, ["rf"], ["rm"])
        kb.tt("dve", rf[:], rf[:], rm[:], add, ["rf", "rm"], ["rf"])
        kb.act(dstt[:], rf[:], AF.Sin, ["rf"], [dst], scale=2 * math.pi)
    kb.barrier()
    kb.pop()

    if stop_after == "P0":
        return nc, kb
    kb.push()
    anw = vec_pk("anw")
    w_in_bf = kb.sb("w_in_bf", [128, 8, INCOLS], BF16)
    wst = [kb.sb(f"wst{i}", [128, INCOLS], F32) for i in range(2)]
    for k in range(8):
        kb.dma("sp", wst[k % 2][:], D["w_in"][k * 128:(k + 1) * 128, :], f"wst{k % 2}", [], [f"wst{k % 2}"])
        kb.ts("dve" if k % 2 == 0 else "pool", w_in_bf[:, k, :], wst[k % 2][:], anw[:, k:k + 1], None, mult, None,
              [f"wst{k % 2}", "vecs"], ["w_in_bf"])
    wsmall = kb.sb("wsmall", [128, 8, 48], BF16)
    kb.cp("dve", wsmall[:, :, 0:32], w_in_bf[:, :, C_KR:C_KR + 32], ["w_in_bf"], ["wsmall"])
    kb.cp("dve", wsmall[:, :, 32:48], w_in_bf[:, :, C_DT:C_DT + 16], ["w_in_bf"], ["wsmall"])

    NX = 3
    xt = [kb.sb(f"xt{i}", [128, 1024], F32) for i in range(NX)]
    hn = [kb.sb(f"hn{i}", [128, 1024], BF16) for i in range(NX)]
    ss = [kb.sb(f"ss{i}", [128, 1], F32) for i in range(NX)]
    junk = kb.sb("junk", [128, 1024], BF16)
    hT = [kb.sb(f"hT{i}", [128, 8, GS], BF16) for i in range(2)]
    sm = kb.sb("sm", [128, 48], F32)
    rot = [kb.sb(f"rot{i}", [128, 16], F32) for i in range(4)]
    dx = kb.sb("dx", [128, 16], F32)
    cqf = kb.sb("cqf", [128, 2, GS], F32)
    kvf = kb.sb("kvf", [128, GS], F32)
    sq = [kb.sb(f"sq{i}", [128, GS], F32) for i in range(3)]
    rs = [kb.sb(f"rs{i}", [128, GS], F32) for i in range(2)]
    cqn = [kb.sb(f"cqn{i}", [128, 2, GS], BF16) for i in range(2)]
    kvn = [kb.sb(f"kvn{i}", [128, GS], BF16) for i in range(2)]
    szs = [kb.sb(f"szs{i}", [128, 4, GS], BF16) for i in range(2)]
    xbs = [kb.sb(f"xbs{i}", [128, 6, GS], F32) for i in range(2)]
    pT = [kb.ps(f"pT{i}", [128, 8, 128], BF16) for i in range(2)]
    pS = kb.ps("pS", [128, 512], F32)
    pM = [kb.ps(f"pM{i}", [128, 512], F32) for i in range(3)]
    pST = [kb.ps(f"pST{i}", [128, 512], F32) for i in range(2)]

    chunks = [("cq", 0, C_CQ), ("cq", 1, C_CQ + 128), ("ckv", 0, C_CKV)]
    chunks += [("z", i, C_Z + 128 * i) for i in range(4)]
    chunks += [("xbc", i, C_XBC + 128 * i) for i in range(6)]
    ci_glob = 0
    for g in range(NG):
        gp = g % 2
        for t in range(TG):
            tt_ = g * TG + t
            s = tt_ % NX
            pp = tt_ % 2
            kb.dma("sp", xt[s][:], D["x"][tt_ * 128:(tt_ + 1) * 128, :], f"xt{s}", [], [f"xt{s}"])
            kb.act(junk[:], xt[s][:], AF.Square, [f"xt{s}"], ["junk", f"ss{s}"], accum_out=ss[s][:])
            kb.ts("dve", ss[s][:], ss[s][:], 1.0 / 1024, EPS, mult, add, [f"ss{s}"], [f"ss{s}"])
            kb.act(ss[s][:], ss[s][:], AF.Ln, [f"ss{s}"], [f"ss{s}"])
            kb.act(ss[s][:], ss[s][:], AF.Exp, [f"ss{s}"], [f"ss{s}"], scale=-0.5)
            kb.ts("dve", hn[s][:], xt[s][:], ss[s][:], None, mult, None, [f"xt{s}", f"ss{s}"], [f"hn{s}"])
            for k in range(8):
                kb.tr(pT[pp][:, k, :], hn[s][:, k * 128:(k + 1) * 128], ident[:], [f"hn{s}", "ident"], [f"pT{pp}"])
            kb.cp("act" if t % 2 else "dve", hT[gp][:, :, t * 128:(t + 1) * 128], pT[pp][:], [f"pT{pp}"], [f"hT{gp}_{t}"])
            if KD and KD < 2:
                continue
            for k in range(8):
                kb.mm(pS[:, :48], hT[gp][:, k, t * 128:(t + 1) * 128], wsmall[:, k, :], k == 0, k == 7,
                      [f"hT{gp}_{t}", "wsmall"], ["pS"])
            kb.cp("dve", sm[:], pS[:, :48], ["pS"], ["sm"])
            if KD and KD < 3:
                continue
            k1, k2 = sm[:, 0:16], sm[:, 16:32]
            cs_, sn_ = cosT[:, tt_, :], sinT[:, tt_, :]
            kb.tt("pool", rot[0][:], k1, cs_, mult, ["sm", "cosT"], ["rot0"])
            kb.tt("pool", rot[1][:], k2, sn_, mult, ["sm", "sinT"], ["rot1"])
            kb.tt("pool", krot[:, tt_, 0:16], rot[0][:], rot[1][:], sub, ["rot0", "rot1"], ["krot"])
            kb.tt("pool", rot[2][:], k2, cs_, mult, ["sm", "cosT"], ["rot2"])
            kb.tt("pool", rot[3][:], k1, sn_, mult, ["sm", "sinT"], ["rot3"])
            kb.tt("pool", krot[:, tt_, 16:32], rot[2][:], rot[3][:], add, ["rot2", "rot3"], ["krot"])
            kb.tt("dve", dx[:], sm[:, 32:48], dtb_bc[:], add, ["sm", "dtb_bc"], ["dx"])
            kb.act(dx[:], dx[:], AF.Exp, ["dx"], ["dx"])
            kb.act(dtp[:, tt_, :], dx[:], AF.Ln, ["dx", "c_one"], ["dtp"], bias=c_one[:])
        hT_bufs = [f"hT{gp}_{t}" for t in range(TG)]
        for (kind, i, c0) in (chunks if not KD else chunks[:max(0, KD - 3)]):
            pi = ci_glob % 3
            ci_glob += 1
            pm = pM[pi]
            for k in range(8):
                kb.mm(pm[:, :GS], w_in_bf[:, k, c0:c0 + 128], hT[gp][:, k, :], k == 0, k == 7,
                      ["w_in_bf"] + hT_bufs, [f"pM{pi}"])
            KD2 = int(os.environ.get("KDBG2", "9"))
            if kind == "cq":
                if KD2 >= 1 and os.environ.get("KNODVE") != "1":
                    kb.cp("dve", cqf[:, i, :], pm[:, :GS], [f"pM{pi}"], [f"cqf{i}"])
                if KD2 >= 2:
                    if os.environ.get("KSQ") == "dve":
                        kb.cp("dve", sq[i][:], pm[:, :GS], [f"pM{pi}"], [f"sq{i}"])
                    elif os.environ.get("KSQ") == "junk":
                        kb.act(junk[:, :GS], pm[:, :GS], AF.Square, [f"pM{pi}"], ["junk"])
                    else:
                        kb.act(sq[i][:], pm[:, :GS], AF.Square, [f"pM{pi}"], [f"sq{i}"])
                if KD2 >= 3:
                    kb.mm(pST[0][:, :GS], onesf[:], sq[i][:], i == 0, i == 1, ["onesf", f"sq{i}"], ["pST0"])
                if i == 1:
                    kb.act(rs[0][:], pST[0][:, :GS], AF.Ln, ["pST0", "c_eps"], ["rs0"], scale=1.0 / 256, bias=c_eps[:])
                    kb.act(rs[0][:], rs[0][:], AF.Exp, ["rs0"], ["rs0"], scale=-0.5)
                    for j in range(2):
                        kb.tt("dve", cqn[gp][:, j, :], cqf[:, j, :], rs[0][:], mult, [f"cqf{j}", "rs0"], [f"cqn{gp}_{j}"])
                    kb.dma("sp", S_cqn.rearrange("(c p) s -> p c s", p=128)[:, :, g * GS:(g + 1) * GS], cqn[gp][:],
                           f"cqn{gp}", [f"cqn{gp}_0", f"cqn{gp}_1"], [])
            elif kind == "ckv":
                kb.cp("dve", kvf[:], pm[:, :GS], [f"pM{pi}"], ["kvf"])
                kb.act(sq[2][:], pm[:, :GS], AF.Square, [f"pM{pi}"], ["sq2"])
                kb.mm(pST[1][:, :GS], onesf[:], sq[2][:], True, True, ["onesf", "sq2"], ["pST1"])
                kb.act(rs[1][:], pST[1][:, :GS], AF.Ln, ["pST1", "c_eps"], ["rs1"], scale=1.0 / 128, bias=c_eps[:])
                kb.act(rs[1][:], rs[1][:], AF.Exp, ["rs1"], ["rs1"], scale=-0.5)
                kb.tt("dve", kvn[gp][:], kvf[:], rs[1][:], mult, ["kvf", "rs1"], [f"kvn{gp}"])
                kb.dma("sp", S_kvn[:, g * GS:(g + 1) * GS], kvn[gp][:], f"kvn{gp}", [f"kvn{gp}"], [])
            elif kind == "z":
                kb.act(szs[gp][:, i, :], pm[:, :GS], AF.Silu, [f"pM{pi}"], [f"szs{gp}_{i}"])
                if i == 3:
                    kb.dma("sp", S_sz.rearrange("(c p) s -> p c s", p=128)[:, :, g * GS:(g + 1) * GS], szs[gp][:],
                           f"szs{gp}", [f"szs{gp}_{j}" for j in range(4)], [])
            else:
                kb.cp("dve" if i % 2 else "act", xbs[gp][:, i, :], pm[:, :GS], [f"pM{pi}"], [f"xbs{gp}_{i}"])
                if i == 5:
                    kb.dma("sp", S_xbc.rearrange("(c p) s -> p c s", p=128)[:, :, g * GS:(g + 1) * GS], xbs[gp][:],
                           f"xbs{gp}", [f"xbs{gp}_{j}" for j in range(6)], [])
    kb.barrier()
    kb.pop()
    if stop_after == "P1":
        return nc, kb

    kb.push()
    cw = vec_pk("cw").rearrange("p (t j) -> p t j", j=6)
    cb = vec_pk("cb")
    xin = [kb.sb(f"xin{i}", [128, 6, GS + 4], F32) for i in range(2)]
    acc = [kb.sb(f"acc{i}", [128, 6, GS], F32) for i in range(2)]
    xc = [kb.sb(f"xc{i}", [128, 6, GS], BF16) for i in range(2)]
    xts = [kb.sb(f"xts{i}", [128, 768], BF16) for i in range(2)]
    pX = [kb.ps(f"pX{i}", [128, 6, 128], BF16) for i in range(2)]
    xbc_v = S_xbc.rearrange("(c p) s -> p c s", p=128)
    for g in range(NG):
        gp = g % 2
        lo, hi = g * GS - 2, g * GS + GS + 2
        clo, chi = max(lo, 0), min(hi, S)
        if lo < 0:
            kb.memset("pool", xin[gp][:, :, 0:2], 0.0, [f"xin{gp}"])
        if hi > S:
            kb.memset("pool", xin[gp][:, :, GS + 2:GS + 4], 0.0, [f"xin{gp}"])
        kb.dma("sp", xin[gp][:, :, clo - lo:chi - lo], xbc_v[:, :, clo:chi], f"xin{gp}", [], [f"xin{gp}"])
        for j in range(6):
            kb.ts("dve", acc[gp][:, j, :], xin[gp][:, j, 0:GS], cw[:, 0, j:j + 1], cb[:, j:j + 1], mult, add,
                  [f"xin{gp}", "vecs"], [f"acc{gp}_{j}"])
            for k in range(1, 5):
                kb.stt(acc[gp][:, j, :], xin[gp][:, j, k:k + GS], cw[:, k, j:j + 1], acc[gp][:, j, :], mult, add,
                       [f"xin{gp}", "vecs", f"acc{gp}_{j}"], [f"acc{gp}_{j}"])
            kb.act(xc[gp][:, j, :], acc[gp][:, j, :], AF.Silu, [f"acc{gp}_{j}"], [f"xc{gp}_{j}"])
        xcb = [f"xc{gp}_{j}" for j in range(6)]
        kb.dma("sp", S_bcT.rearrange("(j p) s -> p j s", p=128)[:, :, g * GS:(g + 1) * GS], xc[gp][:, 4:6, :],
               f"xc{gp}", xcb, [])
        for t in range(TG):
            tt_ = g * TG + t
            pp = tt_ % 2
            for j in range(6):
                kb.tr(pX[pp][:, j, :], xc[gp][:, j, t * 128:(t + 1) * 128], ident[:], [f"xc{gp}_{j}", "ident"], [f"pX{pp}"])
            kb.cp("act" if t % 2 else "dve", xts[pp][:], pX[pp][:], [f"pX{pp}"], [f"xts{pp}"])
            kb.dma("sp", S_xtm[tt_ * 128:(tt_ + 1) * 128, :], xts[pp][:], f"xts{pp}", [f"xts{pp}"], [])
    kb.barrier()
    kb.pop()
    if stop_after == "P1b":
        return nc, kb

    kb.push()
    tri = [kb.sb("triU", [128, 128], F32), kb.sb("triL", [128, 128], F32)]
    for d_, (cm_, pat) in enumerate(((-1, 1), (1, -1))):
        kb.memset("pool", tri[d_][:], 1.0, [f"tri{d_}"])
        Sd.op("pool", lambda e, d_=d_, cm_=cm_, pat=pat: e.affine_select(
            out=tri[d_][:], in_=tri[d_][:], pattern=[[pat, 128]], compare_op=ALU.is_ge, fill=0.0, base=0,
            channel_multiplier=cm_), [f"tri{d_}"], [f"tri{d_}"])
    Esel = kb.sb("Esel", [8, 8, 128], F32)
    kb.memset("pool", Esel[:], 0.0, ["Esel"])
    Sd.op("pool", lambda e: e.affine_select(out=Esel[:], in_=Esel[:], pattern=[[-1, 8], [0, 128]],
                                            compare_op=ALU.not_equal, fill=1.0, base=0, channel_multiplier=1),
          ["Esel"], ["Esel"])
    d_bc = kb.sb("d_bc", [128, 8], F32)
    kb.dma("sp", d_bc[:], D["ssd_d"].partition_broadcast(128), "d_bc", [], ["d_bc"])
    diagD = kb.sb("diagD", [128, 8, 128], BF16)
    for r in range(8):
        kb.ts("dve", diagD[:, r, :], identf[:], d_bc[:, r:r + 1], None, mult, None, ["identf", "d_bc"], ["diagD"])
    xtm = [kb.sb(f"xtm{i}", [128, 768], BF16) for i in range(2)]
    bcT = [kb.sb(f"bcT{i}", [64, 4, 128], BF16) for i in range(2)]
    A_ = kb.sb("A_", [128, 8], F32)
    cs_sb = kb.sb("cs_sb", [128, 8], F32)
    csT_sb = kb.sb("csT_sb", [8, 128], F32)
    seg = kb.sb("seg", [128, 8, 128], F32)
    GTm = kb.sb("GTm", [128, 2, 128], F32)
    MT = kb.sb("MT", [128, 8, 128], BF16)
    ecs = kb.sb("ecs", [64, 8, 128], F32)
    Cdec = kb.sb("Cdec", [64, 8, 128], BF16)
    wex = kb.sb("wex", [128, 8], F32)
    xw = kb.sb("xw", [128, 8, 64], BF16)
    xdt = kb.sb("xdt", [128, 8, 64], BF16)
    dec = kb.sb("dec", [64, 8], F32)
    hst = kb.sb("hst", [64, 8, 64], F32)
    hbf = kb.sb("hbf", [64, 8, 64], BF16)
    ysb = [kb.sb(f"ysb{i}", [64, 8, 128], F32) for i in range(2)]
    yfl = [kb.sb(f"yfl{i}", [64, 8, 128], F32) for i in range(2)]
    szl = [kb.sb(f"szl{i}", [64, 8, 128], BF16) for i in range(2)]
    yg = kb.sb("yg", [64, 8, 128], F32)
    sqy = kb.sb("sqy", [64, 8, 128], F32)
    rsy = kb.sb("rsy", [64, 128], F32)
    ynb = [kb.sb(f"ynb{i}", [64, 8, 128], BF16) for i in range(2)]
    pA = kb.ps("pA", [128, 512], F32)
    pB = kb.ps("pB", [64, 8, 64], F32)
    pC = kb.ps("pC", [64, 512], F32)
    pCS = kb.ps("pCS", [128, 8, 128], F32)
    pG = kb.ps("pG", [128, 512], F32)
    pY = kb.ps("pY", [64, 8, 128], F32)
    yf_v = S_yf.rearrange("(r p) s -> p r s", p=64)
    yn_v = S_yn.rearrange("(r p) s -> p r s", p=64)
    sz_v = S_sz.rearrange("(r p) s -> p r s", p=64)
    bcT_v = S_bcT.rearrange("(q n) s -> n q s", n=64)
    for d_ in range(2):
        kb.memset("pool", hst[:], 0.0, ["hst"])
        kb.memset("pool", hbf[:], 0.0, ["hbf"])
        order = list(range(NT)) if d_ == 0 else list(range(NT - 1, -1, -1))
        for ci, c in enumerate(order):
            b2 = ci % 2
            cs0, cs1 = c * 128, (c + 1) * 128
            kb.dma("sp", xtm[b2][:], S_xtm[cs0:cs1, :], f"xtm{b2}", [], [f"xtm{b2}"])
            kb.dma("sp", bcT[b2][:], bcT_v[:, :, cs0:cs1], f"bcT{b2}", [], [f"bcT{b2}"])
            if d_ == 1:
                kb.dma("sp", yfl[b2][:], yf_v[:, :, cs0:cs1], f"yfl{b2}", [], [f"yfl{b2}"])
                kb.dma("sp", szl[b2][:], sz_v[:, :, cs0:cs1], f"szl{b2}", [], [f"szl{b2}"])
            dts = dtp[:, c, d_ * 8:(d_ + 1) * 8]
            kb.tt("dve", A_[:], dts, a_bc[:, d_ * 8:(d_ + 1) * 8], mult, ["dtp", "a_bc"], ["A_"])
            kb.mm(pA[:, 0:8], tri[d_][:], A_[:], True, True, [f"tri{d_}", "A_"], ["pA"])
            kb.mm(pA[:, 8:16], onesf[:], A_[:], True, True, ["onesf", "A_"], ["pA"])
            kb.cp("dve", cs_sb[:], pA[:, 0:8], ["pA"], ["cs_sb"])
            kb.tr(pA[0:8, 128:256], cs_sb[:], identf[:], ["cs_sb", "identf"], ["pA"])
            kb.cp("dve", csT_sb[:], pA[0:8, 128:256], ["pA"], ["csT_sb"])
            for r in range(8):
                kb.mm(pCS[:, r, :], Esel[:, r, :], csT_sb[:], True, True, ["Esel", "csT_sb"], ["pCS"])
            kb.tt("dve", seg[:], pCS[:], cs_sb[:, :, None].to_broadcast([128, 8, 128]), sub, ["pCS", "cs_sb"], ["seg"])
            kb.ts("dve", seg[:], seg[:], 0.0, None, ALU.min, None, ["seg"], ["seg"])
            kb.act(seg[:], seg[:], AF.Exp, ["seg"], ["seg"])
            kb.act(ecs[:], pCS[0:64, :, :], AF.Exp, ["pCS"], ["ecs"])
            for g_ in range(2):
                kb.mm(pG[:, g_ * 128:(g_ + 1) * 128], bcT[b2][:, g_, :], bcT[b2][:, 2 + g_, :], True, True,
                      [f"bcT{b2}"], ["pG"])
            kb.tt("dve", GTm[:], pG[:, 0:256].rearrange("p (g i) -> p g i", g=2), tri[d_][:, None, :].to_broadcast([128, 2, 128]),
                  mult, ["pG", f"tri{d_}"], ["GTm"])
            kb.tt("dve", xdt[:], xtm[b2][:, 0:512].rearrange("p (r q) -> p r q", r=8), dts[:, :, None].to_broadcast([128, 8, 64]),
                  mult, [f"xtm{b2}", "dtp"], ["xdt"])
            kb.tt("dve", MT[:].rearrange("p (g r) i -> p g r i", g=2), seg[:].rearrange("p (g r) i -> p g r i", g=2),
                  GTm[:, :, None, :].to_broadcast([128, 2, 4, 128]), mult, ["seg", "GTm"], ["MT"])
            kb.tt("dve", Cdec[:].rearrange("p (g r) i -> p g r i", g=2), ecs[:].rearrange("p (g r) i -> p g r i", g=2),
                  bcT[b2][:, 2:4, None, :].to_broadcast([64, 2, 4, 128]), mult, ["ecs", f"bcT{b2}"], ["Cdec"])
            for r in range(8):
                kb.mm(pY[:, r, :], xdt[:, r, :], MT[:, r, :], True, False, ["xdt", "MT"], ["pY"])
                if d_ == 0:
                    kb.mm(pY[:, r, :], xtm[b2][:, r * 64:(r + 1) * 64], diagD[:, r, :], False, False, [f"xtm{b2}", "diagD"], ["pY"])
                kb.mm(pY[:, r, :], hbf[:, r, :], Cdec[:, r, :], False, True, ["hbf", "Cdec"], ["pY"])
            kb.tt("dve", wex[:], pA[:, 8:16], cs_sb[:], sub, ["pA", "cs_sb"], ["wex"])
            kb.act(wex[:], wex[:], AF.Exp, ["wex"], ["wex"])
            kb.tt("dve", xw[:], xdt[:], wex[:, :, None].to_broadcast([128, 8, 64]), mult, ["xdt", "wex"], ["xw"])
            for g_ in range(2):
                kb.mm(pB[:, g_ * 4:(g_ + 1) * 4, :], xtm[b2][:, 512 + g_ * 64:512 + (g_ + 1) * 64],
                      xw[:, g_ * 4:(g_ + 1) * 4, :], True, True, [f"xtm{b2}", "xw"], ["pB"])
            kb.act(dec[:], pA[0:64, 8:16], AF.Exp, ["pA"], ["dec"])
            kb.tt("dve", hst[:], hst[:], dec[:, :, None].to_broadcast([64, 8, 64]), mult, ["hst", "dec"], ["hst"])
            kb.tt("dve", hst[:], hst[:], pB[:], add, ["hst", "pB"], ["hst"])
            kb.cp("act", hbf[:], hst[:], ["hst"], ["hbf"])
            if d_ == 0:
                kb.cp("act", ysb[b2][:], pY[:], ["pY"], [f"ysb{b2}"])
                kb.dma("sp", yf_v[:, :, cs0:cs1], ysb[b2][:], f"ysb{b2}", [f"ysb{b2}"], [])
            else:
                kb.tt("dve", yg[:], pY[:], yfl[b2][:], add, ["pY", f"yfl{b2}"], ["yg"])
                kb.tt("dve", yg[:], yg[:], szl[b2][:], mult, ["yg", f"szl{b2}"], ["yg"])
                kb.act(sqy[:], yg[:], AF.Square, ["yg"], ["sqy"])
                for r in range(8):
                    kb.mm(pC[:, 0:128], onesf[0:64, 0:64], sqy[:, r, :], r == 0, r == 7, ["onesf", "sqy"], ["pC"])
                kb.act(rsy[:], pC[:, 0:128], AF.Ln, ["pC", "c_eps"], ["rsy"], scale=1.0 / 512, bias=c_eps[0:64, :])
                kb.act(rsy[:], rsy[:], AF.Exp, ["rsy"], ["rsy"], scale=-0.5)
                kb.tt("dve", ynb[b2][:], yg[:], rsy[:, None, :].to_broadcast([64, 8, 128]), mult, ["yg", "rsy"], [f"ynb{b2}"])
                kb.dma("sp", yn_v[:, :, cs0:cs1], ynb[b2][:], f"ynb{b2}", [f"ynb{b2}"], [])
        kb.barrier()
    kb.pop()
    if stop_after == "P2":
        return nc, kb

    S_attn = scr("S_attn", [512, S], BF16)
    SCALE = 96.0 ** -0.5
    kb.push()
    KT = kb.sb("KT", [128, 8, S], BF16)
    Vaug = kb.sb("Vaug", [128, NT, 8, 65], BF16)
    kmax = kb.sb("kmax", [128, 1], F32)
    kb.push()
    kvw = vec_pk("kvw")
    wkv_st = kb.sb("wkv_st", [128, 1024], F32)
    wkv = kb.sb("wkv", [128, 8, 128], BF16)
    kb.dma("sp", wkv_st[:], D["w_ukv"], "wkv_st", [], ["wkv_st"])
    kb.ts("dve", wkv[:].rearrange("p h d -> p (h d)"), wkv_st[:], kvw[:, 0:1], None, mult, None, ["wkv_st", "vecs"], ["wkv"])
    kvl = [kb.sb(f"kvl{i}", [128, GS], BF16) for i in range(2)]
    kpad = [kb.sb(f"kpad{i}", [128, 96], BF16) for i in range(2)]
    krs = [kb.sb(f"krs{i}", [128, 128], BF16) for i in range(2)]
    sqk = [kb.sb(f"sqk{i}", [96, GS], BF16) for i in range(2)]
    tmx = kb.sb("tmx", [128, 1], F32)
    pK = [kb.ps(f"pK{i}", [128, 512], F32) for i in range(2)]
    pV = [kb.ps(f"pV{i}", [128, 512], F32) for i in range(2)]
    pR = [kb.ps(f"pR{i}", [128, 1024], BF16) for i in range(2)]
    pN = [kb.ps(f"pN{i}", [128, 512], F32) for i in range(2)]
    kb.memset("pool", Vaug[:, :, :, 64:65], 1.0, ["V"])
    kb.memset("pool", kmax[:], 0.0, ["kmax"])
    for i in range(2):
        kb.memset("pool", kpad[i][:], 0.0, [f"kpad{i}"])
    for g in range(NG):
        gp = g % 2
        gs0, gs1 = g * GS, (g + 1) * GS
        kb.dma("sp", kvl[gp][:], S_kvn[:, gs0:gs1], f"kvl{gp}", [], [f"kvl{gp}"])
        for t in range(TG):
            tt_ = g * TG + t
            pp = tt_ % 2
            kb.cp("pool", kpad[pp][:, 64:96], krot[:, tt_, :], ["krot"], [f"kpad{pp}"])
            kb.tr(pR[pp][0:96, 0:128], kpad[pp][:], ident[:], [f"kpad{pp}", "ident"], [f"pR{pp}"])
            kb.cp("act", krs[pp][64:96, :], pR[pp][64:96, 0:128], [f"pR{pp}"], [f"krs{pp}"])
            kb.cp("pool", KT[64:96, :, tt_ * 128:(tt_ + 1) * 128], krs[pp][64:96, None, :].to_broadcast([32, 8, 128]),
                  [f"krs{pp}"], [f"KTr{g}"])
            kb.mm(pV[pp][:], kvl[gp][:, t * 128:(t + 1) * 128], wkv[:, :, 64:128], True, True, [f"kvl{gp}", "wkv"], [f"pV{pp}"])
            kb.cp("dve", Vaug[:, tt_, :, 0:64], pV[pp][:].rearrange("p (h d) -> p h d", h=8), [f"pV{pp}"], ["V"])
        for h in range(8):
            hp = h % 2
            kb.mm(pK[hp][0:64, :GS], wkv[:, h, 0:64], kvl[gp][:], True, True, ["wkv", f"kvl{gp}"], [f"pK{hp}"])
            kb.cp("act" if h % 2 else "dve", KT[0:64, h, gs0:gs1], pK[hp][0:64, :GS], [f"pK{hp}"], [f"KTn{g}_{h}"])
            kb.act(sqk[hp][:], KT[0:96, h, gs0:gs1], AF.Square, [f"KTn{g}_{h}", f"KTr{g}"], [f"sqk{hp}"])
            kb.mm(pN[hp][:, :GS], onesb[0:96, :], sqk[hp][:], True, True, ["onesb", f"sqk{hp}"], [f"pN{hp}"])
            Sd.op("dve", lambda e, hp=hp: e.tensor_reduce(out=tmx[:], in_=pN[hp][:, :GS], axis=AX.X, op=ALU.max),
                  [f"pN{hp}"], ["tmx"])
            kb.tt("dve", kmax[:], kmax[:], tmx[:], ALU.max, ["kmax", "tmx"], ["kmax"])
    kb.barrier()
    kb.pop()
    if stop_after == "P3":
        return nc, kb

    kb.push()
    qw = vec_pk("qw")
    wuq_st = kb.sb("wuq_st", [128, 2, 768], F32)
    wuq = kb.sb("wuq", [128, 2, 768], BF16)
    kb.dma("sp", wuq_st[:], D["w_uq"].rearrange("(c p) n -> p c n", p=128), "wuq_st", [], ["wuq_st"])
    for c in range(2):
        kb.ts("dve", wuq[:, c, :], wuq_st[:, c, :], qw[:, c:c + 1], None, mult, None, ["wuq_st", "vecs"], ["wuq"])
    sel65 = kb.sb("sel65", [65, 64], F32)
    kb.memset("pool", sel65[:], 0.0, ["sel65"])
    Sd.op("pool", lambda e: e.affine_select(out=sel65[:], in_=sel65[:], pattern=[[0, 64]], compare_op=ALU.not_equal,
                                            fill=1.0, base=-64, channel_multiplier=1), ["sel65"], ["sel65"])
    cql = [kb.sb(f"cql{i}", [128, 2, GS], BF16) for i in range(2)]
    qtm = kb.sb("qtm", [128, 8, 96], F32)
    qrt = [kb.sb(f"qrt{i}", [128, 16], F32) for i in range(4)]
    qrot = [kb.sb(f"qrot{i}", [128, 8, 96], BF16) for i in range(2)]
    qra = [kb.sb(f"qra{i}", [128, 8, 16], F32) for i in range(4)]
    qT = [kb.sb(f"qT{i}", [96, 8, GS], BF16) for i in range(2)]
    sqq = [kb.sb(f"sqq{i}", [96, GS], BF16) for i in range(2)]
    qmax = kb.sb("qmax", [128, 1], F32)
    tmq = kb.sb("tmq", [128, 1], F32)
    bias_g = [kb.sb(f"bias_g{i}", [128, 1], F32) for i in range(2)]
    NPT = 4
    pt = [kb.sb(f"pt{i}", [128, GS], BF16) for i in range(NPT)]
    osb = [kb.sb(f"osb{i}", [65, GS], F32) for i in range(2)]
    rden = [kb.sb(f"rden{i}", [64, GS], F32) for i in range(2)]
    ao = kb.sb("ao", [64, 8, GS], F32)
    sqa = [kb.sb(f"sqa{i}", [64, GS], F32) for i in range(2)]
    rsa = kb.sb("rsa", [64, GS], F32)
    aon = [kb.sb(f"aon{i}", [64, 8, GS], BF16) for i in range(2)]
    psc = [kb.ps(f"psc{i}", [128, 512], F32) for i in range(3)]
    po = [kb.ps(f"po{i}", [128, 512], F32) for i in range(2)]
    pq = [kb.ps(f"pq{i}", [128, 512], F32) for i in range(2)]
    pQT = kb.ps("pQT", [128, 8, 128], BF16)
    sci = 0
    for g in range(NG):
        gp = g % 2
        gs0, gs1 = g * GS, (g + 1) * GS
        kb.dma("sp", cql[gp][:], S_cqn.rearrange("(c p) s -> p c s", p=128)[:, :, gs0:gs1], f"cql{gp}", [], [f"cql{gp}"])
        for t in range(TG):
            tt_ = g * TG + t
            rp = tt_ % 2
            for c in range(2):
                kb.mm(pq[0][:, 0:512], cql[gp][:, c, t * 128:(t + 1) * 128], wuq[:, c, 0:512], c == 0, c == 1,
                      [f"cql{gp}", "wuq"], ["pq0"])
            for c in range(2):
                kb.mm(pq[1][:, 0:256], cql[gp][:, c, t * 128:(t + 1) * 128], wuq[:, c, 512:768], c == 0, c == 1,
                      [f"cql{gp}", "wuq"], ["pq1"])
            qflat = qtm[:].rearrange("p h d -> p (h d)")
            kb.cp("act", qflat[:, 0:512], pq[0][:, 0:512], ["pq0"], ["qtm"])
            kb.cp("dve", qflat[:, 512:768], pq[1][:, 0:256], ["pq1"], ["qtm"])
            cb_ = cosT[:, tt_:tt_ + 1, :].to_broadcast([128, 8, 16])
            sb_ = sinT[:, tt_:tt_ + 1, :].to_broadcast([128, 8, 16])
            q1, q2 = qtm[:, :, 64:80], qtm[:, :, 80:96]
            kb.cp("pool", qrot[rp][:, :, 0:64], qtm[:, :, 0:64], ["qtm"], [f"qrot{rp}"])
            kb.tt("pool", qra[0][:], q1, cb_, mult, ["qtm", "cosT"], ["qra0"])
            kb.tt("dve", qra[1][:], q2, sb_, mult, ["qtm", "sinT"], ["qra1"])
            kb.tt("pool", qrot[rp][:, :, 64:80], qra[0][:], qra[1][:], sub, ["qra0", "qra1"], [f"qrot{rp}"])
            kb.tt("pool", qra[2][:], q2, cb_, mult, ["qtm", "cosT"], ["qra2"])
            kb.tt("dve", qra[3][:], q1, sb_, mult, ["qtm", "sinT"], ["qra3"])
            kb.tt("dve", qrot[rp][:, :, 80:96], qra[2][:], qra[3][:], add, ["qra2", "qra3"], [f"qrot{rp}"])
            for h in range(8):
                kb.tr(pQT[0:96, h, :], qrot[rp][:, h, :], ident[:], [f"qrot{rp}", "ident"], ["pQT"])
            kb.cp("act", qT[gp][:, :, t * 128:(t + 1) * 128], pQT[0:96, :, :], ["pQT"], [f"qT{gp}_{t}"])
        qTb = [f"qT{gp}_{t}" for t in range(TG)]
        kb.memset("pool", qmax[:], 0.0, ["qmax"])
        for h in range(8):
            hp = h % 2
            kb.act(sqq[hp][:], qT[gp][:, h, :], AF.Square, qTb, [f"sqq{hp}"])
            kb.mm(pq[hp][:, :GS], onesb[0:96, :], sqq[hp][:], True, True, ["onesb", f"sqq{hp}"], [f"pq{hp}"])
            Sd.op("dve", lambda e, hp=hp: e.tensor_reduce(out=tmq[:], in_=pq[hp][:, :GS], axis=AX.X, op=ALU.max),
                  [f"pq{hp}"], ["tmq"])
            kb.tt("dve", qmax[:], qmax[:], tmq[:], ALU.max, ["qmax", "tmq"], ["qmax"])
        bg = bias_g[gp]
        kb.tt("dve", bg[:], qmax[:], kmax[:], mult, ["qmax", "kmax"], [f"bias_g{gp}"])
        kb.ts("dve", bg[:], bg[:], 1e-30, None, add, None, [f"bias_g{gp}"], [f"bias_g{gp}"])
        kb.act(bg[:], bg[:], AF.Ln, [f"bias_g{gp}"], [f"bias_g{gp}"])
        kb.act(bg[:], bg[:], AF.Exp, [f"bias_g{gp}"], [f"bias_g{gp}"], scale=0.5)
        kb.ts("dve", bg[:], bg[:], -SCALE * 1.02, None, mult, None, [f"bias_g{gp}"], [f"bias_g{gp}"])
        units = [(h, kbk) for h in range(8) for kbk in range(NT)]
        LOOK = 2
        NPS = 3

        def emit_qk(u):
            h, kbk = units[u]
            si = u % NPS
            kb.mm(psc[si][:, :GS], KT[0:96, h, kbk * 128:(kbk + 1) * 128], qT[gp][:, h, :], True, True,
                  ["KT"] + qTb, [f"psc{si}"])

        for u in range(min(LOOK, len(units))):
            emit_qk(u)
        for u, (h, kbk) in enumerate(units):
            hp = h % 2
            si = u % NPS
            pi = u % NPT
            if u + LOOK < len(units):
                emit_qk(u + LOOK)
            kb.act(pt[pi][:], psc[si][:, :GS], AF.Exp, [f"psc{si}", f"bias_g{gp}"], [f"pt{pi}"], scale=SCALE, bias=bg[:])
            kb.mm(po[hp][0:65, :GS], Vaug[:, kbk, h, :], pt[pi][:], kbk == 0, kbk == NT - 1, ["V", f"pt{pi}"], [f"po{hp}"])
            if kbk == NT - 1:
                kb.cp("dve", osb[hp][:], po[hp][0:65, :GS], [f"po{hp}"], [f"osb{hp}"])
                kb.mm(pq[hp][0:64, :GS], sel65[:], osb[hp][:], True, True, ["sel65", f"osb{hp}"], [f"pq{hp}"])
                Sd.op("dve", lambda e, hp=hp: e.reciprocal(out=rden[hp][:], in_=pq[hp][0:64, :GS]), [f"pq{hp}"], [f"rden{hp}"])
                kb.tt("pool", ao[:, h, :], osb[hp][0:64, :], rden[hp][:], mult, [f"osb{hp}", f"rden{hp}"], [f"ao{h}"])
        for h in range(8):
            hp = h % 2
            kb.act(sqa[hp][:], ao[:, h, :], AF.Square, [f"ao{h}"], [f"sqa{hp}"])
            kb.mm(pq[0][0:64, :GS], onesf[0:64, 0:64], sqa[hp][:], h == 0, h == 7, ["onesf", f"sqa{hp}"], ["pq0"])
        kb.act(rsa[:], pq[0][0:64, :GS], AF.Ln, ["pq0", "c_eps"], ["rsa"], scale=1.0 / 512, bias=c_eps[0:64, :])
        kb.act(rsa[:], rsa[:], AF.Exp, ["rsa"], ["rsa"], scale=-0.5)
        for h in range(8):
            kb.tt("pool" if h % 2 else "dve", aon[gp][:, h, :], ao[:, h, :], rsa[:], mult, [f"ao{h}", "rsa"], [f"aon{gp}_{h}"])
        kb.dma("sp", S_attn.rearrange("(h p) s -> p h s", p=64)[:, :, gs0:gs1], aon[gp][:], f"aon{gp}",
               [f"aon{gp}_{h}" for h in range(8)], [])
    kb.barrier()
    kb.pop()
    kb.pop()
    if stop_after == "P4":
        return nc, kb

    TS = min(512, S)
    NSUB = TS // 128
    NTILES = (2 * S) // TS + NEXP
    NSLOT = NTILES * TS
    BIG = float(1 << 22)
    S_x1 = scr("S_x1", [S, 1024], F32)
    S_h2 = scr("S_h2", [S, 1024], BF16)
    S_slot = scr("S_slot", [NSLOT, 4], F32)
    S_ymoe = scr("S_ymoe", [2 * S, 1024], F32)
    S_tile = scr("S_tile", [2, 128, NTILES], I32)
    kb.push()
    ohall = kb.sb("ohall", [128, NT * 2, 32], BF16)
    posn = kb.sb("posn", [128, NT * 2], F32)
    wcomb = kb.sb("wcomb", [128, NT * 2], F32)
    run_bc = kb.sb("run_bc", [128, 32], F32)
    stri = kb.sb("stri", [128, 128], BF16)
    kb.memset("pool", run_bc[:], 0.0, ["run_bc"])
    kb.memset("pool", stri[:], 1.0, ["stri"])
    Sd.op("pool", lambda e: e.affine_select(out=stri[:], in_=stri[:], pattern=[[1, 128]], compare_op=ALU.is_gt, fill=0.0,
                                            base=0, channel_multiplier=-1), ["stri"], ["stri"])
    kb.push()
    aow = vecs64[:, 0:8]
    snw = vecs64[:, 8:16]
    wo = kb.sb("wo", [64, 16, 1024], BF16)
    wo_st = [kb.sb(f"wo_st{i}", [64, 1024], F32) for i in range(2)]
    for c in range(16):
        kb.dma("sp", wo_st[c % 2][:], D["w_o"][c * 64:(c + 1) * 64, :], f"wo_st{c % 2}", [], [f"wo_st{c % 2}"])
        sc_ = aow[:, c:c + 1] if c < 8 else snw[:, c - 8:c - 7]
        kb.ts("dve" if c % 2 else "pool", wo[:, c, :], wo_st[c % 2][:], sc_, None, mult, None, [f"wo_st{c % 2}", "vecs64"], [f"wo{c}"])
    wob = [f"wo{c}" for c in range(16)]
    fnw_bc = kb.sb("fnw_bc", [128, 1024], F32)
    kb.dma("sp", fnw_bc[:], D["ffn_norm_w"].partition_broadcast(128), "fnw_bc", [], ["fnw_bc"])
    wr = kb.sb("wr", [128, 8, 36], F32)
    kb.dma("sp", wr[:, :, 0:4], D["w_router_group"].rearrange("(k p) n -> p k n", p=128), "wr", [], ["wr"])
    kb.dma("sp", wr[:, :, 4:36], D["w_router_expert"].rearrange("(k p) n -> p k n", p=128), "wr", [], ["wr"])
    br_bc = kb.sb("br_bc", [128, 36], F32)
    kb.dma("sp", br_bc[:, 0:4], D["b_router_group"].partition_broadcast(128), "br_bc", [], ["br_bc"])
    kb.dma("sp", br_bc[:, 4:36], D["b_router_expert"].partition_broadcast(128), "br_bc", [], ["br_bc"])
    xl = [kb.sb(f"xl{i}", [128, 1024], F32) for i in range(2)]
    atl = [kb.sb(f"atl{i}", [64, 8, 128], BF16) for i in range(2)]
    ynl = [kb.sb(f"ynl{i}", [64, 8, 128], BF16) for i in range(2)]
    x1t = [kb.sb(f"x1t{i}", [128, 1024], F32) for i in range(2)]
    jk_2 = [kb.sb(f"jk_{i}", [128, 1024], BF16) for i in range(2)]
    st5 = [kb.sb(f"st5_{i}", [128, 1], F32) for i in range(2)]
    h2f_2 = [kb.sb(f"h2f_{i}", [128, 1024], F32) for i in range(2)]
    h2b = [kb.sb(f"h2b{i}", [128, 1024], BF16) for i in range(2)]
    h2T_2 = [kb.sb(f"h2T_{i}", [128, 8, 128], F32) for i in range(2)]
    rl_2 = [kb.sb(f"rl_{i}", [128, 36], F32) for i in range(2)]
    gmx_2 = [kb.sb(f"gmx_{i}", [128, 1], F32) for i in range(2)]
    ngm_2 = [kb.sb(f"ngm_{i}", [128, 1], F32) for i in range(2)]
    ohg_2 = [kb.sb(f"ohg_{i}", [128, 4], F32) for i in range(2)]
    eg_2 = [kb.sb(f"eg_{i}", [128, 4], F32) for i in range(2)]
    sume_2 = [kb.sb(f"sume_{i}", [128, 1], F32) for i in range(2)]
    gw_2 = [kb.sb(f"gw_{i}", [128, 1], F32) for i in range(2)]
    selt_2 = [kb.sb(f"selt_{i}", [128, 4, 8], F32) for i in range(2)]
    sel_2 = [kb.sb(f"sel_{i}", [128, 8], F32) for i in range(2)]
    m8_2 = [kb.sb(f"m8_{i}", [128, 8], F32) for i in range(2)]
    dd_2 = [kb.sb(f"dd_{i}", [128, 1], F32) for i in range(2)]
    w12_2 = [kb.sb(f"w12_{i}", [128, 2], F32) for i in range(2)]
    ohe_2 = [kb.sb(f"ohe_{i}", [128, 2, 8], F32) for i in range(2)]
    pmat_2 = [kb.sb(f"pmat_{i}", [128, 32], F32) for i in range(2)]
    ptmp_2 = [kb.sb(f"ptmp_{i}", [128, 32], F32) for i in range(2)]
    pmx = kb.ps("pmx", [128, 1024], F32)
    pH = kb.ps("pH", [128, 8, 128], F32)
    pL = kb.ps("pL", [128, 512], F32)
    pP = kb.ps("pP", [128, 512], F32)
    at_v = S_attn.rearrange("(h p) s -> p h s", p=64)
    for tt_ in range(NT):
        b2 = tt_ % 2
        jk = jk_2[b2]
        h2f = h2f_2[b2]
        h2T = h2T_2[b2]
        rl = rl_2[b2]
        gmx = gmx_2[b2]
        ngm = ngm_2[b2]
        ohg = ohg_2[b2]
        eg = eg_2[b2]
        sume = sume_2[b2]
        gw = gw_2[b2]
        selt = selt_2[b2]
        sel = sel_2[b2]
        m8 = m8_2[b2]
        dd = dd_2[b2]
        w12 = w12_2[b2]
        ohe = ohe_2[b2]
        pmat = pmat_2[b2]
        ptmp = ptmp_2[b2]
        ts0, ts1 = tt_ * 128, (tt_ + 1) * 128
        kb.dma("sp", xl[b2][:], D["x"][ts0:ts1, :], f"xl{b2}", [], [f"xl{b2}"])
        kb.dma("sp", atl[b2][:], at_v[:, :, ts0:ts1], f"atl{b2}", [], [f"atl{b2}"])
        kb.dma("sp", ynl[b2][:], yn_v[:, :, ts0:ts1], f"ynl{b2}", [], [f"ynl{b2}"])
        for half in range(2):
            for c in range(16):
                lhs = atl[b2][:, c, :] if c < 8 else ynl[b2][:, c - 8, :]
                kb.mm(pmx[:, half * 512:(half + 1) * 512], lhs, wo[:, c, half * 512:(half + 1) * 512], c == 0, c == 15,
                      [f"atl{b2}", f"ynl{b2}"] + wob, ["pmx"])
        kb.tt("dve", x1t[b2][:], pmx[:], xl[b2][:], add, ["pmx", f"xl{b2}"], [f"x1t{b2}"])
        kb.dma("sp", S_x1[ts0:ts1, :], x1t[b2][:], f"x1t{b2}", [f"x1t{b2}"], [])
        st = st5[b2]
        kb.act(jk[:], x1t[b2][:], AF.Square, [f"x1t{b2}"], [f"jk_{b2}", f"st5_{b2}"], accum_out=st[:])
        kb.ts("dve", st[:], st[:], 1.0 / 1024, EPS, mult, add, [f"st5_{b2}"], [f"st5_{b2}"])
        kb.act(st[:], st[:], AF.Ln, [f"st5_{b2}"], [f"st5_{b2}"])
        kb.act(st[:], st[:], AF.Exp, [f"st5_{b2}"], [f"st5_{b2}"], scale=-0.5)
        kb.stt(h2f[:], x1t[b2][:], st[:, 0:1], fnw_bc[:], mult, mult, [f"x1t{b2}", f"st5_{b2}", "fnw_bc"], [f"h2f_{b2}"])
        kb.cp("act", h2b[b2][:], h2f[:], [f"h2f_{b2}"], [f"h2b{b2}"])
        kb.dma("sp", S_h2[ts0:ts1, :], h2b[b2][:], f"h2b{b2}", [f"h2b{b2}"], [])
        for k in range(8):
            kb.tr(pH[:, k, :], h2f[:, k * 128:(k + 1) * 128], identf[:], [f"h2f_{b2}", "identf"], ["pH"])
        kb.cp("act", h2T[:], pH[:], ["pH"], [f"h2T_{b2}"])
        for k in range(8):
            kb.mm(pL[:, 0:36], h2T[:, k, :], wr[:, k, :], k == 0, k == 7, [f"h2T_{b2}", "wr"], ["pL"])
        kb.tt("dve", rl[:], pL[:, 0:36], br_bc[:], add, ["pL", "br_bc"], [f"rl_{b2}"])
        kb.reduce(gmx[:], rl[:, 0:4], ALU.max, [f"rl_{b2}"], [f"gmx_{b2}"])
        kb.ts("dve", ohg[:], rl[:, 0:4], gmx[:, 0:1], None, ALU.is_equal, None, [f"rl_{b2}", f"gmx_{b2}"], [f"ohg_{b2}"])
        kb.ts("dve", ngm[:], gmx[:], -1.0, None, mult, None, [f"gmx_{b2}"], [f"ngm_{b2}"])
        kb.act(eg[:], rl[:, 0:4], AF.Exp, [f"rl_{b2}", f"ngm_{b2}"], [f"eg_{b2}", f"sume_{b2}"], bias=ngm[:], accum_out=sume[:])
        kb.recip(gw[:], sume[:], [f"sume_{b2}"], [f"gw_{b2}"])
        kb.tt("dve", selt[:], rl[:, 4:36].rearrange("p (g e) -> p g e", g=4), ohg[:, :, None].to_broadcast([128, 4, 8]), mult,
              [f"rl_{b2}", f"ohg_{b2}"], [f"selt_{b2}"])
        kb.reduce(sel[:], selt[:].rearrange("p g e -> p e g"), ALU.add, [f"selt_{b2}"], [f"sel_{b2}"])
        kb.max8(m8[:], sel[:], [f"sel_{b2}"], [f"m8_{b2}"])
        kb.tt("dve", dd[:], m8[:, 1:2], m8[:, 0:1], sub, [f"m8_{b2}"], [f"dd_{b2}"])
        kb.act(dd[:], dd[:], AF.Exp, [f"dd_{b2}"], [f"dd_{b2}"])
        kb.ts("dve", w12[:, 0:1], dd[:], 1.0, None, add, None, [f"dd_{b2}"], [f"w12_{b2}"])
        kb.recip(w12[:, 0:1], w12[:, 0:1], [f"w12_{b2}"], [f"w12_{b2}"])
        kb.tt("dve", w12[:, 1:2], w12[:, 0:1], dd[:], mult, [f"w12_{b2}", f"dd_{b2}"], [f"w12_{b2}"])
        kb.ts("dve", wcomb[:, 2 * tt_:2 * tt_ + 2], w12[:], gw[:, 0:1], None, mult, None, [f"w12_{b2}", f"gw_{b2}"], ["wcomb"])
        for k in range(2):
            kb.ts("dve", ohe[:, k, :], sel[:], m8[:, k:k + 1], None, ALU.is_equal, None, [f"sel_{b2}", f"m8_{b2}"], [f"ohe_{b2}"])
        for k in range(2):
            u = 2 * tt_ + k
            kb.tt("dve", ohall[:, u, :].rearrange("p (g e) -> p g e", g=4), ohg[:, :, None].to_broadcast([128, 4, 8]),
                  ohe[:, k:k + 1, :].to_broadcast([128, 4, 8]), mult, [f"ohg_{b2}", f"ohe_{b2}"], [f"ohall{u}"])
            kb.mm(pP[:, 0:32], stri[:], ohall[:, u, :], True, True, ["stri", f"ohall{u}"], ["pP"])
            kb.mm(pP[:, 32:64], onesb[:], ohall[:, u, :], True, True, ["onesb", f"ohall{u}"], ["pP"])
            kb.tt("dve", pmat[:], pP[:, 0:32], run_bc[:], add, ["pP", "run_bc"], [f"pmat_{b2}"])
            kb.tt("dve", ptmp[:], pmat[:], ohall[:, u, :], mult, [f"pmat_{b2}", f"ohall{u}"], [f"ptmp_{b2}"])
            kb.reduce(posn[:, u:u + 1], ptmp[:], ALU.add, [f"ptmp_{b2}"], ["posn"])
            kb.tt("dve", run_bc[:], run_bc[:], pP[:, 32:64], add, ["run_bc", "pP"], ["run_bc"])
    kb.barrier()
    kb.pop()
    if stop_after == "P5a":
        return nc, kb

    import math as _m
    LOG_TS = int(_m.log2(TS))
    kb.push()
    cntf = kb.sb("cntf", [128, 32], F32)
    cnti = kb.sb("cnti", [128, 32], I32)
    ntf = kb.sb("ntf", [128, 32], F32)
    ones32 = kb.sb("ones32", [128, 32], F32)
    incl = kb.sb("incl", [128, 32], F32)
    base_bc = kb.sb("base_bc", [128, 32], F32)
    tmpb = kb.sb("tmpb", [128, NT * 2, 32], F32)
    slotf = kb.sb("slotf", [128, NT * 2], F32)
    sloti = kb.sb("sloti", [128, NT * 2], I32)
    rowdat = kb.sb("rowdat", [128, NT * 2, 4], F32)
    rowdat_i = rowdat[:].bitcast(I32)
    NDF = NSLOT // 128
    dflt = kb.sb("dflt", [128, NDF, 4], F32)
    dflt_i = dflt[:].bitcast(I32)
    jidx = kb.sb("jidx", [128, NTILES], F32)
    pidx = kb.sb("pidx", [128, 1], F32)
    cmpt = kb.sb("cmpt", [128, NTILES, 32], F32)
    ej = kb.sb("ej", [128, NTILES], F32)
    wgf = kb.sb("wgf", [128, NTILES], F32)
    wgi = kb.sb("wgi", [128, NTILES], I32)
    wdi = kb.sb("wdi", [128, NTILES], I32)
    kb.ts("dve", cntf[:], run_bc[:], float(TS - 1), None, add, None, ["run_bc"], ["cntf"])
    kb.cp("dve", cnti[:], cntf[:], ["cntf"], ["cnti"])
    kb.ts("dve", cnti[:], cnti[:], LOG_TS, None, ALU.arith_shift_right, None, ["cnti"], ["cnti"])
    kb.cp("dve", ntf[:], cnti[:], ["cnti"], ["ntf"])
    kb.memset("pool", ones32[:], 1.0, ["ones32"])
    Sd.op("dve", lambda e: e.tensor_tensor_scan(out=incl[:], data0=ones32[:], data1=ntf[:], initial=0.0, op0=mult, op1=add),
          ["ones32", "ntf"], ["incl"])
    kb.tt("dve", base_bc[:], incl[:], ntf[:], sub, ["incl", "ntf"], ["base_bc"])
    kb.ts("dve", base_bc[:], base_bc[:], float(TS), None, mult, None, ["base_bc"], ["base_bc"])
    kb.tt("pool", tmpb[:], ohall[:], base_bc[:, None, :].to_broadcast([128, NT * 2, 32]), mult,
          [f"ohall{u}" for u in range(NT * 2)] + ["base_bc"], ["tmpb"])
    Sd.op("dve", lambda e: e.tensor_reduce(out=slotf[:], in_=tmpb[:], axis=AX.X, op=ALU.add), ["tmpb"], ["slotf"])
    kb.tt("dve", slotf[:], slotf[:], posn[:], add, ["slotf", "posn"], ["slotf"])
    kb.cp("dve", sloti[:], slotf[:], ["slotf"], ["sloti"])
    kb.memset("pool", rowdat[:], 0.0, ["rowdat"])
    Sd.op("pool", lambda e: e.iota(rowdat_i[:, :, 0].rearrange("p (t k) -> p t k", k=2), pattern=[[128, NT], [0, 2]], base=0,
                                   channel_multiplier=1), ["rowdat"], ["rowdat"])
    Sd.op("pool", lambda e: e.iota(rowdat_i[:, :, 2].rearrange("p (t k) -> p t k", k=2), pattern=[[128, NT], [S, 2]], base=0,
                                   channel_multiplier=1), ["rowdat"], ["rowdat"])
    kb.cp("pool", rowdat[:, :, 1], wcomb[:], ["wcomb", "rowdat"], ["rowdat"])
    kb.memset("pool", dflt[:], 0.0, ["dflt"])
    kb.memset("pool", dflt_i[:, :, 0:1], 1 << 22, ["dflt"])
    kb.memset("pool", dflt_i[:, :, 2:3], 1 << 22, ["dflt"])
    kb.dma("sp", S_slot.rearrange("(n p) c -> p n c", p=128), dflt[:], "dflt", ["dflt"], ["S_slot_init"])
    for u in range(NT * 2):
        Sd.dma("pool", lambda e, u=u: e.indirect_dma_start(
            out=S_slot, out_offset=bass.IndirectOffsetOnAxis(ap=sloti[:, u:u + 1], axis=0), in_=rowdat[:, u, :], in_offset=None,
            bounds_check=BC(e, NSLOT - 1), oob_is_err=False), "slotsc", ["S_slot_init", "sloti", "rowdat"], [])
    Sd.op("pool", lambda e: e.iota(jidx[:], pattern=[[1, NTILES]], base=0, channel_multiplier=0,
                                   allow_small_or_imprecise_dtypes=True), [], ["jidx"])
    Sd.op("pool", lambda e: e.iota(pidx[:], pattern=[[0, 1]], base=0, channel_multiplier=1,
                                   allow_small_or_imprecise_dtypes=True), [], ["pidx"])
    kb.tt("dve", cmpt[:], incl[:, None, :].to_broadcast([128, NTILES, 32]), jidx[:, :, None].to_broadcast([128, NTILES, 32]),
          ALU.is_le, ["incl", "jidx"], ["cmpt"])
    Sd.op("dve", lambda e: e.tensor_reduce(out=ej[:], in_=cmpt[:], axis=AX.X, op=ALU.add), ["cmpt"], ["ej"])
    kb.ts("dve", wgf[:], ej[:], 128.0, pidx[:, 0:1], mult, add, ["ej", "pidx"], ["wgf"])
    kb.cp("dve", wgi[:], wgf[:], ["wgf"], ["wgi"])
    kb.barrier()
    if stop_after == "P5b":
        return nc, kb

    sl = [kb.sb(f"sl{i}", [128, NSUB * 4], F32) for i in range(2)]
    wg = [kb.sb(f"wg{i}", [128, 8, 256], BF16) for i in range(2)]
    wu = [kb.sb(f"wu{i}", [128, 8, 256], BF16) for i in range(2)]
    wd = [kb.sb(f"wd{i}", [128, 2, 1024], BF16) for i in range(2)]
    hg = [kb.sb(f"hg{i}", [128, 1024], BF16) for i in range(2)]
    hTt = [kb.sb(f"hTt{i}", [128, 8, TS], BF16) for i in range(2)]
    sg = [kb.sb(f"sg{i}", [128, TS], F32) for i in range(2)]
    heT = [kb.sb(f"heT{i}", [128, 2, TS], BF16) for i in range(2)]
    ysc = [kb.sb(f"ysc{i}", [128, 1024], F32) for i in range(2)]
    pHT = kb.ps("pHT", [128, 8, 128], BF16)
    pgu = [kb.ps(f"pgu{i}", [128, 512], F32) for i in range(4)]
    py = kb.ps("py", [128, 1024], F32)
    for i in range(2):
        kb.memset("pool", wg[i][:], 0.0, [f"wg{i}"])
        kb.memset("pool", wu[i][:], 0.0, [f"wu{i}"])
        kb.memset("pool", wd[i][:], 0.0, [f"wd{i}"])
        kb.memset("pool", hg[i][:], 0.0, [f"hg{i}"])
    sub_i = 0
    for j in range(NTILES):
        jp = j % 2
        kb.dma("sp", sl[jp][:].rearrange("p (n c) -> p n c", c=4), S_slot[j * TS:(j + 1) * TS, :].rearrange("(n p) c -> p n c", p=128), f"sl{jp}", [], [f"sl{jp}"])
        sl_i = sl[jp][:].bitcast(I32)
        for wt, wname, a_ in ((wg, "w_exp_gate", 8), (wu, "w_exp_up", 8), (wd, "w_exp_down", 2)):
            Sd.dma("pool", lambda e, j=j, jp=jp, wt=wt, wname=wname, a_=a_: e.indirect_dma_start(
                out=wt[jp][:].rearrange("p k c -> p (k c)"), out_offset=None,
                in_=D[wname].rearrange("(r a) c -> r (a c)", a=a_),
                in_offset=bass.IndirectOffsetOnAxis(ap=wgi[:, j:j + 1], axis=0),
                bounds_check=BC(e, 32 * 128 - 1), oob_is_err=False), f"{wname}{jp}", ["wgi"],
                [{"w_exp_gate": "wg", "w_exp_up": "wu", "w_exp_down": "wd"}[wname] + str(jp)])
        for sb_ in range(NSUB):
            s2 = sub_i % 2
            sub_i += 1
            Sd.dma("pool", lambda e, jp=jp, sb_=sb_, s2=s2, sl_i=sl_i: e.indirect_dma_start(
                out=hg[s2][:], out_offset=None, in_=S_h2, in_offset=bass.IndirectOffsetOnAxis(ap=sl_i[:, sb_ * 4:sb_ * 4 + 1], axis=0),
                bounds_check=BC(e, S - 1), oob_is_err=False), f"hg{s2}", [f"sl{jp}"], [f"hg{s2}"])
            for k in range(8):
                kb.tr(pHT[:, k, :], hg[s2][:].rearrange("p (j a) -> p a j", a=8)[:, k, :], ident[:], [f"hg{s2}", "ident"], ["pHT"])
            kb.cp("act" if sb_ % 2 else "dve", hTt[jp][:, :, sb_ * 128:(sb_ + 1) * 128], pHT[:], ["pHT"], [f"hTt{jp}_{sb_}"])
        hb_ = [f"hTt{jp}_{sb_}" for sb_ in range(NSUB)]
        for m in range(2):
            pg_, pu_ = pgu[2 * m], pgu[2 * m + 1]
            for kc in range(8):
                kb.mm(pg_[:, :TS], wg[jp][:].rearrange("p k (j a) -> p k a j", a=2)[:, kc, m, :], hTt[jp][:, kc, :], kc == 0, kc == 7,
                      [f"wg{jp}"] + hb_, [f"pgu{2 * m}"])
            for kc in range(8):
                kb.mm(pu_[:, :TS], wu[jp][:].rearrange("p k (j a) -> p k a j", a=2)[:, kc, m, :], hTt[jp][:, kc, :], kc == 0, kc == 7,
                      [f"wu{jp}"] + hb_, [f"pgu{2 * m + 1}"])
            kb.act(sg[m][:], pg_[:, :TS], AF.Silu, [f"pgu{2 * m}"], [f"sg{m}"])
            kb.tt("dve", heT[jp][:, m, :], sg[m][:], pu_[:, :TS], mult, [f"sg{m}", f"pgu{2 * m + 1}"], [f"heT{jp}_{m}"])
        for sb_ in range(NSUB):
            s2 = sub_i % 2
            sub_i += 1
            for half in range(2):
                for m in range(2):
                    kb.mm(py[:, half * 512:(half + 1) * 512], heT[jp][:, m, sb_ * 128:(sb_ + 1) * 128],
                          wd[jp][:, m, half * 512:(half + 1) * 512], m == 0, m == 1,
                          [f"heT{jp}_0", f"heT{jp}_1", f"wd{jp}"], ["py"])
            kb.act(ysc[s2][:], py[:], AF.Copy, ["py", f"sl{jp}"], [f"ysc{s2}"], scale=sl[jp][:, sb_ * 4 + 1:sb_ * 4 + 2])
            Sd.dma("pool", lambda e, sb_=sb_, s2=s2, sl_i=sl_i: e.indirect_dma_start(
                out=S_ymoe, out_offset=bass.IndirectOffsetOnAxis(ap=sl_i[:, sb_ * 4 + 2:sb_ * 4 + 3], axis=0), in_=ysc[s2][:], in_offset=None,
                bounds_check=BC(e, 2 * S - 1), oob_is_err=False), f"ysc{s2}", [f"ysc{s2}", f"sl{jp}"],
                ["S_ymoe"] if serial_scatter else [])
    kb.barrier()
    kb.pop()
    kb.pop()
    if stop_after == "P5c":
        return nc, kb

    kb.push()
    pnw = vec_pk("pnw")
    wpg = kb.sb("wpg", [128, 8, 1024], BF16)
    wpg_st = [kb.sb(f"wpg_st{i}", [128, 1024], F32) for i in range(2)]
    for k in range(8):
        kb.dma("sp", wpg_st[k % 2][:], D["w_ple_gate"][k * 128:(k + 1) * 128, :], f"wpg_st{k % 2}", [], [f"wpg_st{k % 2}"])
        kb.ts("dve" if k % 2 else "pool", wpg[:, k, :], wpg_st[k % 2][:], pnw[:, k:k + 1], None, mult, None,
              [f"wpg_st{k % 2}", "vecs"], [f"wpg{k}"])
    wpgb = [f"wpg{k}" for k in range(8)]
    wpp = kb.sb("wpp", [128, 2, 1024], BF16)
    Sd.dma("pool", lambda e: e.dma_start(out=wpp[:], in_=D["w_ple_proj"].rearrange("(c p) n -> p c n", p=128)), "wpp", [], ["wpp"])
    bpg_bc = kb.sb("bpg_bc", [128, 1024], F32)
    kb.dma("sp", bpg_bc[:], D["b_ple_gate"].partition_broadcast(128), "bpg_bc", [], ["bpg_bc"])
    ppw_bc = kb.sb("ppw_bc", [128, 1024], F32)
    kb.dma("sp", ppw_bc[:], D["ple_post_norm_w"].partition_broadcast(128), "ppw_bc", [], ["ppw_bc"])
    fin_bc = kb.sb("fin_bc", [128, 1024], F32)
    kb.dma("sp", fin_bc[:], D["final_norm_w"].partition_broadcast(128), "fin_bc", [], ["fin_bc"])
    x1l = [kb.sb(f"x1l{i}", [128, 1024], F32) for i in range(2)]
    y0l = [kb.sb(f"y0l{i}", [128, 1024], F32) for i in range(2)]
    y1l = [kb.sb(f"y1l{i}", [128, 1024], F32) for i in range(2)]
    pl = [kb.sb(f"pl{i}", [128, 256], F32) for i in range(2)]
    plb_2 = [kb.sb(f"plb_{i}", [128, 256], BF16) for i in range(2)]
    pTs_2 = [kb.sb(f"pTs_{i}", [128, 2, 128], BF16) for i in range(2)]
    x2_2 = [kb.sb(f"x2_{i}", [128, 1024], F32) for i in range(2)]
    jk2_2 = [kb.sb(f"jk2_{i}", [128, 1024], BF16) for i in range(2)]
    s6_2 = [[kb.sb(f"s6_{i}_{q}", [128, 1], F32) for i in range(3)] for q in range(2)]
    n3b_2 = [kb.sb(f"n3b_{i}", [128, 1024], BF16) for i in range(2)]
    n3T_2 = [kb.sb(f"n3T_{i}", [128, 8, 128], BF16) for i in range(2)]
    gate_2 = [kb.sb(f"gate_{i}", [128, 1024], F32) for i in range(2)]
    ple_2 = [kb.sb(f"ple_{i}", [128, 1024], F32) for i in range(2)]
    x3_2 = [kb.sb(f"x3_{i}", [128, 1024], F32) for i in range(2)]
    ot = [kb.sb(f"ot{i}", [128, 1024], F32) for i in range(2)]
    pGt = kb.ps("pGt", [128, 1024], F32)
    pPp = kb.ps("pPp", [128, 1024], F32)
    pN3 = kb.ps("pN3", [128, 8, 128], BF16)
    pPT = kb.ps("pPT", [128, 8, 128], BF16)

    def rstd_of(src_ap, src_bufs, st, stname, junk_ap, junkname):
        kb.act(junk_ap, src_ap, AF.Square, src_bufs, [junkname, stname], accum_out=st[:])
        kb.ts("dve", st[:], st[:], 1.0 / 1024, EPS, mult, add, [stname], [stname])
        kb.act(st[:], st[:], AF.Ln, [stname], [stname])
        kb.act(st[:], st[:], AF.Exp, [stname], [stname], scale=-0.5)

    for tt_ in range(NT):
        b2 = tt_ % 2
        plb = plb_2[b2]
        pTs = pTs_2[b2]
        x2 = x2_2[b2]
        jk2 = jk2_2[b2]
        n3b = n3b_2[b2]
        n3T = n3T_2[b2]
        gate = gate_2[b2]
        ple = ple_2[b2]
        x3 = x3_2[b2]
        s6 = s6_2[b2]
        ts0, ts1 = tt_ * 128, (tt_ + 1) * 128
        kb.dma("sp", x1l[b2][:], S_x1[ts0:ts1, :], f"x1l{b2}", [], [f"x1l{b2}"])
        kb.dma("sp", y0l[b2][:], S_ymoe[ts0:ts1, :], f"y0l{b2}", [], [f"y0l{b2}"])
        kb.dma("sp", y1l[b2][:], S_ymoe[S + ts0:S + ts1, :], f"y1l{b2}", [], [f"y1l{b2}"])
        kb.dma("sp", pl[b2][:], D["p"][ts0:ts1, :], f"pl{b2}", [], [f"pl{b2}"])
        kb.tt("dve", x2[:], x1l[b2][:], y0l[b2][:], add, [f"x1l{b2}", f"y0l{b2}"], [f"x2_{b2}"])
        kb.tt("dve", x2[:], x2[:], y1l[b2][:], add, [f"x2_{b2}", f"y1l{b2}"], [f"x2_{b2}"])
        rstd_of(x2[:], [f"x2_{b2}"], s6[0], f"s6_0_{b2}", jk2[:], f"jk2_{b2}")
        kb.ts("dve", n3b[:], x2[:], s6[0][:, 0:1], None, mult, None, [f"x2_{b2}", f"s6_0_{b2}"], [f"n3b_{b2}"])
        for k in range(8):
            kb.tr(pN3[:, k, :], n3b[:, k * 128:(k + 1) * 128], ident[:], [f"n3b_{b2}", "ident"], ["pN3"])
        kb.cp("act", n3T[:], pN3[:], ["pN3"], [f"n3T_{b2}"])
        for half in range(2):
            for k in range(8):
                kb.mm(pGt[:, half * 512:(half + 1) * 512], n3T[:, k, :], wpg[:, k, half * 512:(half + 1) * 512], k == 0, k == 7,
                      [f"n3T_{b2}"] + wpgb, ["pGt"])
        kb.tt("dve", gate[:], pGt[:], bpg_bc[:], add, ["pGt", "bpg_bc"], [f"gate_{b2}"])
        kb.act(gate[:], gate[:], AF.Sigmoid, [f"gate_{b2}"], [f"gate_{b2}"])
        kb.cp("pool", plb[:], pl[b2][:], [f"pl{b2}"], [f"plb_{b2}"])
        for c in range(2):
            kb.tr(pPT[:, c, :], plb[:, c * 128:(c + 1) * 128], ident[:], [f"plb_{b2}", "ident"], ["pPT"])
        kb.cp("dve", pTs[:], pPT[:, 0:2, :], ["pPT"], [f"pTs_{b2}"])
        for half in range(2):
            for c in range(2):
                kb.mm(pPp[:, half * 512:(half + 1) * 512], pTs[:, c, :], wpp[:, c, half * 512:(half + 1) * 512], c == 0, c == 1,
                      [f"pTs_{b2}", "wpp"], ["pPp"])
        rstd_of(pPp[:], ["pPp"], s6[1], f"s6_1_{b2}", jk2[:], f"jk2_{b2}")
        kb.stt(ple[:], pPp[:], s6[1][:, 0:1], ppw_bc[:], mult, mult, ["pPp", f"s6_1_{b2}", "ppw_bc"], [f"ple_{b2}"])
        kb.tt("dve", ple[:], ple[:], gate[:], mult, [f"ple_{b2}", f"gate_{b2}"], [f"ple_{b2}"])
        kb.tt("dve", x3[:], x2[:], ple[:], add, [f"x2_{b2}", f"ple_{b2}"], [f"x3_{b2}"])
        rstd_of(x3[:], [f"x3_{b2}"], s6[2], f"s6_2_{b2}", jk2[:], f"jk2_{b2}")
        kb.stt(ot[b2][:], x3[:], s6[2][:, 0:1], fin_bc[:], mult, mult, [f"x3_{b2}", f"s6_2_{b2}", "fin_bc"], [f"ot{b2}"])
        kb.dma("sp", OUT[ts0:ts1, :], ot[b2][:], f"ot{b2}", [f"ot{b2}"], [])
    kb.barrier()
    kb.pop()
    return nc, kb


_NC_CACHE = {}


def kernel(**inputs):
    x = np.asarray(inputs["x"], dtype=np.float32)
    B, S, _ = x.shape
    assert B == 8
    p = np.asarray(inputs["p"], dtype=np.float32)
    pos = np.asarray(inputs["positions"]).astype(np.int32)
    if S not in _NC_CACHE:
        nc, kb = build(S)
        kb.S.emit(nc, None)
        _NC_CACHE[S] = nc
    nc = _NC_CACHE[S]
    shared = {}
    for name, shape in WEIGHT_SPECS:
        a = np.asarray(inputs[name], dtype=np.float32)
        if name != "final_norm_w":
            a = a[0]
        shp = shape if len(shape) == 2 else [1, shape[0]]
        shared[name] = np.ascontiguousarray(a.reshape(shp))
    in_maps = []
    for b in range(B):
        m = dict(shared)
        m["x"] = np.ascontiguousarray(x[b])
        m["p"] = np.ascontiguousarray(p[0, b])
        m["pos"] = np.ascontiguousarray(pos[b].reshape(S // 128, 128))
        in_maps.append(m)
    res = run_bass_kernel_spmd(nc, in_maps, core_ids=list(range(B)))
    return np.stack([np.asarray(r["out"], dtype=np.float32) for r in res.results], axis=0)
```

```python
import numpy as np
import concourse.bass as bass
import concourse.mybir as mybir
from concourse.bass_utils import run_bass_kernel_spmd

F32 = mybir.dt.float32
BF16 = mybir.dt.bfloat16
I32 = mybir.dt.int32
AF = mybir.ActivationFunctionType
ALU = mybir.AluOpType
AX = mybir.AxisListType

ENGS = ("pe", "act", "dve", "pool", "sp")


class Buf:
    __slots__ = ("name", "w", "r")

    def __init__(self, name):
        self.name = name
        self.w = None
        self.r = []


class _Op:
    __slots__ = ("eng", "fn", "deps", "dma_key", "signal", "count", "kind")

    def __init__(self, eng, fn, deps, dma_key, kind):
        self.eng = eng
        self.fn = fn
        self.deps = deps
        self.dma_key = dma_key
        self.signal = False
        self.count = 0
        self.kind = kind


class Sched:
    def __init__(self):
        self.ops = []
        self.bufs = {}
        self.psum_names = set()

    def buf(self, name):
        b = self.bufs.get(name)
        if b is None:
            b = self.bufs[name] = Buf(name)
        return b

    def _norm(self, xs):
        out = []
        for x in xs:
            if x is None:
                continue
            out.append(self.buf(x) if isinstance(x, str) else x)
        return out

    def op(self, eng, fn, reads=(), writes=(), dma_key=None):
        reads = self._norm(reads)
        writes = self._norm(writes)
        deps = set()
        for b in reads:
            if b.w is not None:
                deps.add(b.w)
            if b.name in self.psum_names:
                deps.update(r for r in b.r if self.ops[r].eng != eng)
        for b in writes:
            if b.w is not None:
                deps.add(b.w)
            deps.update(b.r)
        if eng == "pe":
            deps = set(d for d in deps if self.ops[d].eng != "pe")
        i = len(self.ops)
        self.ops.append(_Op(eng, fn, deps, dma_key, "dma" if dma_key else "op"))
        for b in reads:
            b.r.append(i)
        for b in writes:
            b.w = i
            b.r = []
        return i

    def dma(self, eng, fn, key, reads=(), writes=()):
        return self.op(eng, fn, reads, writes, dma_key=eng + "_" + key)

    def barrier(self):
        self.ops.append(_Op(None, None, set(), None, "barrier"))

    def emit(self, nc, engines):
        ops = self.ops
        last = {e: None for e in ENGS}
        pend_dma = []
        bar_deps = {}
        for i, o in enumerate(ops):
            if o.kind == "barrier":
                d = set(v for v in last.values() if v is not None)
                d.update(pend_dma)
                bar_deps[i] = d
                pend_dma = []
            else:
                last[o.eng] = i
                if o.kind == "dma":
                    pend_dma.append(i)
        for i, o in enumerate(ops):
            for d in o.deps:
                if ops[d].kind == "op":
                    ops[d].signal = True
        for d in bar_deps.values():
            for j in d:
                if ops[j].kind == "op":
                    ops[j].signal = True
        cnt = {e: 0 for e in ENGS}
        dcnt = {}
        for o in ops:
            if o.kind == "op" and o.signal:
                cnt[o.eng] += 1
                o.count = cnt[o.eng]
            elif o.kind == "dma":
                dcnt[o.dma_key] = dcnt.get(o.dma_key, 0) + 16
                o.count = dcnt[o.dma_key]
        sem_names = ["e_" + e for e in ENGS] + ["d_" + k for k in dcnt]
        import contextlib
        with contextlib.ExitStack() as st:
            sems = {n: st.enter_context(nc.semaphore(n)) for n in sem_names}
            seen = {e: {} for e in ENGS}
            streams = {e: [] for e in ENGS}

            def need(e, d):
                p = ops[d]
                name = ("d_" + p.dma_key) if p.kind == "dma" else ("e_" + p.eng)
                if seen[e].get(name, 0) >= p.count:
                    return
                seen[e][name] = p.count
                streams[e].append(("wait", name, p.count))

            for i, o in enumerate(ops):
                if o.kind == "barrier":
                    best = {}
                    for d in bar_deps[i]:
                        p = ops[d]
                        name = ("d_" + p.dma_key) if p.kind == "dma" else ("e_" + p.eng)
                        if name not in best or ops[best[name]].count < p.count:
                            best[name] = d
                    for e in ENGS:
                        for d in sorted(best.values()):
                            need(e, d)
                    continue
                for d in sorted(o.deps):
                    need(o.eng, d)
                streams[o.eng].append(("op", o))
            self.streams = streams
            block = st.enter_context(nc.Block())

            def run(e, eng):
                for it in streams[e]:
                    if it[0] == "wait":
                        eng.wait_ge(sems[it[1]], it[2])
                    else:
                        o = it[1]
                        ins = o.fn(eng)
                        if o.kind == "dma":
                            ins.then_inc(sems["d_" + o.dma_key], 16)
                        elif o.signal:
                            ins.then_inc(sems["e_" + o.eng], 1)

            @block.tensor
            def _(eng):
                run("pe", eng)

            @block.scalar
            def _(eng):
                run("act", eng)

            @block.vector
            def _(eng):
                run("dve", eng)

            @block.gpsimd
            def _(eng):
                run("pool", eng)

            @block.sync
            def _(eng):
                run("sp", eng)
        return {e: len(streams[e]) for e in ENGS}


D_MODEL = 1024
PLE_DIM = 256
EPS = 1e-6
NH = 8
QR, KVR, ROPE, NOPE, VD = 256, 128, 32, 64, 64
SH, SP_, SG, SN, SCONV = 8, 64, 2, 64, 5
SINNER, SXBC = 512, 768
INCOLS = 1712
NEXP, NGRP, EPG, DEXP = 32, 4, 8, 256
C_CQ, C_CKV, C_KR, C_Z, C_XBC, C_DT = 0, 256, 384, 416, 928, 1696

WEIGHT_SPECS = [
    ("attn_norm_w", [1024]), ("w_in", [1024, 1712]), ("q_norm_w", [256]), ("w_uq", [256, 768]),
    ("kv_norm_w", [128]), ("w_ukv", [128, 1024]), ("attn_out_norm_w", [512]), ("conv_w", [5, 768]),
    ("conv_b", [768]), ("dt_bias", [1, 16]), ("a_log", [1, 16]), ("ssd_d", [1, 8]), ("ssd_norm_w", [512]),
    ("w_o", [1024, 1024]), ("ffn_norm_w", [1024]), ("w_router_group", [1024, 4]), ("b_router_group", [1, 4]),
    ("w_router_expert", [1024, 32]), ("b_router_expert", [1, 32]), ("w_exp_gate", [32 * 1024, 256]),
    ("w_exp_up", [32 * 1024, 256]), ("w_exp_down", [32 * 256, 1024]), ("ple_norm_w", [1024]),
    ("w_ple_gate", [1024, 1024]), ("b_ple_gate", [1, 1024]), ("w_ple_proj", [256, 1024]),
    ("ple_post_norm_w", [1024]), ("final_norm_w", [1024]),
]


class KB:
    def __init__(self, nc):
        import contextlib
        self.nc = nc
        self.S = Sched()
        self.root = contextlib.ExitStack()
        self.stack = [self.root]

    def push(self):
        import contextlib
        st = contextlib.ExitStack()
        self.stack.append(st)
        return st

    def pop(self):
        self.stack.pop().close()

    def sb(self, name, shape, dt):
        return self.stack[-1].enter_context(self.nc.sbuf_tensor(name, list(shape), dt))

    def ps(self, name, shape, dt=F32):
        self.S.psum_names.add(name)
        return self.stack[-1].enter_context(self.nc.psum_tensor(name, list(shape), dt))

    def act(self, out, in_, func, r, w, **kw):
        self.S.op("act", lambda e: e.activation(out=out, in_=in_, func=func, **kw), r, w)

    def ts(self, eng, out, in0, s1, s2, op0, op1, r, w):
        if op1 is None:
            self.S.op(eng, lambda e: e.tensor_scalar(out=out, in0=in0, scalar1=s1, scalar2=None, op0=op0), r, w)
        else:
            self.S.op(eng, lambda e: e.tensor_scalar(out=out, in0=in0, scalar1=s1, scalar2=s2, op0=op0, op1=op1), r, w)

    def tt(self, eng, out, in0, in1, op, r, w):
        self.S.op(eng, lambda e: e.tensor_tensor(out=out, in0=in0, in1=in1, op=op), r, w)

    def stt(self, out, in0, scalar, in1, op0, op1, r, w):
        self.S.op("dve", lambda e: e.scalar_tensor_tensor(out=out, in0=in0, scalar=scalar, in1=in1, op0=op0, op1=op1), r, w)

    def cp(self, eng, out, in_, r, w):
        if eng == "act":
            self.S.op("act", lambda e: e.activation(out=out, in_=in_, func=AF.Copy), r, w)
        else:
            self.S.op(eng, lambda e: e.tensor_copy(out=out, in_=in_), r, w)

    def memset(self, eng, ap, val, w):
        self.S.op(eng, lambda e: e.memset(ap, val), (), w)

    def mm(self, out, lhsT, rhs, start, stop, r, w, **kw):
        self.S.op("pe", lambda e: e.matmul(out, lhsT=lhsT, rhs=rhs, start=start, stop=stop, **kw), r, w)

    def tr(self, out, in_, ident, r, w):
        self.S.op("pe", lambda e: e.transpose(out=out, in_=in_, identity=ident), r, w)

    def dma(self, eng, out, in_, key, r, w, **kw):
        self.S.dma(eng, lambda e: e.dma_start(out=out, in_=in_, **kw), key, r, w)

    def barrier(self):
        self.S.barrier()

    def reduce(self, out, in_, op, r, w):
        self.S.op("dve", lambda e: e.tensor_reduce(out=out, in_=in_, axis=AX.X, op=op), r, w)

    def recip(self, out, in_, r, w):
        self.S.op("dve", lambda e: e.reciprocal(out=out, in_=in_), r, w)

    def max8(self, out, in_, r, w):
        self.S.op("dve", lambda e: e.max(out=out, in_=in_), r, w)


def build(S=4096, stop_after=None, serial_scatter=False):
    import math
    nc = bass.Bass("TRN2", target_bir_lowering=False)
    NT = S // 128
    GS = min(512, S)
    NG = S // GS
    TG = GS // 128
    D = {}
    D["x"] = nc.dram_tensor("x", [S, 1024], F32, kind="ExternalInput").ap()
    D["p"] = nc.dram_tensor("p", [S, 256], F32, kind="ExternalInput").ap()
    D["pos"] = nc.dram_tensor("pos", [NT, 128], I32, kind="ExternalInput").ap()
    for name, shape in WEIGHT_SPECS:
        shp = shape if len(shape) == 2 else [1, shape[0]]
        D[name] = nc.dram_tensor(name, shp, F32, kind="ExternalInput").ap()
    OUT = nc.dram_tensor("out", [S, 1024], F32, kind="ExternalOutput").ap()

    def scr(name, shape, dt):
        return nc.dram_tensor(name, shape, dt).ap()

    S_cqn = scr("S_cqn", [256, S], BF16)
    S_kvn = scr("S_kvn", [128, S], BF16)
    S_sz = scr("S_sz", [512, S], BF16)
    S_xbc = scr("S_xbc", [768, S], F32)
    S_xtm = scr("S_xtm", [S, 768], BF16)
    S_bcT = scr("S_bcT", [256, S], BF16)
    S_yf = scr("S_yf", [512, S], F32)
    S_yn = scr("S_yn", [512, S], BF16)

    import os
    KD = int(os.environ.get("KDBG", "0"))
    kb = KB(nc)
    Sd = kb.S
    mult, add, sub = ALU.mult, ALU.add, ALU.subtract
    _regs = {}

    def BC(e, val):
        if val not in _regs:
            _regs[val] = e.to_reg(val)
        return _regs[val]

    VROWS = [("anw", "attn_norm_w", 8), ("cb", "conv_b", 6), ("cw", "conv_w", 30), ("kvw", "kv_norm_w", 1),
             ("qw", "q_norm_w", 2), ("pnw", "ple_norm_w", 8)]
    voff = {}
    _o = 0
    for nm, _, k in VROWS:
        voff[nm] = (_o, k)
        _o += k
    NV = _o

    identf = kb.sb("identf", [128, 128], F32)
    ident = kb.sb("ident", [128, 128], BF16)
    onesf = kb.sb("onesf", [128, 128], F32)
    onesb = kb.sb("onesb", [128, 128], BF16)
    kb.memset("pool", identf[:], 0.0, ["identf"])
    Sd.op("pool", lambda e: e.affine_select(out=identf[:], in_=identf[:], pattern=[[-1, 128]],
                                            compare_op=ALU.not_equal, fill=1.0, base=0,
                                            channel_multiplier=1), ["identf"], ["identf"])
    kb.cp("dve", ident[:], identf[:], ["identf"], ["ident"])
    kb.memset("pool", onesf[:], 1.0, ["onesf"])
    c_eps = kb.sb("c_eps", [128, 1], F32)
    c_one = kb.sb("c_one", [128, 1], F32)
    kb.memset("pool", c_eps[:], EPS, ["c_eps"])
    kb.memset("pool", c_one[:], 1.0, ["c_one"])
    kb.memset("pool", onesb[:], 1.0, ["onesb"])

    vecs = kb.sb("vecs", [128, NV], F32)
    vecs64 = kb.sb("vecs64", [64, 16], F32)
    kb.push()
    vst = kb.sb("vst", [NV, 128], F32)
    vst64 = kb.sb("vst64", [16, 64], F32)
    pvec = kb.ps("pvec", [128, 512], F32)
    for nm, dn, k in VROWS:
        o_ = voff[nm][0]
        src = D[dn]
        src = src.rearrange("o (k p) -> (o k) p", p=128) if dn != "conv_w" else src.rearrange("t (j p) -> (t j) p", p=128)
        kb.dma("sp", vst[o_:o_ + k, :], src, "vst", [], ["vst"])
    kb.dma("sp", vst64[0:8, :], D["attn_out_norm_w"].rearrange("o (k p) -> (o k) p", p=64), "vst64", [], ["vst64"])
    kb.dma("sp", vst64[8:16, :], D["ssd_norm_w"].rearrange("o (k p) -> (o k) p", p=64), "vst64", [], ["vst64"])
    kb.tr(pvec[:, 0:NV], vst[:], identf[:NV, :NV], ["vst", "identf"], ["pvec"])
    kb.cp("dve", vecs[:], pvec[:, 0:NV], ["pvec"], ["vecs"])
    kb.tr(pvec[0:64, 64:80], vst64[:], identf[:16, :16], ["vst64", "identf", "vecs"], ["pvec"])
    kb.cp("dve", vecs64[:], pvec[0:64, 64:80], ["pvec"], ["vecs64"])
    kb.barrier()
    kb.pop()

    def vec_pk(name, dram=None, k=None):
        o_, k_ = voff[name]
        return vecs[:, o_:o_ + k_]

    cosT = kb.sb("cosT", [128, NT, 16], F32)
    sinT = kb.sb("sinT", [128, NT, 16], F32)
    krot = kb.sb("krot", [128, NT, 32], F32)
    dtp = kb.sb("dtp", [128, NT, 16], F32)
    dtb_bc = kb.sb("dtb_bc", [128, 16], F32)
    a_bc = kb.sb("a_bc", [128, 16], F32)
    kb.dma("sp", dtb_bc[:], D["dt_bias"].partition_broadcast(128), "dtb_bc", [], ["dtb_bc"])
    kb.dma("sp", a_bc[:], D["a_log"].partition_broadcast(128), "a_bc", [], ["a_bc"])
    kb.act(a_bc[:], a_bc[:], AF.Exp, ["a_bc"], ["a_bc"])
    kb.ts("dve", a_bc[:], a_bc[:], -1.0, None, mult, None, ["a_bc"], ["a_bc"])
    kb.push()
    posi = kb.sb("posi", [NT, 128], I32)
    posr = kb.sb("posr", [NT, 128], F32)
    posf = kb.sb("posf", [128, NT], F32)
    invf = kb.sb("invf", [128, 16], F32)
    rr = kb.sb("rr", [128, NT, 16], F32)
    rf = kb.sb("rf", [128, NT, 16], F32)
    ri = kb.sb("ri", [128, NT, 16], I32)
    rm = kb.sb("rm", [128, NT, 16], F32)
    ppos = kb.ps("ppos", [128, 512], F32)
    kb.dma("sp", posi[:], D["pos"], "posi", [], ["posi"])
    kb.cp("dve", posr[:], posi[:], ["posi"], ["posr"])
    kb.tr(ppos[:, :NT], posr[:], identf[:NT, :NT], ["posr", "identf"], ["ppos"])
    kb.cp("dve", posf[:], ppos[:, :NT], ["ppos"], ["posf"])
    for i in range(16):
        kb.memset("pool", invf[:, i:i + 1], (10000.0 ** (-(2.0 * i) / 32.0)) / (2 * math.pi), ["invf"])
    for t in range(NT):
        kb.ts("dve", rr[:, t, :], invf[:], posf[:, t:t + 1], None, mult, None, ["invf", "posf"], ["rr"])
    for shift, dst in ((0.0, "sinT"), (0.25, "cosT")):
        dstt = sinT if dst == "sinT" else cosT
        if shift:
            kb.ts("dve", rr[:], rr[:], shift, None, add, None, ["rr"], ["rr"])
        kb.cp("dve", ri[:], rr[:], ["rr"], ["ri"])
        kb.cp("dve", rf[:], ri[:], ["ri"], ["rf"])
        kb.tt("dve", rf[:], rr[:], rf[:], sub, ["rr", "rf"], ["rf"])
        kb.ts("dve", rm[:], rf[:], 0.5, None, ALU.is_gt, None, ["rf"], ["rm"])
        kb.tt("dve", rf[:], rf[:], rm[:], sub, ["rf", "rm"], ["rf"])
        kb.ts("dve", rm[:], rf[:], -0.5, None, ALU.is_lt, None, ["rf"], ["rm"])
        kb.tt("dve", rf[:], rf[:], rm[:], add, ["rf", "rm"], ["rf"])
        kb.act(dstt[:], rf[:], AF.Sin, ["rf"], [dst], scale=2 * math.pi)
    kb.barrier()
    kb.pop()

    if stop_after == "P0":
        return nc, kb
    kb.push()
    anw = vec_pk("anw")
    w_in_bf = kb.sb("w_in_bf", [128, 8, INCOLS], BF16)
    wst = [kb.sb(f"wst{i}", [128, INCOLS], F32) for i in range(2)]
    for k in range(8):
        kb.dma("sp", wst[k % 2][:], D["w_in"][k * 128:(k + 1) * 128, :], f"wst{k % 2}", [], [f"wst{k % 2}"])
        kb.ts("dve" if k % 2 == 0 else "pool", w_in_bf[:, k, :], wst[k % 2][:], anw[:, k:k + 1], None, mult, None,
              [f"wst{k % 2}", "vecs"], ["w_in_bf"])
    wsmall = kb.sb("wsmall", [128, 8, 48], BF16)
    kb.cp("dve", wsmall[:, :, 0:32], w_in_bf[:, :, C_KR:C_KR + 32], ["w_in_bf"], ["wsmall"])
    kb.cp("dve", wsmall[:, :, 32:48], w_in_bf[:, :, C_DT:C_DT + 16], ["w_in_bf"], ["wsmall"])

    NX = 3
    xt = [kb.sb(f"xt{i}", [128, 1024], F32) for i in range(NX)]
    hn = [kb.sb(f"hn{i}", [128, 1024], BF16) for i in range(NX)]
    ss = [kb.sb(f"ss{i}", [128, 1], F32) for i in range(NX)]
    junk = kb.sb("junk", [128, 1024], BF16)
    hT = [kb.sb(f"hT{i}", [128, 8, GS], BF16) for i in range(2)]
    sm = kb.sb("sm", [128, 48], F32)
    rot = [kb.sb(f"rot{i}", [128, 16], F32) for i in range(4)]
    dx = kb.sb("dx", [128, 16], F32)
    cqf = kb.sb("cqf", [128, 2, GS], F32)
    kvf = kb.sb("kvf", [128, GS], F32)
    sq = [kb.sb(f"sq{i}", [128, GS], F32) for i in range(3)]
    rs = [kb.sb(f"rs{i}", [128, GS], F32) for i in range(2)]
    cqn = [kb.sb(f"cqn{i}", [128, 2, GS], BF16) for i in range(2)]
    kvn = [kb.sb(f"kvn{i}", [128, GS], BF16) for i in range(2)]
    szs = [kb.sb(f"szs{i}", [128, 4, GS], BF16) for i in range(2)]
    xbs = [kb.sb(f"xbs{i}", [128, 6, GS], F32) for i in range(2)]
    pT = [kb.ps(f"pT{i}", [128, 8, 128], BF16) for i in range(2)]
    pS = kb.ps("pS", [128, 512], F32)
    pM = [kb.ps(f"pM{i}", [128, 512], F32) for i in range(3)]
    pST = [kb.ps(f"pST{i}", [128, 512], F32) for i in range(2)]

    chunks = [("cq", 0, C_CQ), ("cq", 1, C_CQ + 128), ("ckv", 0, C_CKV)]
    chunks += [("z", i, C_Z + 128 * i) for i in range(4)]
    chunks += [("xbc", i, C_XBC + 128 * i) for i in range(6)]
    ci_glob = 0
    for g in range(NG):
        gp = g % 2
        for t in range(TG):
            tt_ = g * TG + t
            s = tt_ % NX
            pp = tt_ % 2
            kb.dma("sp", xt[s][:], D["x"][tt_ * 128:(tt_ + 1) * 128, :], f"xt{s}", [], [f"xt{s}"])
            kb.act(junk[:], xt[s][:], AF.Square, [f"xt{s}"], ["junk", f"ss{s}"], accum_out=ss[s][:])
            kb.ts("dve", ss[s][:], ss[s][:], 1.0 / 1024, EPS, mult, add, [f"ss{s}"], [f"ss{s}"])
            kb.act(ss[s][:], ss[s][:], AF.Ln, [f"ss{s}"], [f"ss{s}"])
            kb.act(ss[s][:], ss[s][:], AF.Exp, [f"ss{s}"], [f"ss{s}"], scale=-0.5)
            kb.ts("dve", hn[s][:], xt[s][:], ss[s][:], None, mult, None, [f"xt{s}", f"ss{s}"], [f"hn{s}"])
            for k in range(8):
                kb.tr(pT[pp][:, k, :], hn[s][:, k * 128:(k + 1) * 128], ident[:], [f"hn{s}", "ident"], [f"pT{pp}"])
            kb.cp("act" if t % 2 else "dve", hT[gp][:, :, t * 128:(t + 1) * 128], pT[pp][:], [f"pT{pp}"], [f"hT{gp}_{t}"])
            if KD and KD < 2:
                continue
            for k in range(8):
                kb.mm(pS[:, :48], hT[gp][:, k, t * 128:(t + 1) * 128], wsmall[:, k, :], k == 0, k == 7,
                      [f"hT{gp}_{t}", "wsmall"], ["pS"])
            kb.cp("dve", sm[:], pS[:, :48], ["pS"], ["sm"])
            if KD and KD < 3:
                continue
            k1, k2 = sm[:, 0:16], sm[:, 16:32]
            cs_, sn_ = cosT[:, tt_, :], sinT[:, tt_, :]
            kb.tt("pool", rot[0][:], k1, cs_, mult, ["sm", "cosT"], ["rot0"])
            kb.tt("pool", rot[1][:], k2, sn_, mult, ["sm", "sinT"], ["rot1"])
            kb.tt("pool", krot[:, tt_, 0:16], rot[0][:], rot[1][:], sub, ["rot0", "rot1"], ["krot"])
            kb.tt("pool", rot[2][:], k2, cs_, mult, ["sm", "cosT"], ["rot2"])
            kb.tt("pool", rot[3][:], k1, sn_, mult, ["sm", "sinT"], ["rot3"])
            kb.tt("pool", krot[:, tt_, 16:32], rot[2][:], rot[3][:], add, ["rot2", "rot3"], ["krot"])
            kb.tt("dve", dx[:], sm[:, 32:48], dtb_bc[:], add, ["sm", "dtb_bc"], ["dx"])
            kb.act(dx[:], dx[:], AF.Exp, ["dx"], ["dx"])
            kb.act(dtp[:, tt_, :], dx[:], AF.Ln, ["dx", "c_one"], ["dtp"], bias=c_one[:])
        hT_bufs = [f"hT{gp}_{t}" for t in range(TG)]
        for (kind, i, c0) in (chunks if not KD else chunks[:max(0, KD - 3)]):
            pi = ci_glob % 3
            ci_glob += 1
            pm = pM[pi]
            for k in range(8):
                kb.mm(pm[:, :GS], w_in_bf[:, k, c0:c0 + 128], hT[gp][:, k, :], k == 0, k == 7,
                      ["w_in_bf"] + hT_bufs, [f"pM{pi}"])
            KD2 = int(os.environ.get("KDBG2", "9"))
            if kind == "cq":
                if KD2 >= 1 and os.environ.get("KNODVE") != "1":
                    kb.cp("dve", cqf[:, i, :], pm[:, :GS], [f"pM{pi}"], [f"cqf{i}"])
                if KD2 >= 2:
                    if os.environ.get("KSQ") == "dve":
                        kb.cp("dve", sq[i][:], pm[:, :GS], [f"pM{pi}"], [f"sq{i}"])
                    elif os.environ.get("KSQ") == "junk":
                        kb.act(junk[:, :GS], pm[:, :GS], AF.Square, [f"pM{pi}"], ["junk"])
                    else:
                        kb.act(sq[i][:], pm[:, :GS], AF.Square, [f"pM{pi}"], [f"sq{i}"])
                if KD2 >= 3:
                    kb.mm(pST[0][:, :GS], onesf[:], sq[i][:], i == 0, i == 1, ["onesf", f"sq{i}"], ["pST0"])
                if i == 1:
                    kb.act(rs[0][:], pST[0][:, :GS], AF.Ln, ["pST0", "c_eps"], ["rs0"], scale=1.0 / 256, bias=c_eps[:])
                    kb.act(rs[0][:], rs[0][:], AF.Exp, ["rs0"], ["rs0"], scale=-0.5)
                    for j in range(2):
                        kb.tt("dve", cqn[gp][:, j, :], cqf[:, j, :], rs[0][:], mult, [f"cqf{j}", "rs0"], [f"cqn{gp}_{j}"])
                    kb.dma("sp", S_cqn.rearrange("(c p) s -> p c s", p=128)[:, :, g * GS:(g + 1) * GS], cqn[gp][:],
                           f"cqn{gp}", [f"cqn{gp}_0", f"cqn{gp}_1"], [])
            elif kind == "ckv":
                kb.cp("dve", kvf[:], pm[:, :GS], [f"pM{pi}"], ["kvf"])
                kb.act(sq[2][:], pm[:, :GS], AF.Square, [f"pM{pi}"], ["sq2"])
                kb.mm(pST[1][:, :GS], onesf[:], sq[2][:], True, True, ["onesf", "sq2"], ["pST1"])
                kb.act(rs[1][:], pST[1][:, :GS], AF.Ln, ["pST1", "c_eps"], ["rs1"], scale=1.0 / 128, bias=c_eps[:])
                kb.act(rs[1][:], rs[1][:], AF.Exp, ["rs1"], ["rs1"], scale=-0.5)
                kb.tt("dve", kvn[gp][:], kvf[:], rs[1][:], mult, ["kvf", "rs1"], [f"kvn{gp}"])
                kb.dma("sp", S_kvn[:, g * GS:(g + 1) * GS], kvn[gp][:], f"kvn{gp}", [f"kvn{gp}"], [])
            elif kind == "z":
                kb.act(szs[gp][:, i, :], pm[:, :GS], AF.Silu, [f"pM{pi}"], [f"szs{gp}_{i}"])
                if i == 3:
                    kb.dma("sp", S_sz.rearrange("(c p) s -> p c s", p=128)[:, :, g * GS:(g + 1) * GS], szs[gp][:],
                           f"szs{gp}", [f"szs{gp}_{j}" for j in range(4)], [])
            else:
                kb.cp("dve" if i % 2 else "act", xbs[gp][:, i, :], pm[:, :GS], [f"pM{pi}"], [f"xbs{gp}_{i}"])
                if i == 5:
                    kb.dma("sp", S_xbc.rearrange("(c p) s -> p c s", p=128)[:, :, g * GS:(g + 1) * GS], xbs[gp][:],
                           f"xbs{gp}", [f"xbs{gp}_{j}" for j in range(6)], [])
    kb.barrier()
    kb.pop()
    if stop_after == "P1":
        return nc, kb

    kb.push()
    cw = vec_pk("cw").rearrange("p (t j) -> p t j", j=6)
    cb = vec_pk("cb")
    xin = [kb.sb(f"xin{i}", [128, 6, GS + 4], F32) for i in range(2)]
    acc = [kb.sb(f"acc{i}", [128, 6, GS], F32) for i in range(2)]
    xc = [kb.sb(f"xc{i}", [128, 6, GS], BF16) for i in range(2)]
    xts = [kb.sb(f"xts{i}", [128, 768], BF16) for i in range(2)]
    pX = [kb.ps(f"pX{i}", [128, 6, 128], BF16) for i in range(2)]
    xbc_v = S_xbc.rearrange("(c p) s -> p c s", p=128)
    for g in range(NG):
        gp = g % 2
        lo, hi = g * GS - 2, g * GS + GS + 2
        clo, chi = max(lo, 0), min(hi, S)
        if lo < 0:
            kb.memset("pool", xin[gp][:, :, 0:2], 0.0, [f"xin{gp}"])
        if hi > S:
            kb.memset("pool", xin[gp][:, :, GS + 2:GS + 4], 0.0, [f"xin{gp}"])
        kb.dma("sp", xin[gp][:, :, clo - lo:chi - lo], xbc_v[:, :, clo:chi], f"xin{gp}", [], [f"xin{gp}"])
        for j in range(6):
            kb.ts("dve", acc[gp][:, j, :], xin[gp][:, j, 0:GS], cw[:, 0, j:j + 1], cb[:, j:j + 1], mult, add,
                  [f"xin{gp}", "vecs"], [f"acc{gp}_{j}"])
            for k in range(1, 5):
                kb.stt(acc[gp][:, j, :], xin[gp][:, j, k:k + GS], cw[:, k, j:j + 1], acc[gp][:, j, :], mult, add,
                       [f"xin{gp}", "vecs", f"acc{gp}_{j}"], [f"acc{gp}_{j}"])
            kb.act(xc[gp][:, j, :], acc[gp][:, j, :], AF.Silu, [f"acc{gp}_{j}"], [f"xc{gp}_{j}"])
        xcb = [f"xc{gp}_{j}" for j in range(6)]
        kb.dma("sp", S_bcT.rearrange("(j p) s -> p j s", p=128)[:, :, g * GS:(g + 1) * GS], xc[gp][:, 4:6, :],
               f"xc{gp}", xcb, [])
        for t in range(TG):
            tt_ = g * TG + t
            pp = tt_ % 2
            for j in range(6):
                kb.tr(pX[pp][:, j, :], xc[gp][:, j, t * 128:(t + 1) * 128], ident[:], [f"xc{gp}_{j}", "ident"], [f"pX{pp}"])
            kb.cp("act" if t % 2 else "dve", xts[pp][:], pX[pp][:], [f"pX{pp}"], [f"xts{pp}"])
            kb.dma("sp", S_xtm[tt_ * 128:(tt_ + 1) * 128, :], xts[pp][:], f"xts{pp}", [f"xts{pp}"], [])
    kb.barrier()
    kb.pop()
    if stop_after == "P1b":
        return nc, kb

    kb.push()
    tri = [kb.sb("triU", [128, 128], F32), kb.sb("triL", [128, 128], F32)]
    for d_, (cm_, pat) in enumerate(((-1, 1), (1, -1))):
        kb.memset("pool", tri[d_][:], 1.0, [f"tri{d_}"])
        Sd.op("pool", lambda e, d_=d_, cm_=cm_, pat=pat: e.affine_select(
            out=tri[d_][:], in_=tri[d_][:], pattern=[[pat, 128]], compare_op=ALU.is_ge, fill=0.0, base=0,
            channel_multiplier=cm_), [f"tri{d_}"], [f"tri{d_}"])
    Esel = kb.sb("Esel", [8, 8, 128], F32)
    kb.memset("pool", Esel[:], 0.0, ["Esel"])
    Sd.op("pool", lambda e: e.affine_select(out=Esel[:], in_=Esel[:], pattern=[[-1, 8], [0, 128]],
                                            compare_op=ALU.not_equal, fill=1.0, base=0, channel_multiplier=1),
          ["Esel"], ["Esel"])
    d_bc = kb.sb("d_bc", [128, 8], F32)
    kb.dma("sp", d_bc[:], D["ssd_d"].partition_broadcast(128), "d_bc", [], ["d_bc"])
    diagD = kb.sb("diagD", [128, 8, 128], BF16)
    for r in range(8):
        kb.ts("dve", diagD[:, r, :], identf[:], d_bc[:, r:r + 1], None, mult, None, ["identf", "d_bc"], ["diagD"])
    xtm = [kb.sb(f"xtm{i}", [128, 768], BF16) for i in range(2)]
    bcT = [kb.sb(f"bcT{i}", [64, 4, 128], BF16) for i in range(2)]
    A_ = kb.sb("A_", [128, 8], F32)
    cs_sb = kb.sb("cs_sb", [128, 8], F32)
    csT_sb = kb.sb("csT_sb", [8, 128], F32)
    seg = kb.sb("seg", [128, 8, 128], F32)
    GTm = kb.sb("GTm", [128, 2, 128], F32)
    MT = kb.sb("MT", [128, 8, 128], BF16)
    ecs = kb.sb("ecs", [64, 8, 128], F32)
    Cdec = kb.sb("Cdec", [64, 8, 128], BF16)
    wex = kb.sb("wex", [128, 8], F32)
    xw = kb.sb("xw", [128, 8, 64], BF16)
    xdt = kb.sb("xdt", [128, 8, 64], BF16)
    dec = kb.sb("dec", [64, 8], F32)
    hst = kb.sb("hst", [64, 8, 64], F32)
    hbf = kb.sb("hbf", [64, 8, 64], BF16)
    ysb = [kb.sb(f"ysb{i}", [64, 8, 128], F32) for i in range(2)]
    yfl = [kb.sb(f"yfl{i}", [64, 8, 128], F32) for i in range(2)]
    szl = [kb.sb(f"szl{i}", [64, 8, 128], BF16) for i in range(2)]
    yg = kb.sb("yg", [64, 8, 128], F32)
    sqy = kb.sb("sqy", [64, 8, 128], F32)
    rsy = kb.sb("rsy", [64, 128], F32)
    ynb = [kb.sb(f"ynb{i}", [64, 8, 128], BF16) for i in range(2)]
    pA = kb.ps("pA", [128, 512], F32)
    pB = kb.ps("pB", [64, 8, 64], F32)
    pC = kb.ps("pC", [64, 512], F32)
    pCS = kb.ps("pCS", [128, 8, 128], F32)
    pG = kb.ps("pG", [128, 512], F32)
    pY = kb.ps("pY", [64, 8, 128], F32)
    yf_v = S_yf.rearrange("(r p) s -> p r s", p=64)
    yn_v = S_yn.rearrange("(r p) s -> p r s", p=64)
    sz_v = S_sz.rearrange("(r p) s -> p r s", p=64)
    bcT_v = S_bcT.rearrange("(q n) s -> n q s", n=64)
    for d_ in range(2):
        kb.memset("pool", hst[:], 0.0, ["hst"])
        kb.memset("pool", hbf[:], 0.0, ["hbf"])
        order = list(range(NT)) if d_ == 0 else list(range(NT - 1, -1, -1))
        for ci, c in enumerate(order):
            b2 = ci % 2
            cs0, cs1 = c * 128, (c + 1) * 128
            kb.dma("sp", xtm[b2][:], S_xtm[cs0:cs1, :], f"xtm{b2}", [], [f"xtm{b2}"])
            kb.dma("sp", bcT[b2][:], bcT_v[:, :, cs0:cs1], f"bcT{b2}", [], [f"bcT{b2}"])
            if d_ == 1:
                kb.dma("sp", yfl[b2][:], yf_v[:, :, cs0:cs1], f"yfl{b2}", [], [f"yfl{b2}"])
                kb.dma("sp", szl[b2][:], sz_v[:, :, cs0:cs1], f"szl{b2}", [], [f"szl{b2}"])
            dts = dtp[:, c, d_ * 8:(d_ + 1) * 8]
            kb.tt("dve", A_[:], dts, a_bc[:, d_ * 8:(d_ + 1) * 8], mult, ["dtp", "a_bc"], ["A_"])
            kb.mm(pA[:, 0:8], tri[d_][:], A_[:], True, True, [f"tri{d_}", "A_"], ["pA"])
            kb.mm(pA[:, 8:16], onesf[:], A_[:], True, True, ["onesf", "A_"], ["pA"])
            kb.cp("dve", cs_sb[:], pA[:, 0:8], ["pA"], ["cs_sb"])
            kb.tr(pA[0:8, 128:256], cs_sb[:], identf[:], ["cs_sb", "identf"], ["pA"])
            kb.cp("dve", csT_sb[:], pA[0:8, 128:256], ["pA"], ["csT_sb"])
            for r in range(8):
                kb.mm(pCS[:, r, :], Esel[:, r, :], csT_sb[:], True, True, ["Esel", "csT_sb"], ["pCS"])
            kb.tt("dve", seg[:], pCS[:], cs_sb[:, :, None].to_broadcast([128, 8, 128]), sub, ["pCS", "cs_sb"], ["seg"])
            kb.ts("dve", seg[:], seg[:], 0.0, None, ALU.min, None, ["seg"], ["seg"])
            kb.act(seg[:], seg[:], AF.Exp, ["seg"], ["seg"])
            kb.act(ecs[:], pCS[0:64, :, :], AF.Exp, ["pCS"], ["ecs"])
            for g_ in range(2):
                kb.mm(pG[:, g_ * 128:(g_ + 1) * 128], bcT[b2][:, g_, :], bcT[b2][:, 2 + g_, :], True, True,
                      [f"bcT{b2}"], ["pG"])
            kb.tt("dve", GTm[:], pG[:, 0:256].rearrange("p (g i) -> p g i", g=2), tri[d_][:, None, :].to_broadcast([128, 2, 128]),
                  mult, ["pG", f"tri{d_}"], ["GTm"])
            kb.tt("dve", xdt[:], xtm[b2][:, 0:512].rearrange("p (r q) -> p r q", r=8), dts[:, :, None].to_broadcast([128, 8, 64]),
                  mult, [f"xtm{b2}", "dtp"], ["xdt"])
            kb.tt("dve", MT[:].rearrange("p (g r) i -> p g r i", g=2), seg[:].rearrange("p (g r) i -> p g r i", g=2),
                  GTm[:, :, None, :].to_broadcast([128, 2, 4, 128]), mult, ["seg", "GTm"], ["MT"])
            kb.tt("dve", Cdec[:].rearrange("p (g r) i -> p g r i", g=2), ecs[:].rearrange("p (g r) i -> p g r i", g=2),
                  bcT[b2][:, 2:4, None, :].to_broadcast([64, 2, 4, 128]), mult, ["ecs", f"bcT{b2}"], ["Cdec"])
            for r in range(8):
                kb.mm(pY[:, r, :], xdt[:, r, :], MT[:, r, :], True, False, ["xdt", "MT"], ["pY"])
                if d_ == 0:
                    kb.mm(pY[:, r, :], xtm[b2][:, r * 64:(r + 1) * 64], diagD[:, r, :], False, False, [f"xtm{b2}", "diagD"], ["pY"])
                kb.mm(pY[:, r, :], hbf[:, r, :], Cdec[:, r, :], False, True, ["hbf", "Cdec"], ["pY"])
            kb.tt("dve", wex[:], pA[:, 8:16], cs_sb[:], sub, ["pA", "cs_sb"], ["wex"])
            kb.act(wex[:], wex[:], AF.Exp, ["wex"], ["wex"])
            kb.tt("dve", xw[:], xdt[:], wex[:, :, None].to_broadcast([128, 8, 64]), mult, ["xdt", "wex"], ["xw"])
            for g_ in range(2):
                kb.mm(pB[:, g_ * 4:(g_ + 1) * 4, :], xtm[b2][:, 512 + g_ * 64:512 + (g_ + 1) * 64],
                      xw[:, g_ * 4:(g_ + 1) * 4, :], True, True, [f"xtm{b2}", "xw"], ["pB"])
            kb.act(dec[:], pA[0:64, 8:16], AF.Exp, ["pA"], ["dec"])
            kb.tt("dve", hst[:], hst[:], dec[:, :, None].to_broadcast([64, 8, 64]), mult, ["hst", "dec"], ["hst"])
            kb.tt("dve", hst[:], hst[:], pB[:], add, ["hst", "pB"], ["hst"])
            kb.cp("act", hbf[:], hst[:], ["hst"], ["hbf"])
            if d_ == 0:
                kb.cp("act", ysb[b2][:], pY[:], ["pY"], [f"ysb{b2}"])
                kb.dma("sp", yf_v[:, :, cs0:cs1], ysb[b2][:], f"ysb{b2}", [f"ysb{b2}"], [])
            else:
                kb.tt("dve", yg[:], pY[:], yfl[b2][:], add, ["pY", f"yfl{b2}"], ["yg"])
                kb.tt("dve", yg[:], yg[:], szl[b2][:], mult, ["yg", f"szl{b2}"], ["yg"])
                kb.act(sqy[:], yg[:], AF.Square, ["yg"], ["sqy"])
                for r in range(8):
                    kb.mm(pC[:, 0:128], onesf[0:64, 0:64], sqy[:, r, :], r == 0, r == 7, ["onesf", "sqy"], ["pC"])
                kb.act(rsy[:], pC[:, 0:128], AF.Ln, ["pC", "c_eps"], ["rsy"], scale=1.0 / 512, bias=c_eps[0:64, :])
                kb.act(rsy[:], rsy[:], AF.Exp, ["rsy"], ["rsy"], scale=-0.5)
                kb.tt("dve", ynb[b2][:], yg[:], rsy[:, None, :].to_broadcast([64, 8, 128]), mult, ["yg", "rsy"], [f"ynb{b2}"])
                kb.dma("sp", yn_v[:, :, cs0:cs1], ynb[b2][:], f"ynb{b2}", [f"ynb{b2}"], [])
        kb.barrier()
    kb.pop()
    if stop_after == "P2":
        return nc, kb

    S_attn = scr("S_attn", [512, S], BF16)
    SCALE = 96.0 ** -0.5
    kb.push()
    KT = kb.sb("KT", [128, 8, S], BF16)
    Vaug = kb.sb("Vaug", [128, NT, 8, 65], BF16)
    kmax = kb.sb("kmax", [128, 1], F32)
    kb.push()
    kvw = vec_pk("kvw")
    wkv_st = kb.sb("wkv_st", [128, 1024], F32)
    wkv = kb.sb("wkv", [128, 8, 128], BF16)
    kb.dma("sp", wkv_st[:], D["w_ukv"], "wkv_st", [], ["wkv_st"])
    kb.ts("dve", wkv[:].rearrange("p h d -> p (h d)"), wkv_st[:], kvw[:, 0:1], None, mult, None, ["wkv_st", "vecs"], ["wkv"])
    kvl = [kb.sb(f"kvl{i}", [128, GS], BF16) for i in range(2)]
    kpad = [kb.sb(f"kpad{i}", [128, 96], BF16) for i in range(2)]
    krs = [kb.sb(f"krs{i}", [128, 128], BF16) for i in range(2)]
    sqk = [kb.sb(f"sqk{i}", [96, GS], BF16) for i in range(2)]
    tmx = kb.sb("tmx", [128, 1], F32)
    pK = [kb.ps(f"pK{i}", [128, 512], F32) for i in range(2)]
    pV = [kb.ps(f"pV{i}", [128, 512], F32) for i in range(2)]
    pR = [kb.ps(f"pR{i}", [128, 1024], BF16) for i in range(2)]
    pN = [kb.ps(f"pN{i}", [128, 512], F32) for i in range(2)]
    kb.memset("pool", Vaug[:, :, :, 64:65], 1.0, ["V"])
    kb.memset("pool", kmax[:], 0.0, ["kmax"])
    for i in range(2):
        kb.memset("pool", kpad[i][:], 0.0, [f"kpad{i}"])
    for g in range(NG):
        gp = g % 2
        gs0, gs1 = g * GS, (g + 1) * GS
        kb.dma("sp", kvl[gp][:], S_kvn[:, gs0:gs1], f"kvl{gp}", [], [f"kvl{gp}"])
        for t in range(TG):
            tt_ = g * TG + t
            pp = tt_ % 2
            kb.cp("pool", kpad[pp][:, 64:96], krot[:, tt_, :], ["krot"], [f"kpad{pp}"])
            kb.tr(pR[pp][0:96, 0:128], kpad[pp][:], ident[:], [f"kpad{pp}", "ident"], [f"pR{pp}"])
            kb.cp("act", krs[pp][64:96, :], pR[pp][64:96, 0:128], [f"pR{pp}"], [f"krs{pp}"])
            kb.cp("pool", KT[64:96, :, tt_ * 128:(tt_ + 1) * 128], krs[pp][64:96, None, :].to_broadcast([32, 8, 128]),
                  [f"krs{pp}"], [f"KTr{g}"])
            kb.mm(pV[pp][:], kvl[gp][:, t * 128:(t + 1) * 128], wkv[:, :, 64:128], True, True, [f"kvl{gp}", "wkv"], [f"pV{pp}"])
            kb.cp("dve", Vaug[:, tt_, :, 0:64], pV[pp][:].rearrange("p (h d) -> p h d", h=8), [f"pV{pp}"], ["V"])
        for h in range(8):
            hp = h % 2
            kb.mm(pK[hp][0:64, :GS], wkv[:, h, 0:64], kvl[gp][:], True, True, ["wkv", f"kvl{gp}"], [f"pK{hp}"])
            kb.cp("act" if h % 2 else "dve", KT[0:64, h, gs0:gs1], pK[hp][0:64, :GS], [f"pK{hp}"], [f"KTn{g}_{h}"])
            kb.act(sqk[hp][:], KT[0:96, h, gs0:gs1], AF.Square, [f"KTn{g}_{h}", f"KTr{g}"], [f"sqk{hp}"])
            kb.mm(pN[hp][:, :GS], onesb[0:96, :], sqk[hp][:], True, True, ["onesb", f"sqk{hp}"], [f"pN{hp}"])
            Sd.op("dve", lambda e, hp=hp: e.tensor_reduce(out=tmx[:], in_=pN[hp][:, :GS], axis=AX.X, op=ALU.max),
                  [f"pN{hp}"], ["tmx"])
            kb.tt("dve", kmax[:], kmax[:], tmx[:], ALU.max, ["kmax", "tmx"], ["kmax"])
    kb.barrier()
    kb.pop()
    if stop_after == "P3":
        return nc, kb

    kb.push()
    qw = vec_pk("qw")
    wuq_st = kb.sb("wuq_st", [128, 2, 768], F32)
    wuq = kb.sb("wuq", [128, 2, 768], BF16)
    kb.dma("sp", wuq_st[:], D["w_uq"].rearrange("(c p) n -> p c n", p=128), "wuq_st", [], ["wuq_st"])
    for c in range(2):
        kb.ts("dve", wuq[:, c, :], wuq_st[:, c, :], qw[:, c:c + 1], None, mult, None, ["wuq_st", "vecs"], ["wuq"])
    sel65 = kb.sb("sel65", [65, 64], F32)
    kb.memset("pool", sel65[:], 0.0, ["sel65"])
    Sd.op("pool", lambda e: e.affine_select(out=sel65[:], in_=sel65[:], pattern=[[0, 64]], compare_op=ALU.not_equal,
                                            fill=1.0, base=-64, channel_multiplier=1), ["sel65"], ["sel65"])
    cql = [kb.sb(f"cql{i}", [128, 2, GS], BF16) for i in range(2)]
    qtm = kb.sb("qtm", [128, 8, 96], F32)
    qrt = [kb.sb(f"qrt{i}", [128, 16], F32) for i in range(4)]
    qrot = [kb.sb(f"qrot{i}", [128, 8, 96], BF16) for i in range(2)]
    qra = [kb.sb(f"qra{i}", [128, 8, 16], F32) for i in range(4)]
    qT = [kb.sb(f"qT{i}", [96, 8, GS], BF16) for i in range(2)]
    sqq = [kb.sb(f"sqq{i}", [96, GS], BF16) for i in range(2)]
    qmax = kb.sb("qmax", [128, 1], F32)
    tmq = kb.sb("tmq", [128, 1], F32)
    bias_g = [kb.sb(f"bias_g{i}", [128, 1], F32) for i in range(2)]
    NPT = 4
    pt = [kb.sb(f"pt{i}", [128, GS], BF16) for i in range(NPT)]
    osb = [kb.sb(f"osb{i}", [65, GS], F32) for i in range(2)]
    rden = [kb.sb(f"rden{i}", [64, GS], F32) for i in range(2)]
    ao = kb.sb("ao", [64, 8, GS], F32)
    sqa = [kb.sb(f"sqa{i}", [64, GS], F32) for i in range(2)]
    rsa = kb.sb("rsa", [64, GS], F32)
    aon = [kb.sb(f"aon{i}", [64, 8, GS], BF16) for i in range(2)]
    psc = [kb.ps(f"psc{i}", [128, 512], F32) for i in range(3)]
    po = [kb.ps(f"po{i}", [128, 512], F32) for i in range(2)]
    pq = [kb.ps(f"pq{i}", [128, 512], F32) for i in range(2)]
    pQT = kb.ps("pQT", [128, 8, 128], BF16)
    def att_prep(g):
        gp = g % 2
        gs0, gs1 = g * GS, (g + 1) * GS
        kb.dma("sp", cql[gp][:], S_cqn.rearrange("(c p) s -> p c s", p=128)[:, :, gs0:gs1], f"cql{gp}", [], [f"cql{gp}"])
        for t in range(TG):
            tt_ = g * TG + t
            rp = tt_ % 2
            for c in range(2):
                kb.mm(pq[0][:, 0:512], cql[gp][:, c, t * 128:(t + 1) * 128], wuq[:, c, 0:512], c == 0, c == 1,
                      [f"cql{gp}", "wuq"], ["pq0"])
            for c in range(2):
                kb.mm(pq[1][:, 0:256], cql[gp][:, c, t * 128:(t + 1) * 128], wuq[:, c, 512:768], c == 0, c == 1,
                      [f"cql{gp}", "wuq"], ["pq1"])
            qflat = qtm[:].rearrange("p h d -> p (h d)")
            kb.cp("dve", qflat[:, 0:512], pq[0][:, 0:512], ["pq0"], ["qtm"])
            kb.cp("dve", qflat[:, 512:768], pq[1][:, 0:256], ["pq1"], ["qtm"])
            cb_ = cosT[:, tt_:tt_ + 1, :].to_broadcast([128, 8, 16])
            sb_ = sinT[:, tt_:tt_ + 1, :].to_broadcast([128, 8, 16])
            q1, q2 = qtm[:, :, 64:80], qtm[:, :, 80:96]
            kb.cp("pool", qrot[rp][:, :, 0:64], qtm[:, :, 0:64], ["qtm"], [f"qrot{rp}"])
            kb.tt("pool", qra[0][:], q1, cb_, mult, ["qtm", "cosT"], ["qra0"])
            kb.tt("dve", qra[1][:], q2, sb_, mult, ["qtm", "sinT"], ["qra1"])
            kb.tt("pool", qrot[rp][:, :, 64:80], qra[0][:], qra[1][:], sub, ["qra0", "qra1"], [f"qrot{rp}"])
            kb.tt("pool", qra[2][:], q2, cb_, mult, ["qtm", "cosT"], ["qra2"])
            kb.tt("dve", qra[3][:], q1, sb_, mult, ["qtm", "sinT"], ["qra3"])
            kb.tt("dve", qrot[rp][:, :, 80:96], qra[2][:], qra[3][:], add, ["qra2", "qra3"], [f"qrot{rp}"])
            for h in range(8):
                kb.tr(pQT[0:96, h, :], qrot[rp][:, h, :], ident[:], [f"qrot{rp}", "ident"], ["pQT"])
            kb.cp("dve", qT[gp][:, :, t * 128:(t + 1) * 128], pQT[0:96, :, :], ["pQT"], [f"qT{gp}_{t}"])
        qTb = [f"qT{gp}_{t}" for t in range(TG)]
        kb.memset("pool", qmax[:], 0.0, ["qmax"])
        for h in range(8):
            hp = h % 2
            kb.act(sqq[hp][:], qT[gp][:, h, :], AF.Square, qTb, [f"sqq{hp}"])
            kb.mm(pq[hp][:, :GS], onesb[0:96, :], sqq[hp][:], True, True, ["onesb", f"sqq{hp}"], [f"pq{hp}"])
            Sd.op("dve", lambda e, hp=hp: e.tensor_reduce(out=tmq[:], in_=pq[hp][:, :GS], axis=AX.X, op=ALU.max),
                  [f"pq{hp}"], ["tmq"])
            kb.tt("dve", qmax[:], qmax[:], tmq[:], ALU.max, ["qmax", "tmq"], ["qmax"])
        bg = bias_g[gp]
        kb.tt("dve", bg[:], qmax[:], kmax[:], mult, ["qmax", "kmax"], [f"bias_g{gp}"])
        kb.ts("dve", bg[:], bg[:], 1e-30, None, add, None, [f"bias_g{gp}"], [f"bias_g{gp}"])
        kb.act(bg[:], bg[:], AF.Ln, [f"bias_g{gp}"], [f"bias_g{gp}"])
        kb.act(bg[:], bg[:], AF.Exp, [f"bias_g{gp}"], [f"bias_g{gp}"], scale=0.5)
        kb.ts("dve", bg[:], bg[:], -SCALE * 1.02, None, mult, None, [f"bias_g{gp}"], [f"bias_g{gp}"])

    def att_main(g):
        gp = g % 2
        gs0, gs1 = g * GS, (g + 1) * GS
        qTb = [f"qT{gp}_{t}" for t in range(TG)]
        bg = bias_g[gp]
        units = [(h, kbk) for h in range(8) for kbk in range(NT)]
        LOOK = 2
        NPS = 3

        def emit_qk(u):
            h, kbk = units[u]
            si = u % NPS
            kb.mm(psc[si][:, :GS], KT[0:96, h, kbk * 128:(kbk + 1) * 128], qT[gp][:, h, :], True, True,
                  ["KT"] + qTb, [f"psc{si}"])

        for u in range(min(LOOK, len(units))):
            emit_qk(u)
        for u, (h, kbk) in enumerate(units):
            hp = h % 2
            si = u % NPS
            pi = u % NPT
            if u + LOOK < len(units):
                emit_qk(u + LOOK)
            kb.act(pt[pi][:], psc[si][:, :GS], AF.Exp, [f"psc{si}", f"bias_g{gp}"], [f"pt{pi}"], scale=SCALE, bias=bg[:])
            kb.mm(po[hp][0:65, :GS], Vaug[:, kbk, h, :], pt[pi][:], kbk == 0, kbk == NT - 1, ["V", f"pt{pi}"], [f"po{hp}"])
            if kbk == NT - 1:
                kb.cp("dve", osb[hp][:], po[hp][0:65, :GS], [f"po{hp}"], [f"osb{hp}"])
                kb.mm(pq[hp][0:64, :GS], sel65[:], osb[hp][:], True, True, ["sel65", f"osb{hp}"], [f"pq{hp}"])
                Sd.op("dve", lambda e, hp=hp: e.reciprocal(out=rden[hp][:], in_=pq[hp][0:64, :GS]), [f"pq{hp}"], [f"rden{hp}"])
                kb.tt("pool", ao[:, h, :], osb[hp][0:64, :], rden[hp][:], mult, [f"osb{hp}", f"rden{hp}"], [f"ao{h}"])
        for h in range(8):
            hp = h % 2
            kb.act(sqa[hp][:], ao[:, h, :], AF.Square, [f"ao{h}"], [f"sqa{hp}"])
            kb.mm(pq[0][0:64, :GS], onesf[0:64, 0:64], sqa[hp][:], h == 0, h == 7, ["onesf", f"sqa{hp}"], ["pq0"])
        kb.act(rsa[:], pq[0][0:64, :GS], AF.Ln, ["pq0", "c_eps"], ["rsa"], scale=1.0 / 512, bias=c_eps[0:64, :])
        kb.act(rsa[:], rsa[:], AF.Exp, ["rsa"], ["rsa"], scale=-0.5)
        for h in range(8):
            kb.tt("pool" if h % 2 else "dve", aon[gp][:, h, :], ao[:, h, :], rsa[:], mult, [f"ao{h}", "rsa"], [f"aon{gp}_{h}"])
        kb.dma("sp", S_attn.rearrange("(h p) s -> p h s", p=64)[:, :, gs0:gs1], aon[gp][:], f"aon{gp}",
               [f"aon{gp}_{h}" for h in range(8)], [])

    att_prep(0)
    for g in range(NG):
        if g + 1 < NG:
            att_prep(g + 1)
        att_main(g)
    kb.barrier()
    kb.pop()
    kb.pop()
    if stop_after == "P4":
        return nc, kb

    TS = min(512, S)
    NSUB = TS // 128
    NTILES = (2 * S) // TS + NEXP
    NSLOT = NTILES * TS
    BIG = float(1 << 22)
    S_x1 = scr("S_x1", [S, 1024], F32)
    S_h2 = scr("S_h2", [S, 1024], BF16)
    S_slot = scr("S_slot", [NSLOT, 4], F32)
    S_ymoe = scr("S_ymoe", [2 * S, 1024], F32)
    S_tile = scr("S_tile", [2, 128, NTILES], I32)
    kb.push()
    ohall = kb.sb("ohall", [128, NT * 2, 32], BF16)
    posn = kb.sb("posn", [128, NT * 2], F32)
    wcomb = kb.sb("wcomb", [128, NT * 2], F32)
    run_bc = kb.sb("run_bc", [128, 32], F32)
    stri = kb.sb("stri", [128, 128], BF16)
    kb.memset("pool", run_bc[:], 0.0, ["run_bc"])
    kb.memset("pool", stri[:], 1.0, ["stri"])
    Sd.op("pool", lambda e: e.affine_select(out=stri[:], in_=stri[:], pattern=[[1, 128]], compare_op=ALU.is_gt, fill=0.0,
                                            base=0, channel_multiplier=-1), ["stri"], ["stri"])
    kb.push()
    aow = vecs64[:, 0:8]
    snw = vecs64[:, 8:16]
    wo = kb.sb("wo", [64, 16, 1024], BF16)
    wo_st = [kb.sb(f"wo_st{i}", [64, 1024], F32) for i in range(2)]
    for c in range(16):
        kb.dma("sp", wo_st[c % 2][:], D["w_o"][c * 64:(c + 1) * 64, :], f"wo_st{c % 2}", [], [f"wo_st{c % 2}"])
        sc_ = aow[:, c:c + 1] if c < 8 else snw[:, c - 8:c - 7]
        kb.ts("dve" if c % 2 else "pool", wo[:, c, :], wo_st[c % 2][:], sc_, None, mult, None, [f"wo_st{c % 2}", "vecs64"], [f"wo{c}"])
    wob = [f"wo{c}" for c in range(16)]
    fnw_bc = kb.sb("fnw_bc", [128, 1024], F32)
    kb.dma("sp", fnw_bc[:], D["ffn_norm_w"].partition_broadcast(128), "fnw_bc", [], ["fnw_bc"])
    wr = kb.sb("wr", [128, 8, 36], F32)
    kb.dma("sp", wr[:, :, 0:4], D["w_router_group"].rearrange("(k p) n -> p k n", p=128), "wr", [], ["wr"])
    kb.dma("sp", wr[:, :, 4:36], D["w_router_expert"].rearrange("(k p) n -> p k n", p=128), "wr", [], ["wr"])
    br_bc = kb.sb("br_bc", [128, 36], F32)
    kb.dma("sp", br_bc[:, 0:4], D["b_router_group"].partition_broadcast(128), "br_bc", [], ["br_bc"])
    kb.dma("sp", br_bc[:, 4:36], D["b_router_expert"].partition_broadcast(128), "br_bc", [], ["br_bc"])
    xl = [kb.sb(f"xl{i}", [128, 1024], F32) for i in range(2)]
    atl = [kb.sb(f"atl{i}", [64, 8, 128], BF16) for i in range(2)]
    ynl = [kb.sb(f"ynl{i}", [64, 8, 128], BF16) for i in range(2)]
    x1t = [kb.sb(f"x1t{i}", [128, 1024], F32) for i in range(2)]
    jk_2 = [kb.sb(f"jk_{i}", [128, 1024], BF16) for i in range(2)]
    st5 = [kb.sb(f"st5_{i}", [128, 1], F32) for i in range(2)]
    h2f_2 = [kb.sb(f"h2f_{i}", [128, 1024], F32) for i in range(2)]
    h2b = [kb.sb(f"h2b{i}", [128, 1024], BF16) for i in range(2)]
    h2T_2 = [kb.sb(f"h2T_{i}", [128, 8, 128], F32) for i in range(2)]
    rl_2 = [kb.sb(f"rl_{i}", [128, 36], F32) for i in range(2)]
    gmx_2 = [kb.sb(f"gmx_{i}", [128, 1], F32) for i in range(2)]
    ngm_2 = [kb.sb(f"ngm_{i}", [128, 1], F32) for i in range(2)]
    ohg_2 = [kb.sb(f"ohg_{i}", [128, 4], F32) for i in range(2)]
    eg_2 = [kb.sb(f"eg_{i}", [128, 4], F32) for i in range(2)]
    sume_2 = [kb.sb(f"sume_{i}", [128, 1], F32) for i in range(2)]
    gw_2 = [kb.sb(f"gw_{i}", [128, 1], F32) for i in range(2)]
    selt_2 = [kb.sb(f"selt_{i}", [128, 4, 8], F32) for i in range(2)]
    sel_2 = [kb.sb(f"sel_{i}", [128, 8], F32) for i in range(2)]
    m8_2 = [kb.sb(f"m8_{i}", [128, 8], F32) for i in range(2)]
    dd_2 = [kb.sb(f"dd_{i}", [128, 1], F32) for i in range(2)]
    w12_2 = [kb.sb(f"w12_{i}", [128, 2], F32) for i in range(2)]
    ohe_2 = [kb.sb(f"ohe_{i}", [128, 2, 8], F32) for i in range(2)]
    pmat_2 = [kb.sb(f"pmat_{i}", [128, 32], F32) for i in range(2)]
    ptmp_2 = [kb.sb(f"ptmp_{i}", [128, 32], F32) for i in range(2)]
    pmx = kb.ps("pmx", [128, 1024], F32)
    pH = kb.ps("pH", [128, 8, 128], F32)
    pL_2 = [kb.ps(f"pL_{i}", [128, 512], F32) for i in range(2)]
    pP = kb.ps("pP", [128, 512], F32)
    at_v = S_attn.rearrange("(h p) s -> p h s", p=64)
    def p5a_tile(tt_):
        yield
        b2 = tt_ % 2
        pL = pL_2[b2]
        yield
        jk = jk_2[b2]
        yield
        h2f = h2f_2[b2]
        yield
        h2T = h2T_2[b2]
        yield
        rl = rl_2[b2]
        yield
        gmx = gmx_2[b2]
        yield
        ngm = ngm_2[b2]
        yield
        ohg = ohg_2[b2]
        yield
        eg = eg_2[b2]
        yield
        sume = sume_2[b2]
        yield
        gw = gw_2[b2]
        yield
        selt = selt_2[b2]
        yield
        sel = sel_2[b2]
        yield
        m8 = m8_2[b2]
        yield
        dd = dd_2[b2]
        yield
        w12 = w12_2[b2]
        yield
        ohe = ohe_2[b2]
        yield
        pmat = pmat_2[b2]
        yield
        ptmp = ptmp_2[b2]
        yield
        ts0, ts1 = tt_ * 128, (tt_ + 1) * 128
        yield
        kb.dma("sp", xl[b2][:], D["x"][ts0:ts1, :], f"xl{b2}", [], [f"xl{b2}"])
        yield
        kb.dma("sp", atl[b2][:], at_v[:, :, ts0:ts1], f"atl{b2}", [], [f"atl{b2}"])
        yield
        kb.dma("sp", ynl[b2][:], yn_v[:, :, ts0:ts1], f"ynl{b2}", [], [f"ynl{b2}"])
        yield
        for half in range(2):
            for c in range(16):
                lhs = atl[b2][:, c, :] if c < 8 else ynl[b2][:, c - 8, :]
                kb.mm(pmx[:, half * 512:(half + 1) * 512], lhs, wo[:, c, half * 512:(half + 1) * 512], c == 0, c == 15,
                      [f"atl{b2}", f"ynl{b2}"] + wob, ["pmx"])
        yield
        kb.tt("dve", x1t[b2][:], pmx[:], xl[b2][:], add, ["pmx", f"xl{b2}"], [f"x1t{b2}"])
        yield
        kb.dma("sp", S_x1[ts0:ts1, :], x1t[b2][:], f"x1t{b2}", [f"x1t{b2}"], [])
        yield
        st = st5[b2]
        yield
        kb.act(jk[:], x1t[b2][:], AF.Square, [f"x1t{b2}"], [f"jk_{b2}", f"st5_{b2}"], accum_out=st[:])
        yield
        kb.ts("dve", st[:], st[:], 1.0 / 1024, EPS, mult, add, [f"st5_{b2}"], [f"st5_{b2}"])
        yield
        kb.act(st[:], st[:], AF.Ln, [f"st5_{b2}"], [f"st5_{b2}"])
        yield
        kb.act(st[:], st[:], AF.Exp, [f"st5_{b2}"], [f"st5_{b2}"], scale=-0.5)
        yield
        kb.stt(h2f[:], x1t[b2][:], st[:, 0:1], fnw_bc[:], mult, mult, [f"x1t{b2}", f"st5_{b2}", "fnw_bc"], [f"h2f_{b2}"])
        yield
        kb.cp("act", h2b[b2][:], h2f[:], [f"h2f_{b2}"], [f"h2b{b2}"])
        yield
        kb.dma("sp", S_h2[ts0:ts1, :], h2b[b2][:], f"h2b{b2}", [f"h2b{b2}"], [])
        yield
        for k in range(8):
            kb.tr(pH[:, k, :], h2f[:, k * 128:(k + 1) * 128], identf[:], [f"h2f_{b2}", "identf"], ["pH"])
        yield
        kb.cp("act", h2T[:], pH[:], ["pH"], [f"h2T_{b2}"])
        yield
        for k in range(8):
            kb.mm(pL[:, 0:36], h2T[:, k, :], wr[:, k, :], k == 0, k == 7, [f"h2T_{b2}", "wr"], [f"pL_{b2}"])
        yield
        kb.tt("dve", rl[:], pL[:, 0:36], br_bc[:], add, [f"pL_{b2}", "br_bc"], [f"rl_{b2}"])
        yield
        kb.reduce(gmx[:], rl[:, 0:4], ALU.max, [f"rl_{b2}"], [f"gmx_{b2}"])
        yield
        kb.ts("dve", ohg[:], rl[:, 0:4], gmx[:, 0:1], None, ALU.is_equal, None, [f"rl_{b2}", f"gmx_{b2}"], [f"ohg_{b2}"])
        yield
        kb.ts("dve", ngm[:], gmx[:], -1.0, None, mult, None, [f"gmx_{b2}"], [f"ngm_{b2}"])
        yield
        kb.act(eg[:], rl[:, 0:4], AF.Exp, [f"rl_{b2}", f"ngm_{b2}"], [f"eg_{b2}", f"sume_{b2}"], bias=ngm[:], accum_out=sume[:])
        yield
        kb.recip(gw[:], sume[:], [f"sume_{b2}"], [f"gw_{b2}"])
        yield
        kb.tt("dve", selt[:], rl[:, 4:36].rearrange("p (g e) -> p g e", g=4), ohg[:, :, None].to_broadcast([128, 4, 8]), mult,
              [f"rl_{b2}", f"ohg_{b2}"], [f"selt_{b2}"])
        yield
        kb.reduce(sel[:], selt[:].rearrange("p g e -> p e g"), ALU.add, [f"selt_{b2}"], [f"sel_{b2}"])
        yield
        kb.max8(m8[:], sel[:], [f"sel_{b2}"], [f"m8_{b2}"])
        yield
        kb.tt("dve", dd[:], m8[:, 1:2], m8[:, 0:1], sub, [f"m8_{b2}"], [f"dd_{b2}"])
        yield
        kb.act(dd[:], dd[:], AF.Exp, [f"dd_{b2}"], [f"dd_{b2}"])
        yield
        kb.ts("dve", w12[:, 0:1], dd[:], 1.0, None, add, None, [f"dd_{b2}"], [f"w12_{b2}"])
        yield
        kb.recip(w12[:, 0:1], w12[:, 0:1], [f"w12_{b2}"], [f"w12_{b2}"])
        yield
        kb.tt("dve", w12[:, 1:2], w12[:, 0:1], dd[:], mult, [f"w12_{b2}", f"dd_{b2}"], [f"w12_{b2}"])
        yield
        kb.ts("dve", wcomb[:, 2 * tt_:2 * tt_ + 2], w12[:], gw[:, 0:1], None, mult, None, [f"w12_{b2}", f"gw_{b2}"], ["wcomb"])
        yield
        for k in range(2):
            kb.ts("dve", ohe[:, k, :], sel[:], m8[:, k:k + 1], None, ALU.is_equal, None, [f"sel_{b2}", f"m8_{b2}"], [f"ohe_{b2}"])
        yield
        for k in range(2):
            u = 2 * tt_ + k
            kb.tt("dve", ohall[:, u, :].rearrange("p (g e) -> p g e", g=4), ohg[:, :, None].to_broadcast([128, 4, 8]),
                  ohe[:, k:k + 1, :].to_broadcast([128, 4, 8]), mult, [f"ohg_{b2}", f"ohe_{b2}"], [f"ohall{u}"])
            kb.mm(pP[:, 0:32], stri[:], ohall[:, u, :], True, True, ["stri", f"ohall{u}"], ["pP"])
            kb.mm(pP[:, 32:64], onesb[:], ohall[:, u, :], True, True, ["onesb", f"ohall{u}"], ["pP"])
            kb.tt("dve", pmat[:], pP[:, 0:32], run_bc[:], add, ["pP", "run_bc"], [f"pmat_{b2}"])
            kb.tt("dve", ptmp[:], pmat[:], ohall[:, u, :], mult, [f"pmat_{b2}", f"ohall{u}"], [f"ptmp_{b2}"])
            kb.reduce(posn[:, u:u + 1], ptmp[:], ALU.add, [f"ptmp_{b2}"], ["posn"])
            kb.tt("dve", run_bc[:], run_bc[:], pP[:, 32:64], add, ["run_bc", "pP"], ["run_bc"])

    for t0_ in range(NT):
        for _ in p5a_tile(t0_):
            pass
    kb.barrier()
    kb.pop()
    if stop_after == "P5a":
        return nc, kb

    import math as _m
    LOG_TS = int(_m.log2(TS))
    kb.push()
    cntf = kb.sb("cntf", [128, 32], F32)
    cnti = kb.sb("cnti", [128, 32], I32)
    ntf = kb.sb("ntf", [128, 32], F32)
    ones32 = kb.sb("ones32", [128, 32], F32)
    incl = kb.sb("incl", [128, 32], F32)
    base_bc = kb.sb("base_bc", [128, 32], F32)
    tmpb = kb.sb("tmpb", [128, NT * 2, 32], F32)
    slotf = kb.sb("slotf", [128, NT * 2], F32)
    sloti = kb.sb("sloti", [128, NT * 2], I32)
    rowdat = kb.sb("rowdat", [128, NT * 2, 4], F32)
    rowdat_i = rowdat[:].bitcast(I32)
    NDF = NSLOT // 128
    dflt = kb.sb("dflt", [128, NDF, 4], F32)
    dflt_i = dflt[:].bitcast(I32)
    jidx = kb.sb("jidx", [128, NTILES], F32)
    pidx = kb.sb("pidx", [128, 1], F32)
    cmpt = kb.sb("cmpt", [128, NTILES, 32], F32)
    ej = kb.sb("ej", [128, NTILES], F32)
    wgf = kb.sb("wgf", [128, NTILES], F32)
    wgi = kb.sb("wgi", [128, NTILES], I32)
    wdi = kb.sb("wdi", [128, NTILES], I32)
    kb.ts("dve", cntf[:], run_bc[:], float(TS - 1), None, add, None, ["run_bc"], ["cntf"])
    kb.cp("dve", cnti[:], cntf[:], ["cntf"], ["cnti"])
    kb.ts("dve", cnti[:], cnti[:], LOG_TS, None, ALU.arith_shift_right, None, ["cnti"], ["cnti"])
    kb.cp("dve", ntf[:], cnti[:], ["cnti"], ["ntf"])
    kb.memset("pool", ones32[:], 1.0, ["ones32"])
    Sd.op("dve", lambda e: e.tensor_tensor_scan(out=incl[:], data0=ones32[:], data1=ntf[:], initial=0.0, op0=mult, op1=add),
          ["ones32", "ntf"], ["incl"])
    kb.tt("dve", base_bc[:], incl[:], ntf[:], sub, ["incl", "ntf"], ["base_bc"])
    kb.ts("dve", base_bc[:], base_bc[:], float(TS), None, mult, None, ["base_bc"], ["base_bc"])
    kb.tt("pool", tmpb[:], ohall[:], base_bc[:, None, :].to_broadcast([128, NT * 2, 32]), mult,
          [f"ohall{u}" for u in range(NT * 2)] + ["base_bc"], ["tmpb"])
    Sd.op("dve", lambda e: e.tensor_reduce(out=slotf[:], in_=tmpb[:], axis=AX.X, op=ALU.add), ["tmpb"], ["slotf"])
    kb.tt("dve", slotf[:], slotf[:], posn[:], add, ["slotf", "posn"], ["slotf"])
    kb.cp("dve", sloti[:], slotf[:], ["slotf"], ["sloti"])
    kb.memset("pool", rowdat[:], 0.0, ["rowdat"])
    Sd.op("pool", lambda e: e.iota(rowdat_i[:, :, 0].rearrange("p (t k) -> p t k", k=2), pattern=[[128, NT], [0, 2]], base=0,
                                   channel_multiplier=1), ["rowdat"], ["rowdat"])
    Sd.op("pool", lambda e: e.iota(rowdat_i[:, :, 2].rearrange("p (t k) -> p t k", k=2), pattern=[[128, NT], [S, 2]], base=0,
                                   channel_multiplier=1), ["rowdat"], ["rowdat"])
    kb.cp("pool", rowdat[:, :, 1], wcomb[:], ["wcomb", "rowdat"], ["rowdat"])
    kb.memset("pool", dflt[:], 0.0, ["dflt"])
    kb.memset("pool", dflt_i[:, :, 0:1], 1 << 22, ["dflt"])
    kb.memset("pool", dflt_i[:, :, 2:3], 1 << 22, ["dflt"])
    kb.dma("sp", S_slot.rearrange("(n p) c -> p n c", p=128), dflt[:], "dflt", ["dflt"], ["S_slot_init"])
    for u in range(NT * 2):
        Sd.dma("pool", lambda e, u=u: e.indirect_dma_start(
            out=S_slot, out_offset=bass.IndirectOffsetOnAxis(ap=sloti[:, u:u + 1], axis=0), in_=rowdat[:, u, :], in_offset=None,
            bounds_check=BC(e, NSLOT - 1), oob_is_err=False), "slotsc", ["S_slot_init", "sloti", "rowdat"], [])
    Sd.op("pool", lambda e: e.iota(jidx[:], pattern=[[1, NTILES]], base=0, channel_multiplier=0,
                                   allow_small_or_imprecise_dtypes=True), [], ["jidx"])
    Sd.op("pool", lambda e: e.iota(pidx[:], pattern=[[0, 1]], base=0, channel_multiplier=1,
                                   allow_small_or_imprecise_dtypes=True), [], ["pidx"])
    kb.tt("dve", cmpt[:], incl[:, None, :].to_broadcast([128, NTILES, 32]), jidx[:, :, None].to_broadcast([128, NTILES, 32]),
          ALU.is_le, ["incl", "jidx"], ["cmpt"])
    Sd.op("dve", lambda e: e.tensor_reduce(out=ej[:], in_=cmpt[:], axis=AX.X, op=ALU.add), ["cmpt"], ["ej"])
    kb.ts("dve", wgf[:], ej[:], 128.0, pidx[:, 0:1], mult, add, ["ej", "pidx"], ["wgf"])
    kb.cp("dve", wgi[:], wgf[:], ["wgf"], ["wgi"])
    kb.barrier()
    if stop_after == "P5b":
        return nc, kb

    sl = [kb.sb(f"sl{i}", [128, NSUB * 4], F32) for i in range(2)]
    wg = [kb.sb(f"wg{i}", [128, 8, 256], BF16) for i in range(2)]
    wu = [kb.sb(f"wu{i}", [128, 8, 256], BF16) for i in range(2)]
    wd = [kb.sb(f"wd{i}", [128, 2, 1024], BF16) for i in range(2)]
    hg = [kb.sb(f"hg{i}", [128, 1024], BF16) for i in range(2 * NSUB)]
    hTt = [kb.sb(f"hTt{i}", [128, 8, TS], BF16) for i in range(2)]
    sg = [kb.sb(f"sg{i}", [128, TS], F32) for i in range(2)]
    heT = [kb.sb(f"heT{i}", [128, 2, TS], BF16) for i in range(2)]
    ysc = [kb.sb(f"ysc{i}", [128, 1024], F32) for i in range(2)]
    pHT = kb.ps("pHT", [128, 8, 128], BF16)
    pgu = [kb.ps(f"pgu{i}", [128, 512], F32) for i in range(4)]
    py = kb.ps("py", [128, 1024], F32)
    for i in range(2):
        kb.memset("pool", wg[i][:], 0.0, [f"wg{i}"])
        kb.memset("pool", wu[i][:], 0.0, [f"wu{i}"])
        kb.memset("pool", wd[i][:], 0.0, [f"wd{i}"])
        for q_ in range(NSUB):
            kb.memset("pool", hg[i * NSUB + q_][:], 0.0, [f"hg{i * NSUB + q_}"])
    def moe_loads(j):
        jp = j % 2
        kb.dma("sp", sl[jp][:].rearrange("p (n c) -> p n c", c=4), S_slot[j * TS:(j + 1) * TS, :].rearrange("(n p) c -> p n c", p=128),
               f"sl{jp}", [], [f"sl{jp}"])
        sl_i = sl[jp][:].bitcast(I32)
        for wt, wname, a_ in ((wg, "w_exp_gate", 8), (wu, "w_exp_up", 8), (wd, "w_exp_down", 2)):
            Sd.dma("pool", lambda e, j=j, jp=jp, wt=wt, wname=wname, a_=a_: e.indirect_dma_start(
                out=wt[jp][:].rearrange("p k c -> p (k c)"), out_offset=None,
                in_=D[wname].rearrange("(r a) c -> r (a c)", a=a_),
                in_offset=bass.IndirectOffsetOnAxis(ap=wgi[:, j:j + 1], axis=0),
                bounds_check=BC(e, 32 * 128 - 1), oob_is_err=False), f"{wname}{jp}", ["wgi"],
                [{"w_exp_gate": "wg", "w_exp_up": "wu", "w_exp_down": "wd"}[wname] + str(jp)])
        for sb_ in range(NSUB):
            hgt = hg[jp * NSUB + sb_]
            Sd.dma("pool", lambda e, sb_=sb_, hgt=hgt, sl_i=sl_i: e.indirect_dma_start(
                out=hgt[:], out_offset=None, in_=S_h2, in_offset=bass.IndirectOffsetOnAxis(ap=sl_i[:, sb_ * 4:sb_ * 4 + 1], axis=0),
                bounds_check=BC(e, S - 1), oob_is_err=False), f"hg{jp * NSUB + sb_}", [f"sl{jp}"], [f"hg{jp * NSUB + sb_}"])

    def moe_compute(j):
        jp = j % 2
        sl_i = sl[jp][:].bitcast(I32)
        for sb_ in range(NSUB):
            hi_ = jp * NSUB + sb_
            for k in range(8):
                kb.tr(pHT[:, k, :], hg[hi_][:].rearrange("p (j a) -> p a j", a=8)[:, k, :], ident[:], [f"hg{hi_}", "ident"], ["pHT"])
            kb.cp("act" if sb_ % 2 else "dve", hTt[jp][:, :, sb_ * 128:(sb_ + 1) * 128], pHT[:], ["pHT"], [f"hTt{jp}_{sb_}"])
        hb_ = [f"hTt{jp}_{sb_}" for sb_ in range(NSUB)]
        for m in range(2):
            pg_, pu_ = pgu[2 * m], pgu[2 * m + 1]
            for kc in range(8):
                kb.mm(pg_[:, :TS], wg[jp][:].rearrange("p k (j a) -> p k a j", a=2)[:, kc, m, :], hTt[jp][:, kc, :], kc == 0, kc == 7,
                      [f"wg{jp}"] + hb_, [f"pgu{2 * m}"])
            for kc in range(8):
                kb.mm(pu_[:, :TS], wu[jp][:].rearrange("p k (j a) -> p k a j", a=2)[:, kc, m, :], hTt[jp][:, kc, :], kc == 0, kc == 7,
                      [f"wu{jp}"] + hb_, [f"pgu{2 * m + 1}"])
            kb.act(sg[m][:], pg_[:, :TS], AF.Silu, [f"pgu{2 * m}"], [f"sg{m}"])
            kb.tt("dve", heT[jp][:, m, :], sg[m][:], pu_[:, :TS], mult, [f"sg{m}", f"pgu{2 * m + 1}"], [f"heT{jp}_{m}"])
        for sb_ in range(NSUB):
            s2 = sb_ % 2
            for half in range(2):
                for m in range(2):
                    kb.mm(py[:, half * 512:(half + 1) * 512], heT[jp][:, m, sb_ * 128:(sb_ + 1) * 128],
                          wd[jp][:, m, half * 512:(half + 1) * 512], m == 0, m == 1,
                          [f"heT{jp}_0", f"heT{jp}_1", f"wd{jp}"], ["py"])
            kb.act(ysc[s2][:], py[:], AF.Copy, ["py", f"sl{jp}"], [f"ysc{s2}"], scale=sl[jp][:, sb_ * 4 + 1:sb_ * 4 + 2])
            Sd.dma("pool", lambda e, sb_=sb_, s2=s2, sl_i=sl_i: e.indirect_dma_start(
                out=S_ymoe, out_offset=bass.IndirectOffsetOnAxis(ap=sl_i[:, sb_ * 4 + 2:sb_ * 4 + 3], axis=0), in_=ysc[s2][:], in_offset=None,
                bounds_check=BC(e, 2 * S - 1), oob_is_err=False), f"ysc{s2}", [f"ysc{s2}", f"sl{jp}"],
                ["S_ymoe"] if serial_scatter else [])

    moe_loads(0)
    for j in range(NTILES):
        if j + 1 < NTILES:
            moe_loads(j + 1)
        moe_compute(j)
    kb.barrier()
    kb.pop()
    kb.pop()
    if stop_after == "P5c":
        return nc, kb

    kb.push()
    pnw = vec_pk("pnw")
    wpg = kb.sb("wpg", [128, 8, 1024], BF16)
    wpg_st = [kb.sb(f"wpg_st{i}", [128, 1024], F32) for i in range(2)]
    for k in range(8):
        kb.dma("sp", wpg_st[k % 2][:], D["w_ple_gate"][k * 128:(k + 1) * 128, :], f"wpg_st{k % 2}", [], [f"wpg_st{k % 2}"])
        kb.ts("dve" if k % 2 else "pool", wpg[:, k, :], wpg_st[k % 2][:], pnw[:, k:k + 1], None, mult, None,
              [f"wpg_st{k % 2}", "vecs"], [f"wpg{k}"])
    wpgb = [f"wpg{k}" for k in range(8)]
    wpp = kb.sb("wpp", [128, 2, 1024], BF16)
    Sd.dma("pool", lambda e: e.dma_start(out=wpp[:], in_=D["w_ple_proj"].rearrange("(c p) n -> p c n", p=128)), "wpp", [], ["wpp"])
    bpg_bc = kb.sb("bpg_bc", [128, 1024], F32)
    kb.dma("sp", bpg_bc[:], D["b_ple_gate"].partition_broadcast(128), "bpg_bc", [], ["bpg_bc"])
    ppw_bc = kb.sb("ppw_bc", [128, 1024], F32)
    kb.dma("sp", ppw_bc[:], D["ple_post_norm_w"].partition_broadcast(128), "ppw_bc", [], ["ppw_bc"])
    fin_bc = kb.sb("fin_bc", [128, 1024], F32)
    kb.dma("sp", fin_bc[:], D["final_norm_w"].partition_broadcast(128), "fin_bc", [], ["fin_bc"])
    x1l = [kb.sb(f"x1l{i}", [128, 1024], F32) for i in range(2)]
    y0l = [kb.sb(f"y0l{i}", [128, 1024], F32) for i in range(2)]
    y1l = [kb.sb(f"y1l{i}", [128, 1024], F32) for i in range(2)]
    pl = [kb.sb(f"pl{i}", [128, 256], F32) for i in range(2)]
    plb_2 = [kb.sb(f"plb_{i}", [128, 256], BF16) for i in range(2)]
    pTs_2 = [kb.sb(f"pTs_{i}", [128, 2, 128], BF16) for i in range(2)]
    x2_2 = [kb.sb(f"x2_{i}", [128, 1024], F32) for i in range(2)]
    jk2_2 = [kb.sb(f"jk2_{i}", [128, 1024], BF16) for i in range(2)]
    s6_2 = [[kb.sb(f"s6_{i}_{q}", [128, 1], F32) for i in range(3)] for q in range(2)]
    n3b_2 = [kb.sb(f"n3b_{i}", [128, 1024], BF16) for i in range(2)]
    n3T_2 = [kb.sb(f"n3T_{i}", [128, 8, 128], BF16) for i in range(2)]
    gate_2 = [kb.sb(f"gate_{i}", [128, 1024], F32) for i in range(2)]
    ple_2 = [kb.sb(f"ple_{i}", [128, 1024], F32) for i in range(2)]
    x3_2 = [kb.sb(f"x3_{i}", [128, 1024], F32) for i in range(2)]
    ot = [kb.sb(f"ot{i}", [128, 1024], F32) for i in range(2)]
    pGt = kb.ps("pGt", [128, 1024], F32)
    pPp = kb.ps("pPp", [128, 1024], F32)
    pN3_2 = [kb.ps(f"pN3_{i}", [128, 8, 128], BF16) for i in range(2)]
    pPT_2 = [kb.ps(f"pPT_{i}", [128, 8, 128], BF16) for i in range(2)]

    def rstd_of(src_ap, src_bufs, st, stname, junk_ap, junkname):
        kb.act(junk_ap, src_ap, AF.Square, src_bufs, [junkname, stname], accum_out=st[:])
        kb.ts("dve", st[:], st[:], 1.0 / 1024, EPS, mult, add, [stname], [stname])
        kb.act(st[:], st[:], AF.Ln, [stname], [stname])
        kb.act(st[:], st[:], AF.Exp, [stname], [stname], scale=-0.5)

    def p5d_tile(tt_):
        yield
        b2 = tt_ % 2
        pN3 = pN3_2[b2]
        pPT = pPT_2[b2]
        yield
        plb = plb_2[b2]
        yield
        pTs = pTs_2[b2]
        yield
        x2 = x2_2[b2]
        yield
        jk2 = jk2_2[b2]
        yield
        n3b = n3b_2[b2]
        yield
        n3T = n3T_2[b2]
        yield
        gate = gate_2[b2]
        yield
        ple = ple_2[b2]
        yield
        x3 = x3_2[b2]
        yield
        s6 = s6_2[b2]
        yield
        ts0, ts1 = tt_ * 128, (tt_ + 1) * 128
        yield
        kb.dma("sp", x1l[b2][:], S_x1[ts0:ts1, :], f"x1l{b2}", [], [f"x1l{b2}"])
        yield
        kb.dma("sp", y0l[b2][:], S_ymoe[ts0:ts1, :], f"y0l{b2}", [], [f"y0l{b2}"])
        yield
        kb.dma("sp", y1l[b2][:], S_ymoe[S + ts0:S + ts1, :], f"y1l{b2}", [], [f"y1l{b2}"])
        yield
        kb.dma("sp", pl[b2][:], D["p"][ts0:ts1, :], f"pl{b2}", [], [f"pl{b2}"])
        yield
        kb.tt("dve", x2[:], x1l[b2][:], y0l[b2][:], add, [f"x1l{b2}", f"y0l{b2}"], [f"x2_{b2}"])
        yield
        kb.tt("dve", x2[:], x2[:], y1l[b2][:], add, [f"x2_{b2}", f"y1l{b2}"], [f"x2_{b2}"])
        yield
        rstd_of(x2[:], [f"x2_{b2}"], s6[0], f"s6_0_{b2}", jk2[:], f"jk2_{b2}")
        yield
        kb.ts("dve", n3b[:], x2[:], s6[0][:, 0:1], None, mult, None, [f"x2_{b2}", f"s6_0_{b2}"], [f"n3b_{b2}"])
        yield
        for k in range(8):
            kb.tr(pN3[:, k, :], n3b[:, k * 128:(k + 1) * 128], ident[:], [f"n3b_{b2}", "ident"], [f"pN3_{b2}"])
        yield
        kb.cp("act", n3T[:], pN3[:], [f"pN3_{b2}"], [f"n3T_{b2}"])
        yield
        for half in range(2):
            for k in range(8):
                kb.mm(pGt[:, half * 512:(half + 1) * 512], n3T[:, k, :], wpg[:, k, half * 512:(half + 1) * 512], k == 0, k == 7,
                      [f"n3T_{b2}"] + wpgb, ["pGt"])
        yield
        kb.tt("dve", gate[:], pGt[:], bpg_bc[:], add, ["pGt", "bpg_bc"], [f"gate_{b2}"])
        yield
        kb.act(gate[:], gate[:], AF.Sigmoid, [f"gate_{b2}"], [f"gate_{b2}"])
        yield
        kb.cp("pool", plb[:], pl[b2][:], [f"pl{b2}"], [f"plb_{b2}"])
        yield
        for c in range(2):
            kb.tr(pPT[:, c, :], plb[:, c * 128:(c + 1) * 128], ident[:], [f"plb_{b2}", "ident"], [f"pPT_{b2}"])
        yield
        kb.cp("dve", pTs[:], pPT[:, 0:2, :], [f"pPT_{b2}"], [f"pTs_{b2}"])
        yield
        for half in range(2):
            for c in range(2):
                kb.mm(pPp[:, half * 512:(half + 1) * 512], pTs[:, c, :], wpp[:, c, half * 512:(half + 1) * 512], c == 0, c == 1,
                      [f"pTs_{b2}", "wpp"], ["pPp"])
        yield
        rstd_of(pPp[:], ["pPp"], s6[1], f"s6_1_{b2}", jk2[:], f"jk2_{b2}")
        yield
        kb.stt(ple[:], pPp[:], s6[1][:, 0:1], ppw_bc[:], mult, mult, ["pPp", f"s6_1_{b2}", "ppw_bc"], [f"ple_{b2}"])
        yield
        kb.tt("dve", ple[:], ple[:], gate[:], mult, [f"ple_{b2}", f"gate_{b2}"], [f"ple_{b2}"])
        yield
        kb.tt("dve", x3[:], x2[:], ple[:], add, [f"x2_{b2}", f"ple_{b2}"], [f"x3_{b2}"])
        yield
        rstd_of(x3[:], [f"x3_{b2}"], s6[2], f"s6_2_{b2}", jk2[:], f"jk2_{b2}")
        yield
        kb.stt(ot[b2][:], x3[:], s6[2][:, 0:1], fin_bc[:], mult, mult, [f"x3_{b2}", f"s6_2_{b2}", "fin_bc"], [f"ot{b2}"])
        yield
        kb.dma("sp", OUT[ts0:ts1, :], ot[b2][:], f"ot{b2}", [f"ot{b2}"], [])

    for t0_ in range(NT):
        for _ in p5d_tile(t0_):
            pass
    kb.barrier()
    kb.pop()
    return nc, kb


_NC_CACHE = {}


def kernel(**inputs):
    x = np.asarray(inputs["x"], dtype=np.float32)
    B, S, _ = x.shape
    assert B == 8
    p = np.asarray(inputs["p"], dtype=np.float32)
    pos = np.asarray(inputs["positions"]).astype(np.int32)
    if S not in _NC_CACHE:
        nc, kb = build(S)
        kb.S.emit(nc, None)
        _NC_CACHE[S] = nc
    nc = _NC_CACHE[S]
    shared = {}
    for name, shape in WEIGHT_SPECS:
        a = np.asarray(inputs[name], dtype=np.float32)
        if name != "final_norm_w":
            a = a[0]
        shp = shape if len(shape) == 2 else [1, shape[0]]
        shared[name] = np.ascontiguousarray(a.reshape(shp))
    in_maps = []
    for b in range(B):
        m = dict(shared)
        m["x"] = np.ascontiguousarray(x[b])
        m["p"] = np.ascontiguousarray(p[0, b])
        m["pos"] = np.ascontiguousarray(pos[b].reshape(S // 128, 128))
        in_maps.append(m)
    res = run_bass_kernel_spmd(nc, in_maps, core_ids=list(range(B)))
    return np.stack([np.asarray(r["out"], dtype=np.float32) for r in res.results], axis=0)
```

```python
import numpy as np
import concourse.bass as bass
import concourse.mybir as mybir
from concourse.bass_utils import run_bass_kernel_spmd

F32 = mybir.dt.float32
BF16 = mybir.dt.bfloat16
I32 = mybir.dt.int32
AF = mybir.ActivationFunctionType
ALU = mybir.AluOpType
AX = mybir.AxisListType

ENGS = ("pe", "act", "dve", "pool", "sp")


class Buf:
    __slots__ = ("name", "w", "r")

    def __init__(self, name):
        self.name = name
        self.w = None
        self.r = []


class _Op:
    __slots__ = ("eng", "fn", "deps", "dma_key", "signal", "count", "kind")

    def __init__(self, eng, fn, deps, dma_key, kind):
        self.eng = eng
        self.fn = fn
        self.deps = deps
        self.dma_key = dma_key
        self.signal = False
        self.count = 0
        self.kind = kind


class Sched:
    def __init__(self):
        self.ops = []
        self.bufs = {}
        self.psum_names = set()

    def buf(self, name):
        b = self.bufs.get(name)
        if b is None:
            b = self.bufs[name] = Buf(name)
        return b

    def _norm(self, xs):
        out = []
        for x in xs:
            if x is None:
                continue
            out.append(self.buf(x) if isinstance(x, str) else x)
        return out

    def op(self, eng, fn, reads=(), writes=(), dma_key=None):
        reads = self._norm(reads)
        writes = self._norm(writes)
        deps = set()
        for b in reads:
            if b.w is not None:
                deps.add(b.w)
            if b.name in self.psum_names:
                deps.update(r for r in b.r if self.ops[r].eng != eng)
        for b in writes:
            if b.w is not None:
                deps.add(b.w)
            deps.update(b.r)
        if eng == "pe":
            deps = set(d for d in deps if self.ops[d].eng != "pe")
        i = len(self.ops)
        self.ops.append(_Op(eng, fn, deps, dma_key, "dma" if dma_key else "op"))
        for b in reads:
            b.r.append(i)
        for b in writes:
            b.w = i
            b.r = []
        return i

    def dma(self, eng, fn, key, reads=(), writes=()):
        return self.op(eng, fn, reads, writes, dma_key=eng + "_" + key)

    def barrier(self):
        self.ops.append(_Op(None, None, set(), None, "barrier"))

    def emit(self, nc, engines):
        ops = self.ops
        last = {e: None for e in ENGS}
        pend_dma = []
        bar_deps = {}
        for i, o in enumerate(ops):
            if o.kind == "barrier":
                d = set(v for v in last.values() if v is not None)
                d.update(pend_dma)
                bar_deps[i] = d
                pend_dma = []
            else:
                last[o.eng] = i
                if o.kind == "dma":
                    pend_dma.append(i)
        for i, o in enumerate(ops):
            for d in o.deps:
                if ops[d].kind == "op":
                    ops[d].signal = True
        for d in bar_deps.values():
            for j in d:
                if ops[j].kind == "op":
                    ops[j].signal = True
        cnt = {e: 0 for e in ENGS}
        dcnt = {}
        for o in ops:
            if o.kind == "op" and o.signal:
                cnt[o.eng] += 1
                o.count = cnt[o.eng]
            elif o.kind == "dma":
                dcnt[o.dma_key] = dcnt.get(o.dma_key, 0) + 16
                o.count = dcnt[o.dma_key]
        sem_names = ["e_" + e for e in ENGS] + ["d_" + k for k in dcnt]
        import contextlib
        with contextlib.ExitStack() as st:
            sems = {n: st.enter_context(nc.semaphore(n)) for n in sem_names}
            seen = {e: {} for e in ENGS}
            streams = {e: [] for e in ENGS}

            def need(e, d):
                p = ops[d]
                name = ("d_" + p.dma_key) if p.kind == "dma" else ("e_" + p.eng)
                if seen[e].get(name, 0) >= p.count:
                    return
                seen[e][name] = p.count
                streams[e].append(("wait", name, p.count))

            for i, o in enumerate(ops):
                if o.kind == "barrier":
                    best = {}
                    for d in bar_deps[i]:
                        p = ops[d]
                        name = ("d_" + p.dma_key) if p.kind == "dma" else ("e_" + p.eng)
                        if name not in best or ops[best[name]].count < p.count:
                            best[name] = d
                    for e in ENGS:
                        for d in sorted(best.values()):
                            need(e, d)
                    continue
                for d in sorted(o.deps):
                    need(o.eng, d)
                streams[o.eng].append(("op", o))
            self.streams = streams
            block = st.enter_context(nc.Block())

            def run(e, eng):
                for it in streams[e]:
                    if it[0] == "wait":
                        eng.wait_ge(sems[it[1]], it[2])
                    else:
                        o = it[1]
                        ins = o.fn(eng)
                        if o.kind == "dma":
                            ins.then_inc(sems["d_" + o.dma_key], 16)
                        elif o.signal:
                            ins.then_inc(sems["e_" + o.eng], 1)

            @block.tensor
            def _(eng):
                run("pe", eng)

            @block.scalar
            def _(eng):
                run("act", eng)

            @block.vector
            def _(eng):
                run("dve", eng)

            @block.gpsimd
            def _(eng):
                run("pool", eng)

            @block.sync
            def _(eng):
                run("sp", eng)
        return {e: len(streams[e]) for e in ENGS}


D_MODEL = 1024
PLE_DIM = 256
EPS = 1e-6
NH = 8
QR, KVR, ROPE, NOPE, VD = 256, 128, 32, 64, 64
SH, SP_, SG, SN, SCONV = 8, 64, 2, 64, 5
SINNER, SXBC = 512, 768
INCOLS = 1712
NEXP, NGRP, EPG, DEXP = 32, 4, 8, 256
C_CQ, C_CKV, C_KR, C_Z, C_XBC, C_DT = 0, 256, 384, 416, 928, 1696

WEIGHT_SPECS = [
    ("attn_norm_w", [1024]), ("w_in", [1024, 1712]), ("q_norm_w", [256]), ("w_uq", [256, 768]),
    ("kv_norm_w", [128]), ("w_ukv", [128, 1024]), ("attn_out_norm_w", [512]), ("conv_w", [5, 768]),
    ("conv_b", [768]), ("dt_bias", [1, 16]), ("a_log", [1, 16]), ("ssd_d", [1, 8]), ("ssd_norm_w", [512]),
    ("w_o", [1024, 1024]), ("ffn_norm_w", [1024]), ("w_router_group", [1024, 4]), ("b_router_group", [1, 4]),
    ("w_router_expert", [1024, 32]), ("b_router_expert", [1, 32]), ("w_exp_gate", [32 * 1024, 256]),
    ("w_exp_up", [32 * 1024, 256]), ("w_exp_down", [32 * 256, 1024]), ("ple_norm_w", [1024]),
    ("w_ple_gate", [1024, 1024]), ("b_ple_gate", [1, 1024]), ("w_ple_proj", [256, 1024]),
    ("ple_post_norm_w", [1024]), ("final_norm_w", [1024]),
]


class KB:
    def __init__(self, nc):
        import contextlib
        self.nc = nc
        self.S = Sched()
        self.root = contextlib.ExitStack()
        self.stack = [self.root]

    def push(self):
        import contextlib
        st = contextlib.ExitStack()
        self.stack.append(st)
        return st

    def pop(self):
        self.stack.pop().close()

    def sb(self, name, shape, dt):
        return self.stack[-1].enter_context(self.nc.sbuf_tensor(name, list(shape), dt))

    def ps(self, name, shape, dt=F32):
        self.S.psum_names.add(name)
        return self.stack[-1].enter_context(self.nc.psum_tensor(name, list(shape), dt))

    def act(self, out, in_, func, r, w, **kw):
        self.S.op("act", lambda e: e.activation(out=out, in_=in_, func=func, **kw), r, w)

    def ts(self, eng, out, in0, s1, s2, op0, op1, r, w):
        if op1 is None:
            self.S.op(eng, lambda e: e.tensor_scalar(out=out, in0=in0, scalar1=s1, scalar2=None, op0=op0), r, w)
        else:
            self.S.op(eng, lambda e: e.tensor_scalar(out=out, in0=in0, scalar1=s1, scalar2=s2, op0=op0, op1=op1), r, w)

    def tt(self, eng, out, in0, in1, op, r, w):
        self.S.op(eng, lambda e: e.tensor_tensor(out=out, in0=in0, in1=in1, op=op), r, w)

    def stt(self, out, in0, scalar, in1, op0, op1, r, w):
        self.S.op("dve", lambda e: e.scalar_tensor_tensor(out=out, in0=in0, scalar=scalar, in1=in1, op0=op0, op1=op1), r, w)

    def cp(self, eng, out, in_, r, w):
        if eng == "act":
            self.S.op("act", lambda e: e.activation(out=out, in_=in_, func=AF.Copy), r, w)
        else:
            self.S.op(eng, lambda e: e.tensor_copy(out=out, in_=in_), r, w)

    def memset(self, eng, ap, val, w):
        self.S.op(eng, lambda e: e.memset(ap, val), (), w)

    def mm(self, out, lhsT, rhs, start, stop, r, w, **kw):
        self.S.op("pe", lambda e: e.matmul(out, lhsT=lhsT, rhs=rhs, start=start, stop=stop, **kw), r, w)

    def tr(self, out, in_, ident, r, w):
        self.S.op("pe", lambda e: e.transpose(out=out, in_=in_, identity=ident), r, w)

    def dma(self, eng, out, in_, key, r, w, **kw):
        self.S.dma(eng, lambda e: e.dma_start(out=out, in_=in_, **kw), key, r, w)

    def barrier(self):
        self.S.barrier()

    def reduce(self, out, in_, op, r, w):
        self.S.op("dve", lambda e: e.tensor_reduce(out=out, in_=in_, axis=AX.X, op=op), r, w)

    def recip(self, out, in_, r, w):
        self.S.op("dve", lambda e: e.reciprocal(out=out, in_=in_), r, w)

    def max8(self, out, in_, r, w):
        self.S.op("dve", lambda e: e.max(out=out, in_=in_), r, w)


def build(S=4096, stop_after=None, serial_scatter=False):
    import math
    nc = bass.Bass("TRN2", target_bir_lowering=False)
    NT = S // 128
    GS = min(512, S)
    NG = S // GS
    TG = GS // 128
    D = {}
    D["x"] = nc.dram_tensor("x", [S, 1024], F32, kind="ExternalInput").ap()
    D["p"] = nc.dram_tensor("p", [S, 256], F32, kind="ExternalInput").ap()
    D["pos"] = nc.dram_tensor("pos", [NT, 128], I32, kind="ExternalInput").ap()
    for name, shape in WEIGHT_SPECS:
        shp = shape if len(shape) == 2 else [1, shape[0]]
        D[name] = nc.dram_tensor(name, shp, F32, kind="ExternalInput").ap()
    OUT = nc.dram_tensor("out", [S, 1024], F32, kind="ExternalOutput").ap()

    def scr(name, shape, dt):
        return nc.dram_tensor(name, shape, dt).ap()

    S_cqn = scr("S_cqn", [256, S], BF16)
    S_kvn = scr("S_kvn", [128, S], BF16)
    S_sz = scr("S_sz", [512, S], BF16)
    S_xbc = scr("S_xbc", [768, S], F32)
    S_xtm = scr("S_xtm", [S, 768], BF16)
    S_bcT = scr("S_bcT", [256, S], BF16)
    S_yf = scr("S_yf", [512, S], F32)
    S_yn = scr("S_yn", [512, S], BF16)

    import os
    KD = int(os.environ.get("KDBG", "0"))
    kb = KB(nc)
    Sd = kb.S
    mult, add, sub = ALU.mult, ALU.add, ALU.subtract
    _regs = {}

    def BC(e, val):
        if val not in _regs:
            _regs[val] = e.to_reg(val)
        return _regs[val]

    VROWS = [("anw", "attn_norm_w", 8), ("cb", "conv_b", 6), ("cw", "conv_w", 30), ("kvw", "kv_norm_w", 1),
             ("qw", "q_norm_w", 2), ("pnw", "ple_norm_w", 8)]
    voff = {}
    _o = 0
    for nm, _, k in VROWS:
        voff[nm] = (_o, k)
        _o += k
    NV = _o

    identf = kb.sb("identf", [128, 128], F32)
    ident = kb.sb("ident", [128, 128], BF16)
    onesf = kb.sb("onesf", [128, 128], F32)
    onesb = kb.sb("onesb", [128, 128], BF16)
    kb.memset("pool", identf[:], 0.0, ["identf"])
    Sd.op("pool", lambda e: e.affine_select(out=identf[:], in_=identf[:], pattern=[[-1, 128]],
                                            compare_op=ALU.not_equal, fill=1.0, base=0,
                                            channel_multiplier=1), ["identf"], ["identf"])
    kb.cp("dve", ident[:], identf[:], ["identf"], ["ident"])
    kb.memset("pool", onesf[:], 1.0, ["onesf"])
    c_eps = kb.sb("c_eps", [128, 1], F32)
    c_one = kb.sb("c_one", [128, 1], F32)
    kb.memset("pool", c_eps[:], EPS, ["c_eps"])
    kb.memset("pool", c_one[:], 1.0, ["c_one"])
    kb.memset("pool", onesb[:], 1.0, ["onesb"])

    vecs = kb.sb("vecs", [128, NV], F32)
    vecs64 = kb.sb("vecs64", [64, 16], F32)
    kb.push()
    vst = kb.sb("vst", [NV, 128], F32)
    vst64 = kb.sb("vst64", [16, 64], F32)
    pvec = kb.ps("pvec", [128, 512], F32)
    for nm, dn, k in VROWS:
        o_ = voff[nm][0]
        src = D[dn]
        src = src.rearrange("o (k p) -> (o k) p", p=128) if dn != "conv_w" else src.rearrange("t (j p) -> (t j) p", p=128)
        kb.dma("sp", vst[o_:o_ + k, :], src, "vst", [], ["vst"])
    kb.dma("sp", vst64[0:8, :], D["attn_out_norm_w"].rearrange("o (k p) -> (o k) p", p=64), "vst64", [], ["vst64"])
    kb.dma("sp", vst64[8:16, :], D["ssd_norm_w"].rearrange("o (k p) -> (o k) p", p=64), "vst64", [], ["vst64"])
    kb.tr(pvec[:, 0:NV], vst[:], identf[:NV, :NV], ["vst", "identf"], ["pvec"])
    kb.cp("dve", vecs[:], pvec[:, 0:NV], ["pvec"], ["vecs"])
    kb.tr(pvec[0:64, 64:80], vst64[:], identf[:16, :16], ["vst64", "identf", "vecs"], ["pvec"])
    kb.cp("dve", vecs64[:], pvec[0:64, 64:80], ["pvec"], ["vecs64"])
    kb.barrier()
    kb.pop()

    def vec_pk(name, dram=None, k=None):
        o_, k_ = voff[name]
        return vecs[:, o_:o_ + k_]

    cosT = kb.sb("cosT", [128, NT, 16], F32)
    sinT = kb.sb("sinT", [128, NT, 16], F32)
    krot = kb.sb("krot", [128, NT, 32], F32)
    dtp = kb.sb("dtp", [128, NT, 16], F32)
    dtb_bc = kb.sb("dtb_bc", [128, 16], F32)
    a_bc = kb.sb("a_bc", [128, 16], F32)
    kb.dma("sp", dtb_bc[:], D["dt_bias"].partition_broadcast(128), "dtb_bc", [], ["dtb_bc"])
    kb.dma("sp", a_bc[:], D["a_log"].partition_broadcast(128), "a_bc", [], ["a_bc"])
    kb.act(a_bc[:], a_bc[:], AF.Exp, ["a_bc"], ["a_bc"])
    kb.ts("dve", a_bc[:], a_bc[:], -1.0, None, mult, None, ["a_bc"], ["a_bc"])
    kb.push()
    posi = kb.sb("posi", [NT, 128], I32)
    posr = kb.sb("posr", [NT, 128], F32)
    posf = kb.sb("posf", [128, NT], F32)
    invf = kb.sb("invf", [128, 16], F32)
    rr = kb.sb("rr", [128, NT, 16], F32)
    rf = kb.sb("rf", [128, NT, 16], F32)
    ri = kb.sb("ri", [128, NT, 16], I32)
    rm = kb.sb("rm", [128, NT, 16], F32)
    ppos = kb.ps("ppos", [128, 512], F32)
    kb.dma("sp", posi[:], D["pos"], "posi", [], ["posi"])
    kb.cp("dve", posr[:], posi[:], ["posi"], ["posr"])
    kb.tr(ppos[:, :NT], posr[:], identf[:NT, :NT], ["posr", "identf"], ["ppos"])
    kb.cp("dve", posf[:], ppos[:, :NT], ["ppos"], ["posf"])
    for i in range(16):
        kb.memset("pool", invf[:, i:i + 1], (10000.0 ** (-(2.0 * i) / 32.0)) / (2 * math.pi), ["invf"])
    for t in range(NT):
        kb.ts("dve", rr[:, t, :], invf[:], posf[:, t:t + 1], None, mult, None, ["invf", "posf"], ["rr"])
    for shift, dst in ((0.0, "sinT"), (0.25, "cosT")):
        dstt = sinT if dst == "sinT" else cosT
        if shift:
            kb.ts("dve", rr[:], rr[:], shift, None, add, None, ["rr"], ["rr"])
        kb.cp("dve", ri[:], rr[:], ["rr"], ["ri"])
        kb.cp("dve", rf[:], ri[:], ["ri"], ["rf"])
        kb.tt("dve", rf[:], rr[:], rf[:], sub, ["rr", "rf"], ["rf"])
        kb.ts("dve", rm[:], rf[:], 0.5, None, ALU.is_gt, None, ["rf"], ["rm"])
        kb.tt("dve", rf[:], rf[:], rm[:], sub, ["rf", "rm"], ["rf"])
        kb.ts("dve", rm[:], rf[:], -0.5, None, ALU.is_lt, None, ["rf"], ["rm"])
        kb.tt("dve", rf[:], rf[:], rm[:], add, ["rf", "rm"], ["rf"])
        kb.act(dstt[:], rf[:], AF.Sin, ["rf"], [dst], scale=2 * math.pi)
    kb.barrier()
    kb.pop()

    if stop_after == "P0":
        return nc, kb
    kb.push()
    anw = vec_pk("anw")
    w_in_bf = kb.sb("w_in_bf", [128, 8, INCOLS], BF16)
    wst = [kb.sb(f"wst{i}", [128, INCOLS], F32) for i in range(2)]
    for k in range(8):
        kb.dma("sp", wst[k % 2][:], D["w_in"][k * 128:(k + 1) * 128, :], f"wst{k % 2}", [], [f"wst{k % 2}"])
        kb.ts("dve" if k % 2 == 0 else "pool", w_in_bf[:, k, :], wst[k % 2][:], anw[:, k:k + 1], None, mult, None,
              [f"wst{k % 2}", "vecs"], ["w_in_bf"])
    wsmall = kb.sb("wsmall", [128, 8, 48], BF16)
    kb.cp("dve", wsmall[:, :, 0:32], w_in_bf[:, :, C_KR:C_KR + 32], ["w_in_bf"], ["wsmall"])
    kb.cp("dve", wsmall[:, :, 32:48], w_in_bf[:, :, C_DT:C_DT + 16], ["w_in_bf"], ["wsmall"])

    NX = 3
    xt = [kb.sb(f"xt{i}", [128, 1024], F32) for i in range(NX)]
    hn = [kb.sb(f"hn{i}", [128, 1024], BF16) for i in range(NX)]
    ss = [kb.sb(f"ss{i}", [128, 1], F32) for i in range(NX)]
    junk = kb.sb("junk", [128, 1024], BF16)
    hT = [kb.sb(f"hT{i}", [128, 8, GS], BF16) for i in range(2)]
    sm = kb.sb("sm", [128, 48], F32)
    rot = [kb.sb(f"rot{i}", [128, 16], F32) for i in range(4)]
    dx = kb.sb("dx", [128, 16], F32)
    cqf = kb.sb("cqf", [128, 2, GS], F32)
    kvf = kb.sb("kvf", [128, GS], F32)
    sq = [kb.sb(f"sq{i}", [128, GS], F32) for i in range(3)]
    rs = [kb.sb(f"rs{i}", [128, GS], F32) for i in range(2)]
    cqn = [kb.sb(f"cqn{i}", [128, 2, GS], BF16) for i in range(2)]
    kvn = [kb.sb(f"kvn{i}", [128, GS], BF16) for i in range(2)]
    szs = [kb.sb(f"szs{i}", [128, 4, GS], BF16) for i in range(2)]
    xbs = [kb.sb(f"xbs{i}", [128, 6, GS], F32) for i in range(2)]
    pT = [kb.ps(f"pT{i}", [128, 8, 128], BF16) for i in range(2)]
    pS = kb.ps("pS", [128, 512], F32)
    pM = [kb.ps(f"pM{i}", [128, 512], F32) for i in range(3)]
    pST = [kb.ps(f"pST{i}", [128, 512], F32) for i in range(2)]

    chunks = [("cq", 0, C_CQ), ("cq", 1, C_CQ + 128), ("ckv", 0, C_CKV)]
    chunks += [("z", i, C_Z + 128 * i) for i in range(4)]
    chunks += [("xbc", i, C_XBC + 128 * i) for i in range(6)]
    ci_glob = 0
    for g in range(NG):
        gp = g % 2
        for t in range(TG):
            tt_ = g * TG + t
            s = tt_ % NX
            pp = tt_ % 2
            kb.dma("sp", xt[s][:], D["x"][tt_ * 128:(tt_ + 1) * 128, :], f"xt{s}", [], [f"xt{s}"])
            kb.act(junk[:], xt[s][:], AF.Square, [f"xt{s}"], ["junk", f"ss{s}"], accum_out=ss[s][:])
            kb.ts("dve", ss[s][:], ss[s][:], 1.0 / 1024, EPS, mult, add, [f"ss{s}"], [f"ss{s}"])
            kb.act(ss[s][:], ss[s][:], AF.Ln, [f"ss{s}"], [f"ss{s}"])
            kb.act(ss[s][:], ss[s][:], AF.Exp, [f"ss{s}"], [f"ss{s}"], scale=-0.5)
            kb.ts("dve", hn[s][:], xt[s][:], ss[s][:], None, mult, None, [f"xt{s}", f"ss{s}"], [f"hn{s}"])
            for k in range(8):
                kb.tr(pT[pp][:, k, :], hn[s][:, k * 128:(k + 1) * 128], ident[:], [f"hn{s}", "ident"], [f"pT{pp}"])
            kb.cp("act" if t % 2 else "dve", hT[gp][:, :, t * 128:(t + 1) * 128], pT[pp][:], [f"pT{pp}"], [f"hT{gp}_{t}"])
            if KD and KD < 2:
                continue
            for k in range(8):
                kb.mm(pS[:, :48], hT[gp][:, k, t * 128:(t + 1) * 128], wsmall[:, k, :], k == 0, k == 7,
                      [f"hT{gp}_{t}", "wsmall"], ["pS"])
            kb.cp("dve", sm[:], pS[:, :48], ["pS"], ["sm"])
            if KD and KD < 3:
                continue
            k1, k2 = sm[:, 0:16], sm[:, 16:32]
            cs_, sn_ = cosT[:, tt_, :], sinT[:, tt_, :]
            kb.tt("pool", rot[0][:], k1, cs_, mult, ["sm", "cosT"], ["rot0"])
            kb.tt("pool", rot[1][:], k2, sn_, mult, ["sm", "sinT"], ["rot1"])
            kb.tt("pool", krot[:, tt_, 0:16], rot[0][:], rot[1][:], sub, ["rot0", "rot1"], ["krot"])
            kb.tt("pool", rot[2][:], k2, cs_, mult, ["sm", "cosT"], ["rot2"])
            kb.tt("pool", rot[3][:], k1, sn_, mult, ["sm", "sinT"], ["rot3"])
            kb.tt("pool", krot[:, tt_, 16:32], rot[2][:], rot[3][:], add, ["rot2", "rot3"], ["krot"])
            kb.tt("dve", dx[:], sm[:, 32:48], dtb_bc[:], add, ["sm", "dtb_bc"], ["dx"])
            kb.act(dx[:], dx[:], AF.Exp, ["dx"], ["dx"])
            kb.act(dtp[:, tt_, :], dx[:], AF.Ln, ["dx", "c_one"], ["dtp"], bias=c_one[:])
        hT_bufs = [f"hT{gp}_{t}" for t in range(TG)]
        for (kind, i, c0) in (chunks if not KD else chunks[:max(0, KD - 3)]):
            pi = ci_glob % 3
            ci_glob += 1
            pm = pM[pi]
            for k in range(8):
                kb.mm(pm[:, :GS], w_in_bf[:, k, c0:c0 + 128], hT[gp][:, k, :], k == 0, k == 7,
                      ["w_in_bf"] + hT_bufs, [f"pM{pi}"])
            KD2 = int(os.environ.get("KDBG2", "9"))
            if kind == "cq":
                if KD2 >= 1 and os.environ.get("KNODVE") != "1":
                    kb.cp("dve", cqf[:, i, :], pm[:, :GS], [f"pM{pi}"], [f"cqf{i}"])
                if KD2 >= 2:
                    if os.environ.get("KSQ") == "dve":
                        kb.cp("dve", sq[i][:], pm[:, :GS], [f"pM{pi}"], [f"sq{i}"])
                    elif os.environ.get("KSQ") == "junk":
                        kb.act(junk[:, :GS], pm[:, :GS], AF.Square, [f"pM{pi}"], ["junk"])
                    else:
                        kb.act(sq[i][:], pm[:, :GS], AF.Square, [f"pM{pi}"], [f"sq{i}"])
                if KD2 >= 3:
                    kb.mm(pST[0][:, :GS], onesf[:], sq[i][:], i == 0, i == 1, ["onesf", f"sq{i}"], ["pST0"])
                if i == 1:
                    kb.act(rs[0][:], pST[0][:, :GS], AF.Ln, ["pST0", "c_eps"], ["rs0"], scale=1.0 / 256, bias=c_eps[:])
                    kb.act(rs[0][:], rs[0][:], AF.Exp, ["rs0"], ["rs0"], scale=-0.5)
                    for j in range(2):
                        kb.tt("dve", cqn[gp][:, j, :], cqf[:, j, :], rs[0][:], mult, [f"cqf{j}", "rs0"], [f"cqn{gp}_{j}"])
                    kb.dma("sp", S_cqn.rearrange("(c p) s -> p c s", p=128)[:, :, g * GS:(g + 1) * GS], cqn[gp][:],
                           f"cqn{gp}", [f"cqn{gp}_0", f"cqn{gp}_1"], [])
            elif kind == "ckv":
                kb.cp("dve", kvf[:], pm[:, :GS], [f"pM{pi}"], ["kvf"])
                kb.act(sq[2][:], pm[:, :GS], AF.Square, [f"pM{pi}"], ["sq2"])
                kb.mm(pST[1][:, :GS], onesf[:], sq[2][:], True, True, ["onesf", "sq2"], ["pST1"])
                kb.act(rs[1][:], pST[1][:, :GS], AF.Ln, ["pST1", "c_eps"], ["rs1"], scale=1.0 / 128, bias=c_eps[:])
                kb.act(rs[1][:], rs[1][:], AF.Exp, ["rs1"], ["rs1"], scale=-0.5)
                kb.tt("dve", kvn[gp][:], kvf[:], rs[1][:], mult, ["kvf", "rs1"], [f"kvn{gp}"])
                kb.dma("sp", S_kvn[:, g * GS:(g + 1) * GS], kvn[gp][:], f"kvn{gp}", [f"kvn{gp}"], [])
            elif kind == "z":
                kb.act(szs[gp][:, i, :], pm[:, :GS], AF.Silu, [f"pM{pi}"], [f"szs{gp}_{i}"])
                if i == 3:
                    kb.dma("sp", S_sz.rearrange("(c p) s -> p c s", p=128)[:, :, g * GS:(g + 1) * GS], szs[gp][:],
                           f"szs{gp}", [f"szs{gp}_{j}" for j in range(4)], [])
            else:
                kb.cp("dve" if i % 2 else "act", xbs[gp][:, i, :], pm[:, :GS], [f"pM{pi}"], [f"xbs{gp}_{i}"])
                if i == 5:
                    kb.dma("sp", S_xbc.rearrange("(c p) s -> p c s", p=128)[:, :, g * GS:(g + 1) * GS], xbs[gp][:],
                           f"xbs{gp}", [f"xbs{gp}_{j}" for j in range(6)], [])
    kb.barrier()
    kb.pop()
    if stop_after == "P1":
        return nc, kb

    kb.push()
    cw = vec_pk("cw").rearrange("p (t j) -> p t j", j=6)
    cb = vec_pk("cb")
    xin = [kb.sb(f"xin{i}", [128, 6, GS + 4], F32) for i in range(2)]
    acc = [kb.sb(f"acc{i}", [128, 6, GS], F32) for i in range(2)]
    xc = [kb.sb(f"xc{i}", [128, 6, GS], BF16) for i in range(2)]
    xts = [kb.sb(f"xts{i}", [128, 768], BF16) for i in range(2)]
    pX = [kb.ps(f"pX{i}", [128, 6, 128], BF16) for i in range(2)]
    xbc_v = S_xbc.rearrange("(c p) s -> p c s", p=128)
    for g in range(NG):
        gp = g % 2
        lo, hi = g * GS - 2, g * GS + GS + 2
        clo, chi = max(lo, 0), min(hi, S)
        if lo < 0:
            kb.memset("pool", xin[gp][:, :, 0:2], 0.0, [f"xin{gp}"])
        if hi > S:
            kb.memset("pool", xin[gp][:, :, GS + 2:GS + 4], 0.0, [f"xin{gp}"])
        kb.dma("sp", xin[gp][:, :, clo - lo:chi - lo], xbc_v[:, :, clo:chi], f"xin{gp}", [], [f"xin{gp}"])
        for j in range(6):
            kb.ts("dve", acc[gp][:, j, :], xin[gp][:, j, 0:GS], cw[:, 0, j:j + 1], cb[:, j:j + 1], mult, add,
                  [f"xin{gp}", "vecs"], [f"acc{gp}_{j}"])
            for k in range(1, 5):
                kb.stt(acc[gp][:, j, :], xin[gp][:, j, k:k + GS], cw[:, k, j:j + 1], acc[gp][:, j, :], mult, add,
                       [f"xin{gp}", "vecs", f"acc{gp}_{j}"], [f"acc{gp}_{j}"])
            kb.act(xc[gp][:, j, :], acc[gp][:, j, :], AF.Silu, [f"acc{gp}_{j}"], [f"xc{gp}_{j}"])
        xcb = [f"xc{gp}_{j}" for j in range(6)]
        kb.dma("sp", S_bcT.rearrange("(j p) s -> p j s", p=128)[:, :, g * GS:(g + 1) * GS], xc[gp][:, 4:6, :],
               f"xc{gp}", xcb, [])
        for t in range(TG):
            tt_ = g * TG + t
            pp = tt_ % 2
            for j in range(6):
                kb.tr(pX[pp][:, j, :], xc[gp][:, j, t * 128:(t + 1) * 128], ident[:], [f"xc{gp}_{j}", "ident"], [f"pX{pp}"])
            kb.cp("act" if t % 2 else "dve", xts[pp][:], pX[pp][:], [f"pX{pp}"], [f"xts{pp}"])
            kb.dma("sp", S_xtm[tt_ * 128:(tt_ + 1) * 128, :], xts[pp][:], f"xts{pp}", [f"xts{pp}"], [])
    kb.barrier()
    kb.pop()
    if stop_after == "P1b":
        return nc, kb

    kb.push()
    tri = [kb.sb("triU", [128, 128], F32), kb.sb("triL", [128, 128], F32)]
    for d_, (cm_, pat) in enumerate(((-1, 1), (1, -1))):
        kb.memset("pool", tri[d_][:], 1.0, [f"tri{d_}"])
        Sd.op("pool", lambda e, d_=d_, cm_=cm_, pat=pat: e.affine_select(
            out=tri[d_][:], in_=tri[d_][:], pattern=[[pat, 128]], compare_op=ALU.is_ge, fill=0.0, base=0,
            channel_multiplier=cm_), [f"tri{d_}"], [f"tri{d_}"])
    Esel = kb.sb("Esel", [8, 8, 128], F32)
    kb.memset("pool", Esel[:], 0.0, ["Esel"])
    Sd.op("pool", lambda e: e.affine_select(out=Esel[:], in_=Esel[:], pattern=[[-1, 8], [0, 128]],
                                            compare_op=ALU.not_equal, fill=1.0, base=0, channel_multiplier=1),
          ["Esel"], ["Esel"])
    d_bc = kb.sb("d_bc", [128, 8], F32)
    kb.dma("sp", d_bc[:], D["ssd_d"].partition_broadcast(128), "d_bc", [], ["d_bc"])
    diagD = kb.sb("diagD", [128, 8, 128], BF16)
    for r in range(8):
        kb.ts("dve", diagD[:, r, :], identf[:], d_bc[:, r:r + 1], None, mult, None, ["identf", "d_bc"], ["diagD"])
    xtm = [kb.sb(f"xtm{i}", [128, 768], BF16) for i in range(2)]
    bcT = [kb.sb(f"bcT{i}", [64, 4, 128], BF16) for i in range(2)]
    A_ = kb.sb("A_", [128, 8], F32)
    cs_sb = kb.sb("cs_sb", [128, 8], F32)
    csT_sb = kb.sb("csT_sb", [8, 128], F32)
    seg = kb.sb("seg", [128, 8, 128], F32)
    GTm = kb.sb("GTm", [128, 2, 128], F32)
    MT = kb.sb("MT", [128, 8, 128], BF16)
    ecs = kb.sb("ecs", [64, 8, 128], F32)
    Cdec = kb.sb("Cdec", [64, 8, 128], BF16)
    wex = kb.sb("wex", [128, 8], F32)
    xw = kb.sb("xw", [128, 8, 64], BF16)
    xdt = kb.sb("xdt", [128, 8, 64], BF16)
    dec = kb.sb("dec", [64, 8], F32)
    hst = kb.sb("hst", [64, 8, 64], F32)
    hbf = kb.sb("hbf", [64, 8, 64], BF16)
    ysb = [kb.sb(f"ysb{i}", [64, 8, 128], F32) for i in range(2)]
    yfl = [kb.sb(f"yfl{i}", [64, 8, 128], F32) for i in range(2)]
    szl = [kb.sb(f"szl{i}", [64, 8, 128], BF16) for i in range(2)]
    yg = kb.sb("yg", [64, 8, 128], F32)
    sqy = kb.sb("sqy", [64, 8, 128], F32)
    rsy = kb.sb("rsy", [64, 128], F32)
    ynb = [kb.sb(f"ynb{i}", [64, 8, 128], BF16) for i in range(2)]
    pA = kb.ps("pA", [128, 512], F32)
    pB = kb.ps("pB", [64, 8, 64], F32)
    pC = kb.ps("pC", [64, 512], F32)
    pCS = kb.ps("pCS", [128, 8, 128], F32)
    pG = kb.ps("pG", [128, 512], F32)
    pY = kb.ps("pY", [64, 8, 128], F32)
    yf_v = S_yf.rearrange("(r p) s -> p r s", p=64)
    yn_v = S_yn.rearrange("(r p) s -> p r s", p=64)
    sz_v = S_sz.rearrange("(r p) s -> p r s", p=64)
    bcT_v = S_bcT.rearrange("(q n) s -> n q s", n=64)
    for d_ in range(2):
        kb.memset("pool", hst[:], 0.0, ["hst"])
        kb.memset("pool", hbf[:], 0.0, ["hbf"])
        order = list(range(NT)) if d_ == 0 else list(range(NT - 1, -1, -1))
        for ci, c in enumerate(order):
            b2 = ci % 2
            cs0, cs1 = c * 128, (c + 1) * 128
            kb.dma("sp", xtm[b2][:], S_xtm[cs0:cs1, :], f"xtm{b2}", [], [f"xtm{b2}"])
            kb.dma("sp", bcT[b2][:], bcT_v[:, :, cs0:cs1], f"bcT{b2}", [], [f"bcT{b2}"])
            if d_ == 1:
                kb.dma("sp", yfl[b2][:], yf_v[:, :, cs0:cs1], f"yfl{b2}", [], [f"yfl{b2}"])
                kb.dma("sp", szl[b2][:], sz_v[:, :, cs0:cs1], f"szl{b2}", [], [f"szl{b2}"])
            dts = dtp[:, c, d_ * 8:(d_ + 1) * 8]
            kb.tt("dve", A_[:], dts, a_bc[:, d_ * 8:(d_ + 1) * 8], mult, ["dtp", "a_bc"], ["A_"])
            kb.mm(pA[:, 0:8], tri[d_][:], A_[:], True, True, [f"tri{d_}", "A_"], ["pA"])
            kb.mm(pA[:, 8:16], onesf[:], A_[:], True, True, ["onesf", "A_"], ["pA"])
            kb.cp("dve", cs_sb[:], pA[:, 0:8], ["pA"], ["cs_sb"])
            kb.tr(pA[0:8, 128:256], cs_sb[:], identf[:], ["cs_sb", "identf"], ["pA"])
            kb.cp("dve", csT_sb[:], pA[0:8, 128:256], ["pA"], ["csT_sb"])
            for r in range(8):
                kb.mm(pCS[:, r, :], Esel[:, r, :], csT_sb[:], True, True, ["Esel", "csT_sb"], ["pCS"])
            kb.tt("dve", seg[:], pCS[:], cs_sb[:, :, None].to_broadcast([128, 8, 128]), sub, ["pCS", "cs_sb"], ["seg"])
            kb.ts("dve", seg[:], seg[:], 0.0, None, ALU.min, None, ["seg"], ["seg"])
            kb.act(seg[:], seg[:], AF.Exp, ["seg"], ["seg"])
            kb.act(ecs[:], pCS[0:64, :, :], AF.Exp, ["pCS"], ["ecs"])
            for g_ in range(2):
                kb.mm(pG[:, g_ * 128:(g_ + 1) * 128], bcT[b2][:, g_, :], bcT[b2][:, 2 + g_, :], True, True,
                      [f"bcT{b2}"], ["pG"])
            kb.tt("dve", GTm[:], pG[:, 0:256].rearrange("p (g i) -> p g i", g=2), tri[d_][:, None, :].to_broadcast([128, 2, 128]),
                  mult, ["pG", f"tri{d_}"], ["GTm"])
            kb.tt("dve", xdt[:], xtm[b2][:, 0:512].rearrange("p (r q) -> p r q", r=8), dts[:, :, None].to_broadcast([128, 8, 64]),
                  mult, [f"xtm{b2}", "dtp"], ["xdt"])
            kb.tt("dve", MT[:].rearrange("p (g r) i -> p g r i", g=2), seg[:].rearrange("p (g r) i -> p g r i", g=2),
                  GTm[:, :, None, :].to_broadcast([128, 2, 4, 128]), mult, ["seg", "GTm"], ["MT"])
            kb.tt("dve", Cdec[:].rearrange("p (g r) i -> p g r i", g=2), ecs[:].rearrange("p (g r) i -> p g r i", g=2),
                  bcT[b2][:, 2:4, None, :].to_broadcast([64, 2, 4, 128]), mult, ["ecs", f"bcT{b2}"], ["Cdec"])
            for r in range(8):
                kb.mm(pY[:, r, :], xdt[:, r, :], MT[:, r, :], True, False, ["xdt", "MT"], ["pY"])
                if d_ == 0:
                    kb.mm(pY[:, r, :], xtm[b2][:, r * 64:(r + 1) * 64], diagD[:, r, :], False, False, [f"xtm{b2}", "diagD"], ["pY"])
                kb.mm(pY[:, r, :], hbf[:, r, :], Cdec[:, r, :], False, True, ["hbf", "Cdec"], ["pY"])
            kb.tt("dve", wex[:], pA[:, 8:16], cs_sb[:], sub, ["pA", "cs_sb"], ["wex"])
            kb.act(wex[:], wex[:], AF.Exp, ["wex"], ["wex"])
            kb.tt("dve", xw[:], xdt[:], wex[:, :, None].to_broadcast([128, 8, 64]), mult, ["xdt", "wex"], ["xw"])
            for g_ in range(2):
                kb.mm(pB[:, g_ * 4:(g_ + 1) * 4, :], xtm[b2][:, 512 + g_ * 64:512 + (g_ + 1) * 64],
                      xw[:, g_ * 4:(g_ + 1) * 4, :], True, True, [f"xtm{b2}", "xw"], ["pB"])
            kb.act(dec[:], pA[0:64, 8:16], AF.Exp, ["pA"], ["dec"])
            kb.tt("dve", hst[:], hst[:], dec[:, :, None].to_broadcast([64, 8, 64]), mult, ["hst", "dec"], ["hst"])
            kb.tt("dve", hst[:], hst[:], pB[:], add, ["hst", "pB"], ["hst"])
            kb.cp("act", hbf[:], hst[:], ["hst"], ["hbf"])
            if d_ == 0:
                kb.cp("act", ysb[b2][:], pY[:], ["pY"], [f"ysb{b2}"])
                kb.dma("sp", yf_v[:, :, cs0:cs1], ysb[b2][:], f"ysb{b2}", [f"ysb{b2}"], [])
            else:
                kb.tt("dve", yg[:], pY[:], yfl[b2][:], add, ["pY", f"yfl{b2}"], ["yg"])
                kb.tt("dve", yg[:], yg[:], szl[b2][:], mult, ["yg", f"szl{b2}"], ["yg"])
                kb.act(sqy[:], yg[:], AF.Square, ["yg"], ["sqy"])
                for r in range(8):
                    kb.mm(pC[:, 0:128], onesf[0:64, 0:64], sqy[:, r, :], r == 0, r == 7, ["onesf", "sqy"], ["pC"])
                kb.act(rsy[:], pC[:, 0:128], AF.Ln, ["pC", "c_eps"], ["rsy"], scale=1.0 / 512, bias=c_eps[0:64, :])
                kb.act(rsy[:], rsy[:], AF.Exp, ["rsy"], ["rsy"], scale=-0.5)
                kb.tt("dve", ynb[b2][:], yg[:], rsy[:, None, :].to_broadcast([64, 8, 128]), mult, ["yg", "rsy"], [f"ynb{b2}"])
                kb.dma("sp", yn_v[:, :, cs0:cs1], ynb[b2][:], f"ynb{b2}", [f"ynb{b2}"], [])
        kb.barrier()
    kb.pop()
    if stop_after == "P2":
        return nc, kb

    S_attn = scr("S_attn", [512, S], BF16)
    SCALE = 96.0 ** -0.5
    kb.push()
    KT = kb.sb("KT", [128, 8, S], BF16)
    Vaug = kb.sb("Vaug", [128, NT, 8, 65], BF16)
    kmax = kb.sb("kmax", [128, 1], F32)
    kb.push()
    kvw = vec_pk("kvw")
    wkv_st = kb.sb("wkv_st", [128, 1024], F32)
    wkv = kb.sb("wkv", [128, 8, 128], BF16)
    kb.dma("sp", wkv_st[:], D["w_ukv"], "wkv_st", [], ["wkv_st"])
    kb.ts("dve", wkv[:].rearrange("p h d -> p (h d)"), wkv_st[:], kvw[:, 0:1], None, mult, None, ["wkv_st", "vecs"], ["wkv"])
    kvl = [kb.sb(f"kvl{i}", [128, GS], BF16) for i in range(2)]
    kpad = [kb.sb(f"kpad{i}", [128, 96], BF16) for i in range(2)]
    krs = [kb.sb(f"krs{i}", [128, 128], BF16) for i in range(2)]
    sqk = [kb.sb(f"sqk{i}", [96, GS], BF16) for i in range(2)]
    tmx = kb.sb("tmx", [128, 1], F32)
    pK = [kb.ps(f"pK{i}", [128, 512], F32) for i in range(2)]
    pV = [kb.ps(f"pV{i}", [128, 512], F32) for i in range(2)]
    pR = [kb.ps(f"pR{i}", [128, 1024], BF16) for i in range(2)]
    pN = [kb.ps(f"pN{i}", [128, 512], F32) for i in range(2)]
    kb.memset("pool", Vaug[:, :, :, 64:65], 1.0, ["V"])
    kb.memset("pool", kmax[:], 0.0, ["kmax"])
    for i in range(2):
        kb.memset("pool", kpad[i][:], 0.0, [f"kpad{i}"])
    for g in range(NG):
        gp = g % 2
        gs0, gs1 = g * GS, (g + 1) * GS
        kb.dma("sp", kvl[gp][:], S_kvn[:, gs0:gs1], f"kvl{gp}", [], [f"kvl{gp}"])
        for t in range(TG):
            tt_ = g * TG + t
            pp = tt_ % 2
            kb.cp("pool", kpad[pp][:, 64:96], krot[:, tt_, :], ["krot"], [f"kpad{pp}"])
            kb.tr(pR[pp][0:96, 0:128], kpad[pp][:], ident[:], [f"kpad{pp}", "ident"], [f"pR{pp}"])
            kb.cp("act", krs[pp][64:96, :], pR[pp][64:96, 0:128], [f"pR{pp}"], [f"krs{pp}"])
            kb.cp("pool", KT[64:96, :, tt_ * 128:(tt_ + 1) * 128], krs[pp][64:96, None, :].to_broadcast([32, 8, 128]),
                  [f"krs{pp}"], [f"KTr{g}"])
            kb.mm(pV[pp][:], kvl[gp][:, t * 128:(t + 1) * 128], wkv[:, :, 64:128], True, True, [f"kvl{gp}", "wkv"], [f"pV{pp}"])
            kb.cp("dve", Vaug[:, tt_, :, 0:64], pV[pp][:].rearrange("p (h d) -> p h d", h=8), [f"pV{pp}"], ["V"])
        for h in range(8):
            hp = h % 2
            kb.mm(pK[hp][0:64, :GS], wkv[:, h, 0:64], kvl[gp][:], True, True, ["wkv", f"kvl{gp}"], [f"pK{hp}"])
            kb.cp("act" if h % 2 else "dve", KT[0:64, h, gs0:gs1], pK[hp][0:64, :GS], [f"pK{hp}"], [f"KTn{g}_{h}"])
            kb.act(sqk[hp][:], KT[0:96, h, gs0:gs1], AF.Square, [f"KTn{g}_{h}", f"KTr{g}"], [f"sqk{hp}"])
            kb.mm(pN[hp][:, :GS], onesb[0:96, :], sqk[hp][:], True, True, ["onesb", f"sqk{hp}"], [f"pN{hp}"])
            Sd.op("dve", lambda e, hp=hp: e.tensor_reduce(out=tmx[:], in_=pN[hp][:, :GS], axis=AX.X, op=ALU.max),
                  [f"pN{hp}"], ["tmx"])
            kb.tt("dve", kmax[:], kmax[:], tmx[:], ALU.max, ["kmax", "tmx"], ["kmax"])
    kb.barrier()
    kb.pop()
    if stop_after == "P3":
        return nc, kb

    kb.push()
    qw = vec_pk("qw")
    wuq_st = kb.sb("wuq_st", [128, 2, 768], F32)
    wuq = kb.sb("wuq", [128, 2, 768], BF16)
    kb.dma("sp", wuq_st[:], D["w_uq"].rearrange("(c p) n -> p c n", p=128), "wuq_st", [], ["wuq_st"])
    for c in range(2):
        kb.ts("dve", wuq[:, c, :], wuq_st[:, c, :], qw[:, c:c + 1], None, mult, None, ["wuq_st", "vecs"], ["wuq"])
    sel65 = kb.sb("sel65", [65, 64], F32)
    kb.memset("pool", sel65[:], 0.0, ["sel65"])
    Sd.op("pool", lambda e: e.affine_select(out=sel65[:], in_=sel65[:], pattern=[[0, 64]], compare_op=ALU.not_equal,
                                            fill=1.0, base=-64, channel_multiplier=1), ["sel65"], ["sel65"])
    cql = [kb.sb(f"cql{i}", [128, 2, GS], BF16) for i in range(2)]
    qtm = kb.sb("qtm", [128, 8, 96], F32)
    qrt = [kb.sb(f"qrt{i}", [128, 16], F32) for i in range(4)]
    qrot = [kb.sb(f"qrot{i}", [128, 8, 96], BF16) for i in range(2)]
    qra = [kb.sb(f"qra{i}", [128, 8, 16], F32) for i in range(4)]
    qT = [kb.sb(f"qT{i}", [96, 8, GS], BF16) for i in range(2)]
    sqq = [kb.sb(f"sqq{i}", [96, GS], BF16) for i in range(2)]
    qmax = kb.sb("qmax", [128, 1], F32)
    tmq = kb.sb("tmq", [128, 1], F32)
    bias_g = [kb.sb(f"bias_g{i}", [128, 1], F32) for i in range(2)]
    NPT = 4
    pt = [kb.sb(f"pt{i}", [128, GS], BF16) for i in range(NPT)]
    osb = [kb.sb(f"osb{i}", [65, GS], F32) for i in range(2)]
    rden = [kb.sb(f"rden{i}", [64, GS], F32) for i in range(2)]
    ao = kb.sb("ao", [64, 8, GS], F32)
    sqa = [kb.sb(f"sqa{i}", [64, GS], F32) for i in range(2)]
    rsa = kb.sb("rsa", [64, GS], F32)
    aon = [kb.sb(f"aon{i}", [64, 8, GS], BF16) for i in range(2)]
    psc = [kb.ps(f"psc{i}", [128, 512], F32) for i in range(3)]
    po = [kb.ps(f"po{i}", [128, 512], F32) for i in range(2)]
    pq = [kb.ps(f"pq{i}", [128, 512], F32) for i in range(2)]
    pQT = kb.ps("pQT", [128, 8, 128], BF16)
    def att_prep(g):
        gp = g % 2
        gs0, gs1 = g * GS, (g + 1) * GS
        kb.dma("sp", cql[gp][:], S_cqn.rearrange("(c p) s -> p c s", p=128)[:, :, gs0:gs1], f"cql{gp}", [], [f"cql{gp}"])
        for t in range(TG):
            tt_ = g * TG + t
            rp = tt_ % 2
            for c in range(2):
                kb.mm(pq[0][:, 0:512], cql[gp][:, c, t * 128:(t + 1) * 128], wuq[:, c, 0:512], c == 0, c == 1,
                      [f"cql{gp}", "wuq"], ["pq0"])
            for c in range(2):
                kb.mm(pq[1][:, 0:256], cql[gp][:, c, t * 128:(t + 1) * 128], wuq[:, c, 512:768], c == 0, c == 1,
                      [f"cql{gp}", "wuq"], ["pq1"])
            qflat = qtm[:].rearrange("p h d -> p (h d)")
            kb.cp("dve", qflat[:, 0:512], pq[0][:, 0:512], ["pq0"], ["qtm"])
            kb.cp("dve", qflat[:, 512:768], pq[1][:, 0:256], ["pq1"], ["qtm"])
            cb_ = cosT[:, tt_:tt_ + 1, :].to_broadcast([128, 8, 16])
            sb_ = sinT[:, tt_:tt_ + 1, :].to_broadcast([128, 8, 16])
            q1, q2 = qtm[:, :, 64:80], qtm[:, :, 80:96]
            kb.cp("pool", qrot[rp][:, :, 0:64], qtm[:, :, 0:64], ["qtm"], [f"qrot{rp}"])
            kb.tt("pool", qra[0][:], q1, cb_, mult, ["qtm", "cosT"], ["qra0"])
            kb.tt("dve", qra[1][:], q2, sb_, mult, ["qtm", "sinT"], ["qra1"])
            kb.tt("pool", qrot[rp][:, :, 64:80], qra[0][:], qra[1][:], sub, ["qra0", "qra1"], [f"qrot{rp}"])
            kb.tt("pool", qra[2][:], q2, cb_, mult, ["qtm", "cosT"], ["qra2"])
            kb.tt("dve", qra[3][:], q1, sb_, mult, ["qtm", "sinT"], ["qra3"])
            kb.tt("dve", qrot[rp][:, :, 80:96], qra[2][:], qra[3][:], add, ["qra2", "qra3"], [f"qrot{rp}"])
            for h in range(8):
                kb.tr(pQT[0:96, h, :], qrot[rp][:, h, :], ident[:], [f"qrot{rp}", "ident"], ["pQT"])
            kb.cp("dve", qT[gp][:, :, t * 128:(t + 1) * 128], pQT[0:96, :, :], ["pQT"], [f"qT{gp}_{t}"])
        qTb = [f"qT{gp}_{t}" for t in range(TG)]
        kb.memset("pool", qmax[:], 0.0, ["qmax"])
        for h in range(8):
            hp = h % 2
            kb.act(sqq[hp][:], qT[gp][:, h, :], AF.Square, qTb, [f"sqq{hp}"])
            kb.mm(pq[hp][:, :GS], onesb[0:96, :], sqq[hp][:], True, True, ["onesb", f"sqq{hp}"], [f"pq{hp}"])
            Sd.op("dve", lambda e, hp=hp: e.tensor_reduce(out=tmq[:], in_=pq[hp][:, :GS], axis=AX.X, op=ALU.max),
                  [f"pq{hp}"], ["tmq"])
            kb.tt("dve", qmax[:], qmax[:], tmq[:], ALU.max, ["qmax", "tmq"], ["qmax"])
        bg = bias_g[gp]
        kb.tt("dve", bg[:], qmax[:], kmax[:], mult, ["qmax", "kmax"], [f"bias_g{gp}"])
        kb.ts("dve", bg[:], bg[:], 1e-30, None, add, None, [f"bias_g{gp}"], [f"bias_g{gp}"])
        kb.act(bg[:], bg[:], AF.Ln, [f"bias_g{gp}"], [f"bias_g{gp}"])
        kb.act(bg[:], bg[:], AF.Exp, [f"bias_g{gp}"], [f"bias_g{gp}"], scale=0.5)
        kb.ts("dve", bg[:], bg[:], -SCALE * 1.02, None, mult, None, [f"bias_g{gp}"], [f"bias_g{gp}"])

    def att_tail(g):
        gp = g % 2
        gs0, gs1 = g * GS, (g + 1) * GS
        for h in range(8):
            hp = h % 2
            kb.act(sqa[hp][:], ao[:, h, :], AF.Square, [f"ao{h}"], [f"sqa{hp}"])
            kb.mm(pq[0][0:64, :GS], onesf[0:64, 0:64], sqa[hp][:], h == 0, h == 7, ["onesf", f"sqa{hp}"], ["pq0"])
        kb.act(rsa[:], pq[0][0:64, :GS], AF.Ln, ["pq0", "c_eps"], ["rsa"], scale=1.0 / 512, bias=c_eps[0:64, :])
        kb.act(rsa[:], rsa[:], AF.Exp, ["rsa"], ["rsa"], scale=-0.5)
        for h in range(8):
            kb.tt("pool" if h % 2 else "dve", aon[gp][:, h, :], ao[:, h, :], rsa[:], mult, [f"ao{h}", "rsa"], [f"aon{gp}_{h}"])
        kb.dma("sp", S_attn.rearrange("(h p) s -> p h s", p=64)[:, :, gs0:gs1], aon[gp][:], f"aon{gp}",
               [f"aon{gp}_{h}" for h in range(8)], [])


    def att_main(g):
        gp = g % 2
        gs0, gs1 = g * GS, (g + 1) * GS
        qTb = [f"qT{gp}_{t}" for t in range(TG)]
        bg = bias_g[gp]
        units = [(h, kbk) for h in range(8) for kbk in range(NT)]
        LOOK = 2
        NPS = 3

        def emit_qk(u):
            h, kbk = units[u]
            si = u % NPS
            kb.mm(psc[si][:, :GS], KT[0:96, h, kbk * 128:(kbk + 1) * 128], qT[gp][:, h, :], True, True,
                  ["KT"] + qTb, [f"psc{si}"])

        for u in range(min(LOOK, len(units))):
            emit_qk(u)
        for u, (h, kbk) in enumerate(units):
            hp = h % 2
            if u == NT // 2 and g > 0:
                att_tail(g - 1)
            si = u % NPS
            pi = u % NPT
            if u + LOOK < len(units):
                emit_qk(u + LOOK)
            kb.act(pt[pi][:], psc[si][:, :GS], AF.Exp, [f"psc{si}", f"bias_g{gp}"], [f"pt{pi}"], scale=SCALE, bias=bg[:])
            kb.mm(po[hp][0:65, :GS], Vaug[:, kbk, h, :], pt[pi][:], kbk == 0, kbk == NT - 1, ["V", f"pt{pi}"], [f"po{hp}"])
            if kbk == NT - 1:
                kb.cp("dve", osb[hp][:], po[hp][0:65, :GS], [f"po{hp}"], [f"osb{hp}"])
                kb.mm(pq[hp][0:64, :GS], sel65[:], osb[hp][:], True, True, ["sel65", f"osb{hp}"], [f"pq{hp}"])
                Sd.op("dve", lambda e, hp=hp: e.reciprocal(out=rden[hp][:], in_=pq[hp][0:64, :GS]), [f"pq{hp}"], [f"rden{hp}"])
                kb.tt("pool", ao[:, h, :], osb[hp][0:64, :], rden[hp][:], mult, [f"osb{hp}", f"rden{hp}"], [f"ao{h}"])
    att_prep(0)
    for g in range(NG):
        if g + 1 < NG:
            att_prep(g + 1)
        att_main(g)
    att_tail(NG - 1)
    kb.barrier()
    kb.pop()
    kb.pop()
    if stop_after == "P4":
        return nc, kb

    TS = min(512, S)
    NSUB = TS // 128
    NTILES = (2 * S) // TS + NEXP
    NSLOT = NTILES * TS
    BIG = float(1 << 22)
    S_x1 = scr("S_x1", [S, 1024], F32)
    S_h2 = scr("S_h2", [S, 1024], BF16)
    S_slot = scr("S_slot", [NSLOT, 4], F32)
    S_ymoe = scr("S_ymoe", [2 * S, 1024], F32)
    S_tile = scr("S_tile", [2, 128, NTILES], I32)
    kb.push()
    ohall = kb.sb("ohall", [128, NT * 2, 32], BF16)
    posn = kb.sb("posn", [128, NT * 2], F32)
    wcomb = kb.sb("wcomb", [128, NT * 2], F32)
    run_bc = kb.sb("run_bc", [128, 32], F32)
    stri = kb.sb("stri", [128, 128], BF16)
    kb.memset("pool", run_bc[:], 0.0, ["run_bc"])
    kb.memset("pool", stri[:], 1.0, ["stri"])
    Sd.op("pool", lambda e: e.affine_select(out=stri[:], in_=stri[:], pattern=[[1, 128]], compare_op=ALU.is_gt, fill=0.0,
                                            base=0, channel_multiplier=-1), ["stri"], ["stri"])
    kb.push()
    aow = vecs64[:, 0:8]
    snw = vecs64[:, 8:16]
    wo = kb.sb("wo", [64, 16, 1024], BF16)
    wo_st = [kb.sb(f"wo_st{i}", [64, 1024], F32) for i in range(2)]
    for c in range(16):
        kb.dma("sp", wo_st[c % 2][:], D["w_o"][c * 64:(c + 1) * 64, :], f"wo_st{c % 2}", [], [f"wo_st{c % 2}"])
        sc_ = aow[:, c:c + 1] if c < 8 else snw[:, c - 8:c - 7]
        kb.ts("dve" if c % 2 else "pool", wo[:, c, :], wo_st[c % 2][:], sc_, None, mult, None, [f"wo_st{c % 2}", "vecs64"], [f"wo{c}"])
    wob = [f"wo{c}" for c in range(16)]
    fnw_bc = kb.sb("fnw_bc", [128, 1024], F32)
    kb.dma("sp", fnw_bc[:], D["ffn_norm_w"].partition_broadcast(128), "fnw_bc", [], ["fnw_bc"])
    wr = kb.sb("wr", [128, 8, 36], F32)
    kb.dma("sp", wr[:, :, 0:4], D["w_router_group"].rearrange("(k p) n -> p k n", p=128), "wr", [], ["wr"])
    kb.dma("sp", wr[:, :, 4:36], D["w_router_expert"].rearrange("(k p) n -> p k n", p=128), "wr", [], ["wr"])
    br_bc = kb.sb("br_bc", [128, 36], F32)
    kb.dma("sp", br_bc[:, 0:4], D["b_router_group"].partition_broadcast(128), "br_bc", [], ["br_bc"])
    kb.dma("sp", br_bc[:, 4:36], D["b_router_expert"].partition_broadcast(128), "br_bc", [], ["br_bc"])
    xl = [kb.sb(f"xl{i}", [128, 1024], F32) for i in range(2)]
    atl = [kb.sb(f"atl{i}", [64, 8, 128], BF16) for i in range(2)]
    ynl = [kb.sb(f"ynl{i}", [64, 8, 128], BF16) for i in range(2)]
    x1t = [kb.sb(f"x1t{i}", [128, 1024], F32) for i in range(2)]
    jk_2 = [kb.sb(f"jk_{i}", [128, 1024], BF16) for i in range(2)]
    st5 = [kb.sb(f"st5_{i}", [128, 1], F32) for i in range(2)]
    h2f_2 = [kb.sb(f"h2f_{i}", [128, 1024], F32) for i in range(2)]
    h2b = [kb.sb(f"h2b{i}", [128, 1024], BF16) for i in range(2)]
    h2T_2 = [kb.sb(f"h2T_{i}", [128, 8, 128], F32) for i in range(2)]
    rl_2 = [kb.sb(f"rl_{i}", [128, 36], F32) for i in range(2)]
    gmx_2 = [kb.sb(f"gmx_{i}", [128, 1], F32) for i in range(2)]
    ngm_2 = [kb.sb(f"ngm_{i}", [128, 1], F32) for i in range(2)]
    ohg_2 = [kb.sb(f"ohg_{i}", [128, 4], F32) for i in range(2)]
    eg_2 = [kb.sb(f"eg_{i}", [128, 4], F32) for i in range(2)]
    sume_2 = [kb.sb(f"sume_{i}", [128, 1], F32) for i in range(2)]
    gw_2 = [kb.sb(f"gw_{i}", [128, 1], F32) for i in range(2)]
    selt_2 = [kb.sb(f"selt_{i}", [128, 4, 8], F32) for i in range(2)]
    sel_2 = [kb.sb(f"sel_{i}", [128, 8], F32) for i in range(2)]
    m8_2 = [kb.sb(f"m8_{i}", [128, 8], F32) for i in range(2)]
    dd_2 = [kb.sb(f"dd_{i}", [128, 1], F32) for i in range(2)]
    w12_2 = [kb.sb(f"w12_{i}", [128, 2], F32) for i in range(2)]
    ohe_2 = [kb.sb(f"ohe_{i}", [128, 2, 8], F32) for i in range(2)]
    pmat_2 = [kb.sb(f"pmat_{i}", [128, 32], F32) for i in range(2)]
    ptmp_2 = [kb.sb(f"ptmp_{i}", [128, 32], F32) for i in range(2)]
    pmx = kb.ps("pmx", [128, 1024], F32)
    pH = kb.ps("pH", [128, 8, 128], F32)
    pL_2 = [kb.ps(f"pL_{i}", [128, 512], F32) for i in range(2)]
    pP = kb.ps("pP", [128, 512], F32)
    at_v = S_attn.rearrange("(h p) s -> p h s", p=64)
    def p5a_tile(tt_):
        yield
        b2 = tt_ % 2
        pL = pL_2[b2]
        yield
        jk = jk_2[b2]
        yield
        h2f = h2f_2[b2]
        yield
        h2T = h2T_2[b2]
        yield
        rl = rl_2[b2]
        yield
        gmx = gmx_2[b2]
        yield
        ngm = ngm_2[b2]
        yield
        ohg = ohg_2[b2]
        yield
        eg = eg_2[b2]
        yield
        sume = sume_2[b2]
        yield
        gw = gw_2[b2]
        yield
        selt = selt_2[b2]
        yield
        sel = sel_2[b2]
        yield
        m8 = m8_2[b2]
        yield
        dd = dd_2[b2]
        yield
        w12 = w12_2[b2]
        yield
        ohe = ohe_2[b2]
        yield
        pmat = pmat_2[b2]
        yield
        ptmp = ptmp_2[b2]
        yield
        ts0, ts1 = tt_ * 128, (tt_ + 1) * 128
        yield
        kb.dma("sp", xl[b2][:], D["x"][ts0:ts1, :], f"xl{b2}", [], [f"xl{b2}"])
        yield
        kb.dma("sp", atl[b2][:], at_v[:, :, ts0:ts1], f"atl{b2}", [], [f"atl{b2}"])
        yield
        kb.dma("sp", ynl[b2][:], yn_v[:, :, ts0:ts1], f"ynl{b2}", [], [f"ynl{b2}"])
        yield
        for half in range(2):
            for c in range(16):
                lhs = atl[b2][:, c, :] if c < 8 else ynl[b2][:, c - 8, :]
                kb.mm(pmx[:, half * 512:(half + 1) * 512], lhs, wo[:, c, half * 512:(half + 1) * 512], c == 0, c == 15,
                      [f"atl{b2}", f"ynl{b2}"] + wob, ["pmx"])
        yield
        kb.tt("dve", x1t[b2][:], pmx[:], xl[b2][:], add, ["pmx", f"xl{b2}"], [f"x1t{b2}"])
        yield
        kb.dma("sp", S_x1[ts0:ts1, :], x1t[b2][:], f"x1t{b2}", [f"x1t{b2}"], [])
        yield
        st = st5[b2]
        yield
        kb.act(jk[:], x1t[b2][:], AF.Square, [f"x1t{b2}"], [f"jk_{b2}", f"st5_{b2}"], accum_out=st[:])
        yield
        kb.ts("dve", st[:], st[:], 1.0 / 1024, EPS, mult, add, [f"st5_{b2}"], [f"st5_{b2}"])
        yield
        kb.act(st[:], st[:], AF.Ln, [f"st5_{b2}"], [f"st5_{b2}"])
        yield
        kb.act(st[:], st[:], AF.Exp, [f"st5_{b2}"], [f"st5_{b2}"], scale=-0.5)
        yield
        kb.stt(h2f[:], x1t[b2][:], st[:, 0:1], fnw_bc[:], mult, mult, [f"x1t{b2}", f"st5_{b2}", "fnw_bc"], [f"h2f_{b2}"])
        yield
        kb.cp("act", h2b[b2][:], h2f[:], [f"h2f_{b2}"], [f"h2b{b2}"])
        yield
        kb.dma("sp", S_h2[ts0:ts1, :], h2b[b2][:], f"h2b{b2}", [f"h2b{b2}"], [])
        yield
        for k in range(8):
            kb.tr(pH[:, k, :], h2f[:, k * 128:(k + 1) * 128], identf[:], [f"h2f_{b2}", "identf"], ["pH"])
        yield
        kb.cp("act", h2T[:], pH[:], ["pH"], [f"h2T_{b2}"])
        yield
        for k in range(8):
            kb.mm(pL[:, 0:36], h2T[:, k, :], wr[:, k, :], k == 0, k == 7, [f"h2T_{b2}", "wr"], [f"pL_{b2}"])
        yield
        kb.tt("dve", rl[:], pL[:, 0:36], br_bc[:], add, [f"pL_{b2}", "br_bc"], [f"rl_{b2}"])
        yield
        kb.reduce(gmx[:], rl[:, 0:4], ALU.max, [f"rl_{b2}"], [f"gmx_{b2}"])
        yield
        kb.ts("dve", ohg[:], rl[:, 0:4], gmx[:, 0:1], None, ALU.is_equal, None, [f"rl_{b2}", f"gmx_{b2}"], [f"ohg_{b2}"])
        yield
        kb.ts("dve", ngm[:], gmx[:], -1.0, None, mult, None, [f"gmx_{b2}"], [f"ngm_{b2}"])
        yield
        kb.act(eg[:], rl[:, 0:4], AF.Exp, [f"rl_{b2}", f"ngm_{b2}"], [f"eg_{b2}", f"sume_{b2}"], bias=ngm[:], accum_out=sume[:])
        yield
        kb.recip(gw[:], sume[:], [f"sume_{b2}"], [f"gw_{b2}"])
        yield
        kb.tt("dve", selt[:], rl[:, 4:36].rearrange("p (g e) -> p g e", g=4), ohg[:, :, None].to_broadcast([128, 4, 8]), mult,
              [f"rl_{b2}", f"ohg_{b2}"], [f"selt_{b2}"])
        yield
        kb.reduce(sel[:], selt[:].rearrange("p g e -> p e g"), ALU.add, [f"selt_{b2}"], [f"sel_{b2}"])
        yield
        kb.max8(m8[:], sel[:], [f"sel_{b2}"], [f"m8_{b2}"])
        yield
        kb.tt("dve", dd[:], m8[:, 1:2], m8[:, 0:1], sub, [f"m8_{b2}"], [f"dd_{b2}"])
        yield
        kb.act(dd[:], dd[:], AF.Exp, [f"dd_{b2}"], [f"dd_{b2}"])
        yield
        kb.ts("dve", w12[:, 0:1], dd[:], 1.0, None, add, None, [f"dd_{b2}"], [f"w12_{b2}"])
        yield
        kb.recip(w12[:, 0:1], w12[:, 0:1], [f"w12_{b2}"], [f"w12_{b2}"])
        yield
        kb.tt("dve", w12[:, 1:2], w12[:, 0:1], dd[:], mult, [f"w12_{b2}", f"dd_{b2}"], [f"w12_{b2}"])
        yield
        kb.ts("dve", wcomb[:, 2 * tt_:2 * tt_ + 2], w12[:], gw[:, 0:1], None, mult, None, [f"w12_{b2}", f"gw_{b2}"], ["wcomb"])
        yield
        for k in range(2):
            kb.ts("dve", ohe[:, k, :], sel[:], m8[:, k:k + 1], None, ALU.is_equal, None, [f"sel_{b2}", f"m8_{b2}"], [f"ohe_{b2}"])
        yield
        for k in range(2):
            u = 2 * tt_ + k
            kb.tt("dve", ohall[:, u, :].rearrange("p (g e) -> p g e", g=4), ohg[:, :, None].to_broadcast([128, 4, 8]),
                  ohe[:, k:k + 1, :].to_broadcast([128, 4, 8]), mult, [f"ohg_{b2}", f"ohe_{b2}"], [f"ohall{u}"])
            kb.mm(pP[:, 0:32], stri[:], ohall[:, u, :], True, True, ["stri", f"ohall{u}"], ["pP"])
            kb.mm(pP[:, 32:64], onesb[:], ohall[:, u, :], True, True, ["onesb", f"ohall{u}"], ["pP"])
            kb.tt("dve", pmat[:], pP[:, 0:32], run_bc[:], add, ["pP", "run_bc"], [f"pmat_{b2}"])
            kb.tt("dve", ptmp[:], pmat[:], ohall[:, u, :], mult, [f"pmat_{b2}", f"ohall{u}"], [f"ptmp_{b2}"])
            kb.reduce(posn[:, u:u + 1], ptmp[:], ALU.add, [f"ptmp_{b2}"], ["posn"])
            kb.tt("dve", run_bc[:], run_bc[:], pP[:, 32:64], add, ["run_bc", "pP"], ["run_bc"])

    LAG_ = int(os.environ.get("KLAG", "4"))
    for t0_ in range(0, NT, 2):
        gA_ = p5a_tile(t0_)
        gB_ = p5a_tile(t0_ + 1) if t0_ + 1 < NT else iter(())
        aliveA_, aliveB_, nA_ = True, True, 0
        while aliveA_ or aliveB_:
            if aliveA_:
                try:
                    next(gA_)
                    nA_ += 1
                except StopIteration:
                    aliveA_ = False
            if aliveB_ and (nA_ >= LAG_ or not aliveA_):
                try:
                    next(gB_)
                except StopIteration:
                    aliveB_ = False
    kb.barrier()
    kb.pop()
    if stop_after == "P5a":
        return nc, kb

    import math as _m
    LOG_TS = int(_m.log2(TS))
    kb.push()
    cntf = kb.sb("cntf", [128, 32], F32)
    cnti = kb.sb("cnti", [128, 32], I32)
    ntf = kb.sb("ntf", [128, 32], F32)
    ones32 = kb.sb("ones32", [128, 32], F32)
    incl = kb.sb("incl", [128, 32], F32)
    base_bc = kb.sb("base_bc", [128, 32], F32)
    tmpb = kb.sb("tmpb", [128, NT * 2, 32], F32)
    slotf = kb.sb("slotf", [128, NT * 2], F32)
    sloti = kb.sb("sloti", [128, NT * 2], I32)
    rowdat = kb.sb("rowdat", [128, NT * 2, 4], F32)
    rowdat_i = rowdat[:].bitcast(I32)
    NDF = NSLOT // 128
    dflt = kb.sb("dflt", [128, NDF, 4], F32)
    dflt_i = dflt[:].bitcast(I32)
    jidx = kb.sb("jidx", [128, NTILES], F32)
    pidx = kb.sb("pidx", [128, 1], F32)
    cmpt = kb.sb("cmpt", [128, NTILES, 32], F32)
    ej = kb.sb("ej", [128, NTILES], F32)
    wgf = kb.sb("wgf", [128, NTILES], F32)
    wgi = kb.sb("wgi", [128, NTILES], I32)
    wdi = kb.sb("wdi", [128, NTILES], I32)
    kb.ts("dve", cntf[:], run_bc[:], float(TS - 1), None, add, None, ["run_bc"], ["cntf"])
    kb.cp("dve", cnti[:], cntf[:], ["cntf"], ["cnti"])
    kb.ts("dve", cnti[:], cnti[:], LOG_TS, None, ALU.arith_shift_right, None, ["cnti"], ["cnti"])
    kb.cp("dve", ntf[:], cnti[:], ["cnti"], ["ntf"])
    kb.memset("pool", ones32[:], 1.0, ["ones32"])
    Sd.op("dve", lambda e: e.tensor_tensor_scan(out=incl[:], data0=ones32[:], data1=ntf[:], initial=0.0, op0=mult, op1=add),
          ["ones32", "ntf"], ["incl"])
    kb.tt("dve", base_bc[:], incl[:], ntf[:], sub, ["incl", "ntf"], ["base_bc"])
    kb.ts("dve", base_bc[:], base_bc[:], float(TS), None, mult, None, ["base_bc"], ["base_bc"])
    kb.tt("pool", tmpb[:], ohall[:], base_bc[:, None, :].to_broadcast([128, NT * 2, 32]), mult,
          [f"ohall{u}" for u in range(NT * 2)] + ["base_bc"], ["tmpb"])
    Sd.op("dve", lambda e: e.tensor_reduce(out=slotf[:], in_=tmpb[:], axis=AX.X, op=ALU.add), ["tmpb"], ["slotf"])
    kb.tt("dve", slotf[:], slotf[:], posn[:], add, ["slotf", "posn"], ["slotf"])
    kb.cp("dve", sloti[:], slotf[:], ["slotf"], ["sloti"])
    kb.memset("pool", rowdat[:], 0.0, ["rowdat"])
    Sd.op("pool", lambda e: e.iota(rowdat_i[:, :, 0].rearrange("p (t k) -> p t k", k=2), pattern=[[128, NT], [0, 2]], base=0,
                                   channel_multiplier=1), ["rowdat"], ["rowdat"])
    Sd.op("pool", lambda e: e.iota(rowdat_i[:, :, 2].rearrange("p (t k) -> p t k", k=2), pattern=[[128, NT], [S, 2]], base=0,
                                   channel_multiplier=1), ["rowdat"], ["rowdat"])
    kb.cp("pool", rowdat[:, :, 1], wcomb[:], ["wcomb", "rowdat"], ["rowdat"])
    kb.memset("pool", dflt[:], 0.0, ["dflt"])
    kb.memset("pool", dflt_i[:, :, 0:1], 1 << 22, ["dflt"])
    kb.memset("pool", dflt_i[:, :, 2:3], 1 << 22, ["dflt"])
    kb.dma("sp", S_slot.rearrange("(n p) c -> p n c", p=128), dflt[:], "dflt", ["dflt"], ["S_slot_init"])
    for u in range(NT * 2):
        Sd.dma("pool", lambda e, u=u: e.indirect_dma_start(
            out=S_slot, out_offset=bass.IndirectOffsetOnAxis(ap=sloti[:, u:u + 1], axis=0), in_=rowdat[:, u, :], in_offset=None,
            bounds_check=BC(e, NSLOT - 1), oob_is_err=False), "slotsc", ["S_slot_init", "sloti", "rowdat"], [])
    Sd.op("pool", lambda e: e.iota(jidx[:], pattern=[[1, NTILES]], base=0, channel_multiplier=0,
                                   allow_small_or_imprecise_dtypes=True), [], ["jidx"])
    Sd.op("pool", lambda e: e.iota(pidx[:], pattern=[[0, 1]], base=0, channel_multiplier=1,
                                   allow_small_or_imprecise_dtypes=True), [], ["pidx"])
    kb.tt("dve", cmpt[:], incl[:, None, :].to_broadcast([128, NTILES, 32]), jidx[:, :, None].to_broadcast([128, NTILES, 32]),
          ALU.is_le, ["incl", "jidx"], ["cmpt"])
    Sd.op("dve", lambda e: e.tensor_reduce(out=ej[:], in_=cmpt[:], axis=AX.X, op=ALU.add), ["cmpt"], ["ej"])
    kb.ts("dve", wgf[:], ej[:], 128.0, pidx[:, 0:1], mult, add, ["ej", "pidx"], ["wgf"])
    kb.cp("dve", wgi[:], wgf[:], ["wgf"], ["wgi"])
    kb.barrier()
    if stop_after == "P5b":
        return nc, kb

    sl = [kb.sb(f"sl{i}", [128, NSUB * 4], F32) for i in range(2)]
    wg = [kb.sb(f"wg{i}", [128, 8, 256], BF16) for i in range(2)]
    wu = [kb.sb(f"wu{i}", [128, 8, 256], BF16) for i in range(2)]
    wd = [kb.sb(f"wd{i}", [128, 2, 1024], BF16) for i in range(2)]
    hg = [kb.sb(f"hg{i}", [128, 1024], BF16) for i in range(2 * NSUB)]
    hTt = [kb.sb(f"hTt{i}", [128, 8, TS], BF16) for i in range(2)]
    sg = [kb.sb(f"sg{i}", [128, TS], F32) for i in range(2)]
    heT = [kb.sb(f"heT{i}", [128, 2, TS], BF16) for i in range(2)]
    ysc = [kb.sb(f"ysc{i}", [128, 1024], F32) for i in range(2)]
    pHT = kb.ps("pHT", [128, 8, 128], BF16)
    pgu = [kb.ps(f"pgu{i}", [128, 512], F32) for i in range(4)]
    py = kb.ps("py", [128, 1024], F32)
    for i in range(2):
        kb.memset("pool", wg[i][:], 0.0, [f"wg{i}"])
        kb.memset("pool", wu[i][:], 0.0, [f"wu{i}"])
        kb.memset("pool", wd[i][:], 0.0, [f"wd{i}"])
        for q_ in range(NSUB):
            kb.memset("pool", hg[i * NSUB + q_][:], 0.0, [f"hg{i * NSUB + q_}"])
    def moe_loads(j):
        jp = j % 2
        kb.dma("sp", sl[jp][:].rearrange("p (n c) -> p n c", c=4), S_slot[j * TS:(j + 1) * TS, :].rearrange("(n p) c -> p n c", p=128),
               f"sl{jp}", [], [f"sl{jp}"])
        sl_i = sl[jp][:].bitcast(I32)
        for wt, wname, a_ in ((wg, "w_exp_gate", 8), (wu, "w_exp_up", 8), (wd, "w_exp_down", 2)):
            Sd.dma("pool", lambda e, j=j, jp=jp, wt=wt, wname=wname, a_=a_: e.indirect_dma_start(
                out=wt[jp][:].rearrange("p k c -> p (k c)"), out_offset=None,
                in_=D[wname].rearrange("(r a) c -> r (a c)", a=a_),
                in_offset=bass.IndirectOffsetOnAxis(ap=wgi[:, j:j + 1], axis=0),
                bounds_check=BC(e, 32 * 128 - 1), oob_is_err=False), f"{wname}{jp}", ["wgi"],
                [{"w_exp_gate": "wg", "w_exp_up": "wu", "w_exp_down": "wd"}[wname] + str(jp)])
        for sb_ in range(NSUB):
            hgt = hg[jp * NSUB + sb_]
            Sd.dma("pool", lambda e, sb_=sb_, hgt=hgt, sl_i=sl_i: e.indirect_dma_start(
                out=hgt[:], out_offset=None, in_=S_h2, in_offset=bass.IndirectOffsetOnAxis(ap=sl_i[:, sb_ * 4:sb_ * 4 + 1], axis=0),
                bounds_check=BC(e, S - 1), oob_is_err=False), f"hg{jp * NSUB + sb_}", [f"sl{jp}"], [f"hg{jp * NSUB + sb_}"])

    def moe_compute(j):
        jp = j % 2
        sl_i = sl[jp][:].bitcast(I32)
        for sb_ in range(NSUB):
            hi_ = jp * NSUB + sb_
            for k in range(8):
                kb.tr(pHT[:, k, :], hg[hi_][:].rearrange("p (j a) -> p a j", a=8)[:, k, :], ident[:], [f"hg{hi_}", "ident"], ["pHT"])
            kb.cp("act" if sb_ % 2 else "dve", hTt[jp][:, :, sb_ * 128:(sb_ + 1) * 128], pHT[:], ["pHT"], [f"hTt{jp}_{sb_}"])
        hb_ = [f"hTt{jp}_{sb_}" for sb_ in range(NSUB)]
        for m in range(2):
            pg_, pu_ = pgu[2 * m], pgu[2 * m + 1]
            for kc in range(8):
                kb.mm(pg_[:, :TS], wg[jp][:].rearrange("p k (j a) -> p k a j", a=2)[:, kc, m, :], hTt[jp][:, kc, :], kc == 0, kc == 7,
                      [f"wg{jp}"] + hb_, [f"pgu{2 * m}"])
            for kc in range(8):
                kb.mm(pu_[:, :TS], wu[jp][:].rearrange("p k (j a) -> p k a j", a=2)[:, kc, m, :], hTt[jp][:, kc, :], kc == 0, kc == 7,
                      [f"wu{jp}"] + hb_, [f"pgu{2 * m + 1}"])
            kb.act(sg[m][:], pg_[:, :TS], AF.Silu, [f"pgu{2 * m}"], [f"sg{m}"])
            kb.tt("dve", heT[jp][:, m, :], sg[m][:], pu_[:, :TS], mult, [f"sg{m}", f"pgu{2 * m + 1}"], [f"heT{jp}_{m}"])
        for sb_ in range(NSUB):
            s2 = sb_ % 2
            for half in range(2):
                for m in range(2):
                    kb.mm(py[:, half * 512:(half + 1) * 512], heT[jp][:, m, sb_ * 128:(sb_ + 1) * 128],
                          wd[jp][:, m, half * 512:(half + 1) * 512], m == 0, m == 1,
                          [f"heT{jp}_0", f"heT{jp}_1", f"wd{jp}"], ["py"])
            kb.act(ysc[s2][:], py[:], AF.Copy, ["py", f"sl{jp}"], [f"ysc{s2}"], scale=sl[jp][:, sb_ * 4 + 1:sb_ * 4 + 2])
            Sd.dma("pool", lambda e, sb_=sb_, s2=s2, sl_i=sl_i: e.indirect_dma_start(
                out=S_ymoe, out_offset=bass.IndirectOffsetOnAxis(ap=sl_i[:, sb_ * 4 + 2:sb_ * 4 + 3], axis=0), in_=ysc[s2][:], in_offset=None,
                bounds_check=BC(e, 2 * S - 1), oob_is_err=False), f"ysc{s2}", [f"ysc{s2}", f"sl{jp}"],
                ["S_ymoe"] if serial_scatter else [])

    moe_loads(0)
    for j in range(NTILES):
        if j + 1 < NTILES:
            moe_loads(j + 1)
        moe_compute(j)
    kb.barrier()
    kb.pop()
    kb.pop()
    if stop_after == "P5c":
        return nc, kb

    kb.push()
    pnw = vec_pk("pnw")
    wpg = kb.sb("wpg", [128, 8, 1024], BF16)
    wpg_st = [kb.sb(f"wpg_st{i}", [128, 1024], F32) for i in range(2)]
    for k in range(8):
        kb.dma("sp", wpg_st[k % 2][:], D["w_ple_gate"][k * 128:(k + 1) * 128, :], f"wpg_st{k % 2}", [], [f"wpg_st{k % 2}"])
        kb.ts("dve" if k % 2 else "pool", wpg[:, k, :], wpg_st[k % 2][:], pnw[:, k:k + 1], None, mult, None,
              [f"wpg_st{k % 2}", "vecs"], [f"wpg{k}"])
    wpgb = [f"wpg{k}" for k in range(8)]
    wpp = kb.sb("wpp", [128, 2, 1024], BF16)
    Sd.dma("pool", lambda e: e.dma_start(out=wpp[:], in_=D["w_ple_proj"].rearrange("(c p) n -> p c n", p=128)), "wpp", [], ["wpp"])
    bpg_bc = kb.sb("bpg_bc", [128, 1024], F32)
    kb.dma("sp", bpg_bc[:], D["b_ple_gate"].partition_broadcast(128), "bpg_bc", [], ["bpg_bc"])
    ppw_bc = kb.sb("ppw_bc", [128, 1024], F32)
    kb.dma("sp", ppw_bc[:], D["ple_post_norm_w"].partition_broadcast(128), "ppw_bc", [], ["ppw_bc"])
    fin_bc = kb.sb("fin_bc", [128, 1024], F32)
    kb.dma("sp", fin_bc[:], D["final_norm_w"].partition_broadcast(128), "fin_bc", [], ["fin_bc"])
    x1l = [kb.sb(f"x1l{i}", [128, 1024], F32) for i in range(2)]
    y0l = [kb.sb(f"y0l{i}", [128, 1024], F32) for i in range(2)]
    y1l = [kb.sb(f"y1l{i}", [128, 1024], F32) for i in range(2)]
    pl = [kb.sb(f"pl{i}", [128, 256], F32) for i in range(2)]
    plb_2 = [kb.sb(f"plb_{i}", [128, 256], BF16) for i in range(2)]
    pTs_2 = [kb.sb(f"pTs_{i}", [128, 2, 128], BF16) for i in range(2)]
    x2_2 = [kb.sb(f"x2_{i}", [128, 1024], F32) for i in range(2)]
    jk2_2 = [kb.sb(f"jk2_{i}", [128, 1024], BF16) for i in range(2)]
    s6_2 = [[kb.sb(f"s6_{i}_{q}", [128, 1], F32) for i in range(3)] for q in range(2)]
    n3b_2 = [kb.sb(f"n3b_{i}", [128, 1024], BF16) for i in range(2)]
    n3T_2 = [kb.sb(f"n3T_{i}", [128, 8, 128], BF16) for i in range(2)]
    gate_2 = [kb.sb(f"gate_{i}", [128, 1024], F32) for i in range(2)]
    ple_2 = [kb.sb(f"ple_{i}", [128, 1024], F32) for i in range(2)]
    x3_2 = [kb.sb(f"x3_{i}", [128, 1024], F32) for i in range(2)]
    ot = [kb.sb(f"ot{i}", [128, 1024], F32) for i in range(2)]
    pGt = kb.ps("pGt", [128, 1024], F32)
    pPp = kb.ps("pPp", [128, 1024], F32)
    pN3_2 = [kb.ps(f"pN3_{i}", [128, 8, 128], BF16) for i in range(2)]
    pPT_2 = [kb.ps(f"pPT_{i}", [128, 8, 128], BF16) for i in range(2)]

    def rstd_of(src_ap, src_bufs, st, stname, junk_ap, junkname):
        kb.act(junk_ap, src_ap, AF.Square, src_bufs, [junkname, stname], accum_out=st[:])
        kb.ts("dve", st[:], st[:], 1.0 / 1024, EPS, mult, add, [stname], [stname])
        kb.act(st[:], st[:], AF.Ln, [stname], [stname])
        kb.act(st[:], st[:], AF.Exp, [stname], [stname], scale=-0.5)

    def p5d_tile(tt_):
        yield
        b2 = tt_ % 2
        pN3 = pN3_2[b2]
        pPT = pPT_2[b2]
        yield
        plb = plb_2[b2]
        yield
        pTs = pTs_2[b2]
        yield
        x2 = x2_2[b2]
        yield
        jk2 = jk2_2[b2]
        yield
        n3b = n3b_2[b2]
        yield
        n3T = n3T_2[b2]
        yield
        gate = gate_2[b2]
        yield
        ple = ple_2[b2]
        yield
        x3 = x3_2[b2]
        yield
        s6 = s6_2[b2]
        yield
        ts0, ts1 = tt_ * 128, (tt_ + 1) * 128
        yield
        kb.dma("sp", x1l[b2][:], S_x1[ts0:ts1, :], f"x1l{b2}", [], [f"x1l{b2}"])
        yield
        kb.dma("sp", y0l[b2][:], S_ymoe[ts0:ts1, :], f"y0l{b2}", [], [f"y0l{b2}"])
        yield
        kb.dma("sp", y1l[b2][:], S_ymoe[S + ts0:S + ts1, :], f"y1l{b2}", [], [f"y1l{b2}"])
        yield
        kb.dma("sp", pl[b2][:], D["p"][ts0:ts1, :], f"pl{b2}", [], [f"pl{b2}"])
        yield
        kb.tt("dve", x2[:], x1l[b2][:], y0l[b2][:], add, [f"x1l{b2}", f"y0l{b2}"], [f"x2_{b2}"])
        yield
        kb.tt("dve", x2[:], x2[:], y1l[b2][:], add, [f"x2_{b2}", f"y1l{b2}"], [f"x2_{b2}"])
        yield
        rstd_of(x2[:], [f"x2_{b2}"], s6[0], f"s6_0_{b2}", jk2[:], f"jk2_{b2}")
        yield
        kb.ts("dve", n3b[:], x2[:], s6[0][:, 0:1], None, mult, None, [f"x2_{b2}", f"s6_0_{b2}"], [f"n3b_{b2}"])
        yield
        for k in range(8):
            kb.tr(pN3[:, k, :], n3b[:, k * 128:(k + 1) * 128], ident[:], [f"n3b_{b2}", "ident"], [f"pN3_{b2}"])
        yield
        kb.cp("act", n3T[:], pN3[:], [f"pN3_{b2}"], [f"n3T_{b2}"])
        yield
        for half in range(2):
            for k in range(8):
                kb.mm(pGt[:, half * 512:(half + 1) * 512], n3T[:, k, :], wpg[:, k, half * 512:(half + 1) * 512], k == 0, k == 7,
                      [f"n3T_{b2}"] + wpgb, ["pGt"])
        yield
        kb.tt("dve", gate[:], pGt[:], bpg_bc[:], add, ["pGt", "bpg_bc"], [f"gate_{b2}"])
        yield
        kb.act(gate[:], gate[:], AF.Sigmoid, [f"gate_{b2}"], [f"gate_{b2}"])
        yield
        kb.cp("pool", plb[:], pl[b2][:], [f"pl{b2}"], [f"plb_{b2}"])
        yield
        for c in range(2):
            kb.tr(pPT[:, c, :], plb[:, c * 128:(c + 1) * 128], ident[:], [f"plb_{b2}", "ident"], [f"pPT_{b2}"])
        yield
        kb.cp("dve", pTs[:], pPT[:, 0:2, :], [f"pPT_{b2}"], [f"pTs_{b2}"])
        yield
        for half in range(2):
            for c in range(2):
                kb.mm(pPp[:, half * 512:(half + 1) * 512], pTs[:, c, :], wpp[:, c, half * 512:(half + 1) * 512], c == 0, c == 1,
                      [f"pTs_{b2}", "wpp"], ["pPp"])
        yield
        rstd_of(pPp[:], ["pPp"], s6[1], f"s6_1_{b2}", jk2[:], f"jk2_{b2}")
        yield
        kb.stt(ple[:], pPp[:], s6[1][:, 0:1], ppw_bc[:], mult, mult, ["pPp", f"s6_1_{b2}", "ppw_bc"], [f"ple_{b2}"])
        yield
        kb.tt("dve", ple[:], ple[:], gate[:], mult, [f"ple_{b2}", f"gate_{b2}"], [f"ple_{b2}"])
        yield
        kb.tt("dve", x3[:], x2[:], ple[:], add, [f"x2_{b2}", f"ple_{b2}"], [f"x3_{b2}"])
        yield
        rstd_of(x3[:], [f"x3_{b2}"], s6[2], f"s6_2_{b2}", jk2[:], f"jk2_{b2}")
        yield
        kb.stt(ot[b2][:], x3[:], s6[2][:, 0:1], fin_bc[:], mult, mult, [f"x3_{b2}", f"s6_2_{b2}", "fin_bc"], [f"ot{b2}"])
        yield
        kb.dma("sp", OUT[ts0:ts1, :], ot[b2][:], f"ot{b2}", [f"ot{b2}"], [])

    LAG_ = int(os.environ.get("KLAG", "4"))
    for t0_ in range(0, NT, 2):
        gA_ = p5d_tile(t0_)
        gB_ = p5d_tile(t0_ + 1) if t0_ + 1 < NT else iter(())
        aliveA_, aliveB_, nA_ = True, True, 0
        while aliveA_ or aliveB_:
            if aliveA_:
                try:
                    next(gA_)
                    nA_ += 1
                except StopIteration:
                    aliveA_ = False
            if aliveB_ and (nA_ >= LAG_ or not aliveA_):
                try:
                    next(gB_)
                except StopIteration:
                    aliveB_ = False
    kb.barrier()
    kb.pop()
    return nc, kb


_NC_CACHE = {}


def kernel(**inputs):
    x = np.asarray(inputs["x"], dtype=np.float32)
    B, S, _ = x.shape
    assert B == 8
    p = np.asarray(inputs["p"], dtype=np.float32)
    pos = np.asarray(inputs["positions"]).astype(np.int32)
    if S not in _NC_CACHE:
        nc, kb = build(S)
        kb.S.emit(nc, None)
        _NC_CACHE[S] = nc
    nc = _NC_CACHE[S]
    shared = {}
    for name, shape in WEIGHT_SPECS:
        a = np.asarray(inputs[name], dtype=np.float32)
        if name != "final_norm_w":
            a = a[0]
        shp = shape if len(shape) == 2 else [1, shape[0]]
        shared[name] = np.ascontiguousarray(a.reshape(shp))
    in_maps = []
    for b in range(B):
        m = dict(shared)
        m["x"] = np.ascontiguousarray(x[b])
        m["p"] = np.ascontiguousarray(p[0, b])
        m["pos"] = np.ascontiguousarray(pos[b].reshape(S // 128, 128))
        in_maps.append(m)
    res = run_bass_kernel_spmd(nc, in_maps, core_ids=list(range(B)))
    return np.stack([np.asarray(r["out"], dtype=np.float32) for r in res.results], axis=0)
```

```python
import numpy as np
import concourse.bass as bass
import concourse.mybir as mybir
from concourse.bass_utils import run_bass_kernel_spmd

F32 = mybir.dt.float32
BF16 = mybir.dt.bfloat16
I32 = mybir.dt.int32
AF = mybir.ActivationFunctionType
ALU = mybir.AluOpType
AX = mybir.AxisListType

ENGS = ("pe", "act", "dve", "pool", "sp")


class Buf:
    __slots__ = ("name", "w", "r")

    def __init__(self, name):
        self.name = name
        self.w = None
        self.r = []


class _Op:
    __slots__ = ("eng", "fn", "deps", "dma_key", "signal", "count", "kind")

    def __init__(self, eng, fn, deps, dma_key, kind):
        self.eng = eng
        self.fn = fn
        self.deps = deps
        self.dma_key = dma_key
        self.signal = False
        self.count = 0
        self.kind = kind


class Sched:
    def __init__(self):
        self.ops = []
        self.bufs = {}
        self.psum_names = set()

    def buf(self, name):
        b = self.bufs.get(name)
        if b is None:
            b = self.bufs[name] = Buf(name)
        return b

    def _norm(self, xs):
        out = []
        for x in xs:
            if x is None:
                continue
            out.append(self.buf(x) if isinstance(x, str) else x)
        return out

    def op(self, eng, fn, reads=(), writes=(), dma_key=None):
        reads = self._norm(reads)
        writes = self._norm(writes)
        deps = set()
        for b in reads:
            if b.w is not None:
                deps.add(b.w)
            if b.name in self.psum_names:
                deps.update(r for r in b.r if self.ops[r].eng != eng)
        for b in writes:
            if b.w is not None:
                deps.add(b.w)
            deps.update(b.r)
        if eng == "pe":
            deps = set(d for d in deps if self.ops[d].eng != "pe")
        i = len(self.ops)
        self.ops.append(_Op(eng, fn, deps, dma_key, "dma" if dma_key else "op"))
        for b in reads:
            b.r.append(i)
        for b in writes:
            b.w = i
            b.r = []
        return i

    def dma(self, eng, fn, key, reads=(), writes=()):
        return self.op(eng, fn, reads, writes, dma_key=eng + "_" + key)

    def barrier(self):
        self.ops.append(_Op(None, None, set(), None, "barrier"))

    def emit(self, nc, engines):
        ops = self.ops
        last = {e: None for e in ENGS}
        pend_dma = []
        bar_deps = {}
        for i, o in enumerate(ops):
            if o.kind == "barrier":
                d = set(v for v in last.values() if v is not None)
                d.update(pend_dma)
                bar_deps[i] = d
                pend_dma = []
            else:
                last[o.eng] = i
                if o.kind == "dma":
                    pend_dma.append(i)
        for i, o in enumerate(ops):
            for d in o.deps:
                if ops[d].kind == "op":
                    ops[d].signal = True
        for d in bar_deps.values():
            for j in d:
                if ops[j].kind == "op":
                    ops[j].signal = True
        cnt = {e: 0 for e in ENGS}
        dcnt = {}
        for o in ops:
            if o.kind == "op" and o.signal:
                cnt[o.eng] += 1
                o.count = cnt[o.eng]
            elif o.kind == "dma":
                dcnt[o.dma_key] = dcnt.get(o.dma_key, 0) + 16
                o.count = dcnt[o.dma_key]
        sem_names = ["e_" + e for e in ENGS] + ["d_" + k for k in dcnt]
        import contextlib
        with contextlib.ExitStack() as st:
            sems = {n: st.enter_context(nc.semaphore(n)) for n in sem_names}
            seen = {e: {} for e in ENGS}
            streams = {e: [] for e in ENGS}

            def need(e, d):
                p = ops[d]
                name = ("d_" + p.dma_key) if p.kind == "dma" else ("e_" + p.eng)
                if seen[e].get(name, 0) >= p.count:
                    return
                seen[e][name] = p.count
                streams[e].append(("wait", name, p.count))

            for i, o in enumerate(ops):
                if o.kind == "barrier":
                    best = {}
                    for d in bar_deps[i]:
                        p = ops[d]
                        name = ("d_" + p.dma_key) if p.kind == "dma" else ("e_" + p.eng)
                        if name not in best or ops[best[name]].count < p.count:
                            best[name] = d
                    for e in ENGS:
                        for d in sorted(best.values()):
                            need(e, d)
                    continue
                for d in sorted(o.deps):
                    need(o.eng, d)
                streams[o.eng].append(("op", o))
            self.streams = streams
            block = st.enter_context(nc.Block())

            def run(e, eng):
                for it in streams[e]:
                    if it[0] == "wait":
                        eng.wait_ge(sems[it[1]], it[2])
                    else:
                        o = it[1]
                        ins = o.fn(eng)
                        if o.kind == "dma":
                            ins.then_inc(sems["d_" + o.dma_key], 16)
                        elif o.signal:
                            ins.then_inc(sems["e_" + o.eng], 1)

            @block.tensor
            def _(eng):
                run("pe", eng)

            @block.scalar
            def _(eng):
                run("act", eng)

            @block.vector
            def _(eng):
                run("dve", eng)

            @block.gpsimd
            def _(eng):
                run("pool", eng)

            @block.sync
            def _(eng):
                run("sp", eng)
        return {e: len(streams[e]) for e in ENGS}


D_MODEL = 1024
PLE_DIM = 256
EPS = 1e-6
NH = 8
QR, KVR, ROPE, NOPE, VD = 256, 128, 32, 64, 64
SH, SP_, SG, SN, SCONV = 8, 64, 2, 64, 5
SINNER, SXBC = 512, 768
INCOLS = 1712
NEXP, NGRP, EPG, DEXP = 32, 4, 8, 256
C_CQ, C_CKV, C_KR, C_Z, C_XBC, C_DT = 0, 256, 384, 416, 928, 1696

WEIGHT_SPECS = [
    ("attn_norm_w", [1024]), ("w_in", [1024, 1712]), ("q_norm_w", [256]), ("w_uq", [256, 768]),
    ("kv_norm_w", [128]), ("w_ukv", [128, 1024]), ("attn_out_norm_w", [512]), ("conv_w", [5, 768]),
    ("conv_b", [768]), ("dt_bias", [1, 16]), ("a_log", [1, 16]), ("ssd_d", [1, 8]), ("ssd_norm_w", [512]),
    ("w_o", [1024, 1024]), ("ffn_norm_w", [1024]), ("w_router_group", [1024, 4]), ("b_router_group", [1, 4]),
    ("w_router_expert", [1024, 32]), ("b_router_expert", [1, 32]), ("w_exp_gate", [32 * 1024, 256]),
    ("w_exp_up", [32 * 1024, 256]), ("w_exp_down", [32 * 256, 1024]), ("ple_norm_w", [1024]),
    ("w_ple_gate", [1024, 1024]), ("b_ple_gate", [1, 1024]), ("w_ple_proj", [256, 1024]),
    ("ple_post_norm_w", [1024]), ("final_norm_w", [1024]),
]


class KB:
    def __init__(self, nc):
        import contextlib
        self.nc = nc
        self.S = Sched()
        self.root = contextlib.ExitStack()
        self.stack = [self.root]

    def push(self):
        import contextlib
        st = contextlib.ExitStack()
        self.stack.append(st)
        return st

    def pop(self):
        self.stack.pop().close()

    def sb(self, name, shape, dt):
        return self.stack[-1].enter_context(self.nc.sbuf_tensor(name, list(shape), dt))

    def ps(self, name, shape, dt=F32):
        self.S.psum_names.add(name)
        return self.stack[-1].enter_context(self.nc.psum_tensor(name, list(shape), dt))

    def act(self, out, in_, func, r, w, **kw):
        self.S.op("act", lambda e: e.activation(out=out, in_=in_, func=func, **kw), r, w)

    def ts(self, eng, out, in0, s1, s2, op0, op1, r, w):
        if op1 is None:
            self.S.op(eng, lambda e: e.tensor_scalar(out=out, in0=in0, scalar1=s1, scalar2=None, op0=op0), r, w)
        else:
            self.S.op(eng, lambda e: e.tensor_scalar(out=out, in0=in0, scalar1=s1, scalar2=s2, op0=op0, op1=op1), r, w)

    def tt(self, eng, out, in0, in1, op, r, w):
        self.S.op(eng, lambda e: e.tensor_tensor(out=out, in0=in0, in1=in1, op=op), r, w)

    def stt(self, out, in0, scalar, in1, op0, op1, r, w):
        self.S.op("dve", lambda e: e.scalar_tensor_tensor(out=out, in0=in0, scalar=scalar, in1=in1, op0=op0, op1=op1), r, w)

    def cp(self, eng, out, in_, r, w):
        if eng == "act":
            self.S.op("act", lambda e: e.activation(out=out, in_=in_, func=AF.Copy), r, w)
        else:
            self.S.op(eng, lambda e: e.tensor_copy(out=out, in_=in_), r, w)

    def memset(self, eng, ap, val, w):
        self.S.op(eng, lambda e: e.memset(ap, val), (), w)

    def mm(self, out, lhsT, rhs, start, stop, r, w, **kw):
        self.S.op("pe", lambda e: e.matmul(out, lhsT=lhsT, rhs=rhs, start=start, stop=stop, **kw), r, w)

    def tr(self, out, in_, ident, r, w):
        self.S.op("pe", lambda e: e.transpose(out=out, in_=in_, identity=ident), r, w)

    def dma(self, eng, out, in_, key, r, w, **kw):
        self.S.dma(eng, lambda e: e.dma_start(out=out, in_=in_, **kw), key, r, w)

    def barrier(self):
        self.S.barrier()

    def reduce(self, out, in_, op, r, w):
        self.S.op("dve", lambda e: e.tensor_reduce(out=out, in_=in_, axis=AX.X, op=op), r, w)

    def recip(self, out, in_, r, w):
        self.S.op("dve", lambda e: e.reciprocal(out=out, in_=in_), r, w)

    def max8(self, out, in_, r, w):
        self.S.op("dve", lambda e: e.max(out=out, in_=in_), r, w)


def build(S=4096, stop_after=None, serial_scatter=False):
    import math
    nc = bass.Bass("TRN2", target_bir_lowering=False)
    NT = S // 128
    GS = min(512, S)
    NG = S // GS
    TG = GS // 128
    D = {}
    D["x"] = nc.dram_tensor("x", [S, 1024], F32, kind="ExternalInput").ap()
    D["p"] = nc.dram_tensor("p", [S, 256], F32, kind="ExternalInput").ap()
    D["pos"] = nc.dram_tensor("pos", [NT, 128], I32, kind="ExternalInput").ap()
    for name, shape in WEIGHT_SPECS:
        shp = shape if len(shape) == 2 else [1, shape[0]]
        D[name] = nc.dram_tensor(name, shp, F32, kind="ExternalInput").ap()
    OUT = nc.dram_tensor("out", [S, 1024], F32, kind="ExternalOutput").ap()

    def scr(name, shape, dt):
        return nc.dram_tensor(name, shape, dt).ap()

    S_cqn = scr("S_cqn", [256, S], BF16)
    S_kvn = scr("S_kvn", [128, S], BF16)
    S_sz = scr("S_sz", [512, S], BF16)
    S_xbc = scr("S_xbc", [768, S], F32)
    S_xtm = scr("S_xtm", [S, 768], BF16)
    S_bcT = scr("S_bcT", [256, S], BF16)
    S_yf = scr("S_yf", [512, S], F32)
    S_yn = scr("S_yn", [512, S], BF16)

    import os
    KD = int(os.environ.get("KDBG", "0"))
    kb = KB(nc)
    Sd = kb.S
    mult, add, sub = ALU.mult, ALU.add, ALU.subtract
    _regs = {}

    def BC(e, val):
        if val not in _regs:
            _regs[val] = e.to_reg(val)
        return _regs[val]

    VROWS = [("anw", "attn_norm_w", 8), ("cb", "conv_b", 6), ("cw", "conv_w", 30), ("kvw", "kv_norm_w", 1),
             ("qw", "q_norm_w", 2), ("pnw", "ple_norm_w", 8)]
    voff = {}
    _o = 0
    for nm, _, k in VROWS:
        voff[nm] = (_o, k)
        _o += k
    NV = _o

    identf = kb.sb("identf", [128, 128], F32)
    ident = kb.sb("ident", [128, 128], BF16)
    onesf = kb.sb("onesf", [128, 128], F32)
    onesb = kb.sb("onesb", [128, 128], BF16)
    kb.memset("pool", identf[:], 0.0, ["identf"])
    Sd.op("pool", lambda e: e.affine_select(out=identf[:], in_=identf[:], pattern=[[-1, 128]],
                                            compare_op=ALU.not_equal, fill=1.0, base=0,
                                            channel_multiplier=1), ["identf"], ["identf"])
    kb.cp("dve", ident[:], identf[:], ["identf"], ["ident"])
    kb.memset("pool", onesf[:], 1.0, ["onesf"])
    c_eps = kb.sb("c_eps", [128, 1], F32)
    c_one = kb.sb("c_one", [128, 1], F32)
    kb.memset("pool", c_eps[:], EPS, ["c_eps"])
    kb.memset("pool", c_one[:], 1.0, ["c_one"])
    kb.memset("pool", onesb[:], 1.0, ["onesb"])

    vecs = kb.sb("vecs", [128, NV], F32)
    vecs64 = kb.sb("vecs64", [64, 16], F32)
    kb.push()
    vst = kb.sb("vst", [NV, 128], F32)
    vst64 = kb.sb("vst64", [16, 64], F32)
    pvec = kb.ps("pvec", [128, 512], F32)
    for nm, dn, k in VROWS:
        o_ = voff[nm][0]
        src = D[dn]
        src = src.rearrange("o (k p) -> (o k) p", p=128) if dn != "conv_w" else src.rearrange("t (j p) -> (t j) p", p=128)
        kb.dma("sp", vst[o_:o_ + k, :], src, "vst", [], ["vst"])
    kb.dma("sp", vst64[0:8, :], D["attn_out_norm_w"].rearrange("o (k p) -> (o k) p", p=64), "vst64", [], ["vst64"])
    kb.dma("sp", vst64[8:16, :], D["ssd_norm_w"].rearrange("o (k p) -> (o k) p", p=64), "vst64", [], ["vst64"])
    kb.tr(pvec[:, 0:NV], vst[:], identf[:NV, :NV], ["vst", "identf"], ["pvec"])
    kb.cp("dve", vecs[:], pvec[:, 0:NV], ["pvec"], ["vecs"])
    kb.tr(pvec[0:64, 64:80], vst64[:], identf[:16, :16], ["vst64", "identf", "vecs"], ["pvec"])
    kb.cp("dve", vecs64[:], pvec[0:64, 64:80], ["pvec"], ["vecs64"])
    kb.barrier()
    kb.pop()

    def vec_pk(name, dram=None, k=None):
        o_, k_ = voff[name]
        return vecs[:, o_:o_ + k_]

    cosT = kb.sb("cosT", [128, NT, 16], F32)
    sinT = kb.sb("sinT", [128, NT, 16], F32)
    krot = kb.sb("krot", [128, NT, 32], F32)
    dtp = kb.sb("dtp", [128, NT, 16], F32)
    dtb_bc = kb.sb("dtb_bc", [128, 16], F32)
    a_bc = kb.sb("a_bc", [128, 16], F32)
    kb.dma("sp", dtb_bc[:], D["dt_bias"].partition_broadcast(128), "dtb_bc", [], ["dtb_bc"])
    kb.dma("sp", a_bc[:], D["a_log"].partition_broadcast(128), "a_bc", [], ["a_bc"])
    kb.act(a_bc[:], a_bc[:], AF.Exp, ["a_bc"], ["a_bc"])
    kb.ts("dve", a_bc[:], a_bc[:], -1.0, None, mult, None, ["a_bc"], ["a_bc"])
    kb.push()
    posi = kb.sb("posi", [NT, 128], I32)
    posr = kb.sb("posr", [NT, 128], F32)
    posf = kb.sb("posf", [128, NT], F32)
    invf = kb.sb("invf", [128, 16], F32)
    rr = kb.sb("rr", [128, NT, 16], F32)
    rf = kb.sb("rf", [128, NT, 16], F32)
    ri = kb.sb("ri", [128, NT, 16], I32)
    rm = kb.sb("rm", [128, NT, 16], F32)
    ppos = kb.ps("ppos", [128, 512], F32)
    kb.dma("sp", posi[:], D["pos"], "posi", [], ["posi"])
    kb.cp("dve", posr[:], posi[:], ["posi"], ["posr"])
    kb.tr(ppos[:, :NT], posr[:], identf[:NT, :NT], ["posr", "identf"], ["ppos"])
    kb.cp("dve", posf[:], ppos[:, :NT], ["ppos"], ["posf"])
    for i in range(16):
        kb.memset("pool", invf[:, i:i + 1], (10000.0 ** (-(2.0 * i) / 32.0)) / (2 * math.pi), ["invf"])
    for t in range(NT):
        kb.ts("dve", rr[:, t, :], invf[:], posf[:, t:t + 1], None, mult, None, ["invf", "posf"], ["rr"])
    for shift, dst in ((0.0, "sinT"), (0.25, "cosT")):
        dstt = sinT if dst == "sinT" else cosT
        if shift:
            kb.ts("dve", rr[:], rr[:], shift, None, add, None, ["rr"], ["rr"])
        kb.cp("dve", ri[:], rr[:], ["rr"], ["ri"])
        kb.cp("dve", rf[:], ri[:], ["ri"], ["rf"])
        kb.tt("dve", rf[:], rr[:], rf[:], sub, ["rr", "rf"], ["rf"])
        kb.ts("dve", rm[:], rf[:], 0.5, None, ALU.is_gt, None, ["rf"], ["rm"])
        kb.tt("dve", rf[:], rf[:], rm[:], sub, ["rf", "rm"], ["rf"])
        kb.ts("dve", rm[:], rf[:], -0.5, None, ALU.is_lt, None, ["rf"], ["rm"])
        kb.tt("dve", rf[:], rf[:], rm[:], add, ["rf", "rm"], ["rf"])
        kb.act(dstt[:], rf[:], AF.Sin, ["rf"], [dst], scale=2 * math.pi)
    kb.barrier()
    kb.pop()

    if stop_after == "P0":
        return nc, kb
    kb.push()
    anw = vec_pk("anw")
    w_in_bf = kb.sb("w_in_bf", [128, 8, INCOLS], BF16)
    wst = [kb.sb(f"wst{i}", [128, INCOLS], F32) for i in range(2)]
    for k in range(8):
        kb.dma("sp", wst[k % 2][:], D["w_in"][k * 128:(k + 1) * 128, :], f"wst{k % 2}", [], [f"wst{k % 2}"])
        kb.ts("dve" if k % 2 == 0 else "pool", w_in_bf[:, k, :], wst[k % 2][:], anw[:, k:k + 1], None, mult, None,
              [f"wst{k % 2}", "vecs"], ["w_in_bf"])
    wsmall = kb.sb("wsmall", [128, 8, 48], BF16)
    kb.cp("dve", wsmall[:, :, 0:32], w_in_bf[:, :, C_KR:C_KR + 32], ["w_in_bf"], ["wsmall"])
    kb.cp("dve", wsmall[:, :, 32:48], w_in_bf[:, :, C_DT:C_DT + 16], ["w_in_bf"], ["wsmall"])

    NX = 3
    xt = [kb.sb(f"xt{i}", [128, 1024], F32) for i in range(NX)]
    hn = [kb.sb(f"hn{i}", [128, 1024], BF16) for i in range(NX)]
    ss = [kb.sb(f"ss{i}", [128, 1], F32) for i in range(NX)]
    junk = kb.sb("junk", [128, 1024], BF16)
    hT = [kb.sb(f"hT{i}", [128, 8, GS], BF16) for i in range(2)]
    sm = kb.sb("sm", [128, 48], F32)
    rot = [kb.sb(f"rot{i}", [128, 16], F32) for i in range(4)]
    dx = kb.sb("dx", [128, 16], F32)
    cqf = kb.sb("cqf", [128, 2, GS], F32)
    kvf = kb.sb("kvf", [128, GS], F32)
    sq = [kb.sb(f"sq{i}", [128, GS], F32) for i in range(3)]
    rs = [kb.sb(f"rs{i}", [128, GS], F32) for i in range(2)]
    cqn = [kb.sb(f"cqn{i}", [128, 2, GS], BF16) for i in range(2)]
    kvn = [kb.sb(f"kvn{i}", [128, GS], BF16) for i in range(2)]
    szs = [kb.sb(f"szs{i}", [128, 4, GS], BF16) for i in range(2)]
    xbs = [kb.sb(f"xbs{i}", [128, 6, GS], F32) for i in range(2)]
    pT = [kb.ps(f"pT{i}", [128, 8, 128], BF16) for i in range(2)]
    pS = kb.ps("pS", [128, 512], F32)
    pM = [kb.ps(f"pM{i}", [128, 512], F32) for i in range(3)]
    pST = [kb.ps(f"pST{i}", [128, 512], F32) for i in range(2)]

    chunks = [("cq", 0, C_CQ), ("cq", 1, C_CQ + 128), ("ckv", 0, C_CKV)]
    chunks += [("z", i, C_Z + 128 * i) for i in range(4)]
    chunks += [("xbc", i, C_XBC + 128 * i) for i in range(6)]
    ci_glob = 0
    for g in range(NG):
        gp = g % 2
        for t in range(TG):
            tt_ = g * TG + t
            s = tt_ % NX
            pp = tt_ % 2
            kb.dma("sp", xt[s][:], D["x"][tt_ * 128:(tt_ + 1) * 128, :], f"xt{s}", [], [f"xt{s}"])
            kb.act(junk[:], xt[s][:], AF.Square, [f"xt{s}"], ["junk", f"ss{s}"], accum_out=ss[s][:])
            kb.ts("dve", ss[s][:], ss[s][:], 1.0 / 1024, EPS, mult, add, [f"ss{s}"], [f"ss{s}"])
            kb.act(ss[s][:], ss[s][:], AF.Ln, [f"ss{s}"], [f"ss{s}"])
            kb.act(ss[s][:], ss[s][:], AF.Exp, [f"ss{s}"], [f"ss{s}"], scale=-0.5)
            kb.ts("dve", hn[s][:], xt[s][:], ss[s][:], None, mult, None, [f"xt{s}", f"ss{s}"], [f"hn{s}"])
            for k in range(8):
                kb.tr(pT[pp][:, k, :], hn[s][:, k * 128:(k + 1) * 128], ident[:], [f"hn{s}", "ident"], [f"pT{pp}"])
            kb.cp("act" if t % 2 else "dve", hT[gp][:, :, t * 128:(t + 1) * 128], pT[pp][:], [f"pT{pp}"], [f"hT{gp}_{t}"])
            if KD and KD < 2:
                continue
            for k in range(8):
                kb.mm(pS[:, :48], hT[gp][:, k, t * 128:(t + 1) * 128], wsmall[:, k, :], k == 0, k == 7,
                      [f"hT{gp}_{t}", "wsmall"], ["pS"])
            kb.cp("dve", sm[:], pS[:, :48], ["pS"], ["sm"])
            if KD and KD < 3:
                continue
            k1, k2 = sm[:, 0:16], sm[:, 16:32]
            cs_, sn_ = cosT[:, tt_, :], sinT[:, tt_, :]
            kb.tt("pool", rot[0][:], k1, cs_, mult, ["sm", "cosT"], ["rot0"])
            kb.tt("pool", rot[1][:], k2, sn_, mult, ["sm", "sinT"], ["rot1"])
            kb.tt("pool", krot[:, tt_, 0:16], rot[0][:], rot[1][:], sub, ["rot0", "rot1"], ["krot"])
            kb.tt("pool", rot[2][:], k2, cs_, mult, ["sm", "cosT"], ["rot2"])
            kb.tt("pool", rot[3][:], k1, sn_, mult, ["sm", "sinT"], ["rot3"])
            kb.tt("pool", krot[:, tt_, 16:32], rot[2][:], rot[3][:], add, ["rot2", "rot3"], ["krot"])
            kb.tt("dve", dx[:], sm[:, 32:48], dtb_bc[:], add, ["sm", "dtb_bc"], ["dx"])
            kb.act(dx[:], dx[:], AF.Exp, ["dx"], ["dx"])
            kb.act(dtp[:, tt_, :], dx[:], AF.Ln, ["dx", "c_one"], ["dtp"], bias=c_one[:])
        hT_bufs = [f"hT{gp}_{t}" for t in range(TG)]
        for (kind, i, c0) in (chunks if not KD else chunks[:max(0, KD - 3)]):
            pi = ci_glob % 3
            ci_glob += 1
            pm = pM[pi]
            for k in range(8):
                kb.mm(pm[:, :GS], w_in_bf[:, k, c0:c0 + 128], hT[gp][:, k, :], k == 0, k == 7,
                      ["w_in_bf"] + hT_bufs, [f"pM{pi}"])
            KD2 = int(os.environ.get("KDBG2", "9"))
            if kind == "cq":
                if KD2 >= 1 and os.environ.get("KNODVE") != "1":
                    kb.cp("dve", cqf[:, i, :], pm[:, :GS], [f"pM{pi}"], [f"cqf{i}"])
                if KD2 >= 2:
                    if os.environ.get("KSQ") == "dve":
                        kb.cp("dve", sq[i][:], pm[:, :GS], [f"pM{pi}"], [f"sq{i}"])
                    elif os.environ.get("KSQ") == "junk":
                        kb.act(junk[:, :GS], pm[:, :GS], AF.Square, [f"pM{pi}"], ["junk"])
                    else:
                        kb.act(sq[i][:], pm[:, :GS], AF.Square, [f"pM{pi}"], [f"sq{i}"])
                if KD2 >= 3:
                    kb.mm(pST[0][:, :GS], onesf[:], sq[i][:], i == 0, i == 1, ["onesf", f"sq{i}"], ["pST0"])
                if i == 1:
                    kb.act(rs[0][:], pST[0][:, :GS], AF.Ln, ["pST0", "c_eps"], ["rs0"], scale=1.0 / 256, bias=c_eps[:])
                    kb.act(rs[0][:], rs[0][:], AF.Exp, ["rs0"], ["rs0"], scale=-0.5)
                    for j in range(2):
                        kb.tt("dve", cqn[gp][:, j, :], cqf[:, j, :], rs[0][:], mult, [f"cqf{j}", "rs0"], [f"cqn{gp}_{j}"])
                    kb.dma("sp", S_cqn.rearrange("(c p) s -> p c s", p=128)[:, :, g * GS:(g + 1) * GS], cqn[gp][:],
                           f"cqn{gp}", [f"cqn{gp}_0", f"cqn{gp}_1"], [])
            elif kind == "ckv":
                kb.cp("dve", kvf[:], pm[:, :GS], [f"pM{pi}"], ["kvf"])
                kb.act(sq[2][:], pm[:, :GS], AF.Square, [f"pM{pi}"], ["sq2"])
                kb.mm(pST[1][:, :GS], onesf[:], sq[2][:], True, True, ["onesf", "sq2"], ["pST1"])
                kb.act(rs[1][:], pST[1][:, :GS], AF.Ln, ["pST1", "c_eps"], ["rs1"], scale=1.0 / 128, bias=c_eps[:])
                kb.act(rs[1][:], rs[1][:], AF.Exp, ["rs1"], ["rs1"], scale=-0.5)
                kb.tt("dve", kvn[gp][:], kvf[:], rs[1][:], mult, ["kvf", "rs1"], [f"kvn{gp}"])
                kb.dma("sp", S_kvn[:, g * GS:(g + 1) * GS], kvn[gp][:], f"kvn{gp}", [f"kvn{gp}"], [])
            elif kind == "z":
                kb.act(szs[gp][:, i, :], pm[:, :GS], AF.Silu, [f"pM{pi}"], [f"szs{gp}_{i}"])
                if i == 3:
                    kb.dma("sp", S_sz.rearrange("(c p) s -> p c s", p=128)[:, :, g * GS:(g + 1) * GS], szs[gp][:],
                           f"szs{gp}", [f"szs{gp}_{j}" for j in range(4)], [])
            else:
                kb.cp("dve" if i % 2 else "act", xbs[gp][:, i, :], pm[:, :GS], [f"pM{pi}"], [f"xbs{gp}_{i}"])
                if i == 5:
                    kb.dma("sp", S_xbc.rearrange("(c p) s -> p c s", p=128)[:, :, g * GS:(g + 1) * GS], xbs[gp][:],
                           f"xbs{gp}", [f"xbs{gp}_{j}" for j in range(6)], [])
    kb.barrier()
    kb.pop()
    if stop_after == "P1":
        return nc, kb

    kb.push()
    cw = vec_pk("cw").rearrange("p (t j) -> p t j", j=6)
    cb = vec_pk("cb")
    xin = [kb.sb(f"xin{i}", [128, 6, GS + 4], F32) for i in range(2)]
    acc = [kb.sb(f"acc{i}", [128, 6, GS], F32) for i in range(2)]
    xc = [kb.sb(f"xc{i}", [128, 6, GS], BF16) for i in range(2)]
    xts = [kb.sb(f"xts{i}", [128, 768], BF16) for i in range(2)]
    pX = [kb.ps(f"pX{i}", [128, 6, 128], BF16) for i in range(2)]
    xbc_v = S_xbc.rearrange("(c p) s -> p c s", p=128)
    for g in range(NG):
        gp = g % 2
        lo, hi = g * GS - 2, g * GS + GS + 2
        clo, chi = max(lo, 0), min(hi, S)
        if lo < 0:
            kb.memset("pool", xin[gp][:, :, 0:2], 0.0, [f"xin{gp}"])
        if hi > S:
            kb.memset("pool", xin[gp][:, :, GS + 2:GS + 4], 0.0, [f"xin{gp}"])
        kb.dma("sp", xin[gp][:, :, clo - lo:chi - lo], xbc_v[:, :, clo:chi], f"xin{gp}", [], [f"xin{gp}"])
        for j in range(6):
            kb.ts("dve", acc[gp][:, j, :], xin[gp][:, j, 0:GS], cw[:, 0, j:j + 1], cb[:, j:j + 1], mult, add,
                  [f"xin{gp}", "vecs"], [f"acc{gp}_{j}"])
            for k in range(1, 5):
                kb.stt(acc[gp][:, j, :], xin[gp][:, j, k:k + GS], cw[:, k, j:j + 1], acc[gp][:, j, :], mult, add,
                       [f"xin{gp}", "vecs", f"acc{gp}_{j}"], [f"acc{gp}_{j}"])
            kb.act(xc[gp][:, j, :], acc[gp][:, j, :], AF.Silu, [f"acc{gp}_{j}"], [f"xc{gp}_{j}"])
        xcb = [f"xc{gp}_{j}" for j in range(6)]
        kb.dma("sp", S_bcT.rearrange("(j p) s -> p j s", p=128)[:, :, g * GS:(g + 1) * GS], xc[gp][:, 4:6, :],
               f"xc{gp}", xcb, [])
        for t in range(TG):
            tt_ = g * TG + t
            pp = tt_ % 2
            for j in range(6):
                kb.tr(pX[pp][:, j, :], xc[gp][:, j, t * 128:(t + 1) * 128], ident[:], [f"xc{gp}_{j}", "ident"], [f"pX{pp}"])
            kb.cp("act" if t % 2 else "dve", xts[pp][:], pX[pp][:], [f"pX{pp}"], [f"xts{pp}"])
            kb.dma("sp", S_xtm[tt_ * 128:(tt_ + 1) * 128, :], xts[pp][:], f"xts{pp}", [f"xts{pp}"], [])
    kb.barrier()
    kb.pop()
    if stop_after == "P1b":
        return nc, kb

    kb.push()
    tri = [kb.sb("triU", [128, 128], F32), kb.sb("triL", [128, 128], F32)]
    for d_, (cm_, pat) in enumerate(((-1, 1), (1, -1))):
        kb.memset("pool", tri[d_][:], 1.0, [f"tri{d_}"])
        Sd.op("pool", lambda e, d_=d_, cm_=cm_, pat=pat: e.affine_select(
            out=tri[d_][:], in_=tri[d_][:], pattern=[[pat, 128]], compare_op=ALU.is_ge, fill=0.0, base=0,
            channel_multiplier=cm_), [f"tri{d_}"], [f"tri{d_}"])
    Esel = kb.sb("Esel", [8, 8, 128], F32)
    kb.memset("pool", Esel[:], 0.0, ["Esel"])
    Sd.op("pool", lambda e: e.affine_select(out=Esel[:], in_=Esel[:], pattern=[[-1, 8], [0, 128]],
                                            compare_op=ALU.not_equal, fill=1.0, base=0, channel_multiplier=1),
          ["Esel"], ["Esel"])
    d_bc = kb.sb("d_bc", [128, 8], F32)
    kb.dma("sp", d_bc[:], D["ssd_d"].partition_broadcast(128), "d_bc", [], ["d_bc"])
    diagD = kb.sb("diagD", [128, 8, 128], BF16)
    for r in range(8):
        kb.ts("dve", diagD[:, r, :], identf[:], d_bc[:, r:r + 1], None, mult, None, ["identf", "d_bc"], ["diagD"])
    xtm = [kb.sb(f"xtm{i}", [128, 768], BF16) for i in range(2)]
    bcT = [kb.sb(f"bcT{i}", [64, 4, 128], BF16) for i in range(2)]
    A_ = kb.sb("A_", [128, 8], F32)
    cs_sb = kb.sb("cs_sb", [128, 8], F32)
    csT_sb = kb.sb("csT_sb", [8, 128], F32)
    seg = kb.sb("seg", [128, 8, 128], F32)
    GTm = kb.sb("GTm", [128, 2, 128], F32)
    MT = kb.sb("MT", [128, 8, 128], BF16)
    ecs = kb.sb("ecs", [64, 8, 128], F32)
    Cdec = kb.sb("Cdec", [64, 8, 128], BF16)
    wex = kb.sb("wex", [128, 8], F32)
    xw = kb.sb("xw", [128, 8, 64], BF16)
    xdt = kb.sb("xdt", [128, 8, 64], BF16)
    dec = kb.sb("dec", [64, 8], F32)
    hst = kb.sb("hst", [64, 8, 64], F32)
    hbf = kb.sb("hbf", [64, 8, 64], BF16)
    ysb = [kb.sb(f"ysb{i}", [64, 8, 128], F32) for i in range(2)]
    yfl = [kb.sb(f"yfl{i}", [64, 8, 128], F32) for i in range(2)]
    szl = [kb.sb(f"szl{i}", [64, 8, 128], BF16) for i in range(2)]
    yg = kb.sb("yg", [64, 8, 128], F32)
    sqy = kb.sb("sqy", [64, 8, 128], F32)
    rsy = kb.sb("rsy", [64, 128], F32)
    ynb = [kb.sb(f"ynb{i}", [64, 8, 128], BF16) for i in range(2)]
    pA = kb.ps("pA", [128, 512], F32)
    pB = kb.ps("pB", [64, 8, 64], F32)
    pC = kb.ps("pC", [64, 512], F32)
    pCS = kb.ps("pCS", [128, 8, 128], F32)
    pG = kb.ps("pG", [128, 512], F32)
    pY = kb.ps("pY", [64, 8, 128], F32)
    yf_v = S_yf.rearrange("(r p) s -> p r s", p=64)
    yn_v = S_yn.rearrange("(r p) s -> p r s", p=64)
    sz_v = S_sz.rearrange("(r p) s -> p r s", p=64)
    bcT_v = S_bcT.rearrange("(q n) s -> n q s", n=64)
    for d_ in range(2):
        kb.memset("pool", hst[:], 0.0, ["hst"])
        kb.memset("pool", hbf[:], 0.0, ["hbf"])
        order = list(range(NT)) if d_ == 0 else list(range(NT - 1, -1, -1))
        for ci, c in enumerate(order):
            b2 = ci % 2
            cs0, cs1 = c * 128, (c + 1) * 128
            kb.dma("sp", xtm[b2][:], S_xtm[cs0:cs1, :], f"xtm{b2}", [], [f"xtm{b2}"])
            kb.dma("sp", bcT[b2][:], bcT_v[:, :, cs0:cs1], f"bcT{b2}", [], [f"bcT{b2}"])
            if d_ == 1:
                kb.dma("sp", yfl[b2][:], yf_v[:, :, cs0:cs1], f"yfl{b2}", [], [f"yfl{b2}"])
                kb.dma("sp", szl[b2][:], sz_v[:, :, cs0:cs1], f"szl{b2}", [], [f"szl{b2}"])
            dts = dtp[:, c, d_ * 8:(d_ + 1) * 8]
            kb.tt("dve", A_[:], dts, a_bc[:, d_ * 8:(d_ + 1) * 8], mult, ["dtp", "a_bc"], ["A_"])
            kb.mm(pA[:, 0:8], tri[d_][:], A_[:], True, True, [f"tri{d_}", "A_"], ["pA"])
            kb.mm(pA[:, 8:16], onesf[:], A_[:], True, True, ["onesf", "A_"], ["pA"])
            kb.cp("dve", cs_sb[:], pA[:, 0:8], ["pA"], ["cs_sb"])
            kb.tr(pA[0:8, 128:256], cs_sb[:], identf[:], ["cs_sb", "identf"], ["pA"])
            kb.cp("dve", csT_sb[:], pA[0:8, 128:256], ["pA"], ["csT_sb"])
            for r in range(8):
                kb.mm(pCS[:, r, :], Esel[:, r, :], csT_sb[:], True, True, ["Esel", "csT_sb"], ["pCS"])
            kb.tt("dve", seg[:], pCS[:], cs_sb[:, :, None].to_broadcast([128, 8, 128]), sub, ["pCS", "cs_sb"], ["seg"])
            kb.ts("dve", seg[:], seg[:], 0.0, None, ALU.min, None, ["seg"], ["seg"])
            kb.act(seg[:], seg[:], AF.Exp, ["seg"], ["seg"])
            kb.act(ecs[:], pCS[0:64, :, :], AF.Exp, ["pCS"], ["ecs"])
            for g_ in range(2):
                kb.mm(pG[:, g_ * 128:(g_ + 1) * 128], bcT[b2][:, g_, :], bcT[b2][:, 2 + g_, :], True, True,
                      [f"bcT{b2}"], ["pG"])
            kb.tt("dve", GTm[:], pG[:, 0:256].rearrange("p (g i) -> p g i", g=2), tri[d_][:, None, :].to_broadcast([128, 2, 128]),
                  mult, ["pG", f"tri{d_}"], ["GTm"])
            kb.tt("dve", xdt[:], xtm[b2][:, 0:512].rearrange("p (r q) -> p r q", r=8), dts[:, :, None].to_broadcast([128, 8, 64]),
                  mult, [f"xtm{b2}", "dtp"], ["xdt"])
            kb.tt("dve", MT[:].rearrange("p (g r) i -> p g r i", g=2), seg[:].rearrange("p (g r) i -> p g r i", g=2),
                  GTm[:, :, None, :].to_broadcast([128, 2, 4, 128]), mult, ["seg", "GTm"], ["MT"])
            kb.tt("dve", Cdec[:].rearrange("p (g r) i -> p g r i", g=2), ecs[:].rearrange("p (g r) i -> p g r i", g=2),
                  bcT[b2][:, 2:4, None, :].to_broadcast([64, 2, 4, 128]), mult, ["ecs", f"bcT{b2}"], ["Cdec"])
            for r in range(8):
                kb.mm(pY[:, r, :], xdt[:, r, :], MT[:, r, :], True, False, ["xdt", "MT"], ["pY"])
                if d_ == 0:
                    kb.mm(pY[:, r, :], xtm[b2][:, r * 64:(r + 1) * 64], diagD[:, r, :], False, False, [f"xtm{b2}", "diagD"], ["pY"])
                kb.mm(pY[:, r, :], hbf[:, r, :], Cdec[:, r, :], False, True, ["hbf", "Cdec"], ["pY"])
            kb.tt("dve", wex[:], pA[:, 8:16], cs_sb[:], sub, ["pA", "cs_sb"], ["wex"])
            kb.act(wex[:], wex[:], AF.Exp, ["wex"], ["wex"])
            kb.tt("dve", xw[:], xdt[:], wex[:, :, None].to_broadcast([128, 8, 64]), mult, ["xdt", "wex"], ["xw"])
            for g_ in range(2):
                kb.mm(pB[:, g_ * 4:(g_ + 1) * 4, :], xtm[b2][:, 512 + g_ * 64:512 + (g_ + 1) * 64],
                      xw[:, g_ * 4:(g_ + 1) * 4, :], True, True, [f"xtm{b2}", "xw"], ["pB"])
            kb.act(dec[:], pA[0:64, 8:16], AF.Exp, ["pA"], ["dec"])
            kb.tt("dve", hst[:], hst[:], dec[:, :, None].to_broadcast([64, 8, 64]), mult, ["hst", "dec"], ["hst"])
            kb.tt("dve", hst[:], hst[:], pB[:], add, ["hst", "pB"], ["hst"])
            kb.cp("act", hbf[:], hst[:], ["hst"], ["hbf"])
            if d_ == 0:
                kb.cp("act", ysb[b2][:], pY[:], ["pY"], [f"ysb{b2}"])
                kb.dma("sp", yf_v[:, :, cs0:cs1], ysb[b2][:], f"ysb{b2}", [f"ysb{b2}"], [])
            else:
                kb.tt("dve", yg[:], pY[:], yfl[b2][:], add, ["pY", f"yfl{b2}"], ["yg"])
                kb.tt("dve", yg[:], yg[:], szl[b2][:], mult, ["yg", f"szl{b2}"], ["yg"])
                kb.act(sqy[:], yg[:], AF.Square, ["yg"], ["sqy"])
                for r in range(8):
                    kb.mm(pC[:, 0:128], onesf[0:64, 0:64], sqy[:, r, :], r == 0, r == 7, ["onesf", "sqy"], ["pC"])
                kb.act(rsy[:], pC[:, 0:128], AF.Ln, ["pC", "c_eps"], ["rsy"], scale=1.0 / 512, bias=c_eps[0:64, :])
                kb.act(rsy[:], rsy[:], AF.Exp, ["rsy"], ["rsy"], scale=-0.5)
                kb.tt("dve", ynb[b2][:], yg[:], rsy[:, None, :].to_broadcast([64, 8, 128]), mult, ["yg", "rsy"], [f"ynb{b2}"])
                kb.dma("sp", yn_v[:, :, cs0:cs1], ynb[b2][:], f"ynb{b2}", [f"ynb{b2}"], [])
        kb.barrier()
    kb.pop()
    if stop_after == "P2":
        return nc, kb

    S_attn = scr("S_attn", [512, S], BF16)
    SCALE = 96.0 ** -0.5
    kb.push()
    KT = kb.sb("KT", [128, 8, S], BF16)
    Vaug = kb.sb("Vaug", [128, NT, 8, 65], BF16)
    kmax = kb.sb("kmax", [128, 1], F32)
    kb.push()
    kvw = vec_pk("kvw")
    wkv_st = kb.sb("wkv_st", [128, 1024], F32)
    wkv = kb.sb("wkv", [128, 8, 128], BF16)
    kb.dma("sp", wkv_st[:], D["w_ukv"], "wkv_st", [], ["wkv_st"])
    kb.ts("dve", wkv[:].rearrange("p h d -> p (h d)"), wkv_st[:], kvw[:, 0:1], None, mult, None, ["wkv_st", "vecs"], ["wkv"])
    kvl = [kb.sb(f"kvl{i}", [128, GS], BF16) for i in range(2)]
    kpad = [kb.sb(f"kpad{i}", [128, 96], BF16) for i in range(2)]
    krs = [kb.sb(f"krs{i}", [128, 128], BF16) for i in range(2)]
    sqk = [kb.sb(f"sqk{i}", [96, GS], BF16) for i in range(2)]
    tmx = kb.sb("tmx", [128, 1], F32)
    pK = [kb.ps(f"pK{i}", [128, 512], F32) for i in range(2)]
    pV = [kb.ps(f"pV{i}", [128, 512], F32) for i in range(2)]
    pR = [kb.ps(f"pR{i}", [128, 1024], BF16) for i in range(2)]
    pN = [kb.ps(f"pN{i}", [128, 512], F32) for i in range(2)]
    kb.memset("pool", Vaug[:, :, :, 64:65], 1.0, ["V"])
    kb.memset("pool", kmax[:], 0.0, ["kmax"])
    for i in range(2):
        kb.memset("pool", kpad[i][:], 0.0, [f"kpad{i}"])
    for g in range(NG):
        gp = g % 2
        gs0, gs1 = g * GS, (g + 1) * GS
        kb.dma("sp", kvl[gp][:], S_kvn[:, gs0:gs1], f"kvl{gp}", [], [f"kvl{gp}"])
        for t in range(TG):
            tt_ = g * TG + t
            pp = tt_ % 2
            kb.cp("pool", kpad[pp][:, 64:96], krot[:, tt_, :], ["krot"], [f"kpad{pp}"])
            kb.tr(pR[pp][0:96, 0:128], kpad[pp][:], ident[:], [f"kpad{pp}", "ident"], [f"pR{pp}"])
            kb.cp("act", krs[pp][64:96, :], pR[pp][64:96, 0:128], [f"pR{pp}"], [f"krs{pp}"])
            kb.cp("pool", KT[64:96, :, tt_ * 128:(tt_ + 1) * 128], krs[pp][64:96, None, :].to_broadcast([32, 8, 128]),
                  [f"krs{pp}"], [f"KTr{g}"])
            kb.mm(pV[pp][:], kvl[gp][:, t * 128:(t + 1) * 128], wkv[:, :, 64:128], True, True, [f"kvl{gp}", "wkv"], [f"pV{pp}"])
            kb.cp("dve", Vaug[:, tt_, :, 0:64], pV[pp][:].rearrange("p (h d) -> p h d", h=8), [f"pV{pp}"], ["V"])
        for h in range(8):
            hp = h % 2
            kb.mm(pK[hp][0:64, :GS], wkv[:, h, 0:64], kvl[gp][:], True, True, ["wkv", f"kvl{gp}"], [f"pK{hp}"])
            kb.cp("act" if h % 2 else "dve", KT[0:64, h, gs0:gs1], pK[hp][0:64, :GS], [f"pK{hp}"], [f"KTn{g}_{h}"])
            kb.act(sqk[hp][:], KT[0:96, h, gs0:gs1], AF.Square, [f"KTn{g}_{h}", f"KTr{g}"], [f"sqk{hp}"])
            kb.mm(pN[hp][:, :GS], onesb[0:96, :], sqk[hp][:], True, True, ["onesb", f"sqk{hp}"], [f"pN{hp}"])
            Sd.op("dve", lambda e, hp=hp: e.tensor_reduce(out=tmx[:], in_=pN[hp][:, :GS], axis=AX.X, op=ALU.max),
                  [f"pN{hp}"], ["tmx"])
            kb.tt("dve", kmax[:], kmax[:], tmx[:], ALU.max, ["kmax", "tmx"], ["kmax"])
    kb.barrier()
    kb.pop()
    if stop_after == "P3":
        return nc, kb

    kb.push()
    qw = vec_pk("qw")
    wuq_st = kb.sb("wuq_st", [128, 2, 768], F32)
    wuq = kb.sb("wuq", [128, 2, 768], BF16)
    kb.dma("sp", wuq_st[:], D["w_uq"].rearrange("(c p) n -> p c n", p=128), "wuq_st", [], ["wuq_st"])
    for c in range(2):
        kb.ts("dve", wuq[:, c, :], wuq_st[:, c, :], qw[:, c:c + 1], None, mult, None, ["wuq_st", "vecs"], ["wuq"])
    sel65 = kb.sb("sel65", [65, 64], F32)
    kb.memset("pool", sel65[:], 0.0, ["sel65"])
    Sd.op("pool", lambda e: e.affine_select(out=sel65[:], in_=sel65[:], pattern=[[0, 64]], compare_op=ALU.not_equal,
                                            fill=1.0, base=-64, channel_multiplier=1), ["sel65"], ["sel65"])
    cql = [kb.sb(f"cql{i}", [128, 2, GS], BF16) for i in range(2)]
    qtm = kb.sb("qtm", [128, 8, 96], F32)
    qrt = [kb.sb(f"qrt{i}", [128, 16], F32) for i in range(4)]
    qrot = [kb.sb(f"qrot{i}", [128, 8, 96], BF16) for i in range(TG)]
    qra = [kb.sb(f"qra{i}", [128, 8, 16], F32) for i in range(4)]
    qT = [kb.sb(f"qT{i}", [96, 8, GS], BF16) for i in range(2)]
    sqq = [kb.sb(f"sqq{i}", [96, GS], BF16) for i in range(2)]
    qmax = kb.sb("qmax", [128, 1], F32)
    tmq = kb.sb("tmq", [128, 1], F32)
    bias_g = [kb.sb(f"bias_g{i}", [128, 1], F32) for i in range(2)]
    NPT = 4
    pt = [kb.sb(f"pt{i}", [128, GS], BF16) for i in range(NPT)]
    osb = [kb.sb(f"osb{i}", [65, GS], F32) for i in range(2)]
    rden = [kb.sb(f"rden{i}", [64, GS], F32) for i in range(2)]
    ao = kb.sb("ao", [64, 8, GS], F32)
    sqa = [kb.sb(f"sqa{i}", [64, GS], F32) for i in range(2)]
    rsa = kb.sb("rsa", [64, GS], F32)
    aon = [kb.sb(f"aon{i}", [64, 8, GS], BF16) for i in range(2)]
    psc = [kb.ps(f"psc{i}", [128, 512], F32) for i in range(3)]
    po = [kb.ps(f"po{i}", [128, 512], F32) for i in range(2)]
    pq = [kb.ps(f"pq{i}", [128, 512], F32) for i in range(2)]
    pQT = kb.ps("pQT", [128, 8, 128], BF16)
    def att_prepA(g):
        gp = g % 2
        gs0, gs1 = g * GS, (g + 1) * GS
        kb.dma("sp", cql[gp][:], S_cqn.rearrange("(c p) s -> p c s", p=128)[:, :, gs0:gs1], f"cql{gp}", [], [f"cql{gp}"])
        for t in range(TG):
            tt_ = g * TG + t
            rp = t
            for c in range(2):
                kb.mm(pq[0][:, 0:512], cql[gp][:, c, t * 128:(t + 1) * 128], wuq[:, c, 0:512], c == 0, c == 1,
                      [f"cql{gp}", "wuq"], ["pq0"])
            for c in range(2):
                kb.mm(pq[1][:, 0:256], cql[gp][:, c, t * 128:(t + 1) * 128], wuq[:, c, 512:768], c == 0, c == 1,
                      [f"cql{gp}", "wuq"], ["pq1"])
            qflat = qtm[:].rearrange("p h d -> p (h d)")
            kb.cp("dve", qflat[:, 0:512], pq[0][:, 0:512], ["pq0"], ["qtm"])
            kb.cp("dve", qflat[:, 512:768], pq[1][:, 0:256], ["pq1"], ["qtm"])
            cb_ = cosT[:, tt_:tt_ + 1, :].to_broadcast([128, 8, 16])
            sb_ = sinT[:, tt_:tt_ + 1, :].to_broadcast([128, 8, 16])
            q1, q2 = qtm[:, :, 64:80], qtm[:, :, 80:96]
            kb.cp("pool", qrot[rp][:, :, 0:64], qtm[:, :, 0:64], ["qtm"], [f"qrot{rp}"])
            kb.tt("pool", qra[0][:], q1, cb_, mult, ["qtm", "cosT"], ["qra0"])
            kb.tt("dve", qra[1][:], q2, sb_, mult, ["qtm", "sinT"], ["qra1"])
            kb.tt("pool", qrot[rp][:, :, 64:80], qra[0][:], qra[1][:], sub, ["qra0", "qra1"], [f"qrot{rp}"])
            kb.tt("pool", qra[2][:], q2, cb_, mult, ["qtm", "cosT"], ["qra2"])
            kb.tt("dve", qra[3][:], q1, sb_, mult, ["qtm", "sinT"], ["qra3"])
            kb.tt("dve", qrot[rp][:, :, 80:96], qra[2][:], qra[3][:], add, ["qra2", "qra3"], [f"qrot{rp}"])

    def att_prepB(g):
        gp = g % 2
        for t in range(TG):
            for h in range(8):
                kb.tr(pQT[0:96, h, :], qrot[t][:, h, :], ident[:], [f"qrot{t}", "ident"], ["pQT"])
            kb.cp("dve", qT[gp][:, :, t * 128:(t + 1) * 128], pQT[0:96, :, :], ["pQT"], [f"qT{gp}_{t}"])
        qTb = [f"qT{gp}_{t}" for t in range(TG)]
        kb.memset("pool", qmax[:], 0.0, ["qmax"])
        for h in range(8):
            hp = h % 2
            kb.tt("dve", sqq[hp][:], qT[gp][:, h, :], qT[gp][:, h, :], mult, qTb, [f"sqq{hp}"])
            kb.mm(pq[hp][:, :GS], onesb[0:96, :], sqq[hp][:], True, True, ["onesb", f"sqq{hp}"], [f"pq{hp}"])
            Sd.op("dve", lambda e, hp=hp: e.tensor_reduce(out=tmq[:], in_=pq[hp][:, :GS], axis=AX.X, op=ALU.max),
                  [f"pq{hp}"], ["tmq"])
            kb.tt("dve", qmax[:], qmax[:], tmq[:], ALU.max, ["qmax", "tmq"], ["qmax"])
        bg = bias_g[gp]
        kb.tt("dve", bg[:], qmax[:], kmax[:], add, ["qmax", "kmax"], [f"bias_g{gp}"])
        kb.ts("dve", bg[:], bg[:], -SCALE * 1.02 * 0.5, None, mult, None, [f"bias_g{gp}"], [f"bias_g{gp}"])

    def att_tail(g):
        gp = g % 2
        gs0, gs1 = g * GS, (g + 1) * GS
        for h in range(8):
            hp = h % 2
            kb.act(sqa[hp][:], ao[:, h, :], AF.Square, [f"ao{h}"], [f"sqa{hp}"])
            kb.mm(pq[0][0:64, :GS], onesf[0:64, 0:64], sqa[hp][:], h == 0, h == 7, ["onesf", f"sqa{hp}"], ["pq0"])
        kb.act(rsa[:], pq[0][0:64, :GS], AF.Ln, ["pq0", "c_eps"], ["rsa"], scale=1.0 / 512, bias=c_eps[0:64, :])
        kb.act(rsa[:], rsa[:], AF.Exp, ["rsa"], ["rsa"], scale=-0.5)
        for h in range(8):
            kb.tt("pool" if h % 2 else "dve", aon[gp][:, h, :], ao[:, h, :], rsa[:], mult, [f"ao{h}", "rsa"], [f"aon{gp}_{h}"])
        kb.dma("sp", S_attn.rearrange("(h p) s -> p h s", p=64)[:, :, gs0:gs1], aon[gp][:], f"aon{gp}",
               [f"aon{gp}_{h}" for h in range(8)], [])


    def att_main(g):
        gp = g % 2
        gs0, gs1 = g * GS, (g + 1) * GS
        qTb = [f"qT{gp}_{t}" for t in range(TG)]
        bg = bias_g[gp]
        units = [(h, kbk) for h in range(8) for kbk in range(NT)]
        LOOK = 2
        NPS = 3

        def emit_qk(u):
            h, kbk = units[u]
            si = u % NPS
            kb.mm(psc[si][:, :GS], KT[0:96, h, kbk * 128:(kbk + 1) * 128], qT[gp][:, h, :], True, True,
                  ["KT"] + qTb, [f"psc{si}"])

        for u in range(min(LOOK, len(units))):
            emit_qk(u)
        for u, (h, kbk) in enumerate(units):
            hp = h % 2
            if u == NT // 2 and g > 0:
                att_tail(g - 1)
            if u == 2 * NT and g + 1 < NG:
                att_prepA(g + 1)
            si = u % NPS
            pi = u % NPT
            if u + LOOK < len(units):
                emit_qk(u + LOOK)
            kb.act(pt[pi][:], psc[si][:, :GS], AF.Exp, [f"psc{si}", f"bias_g{gp}"], [f"pt{pi}"], scale=SCALE, bias=bg[:])
            kb.mm(po[hp][0:65, :GS], Vaug[:, kbk, h, :], pt[pi][:], kbk == 0, kbk == NT - 1, ["V", f"pt{pi}"], [f"po{hp}"])
            if kbk == NT - 1:
                kb.cp("dve", osb[hp][:], po[hp][0:65, :GS], [f"po{hp}"], [f"osb{hp}"])
                kb.mm(pq[hp][0:64, :GS], sel65[:], osb[hp][:], True, True, ["sel65", f"osb{hp}"], [f"pq{hp}"])
                Sd.op("dve", lambda e, hp=hp: e.reciprocal(out=rden[hp][:], in_=pq[hp][0:64, :GS]), [f"pq{hp}"], [f"rden{hp}"])
                kb.tt("pool", ao[:, h, :], osb[hp][0:64, :], rden[hp][:], mult, [f"osb{hp}", f"rden{hp}"], [f"ao{h}"])
    att_prepA(0)
    att_prepB(0)
    for g in range(NG):
        att_main(g)
        if g + 1 < NG:
            att_prepB(g + 1)
    att_tail(NG - 1)
    kb.barrier()
    kb.pop()
    kb.pop()
    if stop_after == "P4":
        return nc, kb

    TS = min(512, S)
    NSUB = TS // 128
    NTILES = (2 * S) // TS + NEXP
    NSLOT = NTILES * TS
    BIG = float(1 << 22)
    S_x1 = scr("S_x1", [S, 1024], F32)
    S_h2 = scr("S_h2", [S, 1024], BF16)
    S_slot = scr("S_slot", [NSLOT, 4], F32)
    S_ymoe = scr("S_ymoe", [2 * S, 1024], F32)
    S_tile = scr("S_tile", [2, 128, NTILES], I32)
    kb.push()
    ohall = kb.sb("ohall", [128, NT * 2, 32], BF16)
    posn = kb.sb("posn", [128, NT * 2], F32)
    wcomb = kb.sb("wcomb", [128, NT * 2], F32)
    run_bc = kb.sb("run_bc", [128, 32], F32)
    stri = kb.sb("stri", [128, 128], BF16)
    kb.memset("pool", run_bc[:], 0.0, ["run_bc"])
    kb.memset("pool", stri[:], 1.0, ["stri"])
    Sd.op("pool", lambda e: e.affine_select(out=stri[:], in_=stri[:], pattern=[[1, 128]], compare_op=ALU.is_gt, fill=0.0,
                                            base=0, channel_multiplier=-1), ["stri"], ["stri"])
    kb.push()
    aow = vecs64[:, 0:8]
    snw = vecs64[:, 8:16]
    wo = kb.sb("wo", [64, 16, 1024], BF16)
    wo_st = [kb.sb(f"wo_st{i}", [64, 1024], F32) for i in range(2)]
    for c in range(16):
        kb.dma("sp", wo_st[c % 2][:], D["w_o"][c * 64:(c + 1) * 64, :], f"wo_st{c % 2}", [], [f"wo_st{c % 2}"])
        sc_ = aow[:, c:c + 1] if c < 8 else snw[:, c - 8:c - 7]
        kb.ts("dve" if c % 2 else "pool", wo[:, c, :], wo_st[c % 2][:], sc_, None, mult, None, [f"wo_st{c % 2}", "vecs64"], [f"wo{c}"])
    wob = [f"wo{c}" for c in range(16)]
    fnw_bc = kb.sb("fnw_bc", [128, 1024], F32)
    kb.dma("sp", fnw_bc[:], D["ffn_norm_w"].partition_broadcast(128), "fnw_bc", [], ["fnw_bc"])
    wr = kb.sb("wr", [128, 8, 36], F32)
    kb.dma("sp", wr[:, :, 0:4], D["w_router_group"].rearrange("(k p) n -> p k n", p=128), "wr", [], ["wr"])
    kb.dma("sp", wr[:, :, 4:36], D["w_router_expert"].rearrange("(k p) n -> p k n", p=128), "wr", [], ["wr"])
    br_bc = kb.sb("br_bc", [128, 36], F32)
    kb.dma("sp", br_bc[:, 0:4], D["b_router_group"].partition_broadcast(128), "br_bc", [], ["br_bc"])
    kb.dma("sp", br_bc[:, 4:36], D["b_router_expert"].partition_broadcast(128), "br_bc", [], ["br_bc"])
    xl = [kb.sb(f"xl{i}", [128, 1024], F32) for i in range(2)]
    atl = [kb.sb(f"atl{i}", [64, 8, 128], BF16) for i in range(2)]
    ynl = [kb.sb(f"ynl{i}", [64, 8, 128], BF16) for i in range(2)]
    x1t = [kb.sb(f"x1t{i}", [128, 1024], F32) for i in range(2)]
    jk_2 = [kb.sb(f"jk_{i}", [128, 1024], BF16) for i in range(2)]
    st5 = [kb.sb(f"st5_{i}", [128, 1], F32) for i in range(2)]
    h2f_2 = [kb.sb(f"h2f_{i}", [128, 1024], F32) for i in range(2)]
    h2b = [kb.sb(f"h2b{i}", [128, 1024], BF16) for i in range(2)]
    h2T_2 = [kb.sb(f"h2T_{i}", [128, 8, 128], F32) for i in range(2)]
    rl_2 = [kb.sb(f"rl_{i}", [128, 36], F32) for i in range(2)]
    gmx_2 = [kb.sb(f"gmx_{i}", [128, 1], F32) for i in range(2)]
    ngm_2 = [kb.sb(f"ngm_{i}", [128, 1], F32) for i in range(2)]
    ohg_2 = [kb.sb(f"ohg_{i}", [128, 4], F32) for i in range(2)]
    eg_2 = [kb.sb(f"eg_{i}", [128, 4], F32) for i in range(2)]
    sume_2 = [kb.sb(f"sume_{i}", [128, 1], F32) for i in range(2)]
    gw_2 = [kb.sb(f"gw_{i}", [128, 1], F32) for i in range(2)]
    selt_2 = [kb.sb(f"selt_{i}", [128, 4, 8], F32) for i in range(2)]
    sel_2 = [kb.sb(f"sel_{i}", [128, 8], F32) for i in range(2)]
    m8_2 = [kb.sb(f"m8_{i}", [128, 8], F32) for i in range(2)]
    dd_2 = [kb.sb(f"dd_{i}", [128, 1], F32) for i in range(2)]
    w12_2 = [kb.sb(f"w12_{i}", [128, 2], F32) for i in range(2)]
    ohe_2 = [kb.sb(f"ohe_{i}", [128, 2, 8], F32) for i in range(2)]
    pmat_2 = [kb.sb(f"pmat_{i}", [128, 32], F32) for i in range(2)]
    ptmp_2 = [kb.sb(f"ptmp_{i}", [128, 32], F32) for i in range(2)]
    pmx = kb.ps("pmx", [128, 1024], F32)
    pH = kb.ps("pH", [128, 8, 128], F32)
    pL_2 = [kb.ps(f"pL_{i}", [128, 512], F32) for i in range(2)]
    pP = kb.ps("pP", [128, 512], F32)
    at_v = S_attn.rearrange("(h p) s -> p h s", p=64)
    def p5a_tile(tt_):
        yield
        b2 = tt_ % 2
        pL = pL_2[b2]
        yield
        jk = jk_2[b2]
        yield
        h2f = h2f_2[b2]
        yield
        h2T = h2T_2[b2]
        yield
        rl = rl_2[b2]
        yield
        gmx = gmx_2[b2]
        yield
        ngm = ngm_2[b2]
        yield
        ohg = ohg_2[b2]
        yield
        eg = eg_2[b2]
        yield
        sume = sume_2[b2]
        yield
        gw = gw_2[b2]
        yield
        selt = selt_2[b2]
        yield
        sel = sel_2[b2]
        yield
        m8 = m8_2[b2]
        yield
        dd = dd_2[b2]
        yield
        w12 = w12_2[b2]
        yield
        ohe = ohe_2[b2]
        yield
        pmat = pmat_2[b2]
        yield
        ptmp = ptmp_2[b2]
        yield
        ts0, ts1 = tt_ * 128, (tt_ + 1) * 128
        yield
        kb.dma("sp", xl[b2][:], D["x"][ts0:ts1, :], f"xl{b2}", [], [f"xl{b2}"])
        yield
        kb.dma("sp", atl[b2][:], at_v[:, :, ts0:ts1], f"atl{b2}", [], [f"atl{b2}"])
        yield
        kb.dma("sp", ynl[b2][:], yn_v[:, :, ts0:ts1], f"ynl{b2}", [], [f"ynl{b2}"])
        yield
        for half in range(2):
            for c in range(16):
                lhs = atl[b2][:, c, :] if c < 8 else ynl[b2][:, c - 8, :]
                kb.mm(pmx[:, half * 512:(half + 1) * 512], lhs, wo[:, c, half * 512:(half + 1) * 512], c == 0, c == 15,
                      [f"atl{b2}", f"ynl{b2}"] + wob, ["pmx"])
        yield
        kb.tt("dve", x1t[b2][:], pmx[:], xl[b2][:], add, ["pmx", f"xl{b2}"], [f"x1t{b2}"])
        yield
        kb.dma("sp", S_x1[ts0:ts1, :], x1t[b2][:], f"x1t{b2}", [f"x1t{b2}"], [])
        yield
        st = st5[b2]
        yield
        kb.act(jk[:], x1t[b2][:], AF.Square, [f"x1t{b2}"], [f"jk_{b2}", f"st5_{b2}"], accum_out=st[:])
        yield
        kb.ts("dve", st[:], st[:], 1.0 / 1024, EPS, mult, add, [f"st5_{b2}"], [f"st5_{b2}"])
        yield
        kb.act(st[:], st[:], AF.Ln, [f"st5_{b2}"], [f"st5_{b2}"])
        yield
        kb.act(st[:], st[:], AF.Exp, [f"st5_{b2}"], [f"st5_{b2}"], scale=-0.5)
        yield
        kb.stt(h2f[:], x1t[b2][:], st[:, 0:1], fnw_bc[:], mult, mult, [f"x1t{b2}", f"st5_{b2}", "fnw_bc"], [f"h2f_{b2}"])
        yield
        kb.cp("act", h2b[b2][:], h2f[:], [f"h2f_{b2}"], [f"h2b{b2}"])
        yield
        kb.dma("sp", S_h2[ts0:ts1, :], h2b[b2][:], f"h2b{b2}", [f"h2b{b2}"], [])
        yield
        for k in range(8):
            kb.tr(pH[:, k, :], h2f[:, k * 128:(k + 1) * 128], identf[:], [f"h2f_{b2}", "identf"], ["pH"])
        yield
        kb.cp("act", h2T[:], pH[:], ["pH"], [f"h2T_{b2}"])
        yield
        for k in range(8):
            kb.mm(pL[:, 0:36], h2T[:, k, :], wr[:, k, :], k == 0, k == 7, [f"h2T_{b2}", "wr"], [f"pL_{b2}"])
        yield
        kb.tt("dve", rl[:], pL[:, 0:36], br_bc[:], add, [f"pL_{b2}", "br_bc"], [f"rl_{b2}"])
        yield
        kb.reduce(gmx[:], rl[:, 0:4], ALU.max, [f"rl_{b2}"], [f"gmx_{b2}"])
        yield
        kb.ts("dve", ohg[:], rl[:, 0:4], gmx[:, 0:1], None, ALU.is_equal, None, [f"rl_{b2}", f"gmx_{b2}"], [f"ohg_{b2}"])
        yield
        kb.ts("dve", ngm[:], gmx[:], -1.0, None, mult, None, [f"gmx_{b2}"], [f"ngm_{b2}"])
        yield
        kb.act(eg[:], rl[:, 0:4], AF.Exp, [f"rl_{b2}", f"ngm_{b2}"], [f"eg_{b2}", f"sume_{b2}"], bias=ngm[:], accum_out=sume[:])
        yield
        kb.recip(gw[:], sume[:], [f"sume_{b2}"], [f"gw_{b2}"])
        yield
        kb.tt("dve", selt[:], rl[:, 4:36].rearrange("p (g e) -> p g e", g=4), ohg[:, :, None].to_broadcast([128, 4, 8]), mult,
              [f"rl_{b2}", f"ohg_{b2}"], [f"selt_{b2}"])
        yield
        kb.reduce(sel[:], selt[:].rearrange("p g e -> p e g"), ALU.add, [f"selt_{b2}"], [f"sel_{b2}"])
        yield
        kb.max8(m8[:], sel[:], [f"sel_{b2}"], [f"m8_{b2}"])
        yield
        kb.tt("dve", dd[:], m8[:, 1:2], m8[:, 0:1], sub, [f"m8_{b2}"], [f"dd_{b2}"])
        yield
        kb.act(dd[:], dd[:], AF.Exp, [f"dd_{b2}"], [f"dd_{b2}"])
        yield
        kb.ts("dve", w12[:, 0:1], dd[:], 1.0, None, add, None, [f"dd_{b2}"], [f"w12_{b2}"])
        yield
        kb.recip(w12[:, 0:1], w12[:, 0:1], [f"w12_{b2}"], [f"w12_{b2}"])
        yield
        kb.tt("dve", w12[:, 1:2], w12[:, 0:1], dd[:], mult, [f"w12_{b2}", f"dd_{b2}"], [f"w12_{b2}"])
        yield
        kb.ts("dve", wcomb[:, 2 * tt_:2 * tt_ + 2], w12[:], gw[:, 0:1], None, mult, None, [f"w12_{b2}", f"gw_{b2}"], ["wcomb"])
        yield
        for k in range(2):
            kb.ts("dve", ohe[:, k, :], sel[:], m8[:, k:k + 1], None, ALU.is_equal, None, [f"sel_{b2}", f"m8_{b2}"], [f"ohe_{b2}"])
        yield
        for k in range(2):
            u = 2 * tt_ + k
            kb.tt("dve", ohall[:, u, :].rearrange("p (g e) -> p g e", g=4), ohg[:, :, None].to_broadcast([128, 4, 8]),
                  ohe[:, k:k + 1, :].to_broadcast([128, 4, 8]), mult, [f"ohg_{b2}", f"ohe_{b2}"], [f"ohall{u}"])
            kb.mm(pP[:, 0:32], stri[:], ohall[:, u, :], True, True, ["stri", f"ohall{u}"], ["pP"])
            kb.mm(pP[:, 32:64], onesb[:], ohall[:, u, :], True, True, ["onesb", f"ohall{u}"], ["pP"])
            kb.tt("dve", pmat[:], pP[:, 0:32], run_bc[:], add, ["pP", "run_bc"], [f"pmat_{b2}"])
            kb.tt("dve", ptmp[:], pmat[:], ohall[:, u, :], mult, [f"pmat_{b2}", f"ohall{u}"], [f"ptmp_{b2}"])
            kb.reduce(posn[:, u:u + 1], ptmp[:], ALU.add, [f"ptmp_{b2}"], ["posn"])
            kb.tt("dve", run_bc[:], run_bc[:], pP[:, 32:64], add, ["run_bc", "pP"], ["run_bc"])

    LAG_ = int(os.environ.get("KLAG", "4"))
    for t0_ in range(0, NT, 2):
        gA_ = p5a_tile(t0_)
        gB_ = p5a_tile(t0_ + 1) if t0_ + 1 < NT else iter(())
        aliveA_, aliveB_, nA_ = True, True, 0
        while aliveA_ or aliveB_:
            if aliveA_:
                try:
                    next(gA_)
                    nA_ += 1
                except StopIteration:
                    aliveA_ = False
            if aliveB_ and (nA_ >= LAG_ or not aliveA_):
                try:
                    next(gB_)
                except StopIteration:
                    aliveB_ = False
    kb.barrier()
    kb.pop()
    if stop_after == "P5a":
        return nc, kb

    import math as _m
    LOG_TS = int(_m.log2(TS))
    kb.push()
    cntf = kb.sb("cntf", [128, 32], F32)
    cnti = kb.sb("cnti", [128, 32], I32)
    ntf = kb.sb("ntf", [128, 32], F32)
    ones32 = kb.sb("ones32", [128, 32], F32)
    incl = kb.sb("incl", [128, 32], F32)
    base_bc = kb.sb("base_bc", [128, 32], F32)
    tmpb = kb.sb("tmpb", [128, NT * 2, 32], F32)
    slotf = kb.sb("slotf", [128, NT * 2], F32)
    sloti = kb.sb("sloti", [128, NT * 2], I32)
    rowdat = kb.sb("rowdat", [128, NT * 2, 4], F32)
    rowdat_i = rowdat[:].bitcast(I32)
    NDF = NSLOT // 128
    dflt = kb.sb("dflt", [128, NDF, 4], F32)
    dflt_i = dflt[:].bitcast(I32)
    jidx = kb.sb("jidx", [128, NTILES], F32)
    pidx = kb.sb("pidx", [128, 1], F32)
    cmpt = kb.sb("cmpt", [128, NTILES, 32], F32)
    ej = kb.sb("ej", [128, NTILES], F32)
    wgf = kb.sb("wgf", [128, NTILES], F32)
    wgi = kb.sb("wgi", [128, NTILES], I32)
    wdi = kb.sb("wdi", [128, NTILES], I32)
    kb.ts("dve", cntf[:], run_bc[:], float(TS - 1), None, add, None, ["run_bc"], ["cntf"])
    kb.cp("dve", cnti[:], cntf[:], ["cntf"], ["cnti"])
    kb.ts("dve", cnti[:], cnti[:], LOG_TS, None, ALU.arith_shift_right, None, ["cnti"], ["cnti"])
    kb.cp("dve", ntf[:], cnti[:], ["cnti"], ["ntf"])
    kb.memset("pool", ones32[:], 1.0, ["ones32"])
    Sd.op("dve", lambda e: e.tensor_tensor_scan(out=incl[:], data0=ones32[:], data1=ntf[:], initial=0.0, op0=mult, op1=add),
          ["ones32", "ntf"], ["incl"])
    kb.tt("dve", base_bc[:], incl[:], ntf[:], sub, ["incl", "ntf"], ["base_bc"])
    kb.ts("dve", base_bc[:], base_bc[:], float(TS), None, mult, None, ["base_bc"], ["base_bc"])
    kb.tt("pool", tmpb[:], ohall[:], base_bc[:, None, :].to_broadcast([128, NT * 2, 32]), mult,
          [f"ohall{u}" for u in range(NT * 2)] + ["base_bc"], ["tmpb"])
    Sd.op("dve", lambda e: e.tensor_reduce(out=slotf[:], in_=tmpb[:], axis=AX.X, op=ALU.add), ["tmpb"], ["slotf"])
    kb.tt("dve", slotf[:], slotf[:], posn[:], add, ["slotf", "posn"], ["slotf"])
    kb.cp("dve", sloti[:], slotf[:], ["slotf"], ["sloti"])
    kb.memset("pool", rowdat[:], 0.0, ["rowdat"])
    Sd.op("pool", lambda e: e.iota(rowdat_i[:, :, 0].rearrange("p (t k) -> p t k", k=2), pattern=[[128, NT], [0, 2]], base=0,
                                   channel_multiplier=1), ["rowdat"], ["rowdat"])
    Sd.op("pool", lambda e: e.iota(rowdat_i[:, :, 2].rearrange("p (t k) -> p t k", k=2), pattern=[[128, NT], [S, 2]], base=0,
                                   channel_multiplier=1), ["rowdat"], ["rowdat"])
    kb.cp("pool", rowdat[:, :, 1], wcomb[:], ["wcomb", "rowdat"], ["rowdat"])
    kb.memset("pool", dflt[:], 0.0, ["dflt"])
    kb.memset("pool", dflt_i[:, :, 0:1], 1 << 22, ["dflt"])
    kb.memset("pool", dflt_i[:, :, 2:3], 1 << 22, ["dflt"])
    kb.dma("sp", S_slot.rearrange("(n p) c -> p n c", p=128), dflt[:], "dflt", ["dflt"], ["S_slot_init"])
    for u in range(NT * 2):
        Sd.dma("pool", lambda e, u=u: e.indirect_dma_start(
            out=S_slot, out_offset=bass.IndirectOffsetOnAxis(ap=sloti[:, u:u + 1], axis=0), in_=rowdat[:, u, :], in_offset=None,
            bounds_check=BC(e, NSLOT - 1), oob_is_err=False), "slotsc", ["S_slot_init", "sloti", "rowdat"], [])
    Sd.op("pool", lambda e: e.iota(jidx[:], pattern=[[1, NTILES]], base=0, channel_multiplier=0,
                                   allow_small_or_imprecise_dtypes=True), [], ["jidx"])
    Sd.op("pool", lambda e: e.iota(pidx[:], pattern=[[0, 1]], base=0, channel_multiplier=1,
                                   allow_small_or_imprecise_dtypes=True), [], ["pidx"])
    kb.tt("dve", cmpt[:], incl[:, None, :].to_broadcast([128, NTILES, 32]), jidx[:, :, None].to_broadcast([128, NTILES, 32]),
          ALU.is_le, ["incl", "jidx"], ["cmpt"])
    Sd.op("dve", lambda e: e.tensor_reduce(out=ej[:], in_=cmpt[:], axis=AX.X, op=ALU.add), ["cmpt"], ["ej"])
    kb.ts("dve", wgf[:], ej[:], 128.0, pidx[:, 0:1], mult, add, ["ej", "pidx"], ["wgf"])
    kb.cp("dve", wgi[:], wgf[:], ["wgf"], ["wgi"])
    kb.barrier()
    if stop_after == "P5b":
        return nc, kb

    sl = [kb.sb(f"sl{i}", [128, NSUB * 4], F32) for i in range(2)]
    wg = [kb.sb(f"wg{i}", [128, 8, 256], BF16) for i in range(2)]
    wu = [kb.sb(f"wu{i}", [128, 8, 256], BF16) for i in range(2)]
    wd = [kb.sb(f"wd{i}", [128, 2, 1024], BF16) for i in range(2)]
    hg = [kb.sb(f"hg{i}", [128, 1024], BF16) for i in range(2 * NSUB)]
    hTt = [kb.sb(f"hTt{i}", [128, 8, TS], BF16) for i in range(2)]
    sg = [kb.sb(f"sg{i}", [128, TS], F32) for i in range(2)]
    heT = [kb.sb(f"heT{i}", [128, 2, TS], BF16) for i in range(2)]
    ysc = [kb.sb(f"ysc{i}", [128, 1024], F32) for i in range(2)]
    pHT = kb.ps("pHT", [128, 8, 128], BF16)
    pgu = [kb.ps(f"pgu{i}", [128, 512], F32) for i in range(4)]
    py = kb.ps("py", [128, 1024], F32)
    for i in range(2):
        kb.memset("pool", wg[i][:], 0.0, [f"wg{i}"])
        kb.memset("pool", wu[i][:], 0.0, [f"wu{i}"])
        kb.memset("pool", wd[i][:], 0.0, [f"wd{i}"])
        for q_ in range(NSUB):
            kb.memset("pool", hg[i * NSUB + q_][:], 0.0, [f"hg{i * NSUB + q_}"])
    def moe_loads(j):
        jp = j % 2
        kb.dma("sp", sl[jp][:].rearrange("p (n c) -> p n c", c=4), S_slot[j * TS:(j + 1) * TS, :].rearrange("(n p) c -> p n c", p=128),
               f"sl{jp}", [], [f"sl{jp}"])
        sl_i = sl[jp][:].bitcast(I32)
        for wt, wname, a_ in ((wg, "w_exp_gate", 8), (wu, "w_exp_up", 8), (wd, "w_exp_down", 2)):
            Sd.dma("pool", lambda e, j=j, jp=jp, wt=wt, wname=wname, a_=a_: e.indirect_dma_start(
                out=wt[jp][:].rearrange("p k c -> p (k c)"), out_offset=None,
                in_=D[wname].rearrange("(r a) c -> r (a c)", a=a_),
                in_offset=bass.IndirectOffsetOnAxis(ap=wgi[:, j:j + 1], axis=0),
                bounds_check=BC(e, 32 * 128 - 1), oob_is_err=False), f"{wname}{jp}", ["wgi"],
                [{"w_exp_gate": "wg", "w_exp_up": "wu", "w_exp_down": "wd"}[wname] + str(jp)])
        for sb_ in range(NSUB):
            hgt = hg[jp * NSUB + sb_]
            Sd.dma("pool", lambda e, sb_=sb_, hgt=hgt, sl_i=sl_i: e.indirect_dma_start(
                out=hgt[:], out_offset=None, in_=S_h2, in_offset=bass.IndirectOffsetOnAxis(ap=sl_i[:, sb_ * 4:sb_ * 4 + 1], axis=0),
                bounds_check=BC(e, S - 1), oob_is_err=False), f"hg{jp * NSUB + sb_}", [f"sl{jp}"], [f"hg{jp * NSUB + sb_}"])

    def moe_compute(j):
        jp = j % 2
        sl_i = sl[jp][:].bitcast(I32)
        for sb_ in range(NSUB):
            hi_ = jp * NSUB + sb_
            for k in range(8):
                kb.tr(pHT[:, k, :], hg[hi_][:].rearrange("p (j a) -> p a j", a=8)[:, k, :], ident[:], [f"hg{hi_}", "ident"], ["pHT"])
            kb.cp("act" if sb_ % 2 else "dve", hTt[jp][:, :, sb_ * 128:(sb_ + 1) * 128], pHT[:], ["pHT"], [f"hTt{jp}_{sb_}"])
        hb_ = [f"hTt{jp}_{sb_}" for sb_ in range(NSUB)]
        for m in range(2):
            pg_, pu_ = pgu[2 * m], pgu[2 * m + 1]
            for kc in range(8):
                kb.mm(pg_[:, :TS], wg[jp][:].rearrange("p k (j a) -> p k a j", a=2)[:, kc, m, :], hTt[jp][:, kc, :], kc == 0, kc == 7,
                      [f"wg{jp}"] + hb_, [f"pgu{2 * m}"])
            for kc in range(8):
                kb.mm(pu_[:, :TS], wu[jp][:].rearrange("p k (j a) -> p k a j", a=2)[:, kc, m, :], hTt[jp][:, kc, :], kc == 0, kc == 7,
                      [f"wu{jp}"] + hb_, [f"pgu{2 * m + 1}"])
            kb.act(sg[m][:], pg_[:, :TS], AF.Silu, [f"pgu{2 * m}"], [f"sg{m}"])
            kb.tt("dve", heT[jp][:, m, :], sg[m][:], pu_[:, :TS], mult, [f"sg{m}", f"pgu{2 * m + 1}"], [f"heT{jp}_{m}"])
        for sb_ in range(NSUB):
            s2 = sb_ % 2
            for half in range(2):
                for m in range(2):
                    kb.mm(py[:, half * 512:(half + 1) * 512], heT[jp][:, m, sb_ * 128:(sb_ + 1) * 128],
                          wd[jp][:, m, half * 512:(half + 1) * 512], m == 0, m == 1,
                          [f"heT{jp}_0", f"heT{jp}_1", f"wd{jp}"], ["py"])
            kb.act(ysc[s2][:], py[:], AF.Copy, ["py", f"sl{jp}"], [f"ysc{s2}"], scale=sl[jp][:, sb_ * 4 + 1:sb_ * 4 + 2])
            Sd.dma("pool", lambda e, sb_=sb_, s2=s2, sl_i=sl_i: e.indirect_dma_start(
                out=S_ymoe, out_offset=bass.IndirectOffsetOnAxis(ap=sl_i[:, sb_ * 4 + 2:sb_ * 4 + 3], axis=0), in_=ysc[s2][:], in_offset=None,
                bounds_check=BC(e, 2 * S - 1), oob_is_err=False), f"ysc{s2}", [f"ysc{s2}", f"sl{jp}"],
                ["S_ymoe"] if serial_scatter else [])

    moe_loads(0)
    for j in range(NTILES):
        if j + 1 < NTILES:
            moe_loads(j + 1)
        moe_compute(j)
    kb.barrier()
    kb.pop()
    kb.pop()
    if stop_after == "P5c":
        return nc, kb

    kb.push()
    pnw = vec_pk("pnw")
    wpg = kb.sb("wpg", [128, 8, 1024], BF16)
    wpg_st = [kb.sb(f"wpg_st{i}", [128, 1024], F32) for i in range(2)]
    for k in range(8):
        kb.dma("sp", wpg_st[k % 2][:], D["w_ple_gate"][k * 128:(k + 1) * 128, :], f"wpg_st{k % 2}", [], [f"wpg_st{k % 2}"])
        kb.ts("dve" if k % 2 else "pool", wpg[:, k, :], wpg_st[k % 2][:], pnw[:, k:k + 1], None, mult, None,
              [f"wpg_st{k % 2}", "vecs"], [f"wpg{k}"])
    wpgb = [f"wpg{k}" for k in range(8)]
    wpp = kb.sb("wpp", [128, 2, 1024], BF16)
    Sd.dma("pool", lambda e: e.dma_start(out=wpp[:], in_=D["w_ple_proj"].rearrange("(c p) n -> p c n", p=128)), "wpp", [], ["wpp"])
    bpg_bc = kb.sb("bpg_bc", [128, 1024], F32)
    kb.dma("sp", bpg_bc[:], D["b_ple_gate"].partition_broadcast(128), "bpg_bc", [], ["bpg_bc"])
    ppw_bc = kb.sb("ppw_bc", [128, 1024], F32)
    kb.dma("sp", ppw_bc[:], D["ple_post_norm_w"].partition_broadcast(128), "ppw_bc", [], ["ppw_bc"])
    fin_bc = kb.sb("fin_bc", [128, 1024], F32)
    kb.dma("sp", fin_bc[:], D["final_norm_w"].partition_broadcast(128), "fin_bc", [], ["fin_bc"])
    x1l = [kb.sb(f"x1l{i}", [128, 1024], F32) for i in range(2)]
    y0l = [kb.sb(f"y0l{i}", [128, 1024], F32) for i in range(2)]
    y1l = [kb.sb(f"y1l{i}", [128, 1024], F32) for i in range(2)]
    pl = [kb.sb(f"pl{i}", [128, 256], F32) for i in range(2)]
    plb_2 = [kb.sb(f"plb_{i}", [128, 256], BF16) for i in range(2)]
    pTs_2 = [kb.sb(f"pTs_{i}", [128, 2, 128], BF16) for i in range(2)]
    x2_2 = [kb.sb(f"x2_{i}", [128, 1024], F32) for i in range(2)]
    jk2_2 = [kb.sb(f"jk2_{i}", [128, 1024], BF16) for i in range(2)]
    s6_2 = [[kb.sb(f"s6_{i}_{q}", [128, 1], F32) for i in range(3)] for q in range(2)]
    n3b_2 = [kb.sb(f"n3b_{i}", [128, 1024], BF16) for i in range(2)]
    n3T_2 = [kb.sb(f"n3T_{i}", [128, 8, 128], BF16) for i in range(2)]
    gate_2 = [kb.sb(f"gate_{i}", [128, 1024], F32) for i in range(2)]
    ple_2 = [kb.sb(f"ple_{i}", [128, 1024], F32) for i in range(2)]
    x3_2 = [kb.sb(f"x3_{i}", [128, 1024], F32) for i in range(2)]
    ot = [kb.sb(f"ot{i}", [128, 1024], F32) for i in range(2)]
    pGt = kb.ps("pGt", [128, 1024], F32)
    pPp = kb.ps("pPp", [128, 1024], F32)
    pN3_2 = [kb.ps(f"pN3_{i}", [128, 8, 128], BF16) for i in range(2)]
    pPT_2 = [kb.ps(f"pPT_{i}", [128, 8, 128], BF16) for i in range(2)]

    def rstd_of(src_ap, src_bufs, st, stname, junk_ap, junkname):
        kb.act(junk_ap, src_ap, AF.Square, src_bufs, [junkname, stname], accum_out=st[:])
        kb.ts("dve", st[:], st[:], 1.0 / 1024, EPS, mult, add, [stname], [stname])
        kb.act(st[:], st[:], AF.Ln, [stname], [stname])
        kb.act(st[:], st[:], AF.Exp, [stname], [stname], scale=-0.5)

    def p5d_tile(tt_):
        yield
        b2 = tt_ % 2
        pN3 = pN3_2[b2]
        pPT = pPT_2[b2]
        yield
        plb = plb_2[b2]
        yield
        pTs = pTs_2[b2]
        yield
        x2 = x2_2[b2]
        yield
        jk2 = jk2_2[b2]
        yield
        n3b = n3b_2[b2]
        yield
        n3T = n3T_2[b2]
        yield
        gate = gate_2[b2]
        yield
        ple = ple_2[b2]
        yield
        x3 = x3_2[b2]
        yield
        s6 = s6_2[b2]
        yield
        ts0, ts1 = tt_ * 128, (tt_ + 1) * 128
        yield
        kb.dma("sp", x1l[b2][:], S_x1[ts0:ts1, :], f"x1l{b2}", [], [f"x1l{b2}"])
        yield
        kb.dma("sp", y0l[b2][:], S_ymoe[ts0:ts1, :], f"y0l{b2}", [], [f"y0l{b2}"])
        yield
        kb.dma("sp", y1l[b2][:], S_ymoe[S + ts0:S + ts1, :], f"y1l{b2}", [], [f"y1l{b2}"])
        yield
        kb.dma("sp", pl[b2][:], D["p"][ts0:ts1, :], f"pl{b2}", [], [f"pl{b2}"])
        yield
        kb.tt("dve", x2[:], x1l[b2][:], y0l[b2][:], add, [f"x1l{b2}", f"y0l{b2}"], [f"x2_{b2}"])
        yield
        kb.tt("dve", x2[:], x2[:], y1l[b2][:], add, [f"x2_{b2}", f"y1l{b2}"], [f"x2_{b2}"])
        yield
        rstd_of(x2[:], [f"x2_{b2}"], s6[0], f"s6_0_{b2}", jk2[:], f"jk2_{b2}")
        yield
        kb.ts("dve", n3b[:], x2[:], s6[0][:, 0:1], None, mult, None, [f"x2_{b2}", f"s6_0_{b2}"], [f"n3b_{b2}"])
        yield
        for k in range(8):
            kb.tr(pN3[:, k, :], n3b[:, k * 128:(k + 1) * 128], ident[:], [f"n3b_{b2}", "ident"], [f"pN3_{b2}"])
        yield
        kb.cp("act", n3T[:], pN3[:], [f"pN3_{b2}"], [f"n3T_{b2}"])
        yield
        for half in range(2):
            for k in range(8):
                kb.mm(pGt[:, half * 512:(half + 1) * 512], n3T[:, k, :], wpg[:, k, half * 512:(half + 1) * 512], k == 0, k == 7,
                      [f"n3T_{b2}"] + wpgb, ["pGt"])
        yield
        kb.tt("dve", gate[:], pGt[:], bpg_bc[:], add, ["pGt", "bpg_bc"], [f"gate_{b2}"])
        yield
        kb.act(gate[:], gate[:], AF.Sigmoid, [f"gate_{b2}"], [f"gate_{b2}"])
        yield
        kb.cp("pool", plb[:], pl[b2][:], [f"pl{b2}"], [f"plb_{b2}"])
        yield
        for c in range(2):
            kb.tr(pPT[:, c, :], plb[:, c * 128:(c + 1) * 128], ident[:], [f"plb_{b2}", "ident"], [f"pPT_{b2}"])
        yield
        kb.cp("dve", pTs[:], pPT[:, 0:2, :], [f"pPT_{b2}"], [f"pTs_{b2}"])
        yield
        for half in range(2):
            for c in range(2):
                kb.mm(pPp[:, half * 512:(half + 1) * 512], pTs[:, c, :], wpp[:, c, half * 512:(half + 1) * 512], c == 0, c == 1,
                      [f"pTs_{b2}", "wpp"], ["pPp"])
        yield
        rstd_of(pPp[:], ["pPp"], s6[1], f"s6_1_{b2}", jk2[:], f"jk2_{b2}")
        yield
        kb.stt(ple[:], pPp[:], s6[1][:, 0:1], ppw_bc[:], mult, mult, ["pPp", f"s6_1_{b2}", "ppw_bc"], [f"ple_{b2}"])
        yield
        kb.tt("dve", ple[:], ple[:], gate[:], mult, [f"ple_{b2}", f"gate_{b2}"], [f"ple_{b2}"])
        yield
        kb.tt("dve", x3[:], x2[:], ple[:], add, [f"x2_{b2}", f"ple_{b2}"], [f"x3_{b2}"])
        yield
        rstd_of(x3[:], [f"x3_{b2}"], s6[2], f"s6_2_{b2}", jk2[:], f"jk2_{b2}")
        yield
        kb.stt(ot[b2][:], x3[:], s6[2][:, 0:1], fin_bc[:], mult, mult, [f"x3_{b2}", f"s6_2_{b2}", "fin_bc"], [f"ot{b2}"])
        yield
        kb.dma("sp", OUT[ts0:ts1, :], ot[b2][:], f"ot{b2}", [f"ot{b2}"], [])

    LAG_ = int(os.environ.get("KLAG", "4"))
    for t0_ in range(0, NT, 2):
        gA_ = p5d_tile(t0_)
        gB_ = p5d_tile(t0_ + 1) if t0_ + 1 < NT else iter(())
        aliveA_, aliveB_, nA_ = True, True, 0
        while aliveA_ or aliveB_:
            if aliveA_:
                try:
                    next(gA_)
                    nA_ += 1
                except StopIteration:
                    aliveA_ = False
            if aliveB_ and (nA_ >= LAG_ or not aliveA_):
                try:
                    next(gB_)
                except StopIteration:
                    aliveB_ = False
    kb.barrier()
    kb.pop()
    return nc, kb


_NC_CACHE = {}


def kernel(**inputs):
    x = np.asarray(inputs["x"], dtype=np.float32)
    B, S, _ = x.shape
    assert B == 8
    p = np.asarray(inputs["p"], dtype=np.float32)
    pos = np.asarray(inputs["positions"]).astype(np.int32)
    if S not in _NC_CACHE:
        nc, kb = build(S)
        kb.S.emit(nc, None)
        _NC_CACHE[S] = nc
    nc = _NC_CACHE[S]
    shared = {}
    for name, shape in WEIGHT_SPECS:
        a = np.asarray(inputs[name], dtype=np.float32)
        if name != "final_norm_w":
            a = a[0]
        shp = shape if len(shape) == 2 else [1, shape[0]]
        shared[name] = np.ascontiguousarray(a.reshape(shp))
    in_maps = []
    for b in range(B):
        m = dict(shared)
        m["x"] = np.ascontiguousarray(x[b])
        m["p"] = np.ascontiguousarray(p[0, b])
        m["pos"] = np.ascontiguousarray(pos[b].reshape(S // 128, 128))
        in_maps.append(m)
    res = run_bass_kernel_spmd(nc, in_maps, core_ids=list(range(B)))
    return np.stack([np.asarray(r["out"], dtype=np.float32) for r in res.results], axis=0)
```
